# Optimizing a Trainium2 kernel written in Bass

```python
import jax, jax.numpy as jnp
from jax import lax
import numpy as np

D_MODEL = 1024
BATCH = 2
SEQ = 8192
DEPTH = 2

GRID_W = 64
CTX_LEN = 256
HEAD_DIM = 64
HGRN_HEADS = 4
HGRN_DK = 64
HGRN_DV = 64
HGRN_CHUNK = 64
HGRN_QK = HGRN_HEADS * HGRN_DK
HGRN_W = HGRN_HEADS * HGRN_DV
LB_MIN = 1e-6
NA_HEADS = 4
NA_WIN_ROWS = 8
NA_WIN_COLS = 16
NA_KEY_COLS = 2 * NA_WIN_COLS
NA_W = NA_HEADS * HEAD_DIM
SWA_Q_HEADS = 8
SWA_KV_HEADS = 2
SWA_GROUP = SWA_Q_HEADS // SWA_KV_HEADS
SWA_WINDOW = 128
SWA_BLOCK = 128
SWA_W = SWA_Q_HEADS * HEAD_DIM
SWA_KV_W = SWA_KV_HEADS * HEAD_DIM
ROPE_BASE = 10000.0
MIX_W = HGRN_W + NA_W + SWA_W
IN_W = 3 * HGRN_QK + 2 * HGRN_W + 3 * NA_W + SWA_W + 2 * SWA_KV_W
N_EXPERTS = 16
N_GROUPS = 4
TOP_K = 2
D_EXPERT = 512
LN_EPS = 1e-5
RMS_EPS = 1e-6
NEG = -1e30

kernel_name = 'hybrid_hgrn2_natten_swa_moe_dit'

F32 = jnp.float32


def layer_norm(x, g, b):
    xf = x.astype(F32)
    mu = jnp.mean(xf, axis=-1, keepdims=True)
    var = jnp.mean(jnp.square(xf - mu), axis=-1, keepdims=True)
    y = (xf - mu) * lax.rsqrt(var + LN_EPS) * g.astype(F32) + b.astype(F32)
    return y.astype(x.dtype)


def rms_norm(x, g):
    xf = x.astype(F32)
    y = xf * lax.rsqrt(jnp.mean(jnp.square(xf), axis=-1, keepdims=True) + RMS_EPS) * g.astype(F32)
    return y.astype(x.dtype)


def heads(a, n):
    return a.reshape(a.shape[0], a.shape[1], n, -1).transpose(0, 2, 1, 3)


def merge_heads(a):
    return a.transpose(0, 2, 1, 3).reshape(a.shape[0], a.shape[2], -1)


def flip_t(a):
    return jnp.flip(a, axis=2)


def axial_rope(x, row, col):
    half = HEAD_DIM // 2
    nf = half // 2
    inv = ROPE_BASE ** (-jnp.arange(nf, dtype=F32) / nf)

    def rot(xp, pos):
        ang = pos.astype(F32)[:, None] * inv
        cos = jnp.cos(ang).astype(x.dtype)
        sin = jnp.sin(ang).astype(x.dtype)
        x1, x2 = xp[..., :nf], xp[..., nf:]
        return jnp.concatenate([x1 * cos - x2 * sin, x1 * sin + x2 * cos], axis=-1)

    return jnp.concatenate([rot(x[..., :half], row), rot(x[..., half:], col)], axis=-1)


def hgrn_gates(z, lb):
    zf = z.astype(F32)
    logf = jnp.logaddexp(jnp.log(lb), jnp.log1p(-lb) + jax.nn.log_sigmoid(zf))
    k = (1.0 - lb) * jax.nn.sigmoid(-zf)
    return heads(logf, HGRN_HEADS), heads(k, HGRN_HEADS)


def hgrn_scan(q, k, v, logf, s0):
    b_, h_, t_, _ = q.shape
    dv = v.shape[-1]
    nc = t_ // HGRN_CHUNK

    def chunks(a):
        return a.reshape(b_, h_, nc, HGRN_CHUNK, a.shape[-1]).transpose(2, 0, 1, 3, 4)

    lower = jnp.tril(jnp.ones((HGRN_CHUNK, HGRN_CHUNK), dtype=bool))[:, :, None]

    def step(state, inp):
        qc, kc, vc, lf = inp
        cum = jnp.cumsum(lf, axis=2)
        inter = jnp.einsum('bhtd,bhde->bhte', qc * jnp.exp(cum), state)
        decay = jnp.exp(jnp.where(lower, cum[:, :, :, None, :] - cum[:, :, None, :, :], NEG))
        scores = jnp.einsum('bhtd,bhsd,bhtsd->bhts', qc, kc, decay)
        out = inter + jnp.einsum('bhts,bhse->bhte', scores, vc)
        last = cum[:, :, -1:, :]
        new_state = jnp.exp(last[:, :, 0, :, None]) * state + jnp.einsum(
            'bhsd,bhse->bhde', kc * jnp.exp(last - cum), vc)
        return new_state, out

    s_fin, out = lax.scan(step, s0, (chunks(q), chunks(k), chunks(v), chunks(logf)))
    return out.transpose(1, 2, 0, 3, 4).reshape(b_, h_, t_, dv), s_fin


def ctx_attention(q, k, v, sink):
    s = jnp.einsum('bngtd,bnkd->bngtk', q, k).astype(F32) * q.shape[-1] ** -0.5
    if sink is None:
        p = jax.nn.softmax(s, axis=-1)
    else:
        s_sink = jnp.broadcast_to(sink.astype(F32)[None, :, :, None, None], s.shape[:-1] + (1,))
        p = jax.nn.softmax(jnp.concatenate([s, s_sink], axis=-1), axis=-1)[..., :-1]
    return jnp.einsum('bngtk,bnkd->bngtd', p.astype(v.dtype), v)


def neighborhood_attention(q, k, v, kc, vc, rpb):
    b_, h_, l_, dh = q.shape
    rows = l_ // GRID_W
    wr = min(NA_WIN_ROWS, rows)
    ncb = GRID_W // NA_WIN_COLS
    scale = dh ** -0.5
    qcol = jnp.arange(GRID_W).reshape(ncb, NA_WIN_COLS)
    cs = jnp.clip(qcol - NA_WIN_COLS // 2, 0, GRID_W - NA_WIN_COLS)
    blk = jnp.clip(jnp.arange(ncb) * NA_WIN_COLS - NA_WIN_COLS // 2, 0, GRID_W - NA_KEY_COLS)
    kcol = blk[:, None] + jnp.arange(NA_KEY_COLS)
    colmask = (kcol[:, None, :] >= cs[:, :, None]) & (kcol[:, None, :] < cs[:, :, None] + NA_WIN_COLS)
    coff = jnp.clip(kcol[:, None, :] - qcol[:, :, None] + NA_WIN_COLS - 1, 0, 2 * NA_WIN_COLS - 2)
    rpb_c = rpb[:, :, coff].astype(F32)
    kg = k.reshape(b_, h_, rows, GRID_W, dh)
    vg = v.reshape(b_, h_, rows, GRID_W, dh)
    qg = q.reshape(b_, h_, rows, ncb, NA_WIN_COLS, dh).transpose(2, 0, 1, 3, 4, 5)
    nw = wr * NA_KEY_COLS

    def row_block(inp):
        r, qr = inp
        rs = jnp.clip(r - wr // 2, 0, rows - wr)
        kw = jnp.take(lax.dynamic_slice_in_dim(kg, rs, wr, axis=2), kcol, axis=3)
        vw = jnp.take(lax.dynamic_slice_in_dim(vg, rs, wr, axis=2), kcol, axis=3)
        ridx = rs + jnp.arange(wr) - r + NA_WIN_ROWS - 1
        bias = jnp.take(rpb_c, ridx, axis=1).transpose(0, 2, 3, 1, 4)
        s_win = jnp.einsum('bhjud,bhrjvd->bhjurv', qr, kw).astype(F32) * scale + bias
        s_win = jnp.where(colmask[:, :, None, :], s_win, NEG)
        s_ctx = jnp.einsum('bhjud,bhkd->bhjuk', qr, kc).astype(F32) * scale
        p = jax.nn.softmax(jnp.concatenate(
            [s_win.reshape(b_, h_, ncb, NA_WIN_COLS, nw), s_ctx], axis=-1), axis=-1).astype(q.dtype)
        p_win = p[..., :nw].reshape(b_, h_, ncb, NA_WIN_COLS, wr, NA_KEY_COLS)
        return (jnp.einsum('bhjurv,bhrjvd->bhjud', p_win, vw)
                + jnp.einsum('bhjuk,bhkd->bhjud', p[..., nw:], vc))

    o = lax.map(row_block, (jnp.arange(rows), qg))
    return o.transpose(1, 2, 0, 3, 4, 5).reshape(b_, h_, l_, dh)


def swa_attention(q, k, v, kc, vc, sink):
    b_, n_, g_, l_, dh = q.shape
    nb = l_ // SWA_BLOCK
    nctx = kc.shape[2]
    scale = dh ** -0.5
    qb = q.reshape(b_, n_, g_, nb, SWA_BLOCK, dh)

    def band(a):
        ap = jnp.pad(a, ((0, 0), (0, 0), (SWA_BLOCK, SWA_BLOCK), (0, 0))).reshape(b_, n_, nb + 2, SWA_BLOCK, dh)
        return jnp.concatenate([ap[:, :, :-2], ap[:, :, 1:-1], ap[:, :, 2:]], axis=3)

    kb, vb = band(k), band(v)
    qpos = jnp.arange(nb)[:, None] * SWA_BLOCK + jnp.arange(SWA_BLOCK)[None, :]
    kpos = (jnp.arange(nb)[:, None] - 1) * SWA_BLOCK + jnp.arange(3 * SWA_BLOCK)[None, :]
    mask = ((kpos[:, None, :] >= 0) & (kpos[:, None, :] < l_)
            & (jnp.abs(qpos[:, :, None] - kpos[:, None, :]) <= SWA_WINDOW))
    s_band = jnp.where(mask, jnp.einsum('bngjud,bnjvd->bngjuv', qb, kb).astype(F32) * scale, NEG)
    s_ctx = jnp.einsum('bngjud,bnkd->bngjuk', qb, kc).astype(F32) * scale
    s_sink = jnp.broadcast_to(sink.astype(F32)[None, :, :, None, None, None], s_ctx.shape[:-1] + (1,))
    p = jax.nn.softmax(jnp.concatenate([s_band, s_ctx, s_sink], axis=-1), axis=-1).astype(q.dtype)
    nk = 3 * SWA_BLOCK
    o = (jnp.einsum('bngjuv,bnjvd->bngjud', p[..., :nk], vb)
         + jnp.einsum('bngjuk,bnkd->bngjud', p[..., nk:nk + nctx], vc))
    return o.reshape(b_, n_, g_, l_, dh)


def mixer(h, hc, w_in, lb_fwd, lb_bwd, hgrn_norm, rpb, sink, w_out, with_ctx_out):
    b_, l_, _ = h.shape
    t = jnp.arange(l_)
    row, col = t // GRID_W, t % GRID_W
    sizes = [HGRN_QK, HGRN_QK, HGRN_QK, HGRN_W, HGRN_W, NA_W, NA_W, NA_W, SWA_W, SWA_KV_W, SWA_KV_W]
    splits = np.cumsum(sizes)[:-1].tolist()
    aq, af, ab, ai, ag, nq, nk, nv, sq, sk, sv = jnp.split(h @ w_in, splits, axis=-1)
    caq, caf, cab, cai, cag, cnq, cnk, cnv, csq, csk, csv = jnp.split(hc @ w_in, splits, axis=-1)

    lf_f, k_f = hgrn_gates(af, lb_fwd)
    lf_b, k_b = hgrn_gates(ab, lb_bwd)
    clf_f, ck_f = hgrn_gates(caf, lb_fwd)
    clf_b, ck_b = hgrn_gates(cab, lb_bwd)
    q_a, v_a = heads(aq, HGRN_HEADS).astype(F32), heads(ai, HGRN_HEADS).astype(F32)
    cq_a, cv_a = heads(caq, HGRN_HEADS).astype(F32), heads(cai, HGRN_HEADS).astype(F32)
    s0 = jnp.zeros((b_, HGRN_HEADS, HGRN_DK, HGRN_DV), F32)
    oc_f, sc_f = hgrn_scan(cq_a, ck_f, cv_a, clf_f, s0)
    oc_b, sc_b = hgrn_scan(flip_t(cq_a), flip_t(ck_b), flip_t(cv_a), flip_t(clf_b), s0)
    o_f, _ = hgrn_scan(q_a, k_f, v_a, lf_f, sc_f)
    o_b, _ = hgrn_scan(flip_t(q_a), flip_t(k_b), flip_t(v_a), flip_t(lf_b), sc_b)
    y_a = merge_heads(rms_norm(o_f + flip_t(o_b), hgrn_norm)).astype(h.dtype) * jax.nn.silu(ag)

    kc_n, vc_n = heads(cnk, NA_HEADS), heads(cnv, NA_HEADS)
    y_b = merge_heads(neighborhood_attention(heads(nq, NA_HEADS), heads(nk, NA_HEADS),
                                             heads(nv, NA_HEADS), kc_n, vc_n, rpb))

    q_s = axial_rope(heads(sq, SWA_Q_HEADS), row, col).reshape(b_, SWA_KV_HEADS, SWA_GROUP, l_, HEAD_DIM)
    k_s = axial_rope(heads(sk, SWA_KV_HEADS), row, col)
    v_s = heads(sv, SWA_KV_HEADS)
    kc_s, vc_s = heads(csk, SWA_KV_HEADS), heads(csv, SWA_KV_HEADS)
    sink_g = sink.reshape(SWA_KV_HEADS, SWA_GROUP)
    y_c = merge_heads(swa_attention(q_s, k_s, v_s, kc_s, vc_s, sink_g).reshape(b_, SWA_Q_HEADS, l_, HEAD_DIM))

    y = jnp.concatenate([y_a, y_b, y_c], axis=-1) @ w_out
    if not with_ctx_out:
        return y, None
    nctx = hc.shape[1]
    yc_a = merge_heads(rms_norm(oc_f + flip_t(oc_b), hgrn_norm)).astype(hc.dtype) * jax.nn.silu(cag)
    yc_b = merge_heads(ctx_attention(heads(cnq, NA_HEADS)[:, :, None], kc_n, vc_n, None)[:, :, 0])
    cq_s = heads(csq, SWA_Q_HEADS).reshape(b_, SWA_KV_HEADS, SWA_GROUP, nctx, HEAD_DIM)
    yc_c = merge_heads(ctx_attention(cq_s, kc_s, vc_s, sink_g).reshape(b_, SWA_Q_HEADS, nctx, HEAD_DIM))
    yc = jnp.concatenate([yc_a, yc_b, yc_c], axis=-1) @ w_out
    return y, yc


def moe(h, w_router, router_bias, w_gate, w_up, w_down):
    n = h.shape[0]
    epg = N_EXPERTS // N_GROUPS
    s = jax.nn.sigmoid((h @ w_router).astype(F32))
    sel = s + router_bias.astype(F32)
    grp = lax.top_k(sel.reshape(n, N_GROUPS, epg), TOP_K)[0].sum(-1)
    best = jnp.argmax(grp, axis=-1)
    in_group = (jnp.arange(N_EXPERTS) // epg)[None, :] == best[:, None]
    _, idx = lax.top_k(jnp.where(in_group, sel, NEG), TOP_K)
    w = jnp.take_along_axis(s, idx, axis=-1)
    w = w / jnp.sum(w, axis=-1, keepdims=True)
    gates = jnp.sum(jax.nn.one_hot(idx, N_EXPERTS, dtype=F32) * w[..., None], axis=1).astype(h.dtype)
    y = jnp.zeros_like(h)
    for e in range(N_EXPERTS):
        he = jax.nn.silu(h @ w_gate[e]) * (h @ w_up[e])
        y = y + gates[:, e:e + 1] * (he @ w_down[e])
    return y


def ada_mod(cond, w, b):
    m = (jax.nn.silu(cond) @ w + b)[..., None, :]
    return jnp.split(m, 6, axis=-1)


def setup_inputs(seed: int = 0) -> dict:
    key = jax.random.key(seed)
    ks = jax.random.split(key, 21)
    nrm = jax.random.normal
    beta = (8.0 * DEPTH) ** -0.25
    d = D_MODEL
    return {
        'x': nrm(ks[0], (BATCH, SEQ, d), F32),
        'c': nrm(ks[1], (BATCH, d), F32),
        'ctx': nrm(ks[2], (BATCH, CTX_LEN, d), F32),
        'c_ctx': nrm(ks[3], (d,), F32),
        'w_ada': nrm(ks[4], (DEPTH, d, 6 * d), F32) * (0.5 * d ** -0.5),
        'b_ada': nrm(ks[5], (DEPTH, 6 * d), F32) * 0.02,
        'w_in': nrm(ks[6], (DEPTH, d, IN_W), F32) * d ** -0.5,
        'lb_logits': nrm(ks[7], (DEPTH, 2, HGRN_QK), F32),
        'hgrn_norm': 1.0 + 0.1 * nrm(ks[8], (DEPTH, HGRN_DV), F32),
        'na_rpb': 0.02 * nrm(ks[9], (DEPTH, NA_HEADS, 2 * NA_WIN_ROWS - 1, 2 * NA_WIN_COLS - 1), F32),
        'swa_sink': nrm(ks[10], (DEPTH, SWA_Q_HEADS), F32),
        'w_out': nrm(ks[11], (DEPTH, MIX_W, d), F32) * (MIX_W ** -0.5 * beta),
        'ln1_g': 1.0 + 0.1 * nrm(ks[12], (DEPTH, d), F32),
        'ln1_b': 0.02 * nrm(ks[13], (DEPTH, d), F32),
        'ln2_g': 1.0 + 0.1 * nrm(ks[14], (DEPTH, d), F32),
        'ln2_b': 0.02 * nrm(ks[15], (DEPTH, d), F32),
        'w_router': nrm(ks[16], (d, N_EXPERTS), F32) * d ** -0.5,
        'router_bias': 0.01 * nrm(ks[17], (N_EXPERTS,), F32),
        'w_gate': nrm(ks[18], (DEPTH, N_EXPERTS, d, D_EXPERT), F32) * d ** -0.5,
        'w_up': nrm(ks[19], (DEPTH, N_EXPERTS, d, D_EXPERT), F32) * d ** -0.5,
        'w_down': nrm(ks[20], (DEPTH, N_EXPERTS, D_EXPERT, d), F32) * (D_EXPERT ** -0.5 * beta),
    }


def reference(x, c, ctx, c_ctx, w_ada, b_ada, w_in, lb_logits, hgrn_norm, na_rpb, swa_sink, w_out,
              ln1_g, ln1_b, ln2_g, ln2_b, w_router, router_bias, w_gate, w_up, w_down):
    b_, l_, d = x.shape
    alpha = (2.0 * DEPTH) ** 0.25
    p_lb = jax.nn.softmax(lb_logits.astype(F32), axis=0)
    lb_all = jnp.maximum(jnp.cumsum(p_lb, axis=0) - p_lb[0:1], LB_MIN)
    for l in range(DEPTH):
        last = l == DEPTH - 1
        sh1, sc1, g1, sh2, sc2, g2 = ada_mod(c, w_ada[l], b_ada[l])
        csh1, csc1, cg1, csh2, csc2, cg2 = ada_mod(c_ctx, w_ada[l], b_ada[l])
        h = x * (1.0 + sc1) + sh1
        hc = ctx * (1.0 + csc1) + csh1
        y, yc = mixer(h, hc, w_in[l], lb_all[l, 0], lb_all[l, 1], hgrn_norm[l], na_rpb[l], swa_sink[l],
                      w_out[l], not last)
        x = layer_norm(alpha * x + g1 * y, ln1_g[l], ln1_b[l])
        h2 = x * (1.0 + sc2) + sh2
        if last:
            f = moe(h2.reshape(-1, d), w_router, router_bias, w_gate[l], w_up[l], w_down[l]).reshape(x.shape)
            x = layer_norm(alpha * x + g2 * f, ln2_g[l], ln2_b[l])
        else:
            ctx = layer_norm(alpha * ctx + cg1 * yc, ln1_g[l], ln1_b[l])
            hc2 = ctx * (1.0 + csc2) + csh2
            f = moe(jnp.concatenate([h2, hc2], axis=1).reshape(-1, d), w_router, router_bias,
                    w_gate[l], w_up[l], w_down[l]).reshape(b_, l_ + ctx.shape[1], d)
            x = layer_norm(alpha * x + g2 * f[:, :l_], ln2_g[l], ln2_b[l])
            ctx = layer_norm(alpha * ctx + cg2 * f[:, l_:], ln2_g[l], ln2_b[l])
    return x
```

```python
import contextlib
import numpy as np
import concourse.bass as bass
import concourse.mybir as mybir
from concourse.bass_utils import run_bass_kernel_spmd


F32 = mybir.dt.float32
BF16 = mybir.dt.bfloat16
AF = mybir.ActivationFunctionType
ALU = mybir.AluOpType
AX = mybir.AxisListType


class Sched:
    ENG = ("pe", "act", "dve", "pool", "sp")

    def __init__(self, nc, es, n_dma_sems=12):
        self.nc = nc
        self.e = {"pe": nc.tensor, "act": nc.scalar, "dve": nc.vector, "pool": nc.gpsimd, "sp": nc.sync}
        self.sem = {}
        self.cnt = {}
        for k in self.ENG:
            self.sem[k] = es.enter_context(nc.semaphore("s_" + k))
            self.cnt[k] = 0
        self.dq = {}
        for q in ("sp", "pool", "act"):
            n = n_dma_sems if q != "act" else 4
            self.dq[q] = {"sems": [], "vals": [0] * n, "rr": 0}
            for i in range(n):
                key = "d_%s_%d" % (q, i)
                self.sem[key] = es.enter_context(nc.semaphore(key))
                self.dq[q]["sems"].append(key)
        self.waited = {k: {} for k in self.ENG}
        self.lastw = {}
        self.readers = {}
        self.n_inst = 0
        self.n_wait = 0

    def _wait(self, eng, ev):
        if ev is None:
            return
        s, v = ev
        if s == eng and eng == "pe":
            return
        if s == eng and v <= 0:
            return
        if self.waited[eng].get(s, 0) >= v:
            return
        self.e[eng].wait_ge(self.sem[s], v)
        self.waited[eng][s] = v
        self.n_wait += 1

    def _deps(self, eng, reads, writes):
        for t in reads:
            self._wait(eng, self.lastw.get(t))
        for t in writes:
            self._wait(eng, self.lastw.get(t))
            for ev in self.readers.get(t, {}).items():
                self._wait(eng, ev)

    def _record(self, ev, reads, writes):
        s, v = ev
        for t in reads:
            r = self.readers.setdefault(t, {})
            if r.get(s, 0) < v:
                r[s] = v
        for t in writes:
            self.lastw[t] = ev
            self.readers[t] = {}

    def op(self, eng, fn, reads=(), writes=()):
        self._deps(eng, reads, writes)
        inst = fn(self.e[eng])
        self.cnt[eng] += 1
        inst.then_inc(self.sem[eng], 1)
        self._record((eng, self.cnt[eng]), reads, writes)
        self.n_inst += 1
        return inst

    def dma(self, q, out, in_, reads=(), writes=(), **kw):
        d = self.dq[q]
        i = d["rr"]
        d["rr"] = (i + 1) % len(d["sems"])
        key = d["sems"][i]
        if d["vals"][i] > 0:
            self._wait(q, (key, d["vals"][i]))
        self._deps(q, reads, writes)
        inst = self.e[q].dma_start(out=out, in_=in_, **kw)
        d["vals"][i] += 16
        inst.then_inc(self.sem[key], 16)
        self._record((key, d["vals"][i]), reads, writes)
        self.n_inst += 1
        return inst

    def all_events(self):
        evs = [(k, self.cnt[k]) for k in self.ENG if self.cnt[k] > 0]
        for q, d in self.dq.items():
            for key, v in zip(d["sems"], d["vals"]):
                if v > 0:
                    evs.append((key, v))
        return evs

    def barrier(self, engines=None):
        evs = self.all_events()
        for eng in (engines or self.ENG):
            for ev in evs:
                self._wait(eng, ev)

    def finish(self):
        self.barrier(engines=("sp",))


RMS_EPS = 1e-6
NCOL = 960
CTX = 256


def bc_mid(ap2d, n):
    p, k = ap2d.shape
    return ap2d.unsqueeze(2).broadcast_to([p, k, n])


def na_configs(nrows=128):
    cfg = {}
    mats = []
    interior = {}
    for pq in range(nrows // 2):
        rows = set()
        for r in (2 * pq, 2 * pq + 1):
            rs = min(max(r - 4, 0), nrows - 8)
            rows |= set(range(rs, rs + 8))
        pks = sorted(set(k // 2 for k in rows))
        lst = []
        for pk in pks:
            if 2 <= pq <= nrows // 2 - 3:
                key = ("i", pk - pq)
            else:
                key = (pq, pk)
            if key not in interior:
                interior[key] = len(mats)
                mats.append((pq, pk))
            lst.append((pk, interior[key]))
        cfg[pq] = lst
    return cfg, mats


def emit_phase_a(nc, S, io, layer, with_ctx_out, uid, n_lat=8192):
    T = CTX + n_lat
    D = 1024
    xT = io["xT"]; wcore = io["wcore"]; ccol = io["ccol"]; wada = io["wada"]; badac = io["badac"]; lbl = io["lbl"]
    gnorm_d = io["gnorm"]; nab_d = io["nab"]; swm_d = io["swm"]; sink_d = io["sink"]; ropeC_d = io["ropeC"]; ropeS_d = io["ropeS"]
    cst_d = io["cst"]; vmask_d = io["vmask"]; yT = io["yT"]; ofs = io["ofs"]
    stop = 99

    cfgs, mats = na_configs(n_lat // 64)
    assert len(mats) == 21
    blocks = [(0, CTX)] + [(CTX + i * 512, 512) for i in range(n_lat // 512)]
    NTILE = T // 128

    es = contextlib.ExitStack()
    with es:
        sb = lambda name, shape, dt=F32, st=es: st.enter_context(nc.sbuf_tensor(uid + "s_" + name, shape, dt))
        pst = lambda name, shape, dt=F32, st=es: st.enter_context(nc.psum_tensor(uid + "p_" + name, shape, dt))
        cst = sb("cst", [128, 128 * 4 + 512])
        identb = sb("identb", [128, 128], BF16)
        ident8b = sb("ident8b", [128, 128], BF16)
        hmF = cst[:, 256:384]; hmB = cst[:, 384:512]; rmask = cst[0:64, 512:1024]
        vmask = sb("vmask", [128, 4])
        wb = sb("wb", [128, 8, NCOL], BF16)
        modc = sb("modc", [128, 16, 2])
        lb = sb("lb", [64, 2]); oml = sb("oml", [64, 2])
        gnorm = sb("gnorm_sb", [64, 1])
        ones64 = sb("ones64", [64, 64])
        epsr = sb("epsr", [64, 1])
        nqT = sb("nqT", [64, T], BF16); nkT = sb("nkT", [64, T], BF16); nv1 = sb("nv1", [128, NTILE, 65], BF16)
        sq0T = sb("sq0T", [64, T], BF16); sq1T = sb("sq1T", [64, T], BF16); skT = sb("skT", [64, T], BF16)
        sv1 = sb("sv1", [128, NTILE, 65], BF16)
        S.dma("sp", cst[:], cst_d[:], writes=["cst"])
        S.dma("sp", vmask[:], vmask_d[:], writes=["vmask"])
        S.dma("sp", gnorm[:], gnorm_d[:], writes=["gnorm"])
        S.dma("pool", wb[:], wcore.rearrange("(k p) n -> p k n", p=128), writes=["wb"])
        S.op("dve", lambda e: e.tensor_copy(out=identb[:], in_=cst[:, 0:128]), reads=["cst"], writes=["identb"])
        S.op("dve", lambda e: e.tensor_copy(out=ident8b[:], in_=cst[:, 128:256]), reads=["cst"], writes=["ident8b"])
        S.op("dve", lambda e: e.memset(ones64[:], 1.0), writes=["ones64"])
        S.op("dve", lambda e: e.memset(epsr[:], RMS_EPS), writes=["epsr"])
        S.op("pool", lambda e: e.memset(nv1[:, :, 64:65], 1.0), writes=["nv1ones"])
        S.op("pool", lambda e: e.memset(sv1[:, :, 64:65], 1.0), writes=["sv1ones"])
        st0 = contextlib.ExitStack()
        with st0:
            lbt = sb("lbt", [64, 4], st=st0)
            S.dma("sp", lbt[:], lbl[:], writes=["lbt"])
            if layer == 0:
                S.op("dve", lambda e: e.memset(lb[:], 1e-6), writes=["lb"])
            else:
                lbv = lbt[:].rearrange("p (d l) -> p d l", l=2)
                S.op("dve", lambda e: e.tensor_tensor(out=lb[:], in0=lbv[:, :, 0], in1=lbv[:, :, 1], op=ALU.subtract),
                     reads=["lbt"], writes=["lb"])
                S.op("act", lambda e: e.activation(out=lb[:], in_=lb[:], func=AF.Exp), reads=["lb"], writes=["lb"])
                S.op("dve", lambda e: e.tensor_scalar(out=lb[:], in0=lb[:], scalar1=1.0, scalar2=None, op0=ALU.add), reads=["lb"], writes=["lb"])
                S.op("dve", lambda e: e.reciprocal(out=lb[:], in_=lb[:]), reads=["lb"], writes=["lb"])
                S.op("dve", lambda e: e.tensor_scalar(out=lb[:], in0=lb[:], scalar1=1e-6, scalar2=None, op0=ALU.max), reads=["lb"], writes=["lb"])
            S.op("dve", lambda e: e.tensor_scalar(out=oml[:], in0=lb[:], scalar1=-1.0, scalar2=1.0, op0=ALU.mult, op1=ALU.add),
                 reads=["lb"], writes=["oml"])
            cc = sb("cc", [128, 16], st=st0); scc = sb("scc", [128, 8, 2], st=st0)
            wa = sb("wa", [128, 8, 2048], st=st0)
            bdc = sb("bdc", [128, 16], st=st0)
            psm = pst("psm", [128, 16, 2], st=st0)
            S.dma("sp", cc[:], ccol[:], writes=["cc"])
            S.dma("sp", bdc[:], badac[:], writes=["bdc"])
            S.dma("sp", wa[:], wada.rearrange("(k p) n -> p k n", p=128), writes=["wa"])
            S.op("act", lambda e: e.activation(out=scc[:].rearrange("p k w -> p w k"), in_=cc[:].rearrange("p (w k) -> p w k", w=2), func=AF.Silu),
                 reads=["cc"], writes=["scc"])
            for dch in range(16):
                for k in range(8):
                    S.op("pe", lambda e, dch=dch, k=k: e.matmul(psm[:, dch, :], lhsT=wa[:, k, dch * 128:(dch + 1) * 128], rhs=scc[:, k, :],
                                                                start=(k == 0), stop=(k == 7)),
                         reads=["wa", "scc"], writes=["psm"])
            S.op("dve", lambda e: e.tensor_tensor(out=modc[:], in0=psm[:], in1=bc_mid(bdc[:], 2), op=ALU.add),
                 reads=["psm", "bdc"], writes=["modc"])
            S.op("dve", lambda e: e.tensor_scalar(out=modc[:, 8:16, :], in0=modc[:, 8:16, :], scalar1=1.0, scalar2=None, op0=ALU.add),
                 reads=["modc"], writes=["modc"])
            S.barrier()
        if stop <= 0:
            pass

        UB = io["UB"]; QB = io["QB"]; SG = io["SG"]
        dcyB = sb("dcyB", [64, len(blocks), 16])
        Sall = sb("Sall", [64, 17, 64])
        Sbf = sb("Sbf", [64, 16, 64], BF16)
        Ub = sb("Ub", [64, 16, 64])
        sgt = sb("sgt", [64, 512])
        ps_oi = pst("ps_oi", [64, 512])
        oi = sb("oi", [64, 512]); oo = sb("oo", [64, 512]); ofb = sb("ofb", [64, 512])
        qtl = sb("qtl", [64, 512], BF16)
        stp = contextlib.ExitStack()
        with stp:
            xt = sb("xt0", [128, 8, 512], st=stp)
            hT = [sb("hT%d" % i, [128, 8, 512], BF16, st=stp) for i in range(2)]
            rC = sb("rC", [64, 512], st=stp); rS = sb("rS", [64, 512], st=stp)
            g_sb = {}
            for nm in ("aq", "az", "az2", "ag", "r0", "r1"):
                g_sb[nm] = sb("g_" + nm, [64, 512], st=stp)
            tA = sb("tA", [64, 512], st=stp); tB = sb("tB", [64, 512], st=stp); tC = sb("tC", [64, 512], st=stp)
            tD = sb("tD", [64, 512], st=stp); tE = sb("tE", [64, 512], st=stp)
            tot = sb("tot", [64, 16], st=stp); dcy = sb("dcy", [64, 16], st=stp)
            ktl = sb("ktl", [64, 512], BF16, st=stp); khT = sb("khT", [64, 512], BF16, st=stp)
            qtlb = sb("qtlb", [64, 512], BF16, st=stp); ktlb = sb("ktlb", [64, 512], BF16, st=stp); khTb = sb("khTb", [64, 512], BF16, st=stp)
            dcy2 = sb("dcy2", [64, 16], st=stp)
            kh = sb("kh", [128, 4, 64], BF16, st=stp)
            vt = sb("vt", [128, 4, 64], BF16, st=stp)
            vblk = sb("vblk", [128, 4, 4, 64], BF16, st=stp)
            scT = sb("scT", [128, 128], BF16, st=stp)
            ps_f = [pst("ps_f%d" % i, [64, 512], st=stp) for i in range(2)]
            ps_tm = pst("ps_tm", [128, 192], st=stp)
            ps_kh = pst("ps_kh", [128, 64], BF16, st=stp)
            ps_U = pst("ps_U", [64, 4, 64], st=stp)
            ps_sc = pst("ps_sc", [128, 128], st=stp)
            ps_oa = pst("ps_oa", [64, 512], st=stp)
            S.op("dve", lambda e: e.memset(Sall[:, 0, :], 0.0), writes=["Sall"])
            fcnt = [0]

            def load_block(bi, par):
                t0, n = blocks[bi]
                S.dma("sp", xt[:, :, :n], xT.rearrange("(k p) t -> p k t", p=128)[:, :, t0:t0 + n], writes=["xt0"])
                w = 1 if t0 < CTX else 0
                for k in range(8):
                    eng = "dve" if k % 2 == 0 else "pool"
                    S.op(eng, lambda e, k=k, w=w, par=par, n=n: e.tensor_scalar(
                        out=hT[par][:, k, :n], in0=xt[:, k, :n], scalar1=modc[:, 8 + k, w:w + 1], scalar2=modc[:, k, w:w + 1],
                        op0=ALU.mult, op1=ALU.add), reads=["xt0", "modc"], writes=["hT%d_%d" % (par, k)])

            def proj_fm(par, n, g):
                i = fcnt[0] % 2; fcnt[0] += 1
                for k in range(8):
                    S.op("pe", lambda e, k=k, i=i, g=g, par=par, n=n: e.matmul(ps_f[i][:, :n], lhsT=wb[:, k, g * 64:(g + 1) * 64],
                                                                             rhs=hT[par][:, k, :n], start=(k == 0), stop=(k == 7)),
                         reads=["wb", "hT%d_%d" % (par, k)], writes=["ps_f%d" % i])
                return ps_f[i], "ps_f%d" % i

            def hg_elem(n, d, zname, qo, ko, kho, sfx):
                nch = n // 32
                q_ = g_sb["aq"]; z_ = g_sb[zname]; ztok = "g_" + zname
                dc = dcy if d == 0 else dcy2
                dct = "dcy" if d == 0 else "dcy2"
                S.op("act", lambda e: e.activation(out=tA[:, :n], in_=z_[:, :n], func=AF.Sigmoid), reads=[ztok], writes=["tA"]); yield
                S.op("dve", lambda e: e.tensor_scalar(out=tA[:, :n], in0=tA[:, :n], scalar1=oml[:, d:d + 1], scalar2=lb[:, d:d + 1],
                                                      op0=ALU.mult, op1=ALU.add), reads=["tA", "oml", "lb"], writes=["tA"]); yield
                S.op("act", lambda e: e.activation(out=tB[:, :n], in_=tA[:, :n], func=AF.Ln), reads=["tA"], writes=["tB"]); yield
                S.op("dve", lambda e: e.tensor_scalar(out=tA[:, :n], in0=tA[:, :n], scalar1=-1.0, scalar2=1.0, op0=ALU.mult, op1=ALU.add),
                     reads=["tA", "tB"], writes=["tA"]); yield
                S.op("dve", lambda e: e.tensor_tensor_scan(out=tC[:, :n], data0=rmask[:, :n], data1=tB[:, :n], initial=0.0,
                                                           op0=ALU.mult, op1=ALU.add), reads=["tB", "cst"], writes=["tC"]); yield
                cumv = tC[:, :n].rearrange("p (c j) -> p c j", j=32)
                S.op("dve", lambda e: e.tensor_copy(out=tot[:, :nch], in_=cumv[:, :, 31]), reads=["tC"], writes=["tot"]); yield
                S.op("act", lambda e: e.activation(out=dc[:, :nch], in_=tot[:, :nch], func=AF.Exp), reads=["tot"], writes=[dct]); yield
                S.op("dve", lambda e: e.tensor_tensor(out=tD[:, :n].rearrange("p (c j) -> p c j", j=32), in0=bc_mid(tot[:, :nch], 32), in1=cumv,
                                                      op=ALU.subtract), reads=["tot", "tC"], writes=["tD"]); yield
                if d == 0:
                    e1, e3, e1n, e3n = tC, tD, "tC", "tD"
                else:
                    S.op("dve", lambda e: e.tensor_tensor(out=tE[:, :n], in0=tD[:, :n], in1=tB[:, :n], op=ALU.add), reads=["tD", "tB"], writes=["tE"]); yield
                    S.op("dve", lambda e: e.tensor_tensor(out=tC[:, :n], in0=tC[:, :n], in1=tB[:, :n], op=ALU.subtract), reads=["tC", "tB", "tD"], writes=["tC"]); yield
                    e1, e3, e1n, e3n = tE, tC, "tE", "tC"
                S.op("act", lambda e: e.activation(out=tB[:, :n], in_=e1[:, :n], func=AF.Exp, scale=-1.0), reads=[e1n, "tD", "tE", "tC"], writes=["tB"]); yield
                S.op("act", lambda e: e.activation(out=e1[:, :n], in_=e1[:, :n], func=AF.Exp), reads=[e1n, "tB"], writes=[e1n]); yield
                S.op("act", lambda e: e.activation(out=e3[:, :n], in_=e3[:, :n], func=AF.Exp), reads=[e3n], writes=[e3n]); yield
                S.op("dve", lambda e: e.tensor_tensor(out=qo[:, :n], in0=q_[:, :n], in1=e1[:, :n], op=ALU.mult), reads=["g_aq", e1n], writes=["qtl" + sfx]); yield
                S.op("dve", lambda e: e.tensor_tensor(out=ko[:, :n], in0=tA[:, :n], in1=tB[:, :n], op=ALU.mult), reads=["tA", "tB"], writes=["ktl" + sfx]); yield
                S.op("pool", lambda e: e.tensor_tensor(out=kho[:, :n], in0=tA[:, :n], in1=e3[:, :n], op=ALU.mult), reads=["tA", e3n], writes=["khT" + sfx]); yield

            def hg_tiles_U(n, store, kho=None, sfx="", tiles=None):
                kho = khT if kho is None else kho
                ntl = n // 128
                for tl in (range(ntl) if tiles is None else tiles):
                    S.op("pe", lambda e, tl=tl: e.transpose(out=ps_kh[:], in_=kho[:, tl * 128:(tl + 1) * 128], identity=identb[0:64, 0:64]),
                         reads=["khT" + sfx, "identb"], writes=["ps_kh"])
                    S.op("act", lambda e, tl=tl: e.activation(out=kh[:, tl, :], in_=ps_kh[:], func=AF.Copy), reads=["ps_kh"], writes=["kh%d" % tl])
                    S.op("pe", lambda e, tl=tl: e.matmul(ps_U[:].rearrange("p c e -> p (c e)"), lhsT=kh[:, tl, :],
                                                         rhs=vblk[:, tl, :, :].rearrange("p c e -> p (c e)"), start=True, stop=True),
                         reads=["kh%d" % tl, "vblk"], writes=["ps_U"])
                    if store:
                        S.op("act", lambda e, tl=tl: e.activation(out=Ub[:, tl * 4:(tl + 1) * 4, :], in_=ps_U[:], func=AF.Copy),
                             reads=["ps_U"], writes=["Ub"])
                    else:
                        for cc_ in range(4):
                            c = tl * 4 + cc_
                            S.op("dve", lambda e, c=c, cc_=cc_: e.scalar_tensor_tensor(
                                out=Sall[:, c + 1, :], in0=Sall[:, c, :], scalar=dcy[:, c:c + 1], in1=ps_U[:, cc_, :], op0=ALU.mult, op1=ALU.add),
                                reads=["ps_U", "Sall", "dcy"], writes=["Sall"])

            def hg_inter(n, off, qo=None, sfx=""):
                qo = qtl if qo is None else qo
                nch = n // 32
                S.op("act", lambda e: e.activation(out=Sbf[:, :nch, :], in_=Sall[:, off:off + nch, :], func=AF.Copy), reads=["Sall"], writes=["Sbf"])
                for c in range(nch):
                    S.op("pe", lambda e, c=c: e.matmul(ps_oi[:, c * 32:(c + 1) * 32], lhsT=Sbf[:, c, :], rhs=qo[:, c * 32:(c + 1) * 32],
                                                       start=True, stop=True), reads=["Sbf", "qtl" + sfx], writes=["ps_oi"])

            def hg_intra(n, d, qo=None, ko=None, sfx="", tiles=None):
                qo = qtl if qo is None else qo
                ko = ktl if ko is None else ko
                ntl = n // 128
                hm = hmF if d == 0 else hmB
                for tl in (range(ntl) if tiles is None else tiles):
                    S.op("pe", lambda e, tl=tl: e.matmul(ps_sc[:], lhsT=ko[:, tl * 128:(tl + 1) * 128], rhs=qo[:, tl * 128:(tl + 1) * 128],
                                                         start=True, stop=True), reads=["ktl" + sfx, "qtl" + sfx], writes=["ps_sc"])
                    S.op("dve", lambda e: e.tensor_tensor(out=scT[:], in0=ps_sc[:], in1=hm, op=ALU.mult), reads=["ps_sc", "cst"], writes=["scT"])
                    S.op("pe", lambda e, tl=tl: e.matmul(ps_oa[:, tl * 128:(tl + 1) * 128], lhsT=vt[:, tl, :], rhs=scT[:], start=True, stop=True),
                         reads=["vt", "scT"], writes=["ps_oa"])

            load_block(0, 0)
            for bi in range(len(blocks)):
                par = bi % 2
                t0, n = blocks[bi]
                ntl = n // 128; nch = n // 32
                is_ctx = t0 < CTX
                if bi + 1 < len(blocks):
                    load_block(bi + 1, 1 - par)
                if not is_ctx:
                    S.dma("sp", rC[:, :n], ropeC_d[:, t0 - CTX:t0 - CTX + n], writes=["rC"])
                    S.dma("sp", rS[:, :n], ropeS_d[:, t0 - CTX:t0 - CTX + n], writes=["rS"])
                def evac(g, dst, dtok, eng="act"):
                    p_, ptok = proj_fm(par, n, g)
                    o_ = dst[:, :n] if dst.shape[1] == 512 else dst[:, t0:t0 + n]
                    if eng == "act":
                        S.op("act", lambda e: e.activation(out=o_, in_=p_[:, :n], func=AF.Copy), reads=[ptok], writes=[dtok])
                    else:
                        S.op("dve", lambda e: e.tensor_copy(out=o_, in_=p_[:, :n]), reads=[ptok], writes=[dtok])

                def rope_group(ga, gb, dst, dtok):
                    pa, patok = proj_fm(par, n, ga)
                    S.op("dve", lambda e, pa=pa: e.tensor_tensor(out=g_sb["r0"][:, :n], in0=pa[:, :n], in1=rC[:, :n], op=ALU.mult),
                         reads=[patok, "rC"], writes=["g_r0"])
                    pb, pbtok = proj_fm(par, n, gb)
                    S.op("dve", lambda e, pb=pb: e.tensor_tensor(out=g_sb["r1"][:, :n], in0=pb[:, :n], in1=rS[:, :n], op=ALU.mult),
                         reads=[pbtok, "rS"], writes=["g_r1"])
                    S.op("pool", lambda e, dst=dst: e.tensor_tensor(out=dst[:, t0:t0 + n], in0=g_sb["r0"][:, :n], in1=g_sb["r1"][:, :n], op=ALU.add),
                         reads=["g_r0", "g_r1"], writes=[dtok])

                evac(0, g_sb["aq"], "g_aq", "act")
                evac(1, g_sb["az"], "g_az", "dve")
                evac(2, g_sb["az2"], "g_az2", "act")
                for tl in range(ntl):
                    gt = t0 // 128 + tl
                    for k in range(8):
                        S.op("pe", lambda e, k=k, tl=tl, par=par: e.matmul(ps_tm[:], lhsT=hT[par][:, k, tl * 128:(tl + 1) * 128],
                                                                           rhs=wb[:, k, 768:960], start=(k == 0), stop=(k == 7)),
                             reads=["wb", "hT%d_%d" % (par, k)], writes=["ps_tm"])
                    S.op("act", lambda e, tl=tl: e.activation(out=vt[:, tl, :], in_=ps_tm[:, 0:64], func=AF.Copy), reads=["ps_tm"], writes=["vt"])
                    S.op("act", lambda e, gt=gt: e.activation(out=nv1[:, gt, 0:64], in_=ps_tm[:, 64:128], func=AF.Copy), reads=["ps_tm", "vt"], writes=["nv1_%d" % gt])
                    S.op("act", lambda e, gt=gt: e.activation(out=sv1[:, gt, 0:64], in_=ps_tm[:, 128:192], func=AF.Copy), reads=["ps_tm", "nv1_%d" % gt], writes=["sv1_%d" % gt])
                    for c in range(4):
                        S.op("pool", lambda e, tl=tl, c=c: e.tensor_scalar(out=vblk[:, tl, c, :], in0=vt[:, tl, :], scalar1=vmask[:, c:c + 1],
                                                                          scalar2=None, op0=ALU.mult), reads=["vt", "vmask"], writes=["vblk"])
                import itertools
                chain_f = hg_elem(n, 0, "az", qtl, ktl, khT, "")
                chain_b = hg_elem(n, 1, "az2", qtlb, ktlb, khTb, "b")
                others = [lambda: evac(3, g_sb["ag"], "g_ag", "dve"), lambda: evac(4, nqT, "nqT", "act"), lambda: evac(5, nkT, "nkT", "dve")]
                if is_ctx:
                    others += [lambda: evac(6, sq0T, "sq0T", "act"), lambda: evac(8, sq1T, "sq1T", "dve"), lambda: evac(10, skT, "skT", "act")]
                else:
                    others += [lambda: rope_group(6, 7, sq0T, "sq0T"), lambda: rope_group(8, 9, sq1T, "sq1T"), lambda: rope_group(10, 11, skT, "skT")]
                for oth in others:
                    for _ in range(3):
                        next(chain_f, None)
                    oth()
                for _ in chain_f:
                    pass

                def work_f():
                    for tl in range(ntl):
                        hg_tiles_U(n, store=False, tiles=[tl]); yield
                    hg_inter(n, 0); yield
                    for tl in range(ntl):
                        hg_intra(n, 0, tiles=[tl]); yield
                    S.op("act", lambda e: e.activation(out=oi[:, :n], in_=ps_oa[:, :n], func=AF.Copy), reads=["ps_oa"], writes=["oi"])
                    S.op("dve", lambda e: e.tensor_tensor(out=oo[:, :n], in0=ps_oi[:, :n], in1=oi[:, :n], op=ALU.add), reads=["ps_oi", "oi"], writes=["oo"])
                    S.op("dve", lambda e: e.tensor_copy(out=Sall[:, 0, :], in_=Sall[:, nch, :]), reads=["Sall", "Sbf"], writes=["Sall"])
                    yield
                for _ in work_f():
                    next(chain_b, None); next(chain_b, None)
                for _ in chain_b:
                    pass
                hg_tiles_U(n, store=True, kho=khTb, sfx="b")
                hg_intra(n, 1, qo=qtlb, ko=ktlb, sfx="b")
                S.op("dve", lambda e: e.tensor_tensor(out=oo[:, :n], in0=ps_oa[:, :n], in1=oo[:, :n], op=ALU.add), reads=["ps_oa", "oo"], writes=["oo"])
                S.op("dve", lambda e, bi=bi: e.tensor_copy(out=dcyB[:, bi, :nch], in_=dcy2[:, :nch]), reads=["dcy2"], writes=["dcyB"])
                S.dma("sp", ofs[:, t0:t0 + n], oo[:, :n], reads=["oo"], writes=["ofs%d" % bi])
                S.dma("sp", UB[:, bi, :nch * 64], Ub[:, :nch, :].rearrange("p c e -> p (c e)"), reads=["Ub"], writes=["UB%d" % bi])
                S.dma("sp", QB[:, t0:t0 + n], qtlb[:, :n], reads=["qtlb"], writes=["QB%d" % bi])
                S.op("act", lambda e: e.activation(out=sgt[:, :n], in_=g_sb["ag"][:, :n], func=AF.Silu), reads=["g_ag"], writes=["sgt"])
                S.op("dve", lambda e: e.tensor_scalar(out=sgt[:, :n], in0=sgt[:, :n], scalar1=gnorm[:, 0:1], scalar2=None, op0=ALU.mult),
                     reads=["sgt", "gnorm"], writes=["sgt"])
                S.dma("sp", SG[:, t0:t0 + n], sgt[:, :n], reads=["sgt"], writes=["SG%d" % bi])
            S.barrier()

        def pass2_gen():
            S.op("dve", lambda e: e.memset(Sall[:, 0, :], 0.0), writes=["Sall"])
            order2 = [0] + list(range(len(blocks) - 1, 0, -1))
            for oi_, bi in enumerate(order2):
                t0, n = blocks[bi]
                ntl = n // 128; nch = n // 32
                S.dma("sp", Ub[:, :nch, :].rearrange("p c e -> p (c e)"), UB[:, bi, :nch * 64], writes=["Ub"])
                S.dma("sp", qtl[:, :n], QB[:, t0:t0 + n], writes=["qtl"])
                S.dma("sp", ofb[:, :n], ofs[:, t0:t0 + n], writes=["ofb"])
                S.dma("sp", sgt[:, :n], SG[:, t0:t0 + n], writes=["sgt"])
                S.op("dve", lambda e: e.tensor_copy(out=Sall[:, nch, :], in_=Sall[:, 0, :]), reads=["Sall"], writes=["Sall"])
                for c in range(nch - 1, -1, -1):
                    S.op("dve", lambda e, c=c, bi=bi: e.scalar_tensor_tensor(
                        out=Sall[:, c, :], in0=Sall[:, c + 1, :], scalar=dcyB[:, bi, c:c + 1], in1=Ub[:, c, :], op0=ALU.mult, op1=ALU.add),
                        reads=["Ub", "Sall", "dcyB"], writes=["Sall"])
                hg_inter(n, 1)
                S.op("dve", lambda e: e.tensor_tensor(out=oo[:, :n], in0=ps_oi[:, :n], in1=ofb[:, :n], op=ALU.add), reads=["ps_oi", "ofb"], writes=["oo"])
                S.op("act", lambda e: e.activation(out=oi[:, :n], in_=oo[:, :n], func=AF.Square), reads=["oo", "oi"], writes=["oi"])
                pss, psstok = ps_oi, "ps_oi"
                S.op("pe", lambda e, pss=pss: e.matmul(pss[:, :n], lhsT=ones64[:], rhs=oi[:, :n], start=True, stop=True),
                     reads=["ones64", "oi"], writes=[psstok])
                S.op("act", lambda e, pss=pss: e.activation(out=oi[:, :n], in_=pss[:, :n], func=AF.Sqrt, bias=epsr[:, 0:1], scale=1.0 / 64.0),
                     reads=[psstok, "epsr"], writes=["oi"])
                S.op("dve", lambda e: e.reciprocal(out=oi[:, :n], in_=oi[:, :n]), reads=["oi"], writes=["oi"])
                S.op("dve", lambda e: e.tensor_tensor(out=oo[:, :n], in0=oo[:, :n], in1=oi[:, :n], op=ALU.mult), reads=["oo", "oi"], writes=["oo"])
                S.op("dve", lambda e: e.tensor_tensor(out=oo[:, :n], in0=oo[:, :n], in1=sgt[:, :n], op=ALU.mult), reads=["oo", "sgt"], writes=["oo"])
                S.dma("sp", yT[0:64, t0:t0 + n], oo[:, :n], reads=["oo"], writes=["yTa%d" % bi])
                yield bi
        sta = contextlib.ExitStack()
        with sta:
            nab = sb("nab", [128, 21, 128], BF16, st=sta)
            swm = sb("swm", [128, 2, 128], BF16, st=sta)
            snk = sb("snk", [1, 2], st=sta); snkB = sb("snkB", [128, 2], st=sta)
            ones1 = sb("ones1", [1, 128], st=sta)
            pT = [sb("pT%d" % i, [128, 7, 128], BF16, st=sta) for i in range(2)]
            ot = [sb("ot%d" % i, [128, 64], BF16, st=sta) for i in range(2)]
            rinv = sb("rinv", [128, 1], st=sta)
            yblk = [sb("yblk%d" % i, [64, 512], st=sta) for i in range(2)]
            ps_s = [[pst("ps_s%d_%d" % (i, j), [128, 4, 128], st=sta) for j in range(2)] for i in range(2)]
            ps_o = [pst("ps_o%d" % i, [128, 65], st=sta) for i in range(2)]
            ps_y = pst("ps_y", [64, 128], BF16, st=sta)
            S.dma("pool", nab[:], nab_d.rearrange("c k q -> k c q"), writes=["nab"])
            S.dma("pool", swm[:], swm_d.rearrange("c k q -> k c q"), writes=["swm"])
            S.dma("sp", snk[:], sink_d[:], writes=["snk"])
            S.op("dve", lambda e: e.memset(ones1[:], 1.0), writes=["ones1"])
            S.op("pe", lambda e: e.matmul(ps_o[0][:, 0:2], lhsT=ones1[:], rhs=snk[:], start=True, stop=True), reads=["ones1", "snk"], writes=["ps_o0"])
            S.op("act", lambda e: e.activation(out=snkB[:], in_=ps_o[0][:, 0:2], func=AF.Exp), reads=["ps_o0"], writes=["snkB"])
            acnt = [0]
            p2 = pass2_gen()
            p2cnt = [0]

            tiles = []

            def add_tile(qT, qtok_tile, keys, v1, sink_col, yb_, ybtok, ycol, after=None):
                tiles.append(dict(qT=qT, qt=qtok_tile, keys=keys, v1=v1, sink=sink_col, yb=yb_, ybtok=ybtok, ycol=ycol, after=after))

            def st_scores(t, i):
                keys = t["keys"]; nk = len(keys); qT = t["qT"]; qt = t["qt"]
                for j, (kT_, kt, bias) in enumerate(keys):
                    pp = ps_s[i][j // 4]; ptok = "ps_s%d_%d" % (i, j // 4)
                    S.op("pe", lambda e, pp=pp, j=j, kT_=kT_, kt=kt, bias=bias: e.matmul(
                        pp[:, j % 4, :], lhsT=kT_[:, kt * 128:(kt + 1) * 128], rhs=qT[:, qt * 128:(qt + 1) * 128],
                        start=True, stop=(bias is None)), writes=[ptok])
                    if bias is not None:
                        S.op("pe", lambda e, pp=pp, j=j, bias=bias: e.matmul(pp[:, j % 4, :], lhsT=ident8b[:], rhs=bias, start=False, stop=True),
                             reads=["nab", "swm", "ident8b"], writes=[ptok])
                n0 = min(4, nk)
                S.op("act", lambda e: e.activation(out=pT[i][:, 0:n0, :], in_=ps_s[i][0][:, 0:n0, :], func=AF.Exp, scale=0.125),
                     reads=["ps_s%d_0" % i], writes=["pT%d" % i])
                if nk > 4:
                    S.op("act", lambda e: e.activation(out=pT[i][:, 4:nk, :], in_=ps_s[i][1][:, 0:nk - 4, :], func=AF.Exp, scale=0.125),
                         reads=["ps_s%d_1" % i], writes=["pT%d" % i])

            def st_pv(t, i):
                keys = t["keys"]; nk = len(keys); v1 = t["v1"]; sink_col = t["sink"]
                for j, (kT_, kt, bias) in enumerate(keys):
                    S.op("pe", lambda e, j=j, kt=kt: e.matmul(ps_o[i][:], lhsT=pT[i][:, j, :], rhs=v1[:, kt, :], start=(j == 0), stop=(j == nk - 1)),
                         reads=["pT%d" % i], writes=["ps_o%d" % i])
                if sink_col is None:
                    S.op("dve", lambda e: e.reciprocal(out=rinv[:], in_=ps_o[i][:, 64:65]), reads=["ps_o%d" % i], writes=["rinv"])
                else:
                    S.op("dve", lambda e: e.tensor_tensor(out=rinv[:], in0=ps_o[i][:, 64:65], in1=snkB[:, sink_col:sink_col + 1], op=ALU.add),
                         reads=["ps_o%d" % i, "snkB"], writes=["rinv"])
                    S.op("dve", lambda e: e.reciprocal(out=rinv[:], in_=rinv[:]), reads=["rinv"], writes=["rinv"])
                S.op("dve", lambda e: e.tensor_scalar(out=ot[i][:], in0=ps_o[i][:, 0:64], scalar1=rinv[:, 0:1], scalar2=None, op0=ALU.mult),
                     reads=["ps_o%d" % i, "rinv"], writes=["ot%d" % i])

            def st_out(t, i):
                yb_ = t["yb"]; ycol = t["ycol"]
                S.op("pe", lambda e: e.transpose(out=ps_y[:], in_=ot[i][:], identity=identb[:]), reads=["ot%d" % i, "identb"], writes=["ps_y"])
                S.op("act", lambda e: e.activation(out=yb_[:, ycol:ycol + 128], in_=ps_y[:], func=AF.Copy), reads=["ps_y"], writes=[t["ybtok"]])
                if t["after"] is not None:
                    t["after"]()

            CT = CTX // 128
            ctx_keys_n = [(nkT, 0, None), (nkT, 1, None)]
            ctx_keys_s = [(skT, 0, None), (skT, 1, None)]
            ybc = 0
            heads_ = [("n", nqT, None, 64), ("s0", sq0T, 0, 128), ("s1", sq1T, 1, 192)]

            def mk_after(dst, src, ybtok, wtok):
                def f():
                    S.dma("sp", dst, src, reads=[ybtok], writes=[wtok])
                    p2cnt[0] += 1
                    if p2cnt[0] % 2 == 0:
                        next(p2, None)
                return f

            for (hk, qT, sink_col, yrow0) in heads_:
                if with_ctx_out:
                    yb_ = yblk[ybc % 2]; ybtok = "yblk%d" % (ybc % 2); ybc += 1
                    for qt in range(CT):
                        aft = mk_after(yT[yrow0:yrow0 + 64, 0:CTX], yb_[:, 0:CTX], ybtok, "yTc_" + hk) if qt == CT - 1 else None
                        if hk == "n":
                            add_tile(qT, qt, ctx_keys_n, nv1, None, yb_, ybtok, qt * 128, aft)
                        else:
                            add_tile(qT, qt, ctx_keys_s, sv1, sink_col, yb_, ybtok, qt * 128, aft)
                nql = n_lat // 128
                for qb in range(nql // 4):
                    yb_ = yblk[ybc % 2]; ybtok = "yblk%d" % (ybc % 2); ybc += 1
                    for qq in range(4):
                        pq = qb * 4 + qq
                        aft = mk_after(yT[yrow0:yrow0 + 64, CTX + qb * 512:CTX + (qb + 1) * 512], yb_[:], ybtok, "yT_%s_%d" % (hk, qb)) if qq == 3 else None
                        if hk == "n":
                            keys = [(nkT, CT + pk, nab[:, mi, :]) for (pk, mi) in cfgs[pq]] + ctx_keys_n
                            add_tile(qT, CT + pq, keys, nv1, None, yb_, ybtok, qq * 128, aft)
                        else:
                            keys = []
                            if pq > 0:
                                keys.append((skT, CT + pq - 1, swm[:, 0, :]))
                            keys.append((skT, CT + pq, None))
                            if pq < nql - 1:
                                keys.append((skT, CT + pq + 1, swm[:, 1, :]))
                            keys += ctx_keys_s
                            add_tile(qT, CT + pq, keys, sv1, sink_col, yb_, ybtok, qq * 128, aft)
            NTL = len(tiles)
            for n_ in range(NTL + 2):
                if n_ < NTL:
                    st_scores(tiles[n_], n_ % 2)
                if 0 <= n_ - 1 < NTL:
                    st_pv(tiles[n_ - 1], (n_ - 1) % 2)
                if 0 <= n_ - 2 < NTL:
                    st_out(tiles[n_ - 2], (n_ - 2) % 2)
            for _ in p2:
                pass
            S.barrier()

NEG = -30000.0
def rope_tables(n_lat=8192):
    t = np.arange(n_lat); row = (t // 64).astype(np.float32); col = (t % 64).astype(np.float32)
    nf = 16
    inv = (np.float32(10000.0) ** (-np.arange(nf, dtype=np.float32) / np.float32(nf))).astype(np.float32)
    C = np.zeros((64, n_lat), np.float32); S = np.zeros((64, n_lat), np.float32)
    for half, pos in ((0, row), (1, col)):
        ang = (pos[:, None] * inv[None, :]).astype(np.float32)
        c = np.cos(ang).astype(np.float32).T; s_ = np.sin(ang).astype(np.float32).T
        b = half * 32
        C[b:b+16] = c; C[b+16:b+32] = c
        S[b:b+16] = -s_; S[b+16:b+32] = s_
    return C, S
def rope_perm():
    p = np.arange(64)
    for b in (0, 32):
        p[b:b+16] = np.arange(b+16, b+32); p[b+16:b+32] = np.arange(b, b+16)
    return p
def na_index_tables(nrows=128):
    cfgs, mats = na_configs(nrows)
    idx = np.zeros((21, 128, 128), np.int64)
    k = np.arange(128); q = np.arange(128)
    for mi, (pq, pk) in enumerate(mats):
        krow = 2 * pk + k // 64; kcol = k % 64
        qrow = 2 * pq + q // 64; qcol = q % 64
        rs = np.clip(qrow - 4, 0, nrows - 8); cs = np.clip(qcol - 8, 0, 48)
        valid = (krow[:, None] >= rs[None]) & (krow[:, None] < rs[None] + 8) & (kcol[:, None] >= cs[None]) & (kcol[:, None] < cs[None] + 16)
        ridx = krow[:, None] - qrow[None] + 7
        coff = np.clip(kcol[:, None] - qcol[None] + 15, 0, 30)
        ii = np.clip(ridx, 0, 14) * 31 + coff
        idx[mi] = np.where(valid, ii, 15 * 31)
    return idx
def const_tables():
    ident = np.eye(128, dtype=np.float32)
    k = np.arange(128)
    same = (k[:, None] // 32) == (k[None] // 32)
    hmF = (same & (k[:, None] <= k[None])).astype(np.float32)
    hmB = (same & (k[:, None] >= k[None])).astype(np.float32)
    rm = np.ones((128, 512), np.float32); rm[:, ::32] = 0.0
    cst = np.concatenate([ident, 8 * ident, hmF, hmB, rm], axis=1)
    vmask = (k[:, None] // 32 == np.arange(4)[None]).astype(np.float32)
    swm = np.zeros((2, 128, 128), np.float32)
    swm[0] = np.where(k[:, None] >= k[None], 0.0, NEG)
    swm[1] = np.where(k[:, None] <= k[None], 0.0, NEG)
    return cst, vmask, swm
_NAIDX = None
def core_inputs_a(inp, l, b, j, xT_b, n_lat=8192):
    global _NAIDX
    if _NAIDX is None: _NAIDX = na_index_tables(n_lat // 64)
    w = inp['w_in'][l]
    perm = rope_perm()
    def cols(base, width=64, idx=j): return w[:, base + width * idx: base + width * (idx + 1)]
    sq0 = w[:, 2048 + 128 * j: 2048 + 128 * j + 64]; sq1 = w[:, 2048 + 128 * j + 64: 2048 + 128 * j + 128]
    n = j // 2
    sk = w[:, 2560 + 64 * n: 2560 + 64 * n + 64]; sv = w[:, 2688 + 64 * n: 2688 + 64 * n + 64]
    wcore = np.concatenate([cols(0), cols(256), cols(512), cols(1024), cols(1280), cols(1536),
                            sq0, sq0[:, perm], sq1, sq1[:, perm], sk, sk[:, perm],
                            cols(768), cols(1792), sv], axis=1)
    c = inp['c'][b]; cctx = inp['c_ctx']
    ccol = np.concatenate([c.reshape(8, 128).T, cctx.reshape(8, 128).T], axis=1)
    lbl = np.stack([inp['lb_logits'][0, 0, 64*j:64*j+64], inp['lb_logits'][1, 0, 64*j:64*j+64],
                    inp['lb_logits'][0, 1, 64*j:64*j+64], inp['lb_logits'][1, 1, 64*j:64*j+64]], axis=1)
    rpb_ext = np.concatenate([inp['na_rpb'][l, j].reshape(-1), np.array([NEG], np.float32)])
    nab = rpb_ext[_NAIDX]
    C, S = rope_tables(n_lat)
    cst, vmask, swm = const_tables()
    f = lambda a: np.ascontiguousarray(a, dtype=np.float32)
    return {"xT": f(xT_b), "wcore": f(wcore), "ccol": f(ccol), "wada": f(inp['w_ada'][l][:, :2048]),
            "badac": f(inp['b_ada'][l][:2048].reshape(16, 128).T), "lbl": f(lbl), "gnorm": f(inp['hgrn_norm'][l][:, None]),
            "nab": f(nab), "swm": f(swm), "sink": f(inp['swa_sink'][l][None, 2*j:2*j+2]), "ropeC": f(C), "ropeS": f(S),
            "cst": f(cst), "vmask": f(vmask)}


ALPHA = float((2.0 * 2) ** 0.25)
LN_EPS = 1e-5
NEGBIG = 1e30


def bc_mid(ap2d, n):
    p, k = ap2d.shape
    return ap2d.unsqueeze(2).broadcast_to([p, k, n])


def emit_phase_b(nc, S, io, has_ctx, uid, n_lat_tiles=16, n_exp=16):
    NTI = n_lat_tiles + (1 if has_ctx else 0)
    NT = NTI * 128
    D = 1024
    stop = 99
    yT_lat = io["yT_lat"]; yT_ctx = io.get("yT_ctx"); x = io["x"]; ccol = io["ccol"]; wada = io["wada"]; bada = io["bada"]; lnp = io["lnp"]
    wout = io["wout"]; wr = io["wr"]; rb = io["rb"]; wg = io["wg"]; wu = io["wu"]; wd = io["wd"]; ident_d = io["ident"]
    xo = io["xo"]; x1s = io["x1s"]; xT_lat_o = io.get("xT_lat_o"); xT_ctx_o = io.get("xT_ctx_o")
    es = contextlib.ExitStack()
    with es:
        sb = lambda name, shape, dt=F32, st=es: st.enter_context(nc.sbuf_tensor(uid + name, shape, dt))
        pst = lambda name, shape, dt=F32, st=es: st.enter_context(nc.psum_tensor(uid + name, shape, dt))

        h2T = sb("h2T", [128, 8, NT], BF16)
        gates = sb("gates", [128, NTI, 16])
        G2 = sb("G2", [128, D]); G2c = sb("G2c", [128, D]); LN2G = sb("LN2G", [128, D]); LN2B = sb("LN2B", [128, D])
        ident = sb("ident_sb", [128, 128])
        ones1 = sb("ones1", [1, 128])
        epsT = sb("epsT", [128, 1])
        S.dma("sp", ident[:], ident_d[:], writes=["ident"])
        S.op("dve", lambda e: e.memset(ones1[:], 1.0), writes=["ones1"])
        S.op("dve", lambda e: e.memset(epsT[:], LN_EPS), writes=["epsT"])

        st1 = contextlib.ExitStack()
        with st1:
            GA = sb("GA", [128, D], st=st1); BA = sb("BA", [128, D], st=st1)
            A2 = sb("A2", [128, D], st=st1); B2 = sb("B2", [128, D], st=st1)
            A2c = sb("A2c", [128, D], st=st1); B2c = sb("B2c", [128, D], st=st1)
            G1 = sb("G1", [128, D], st=st1); G1c = sb("G1c", [128, D], st=st1)
            st0 = contextlib.ExitStack()
            with st0:
                cc = sb("cc", [128, 16], st=st0)
                scc = sb("scc", [128, 16], st=st0)
                cb = sb("cb", [128, 16, 128], st=st0)
                wa = [sb("wa%d" % i, [128, 8, 512], st=st0) for i in range(2)]
                bd = [sb("bd%d" % i, [1, 512], st=st0) for i in range(2)]
                SC2 = sb("SC2", [128, D], st=st0); SC2c = sb("SC2c", [128, D], st=st0)
                SH2 = sb("SH2", [128, D], st=st0); SH2c = sb("SH2c", [128, D], st=st0)
                LN1G = sb("LN1G", [128, D], st=st0); LN1B = sb("LN1B", [128, D], st=st0)
                psm = [pst("psm%d" % i, [128, 512], st=st0) for i in range(4)]
                S.dma("sp", cc[:], ccol[:], writes=["cc"])
                S.op("act", lambda e: e.activation(out=scc[:], in_=cc[:], func=AF.Silu), reads=["cc"], writes=["scc"])
                S.op("dve", lambda e: e.tensor_copy(out=cb[:], in_=bc_mid(scc[:], 128)), reads=["scc"], writes=["cb"])
                wada_v = wada.rearrange("(k p) n -> p k n", p=128)
                dests = [(G1, G1c), (SH2, SH2c), (SC2, SC2c), (G2, G2c)]
                pi = 0
                hi = 0
                for ch in range(4):
                    for half in range(2):
                        cs = ch * 1024 + half * 512
                        wb = wa[hi % 2]; wtok = "wa%d" % (hi % 2); bdt = bd[hi % 2]; btok = "bd%d" % (hi % 2); hi += 1
                        S.dma("sp", wb[:], wada_v[:, :, cs:cs + 512], writes=[wtok])
                        S.dma("sp", bdt[:], bada[:, cs:cs + 512], writes=[btok])
                        for which in range(2):
                            p_ = psm[pi % 4]; ptok = "psm%d" % (pi % 4); pi += 1
                            for k in range(8):
                                S.op("pe", lambda e, k=k, p_=p_, which=which, wb=wb: e.matmul(
                                    p_[:], lhsT=cb[:, which * 8 + k, :], rhs=wb[:, k, :],
                                    start=(k == 0), stop=False),
                                    reads=["cb", wtok], writes=[ptok])
                            S.op("pe", lambda e, p_=p_, bdt=bdt: e.matmul(p_[:], lhsT=ones1[:], rhs=bdt[:],
                                                                    start=False, stop=True),
                                 reads=["ones1", btok], writes=[ptok])
                            dst = dests[ch][which]
                            S.op("act", lambda e, dst=dst, p_=p_, half=half: e.activation(
                                out=dst[:, half * 512:(half + 1) * 512], in_=p_[:], func=AF.Copy),
                                reads=[ptok], writes=["mod%d_%d" % (ch, which)])
                if stop <= -1:
                    pass
                lnd = [LN1G, LN1B, LN2G, LN2B]
                for j in range(4):
                    for half in range(2):
                        p_ = psm[pi % 4]; ptok = "psm%d" % (pi % 4); pi += 1
                        cs = j * 1024 + half * 512
                        bdt = bd[hi % 2]; btok = "bd%d" % (hi % 2); hi += 1
                        S.dma("sp", bdt[:], lnp[:, cs:cs + 512], writes=[btok])
                        S.op("pe", lambda e, p_=p_, bdt=bdt: e.matmul(p_[:], lhsT=ones1[:], rhs=bdt[:],
                                                                start=True, stop=True),
                             reads=["ones1", btok], writes=[ptok])
                        S.op("act", lambda e, j=j, p_=p_, half=half: e.activation(
                            out=lnd[j][:, half * 512:(half + 1) * 512], in_=p_[:], func=AF.Copy),
                            reads=[ptok], writes=["lnd%d" % j])
                if stop <= -0.5:
                    pass
                S.barrier()
                S.op("dve", lambda e: e.tensor_scalar(out=GA[:], in0=LN1G[:], scalar1=ALPHA, scalar2=None, op0=ALU.mult))
                S.op("dve", lambda e: e.tensor_scalar(out=BA[:], in0=LN1B[:], scalar1=ALPHA, scalar2=None, op0=ALU.mult))
                for ci, (sc, sh, a2, b2) in enumerate(((SC2, SH2, A2, B2), (SC2c, SH2c, A2c, B2c))):
                    S.op("dve", lambda e, sc=sc: e.tensor_scalar(out=sc[:], in0=sc[:], scalar1=1.0, scalar2=None, op0=ALU.add),
                         writes=["sc1p%d" % ci])
                    S.op("dve", lambda e, sc=sc, a2=a2: e.tensor_tensor(out=a2[:], in0=LN1G[:], in1=sc[:], op=ALU.mult),
                         reads=["sc1p%d" % ci])
                    S.op("dve", lambda e, sc=sc, b2=b2: e.tensor_tensor(out=b2[:], in0=LN1B[:], in1=sc[:], op=ALU.mult),
                         reads=["sc1p%d" % ci], writes=["b2t%d" % ci])
                    S.op("dve", lambda e, sh=sh, b2=b2: e.tensor_tensor(out=b2[:], in0=b2[:], in1=sh[:], op=ALU.add),
                         reads=["b2t%d" % ci], writes=["b2t%d" % ci])
                S.barrier()
            if stop <= 0:
                pass
            wo = sb("wo", [128, 8, D], BF16, st=st1)
            wrt = sb("wrt", [128, 8, 16], st=st1)
            rbB = sb("rbB", [128, 16], st=st1)
            rb1 = sb("rb1", [1, 16], st=st1)
            xt = [sb("xt%d" % i, [128, D], st=st1) for i in range(2)]
            yb = [sb("yb%d" % i, [128, 8, 512], BF16, st=st1) for i in range(2)]
            uu = [sb("uu%d" % i, [128, D], st=st1) for i in range(2)]
            xn = [sb("xn%d" % i, [128, D], st=st1) for i in range(2)]
            h2 = [sb("h2_%d" % i, [128, D], st=st1) for i in range(2)]
            x1a = [sb("x1a%d" % i, [128, D], st=st1) for i in range(2)]
            h2f = sb("h2f", [128, 8, 128], st=st1)
            stt = sb("stt", [128, 2, 6], st=st1)
            mv = sb("mv", [128, 2], st=st1)
            rstd = sb("rstd", [128, 1], st=st1)
            nmr = sb("nmr", [128, 1], st=st1)
            rt = [sb("rt%d" % i, [128, 16], st=st1) for i in range(6)]
            rs4 = [sb("rs4_%d" % i, [128, 4], st=st1) for i in range(4)]
            r1 = [sb("r1_%d" % i, [128, 1], st=st1) for i in range(2)]
            ps_y = [pst("ps_y%d" % i, [128, D], st=st1) for i in range(2)]
            ps_t = pst("ps_t", [128, 8, 128], st=st1)
            ps_r = pst("ps_r", [128, 16], st=st1)

            S.dma("pool", wo[:], wout.rearrange("(k p) n -> p k n", p=128), writes=["wo"])
            S.dma("sp", wrt[:], wr.rearrange("(k p) n -> p k n", p=128), writes=["wrt"])
            S.dma("sp", rb1[:], rb[:], writes=["rb1"])
            S.op("pe", lambda e: e.matmul(ps_r[:], lhsT=ones1[:], rhs=rb1[:], start=True, stop=True),
                 reads=["ones1", "rb1"], writes=["ps_r"])
            S.op("act", lambda e: e.activation(out=rbB[:], in_=ps_r[:], func=AF.Copy), reads=["ps_r"], writes=["rbB"])
            yT_v = yT_lat.rearrange("(k p) t -> p k t", p=128)
            for i in range(NTI):
                is_ctx = has_ctx and i == NTI - 1
                b2_ = i % 2
                blk = i // 4
                if i % 4 == 0:
                    ntb = min(4, NTI - i)
                    if is_ctx:
                        S.dma("pool", yb[blk % 2][:, :, :64], yT_ctx.rearrange("(k p) t -> p k t", p=128),
                              writes=["yb%d" % (blk % 2)])
                    else:
                        S.dma("pool", yb[blk % 2][:, :, :ntb * 128], yT_v[:, :, i * 128:(i + ntb) * 128],
                              writes=["yb%d" % (blk % 2)])
                S.dma("sp", xt[b2_][:], x[i * 128:(i + 1) * 128, :], writes=["xt%d" % b2_])
                ybt = yb[blk % 2]
                off = (i % 4) * 128
                for half in range(2):
                    for k in range(8):
                        S.op("pe", lambda e, k=k, half=half, ybt=ybt, off=off, b2_=b2_: e.matmul(
                            ps_y[b2_][:, half * 512:(half + 1) * 512], lhsT=ybt[:, k, off:off + 128],
                            rhs=wo[:, k, half * 512:(half + 1) * 512], start=(k == 0), stop=(k == 7)),
                            reads=["yb%d" % (blk % 2), "wo"], writes=["ps_y%d" % b2_])
                if stop <= 0.1:
                    pass
                g1t = G1c if is_ctx else G1
                a2t = A2c if is_ctx else A2
                b2t = B2c if is_ctx else B2
                u_ = uu[b2_]; utok = "uu%d" % b2_
                S.op("dve", lambda e, u_=u_, b2_=b2_, g1t=g1t: e.tensor_tensor(out=u_[:], in0=ps_y[b2_][:], in1=g1t[:], op=ALU.mult),
                     reads=["ps_y%d" % b2_], writes=[utok])
                S.op("dve", lambda e, u_=u_, b2_=b2_: e.scalar_tensor_tensor(out=u_[:], in0=xt[b2_][:], scalar=ALPHA, in1=u_[:],
                                                                   op0=ALU.mult, op1=ALU.add),
                     reads=["xt%d" % b2_, utok], writes=[utok])
                if stop <= 0.2:
                    pass
                for hh in range(2):
                    S.op("dve", lambda e, hh=hh, u_=u_: e.bn_stats(out=stt[:, hh, :], in_=u_[:, hh * 512:(hh + 1) * 512]),
                         reads=[utok], writes=["stt%d" % hh])
                S.op("dve", lambda e: e.bn_aggr(out=mv[:], in_=stt[:].rearrange("p a b -> p (a b)")),
                     reads=["stt0", "stt1"], writes=["mv"])
                S.op("act", lambda e: e.activation(out=rstd[:], in_=mv[:, 1:2], func=AF.Sqrt, bias=epsT[:, 0:1], scale=1.0),
                     reads=["mv", "epsT"], writes=["rstd"])
                S.op("dve", lambda e: e.reciprocal(out=rstd[:], in_=rstd[:]), reads=["rstd"], writes=["rstd"])
                S.op("dve", lambda e: e.scalar_tensor_tensor(out=nmr[:], in0=mv[:, 0:1], scalar=-1.0, in1=rstd[:],
                                                             op0=ALU.mult, op1=ALU.mult),
                     reads=["mv", "rstd"], writes=["nmr"])
                if stop <= 0.3:
                    pass
                xn_ = xn[b2_]; xtok = "xn%d" % b2_
                S.op("act", lambda e, xn_=xn_, u_=u_: e.activation(out=xn_[:], in_=u_[:], func=AF.Identity,
                                                                   bias=nmr[:, 0:1], scale=rstd[:, 0:1]),
                     reads=[utok, "nmr", "rstd"], writes=[xtok])
                if stop <= 0.4:
                    pass
                xa_ = x1a[b2_]; xatok = "x1a%d" % b2_
                S.op("pool", lambda e, xa_=xa_, xn_=xn_: e.tensor_tensor(out=xa_[:], in0=xn_[:], in1=GA[:], op=ALU.mult),
                     reads=[xtok], writes=[xatok])
                S.op("pool", lambda e, xa_=xa_: e.tensor_tensor(out=xa_[:], in0=xa_[:], in1=BA[:], op=ALU.add),
                     reads=[xatok], writes=[xatok])
                S.dma("sp", x1s[i * 128:(i + 1) * 128, :], xa_[:], reads=[xatok], writes=["x1s%d" % i])
                h_ = h2[b2_]; htok = "h2_%d" % b2_
                S.op("dve", lambda e, h_=h_, xn_=xn_, a2t=a2t: e.tensor_tensor(out=h_[:], in0=xn_[:], in1=a2t[:], op=ALU.mult),
                     reads=[xtok], writes=[htok])
                S.op("dve", lambda e, h_=h_, b2t=b2t: e.tensor_tensor(out=h_[:], in0=h_[:], in1=b2t[:], op=ALU.add),
                     reads=[htok], writes=[htok])
                if stop <= 0.5:
                    pass
                for k in range(8):
                    S.op("pe", lambda e, k=k, h_=h_: e.transpose(out=ps_t[:, k, :], in_=h_[:, k * 128:(k + 1) * 128], identity=ident[:]),
                         reads=[htok, "ident"], writes=["ps_t"])
                S.op("dve", lambda e: e.tensor_copy(out=h2f[:], in_=ps_t[:]), reads=["ps_t"], writes=["h2f"])
                S.op("act", lambda e, i=i: e.activation(out=h2T[:, :, i * 128:(i + 1) * 128], in_=h2f[:], func=AF.Copy),
                     reads=["h2f"], writes=["h2T_%d" % i])
                if stop <= 0.6:
                    pass
                for k in range(8):
                    S.op("pe", lambda e, k=k: e.matmul(ps_r[:], lhsT=h2f[:, k, :], rhs=wrt[:, k, :], start=(k == 0), stop=(k == 7)),
                         reads=["h2f", "wrt"], writes=["ps_r"])
                if stop <= 0.7:
                    pass
                s_, sel_, msk, t16, w_, selm = rt
                m1, m2, grp, ing = rs4
                gm, wsum = r1
                V3 = lambda a: a[:].rearrange("p (g i) -> p g i", g=4)
                S.op("act", lambda e: e.activation(out=s_[:], in_=ps_r[:], func=AF.Sigmoid), reads=["ps_r"], writes=["r_s"])
                S.op("dve", lambda e: e.tensor_tensor(out=sel_[:], in0=s_[:], in1=rbB[:], op=ALU.add), reads=["r_s", "rbB"], writes=["r_sel"])
                S.op("dve", lambda e: e.tensor_reduce(out=m1[:], in_=V3(sel_), axis=AX.X, op=ALU.max), reads=["r_sel"], writes=["r_m1"])
                S.op("dve", lambda e: e.tensor_tensor(out=V3(msk), in0=V3(sel_), in1=bc_mid(m1[:], 4), op=ALU.is_ge),
                     reads=["r_sel", "r_m1"], writes=["r_msk"])
                S.op("dve", lambda e: e.scalar_tensor_tensor(out=t16[:], in0=msk[:], scalar=-NEGBIG, in1=sel_[:], op0=ALU.mult, op1=ALU.add),
                     reads=["r_msk", "r_sel"], writes=["r_t16"])
                S.op("dve", lambda e: e.tensor_reduce(out=m2[:], in_=V3(t16), axis=AX.X, op=ALU.max), reads=["r_t16"], writes=["r_m2"])
                S.op("dve", lambda e: e.tensor_tensor(out=grp[:], in0=m1[:], in1=m2[:], op=ALU.add), reads=["r_m1", "r_m2"], writes=["r_grp"])
                S.op("dve", lambda e: e.tensor_reduce(out=gm[:], in_=grp[:], axis=AX.X, op=ALU.max), reads=["r_grp"], writes=["r_gm"])
                S.op("dve", lambda e: e.tensor_scalar(out=ing[:], in0=grp[:], scalar1=gm[:, 0:1], scalar2=None, op0=ALU.is_ge),
                     reads=["r_grp", "r_gm"], writes=["r_ing"])
                S.op("dve", lambda e: e.tensor_tensor(out=V3(selm), in0=V3(sel_), in1=bc_mid(m2[:], 4), op=ALU.is_ge),
                     reads=["r_sel", "r_m2"], writes=["r_selm"])
                S.op("dve", lambda e: e.tensor_tensor(out=V3(selm), in0=V3(selm), in1=bc_mid(ing[:], 4), op=ALU.mult),
                     reads=["r_selm", "r_ing"], writes=["r_selm"])
                S.op("dve", lambda e: e.tensor_tensor(out=w_[:], in0=s_[:], in1=selm[:], op=ALU.mult), reads=["r_s", "r_selm"], writes=["r_w"])
                S.op("dve", lambda e: e.tensor_reduce(out=wsum[:], in_=w_[:], axis=AX.X, op=ALU.add), reads=["r_w"], writes=["r_ws"])
                S.op("dve", lambda e: e.reciprocal(out=wsum[:], in_=wsum[:]), reads=["r_ws"], writes=["r_ws"])
                S.op("dve", lambda e, i=i: e.tensor_scalar(out=gates[:, i, :], in0=w_[:], scalar1=wsum[:, 0:1], scalar2=None, op0=ALU.mult),
                     reads=["r_w", "r_ws"], writes=["gates%d" % i])
            S.barrier()
        if stop <= 1:
            pass
        st2 = contextlib.ExitStack()
        with st2:
            acc = sb("acc", [128, NTI, D], st=st2)
            st2w = contextlib.ExitStack()
            wgb = [sb("wgb%d" % i, [128, 8, 512], BF16, st=st2w) for i in range(2)]
            wub = [sb("wub%d" % i, [128, 8, 512], BF16, st=st2w) for i in range(2)]
            wdb = [sb("wdb%d" % i, [128, 4, D], BF16, st=st2w) for i in range(2)]
            sg = [sb("sg%d" % i, [128, 512], st=st2w) for i in range(2)]
            heT = [sb("heT%d" % i, [128, 4, 512], BF16, st=st2w) for i in range(2)]
            ps_g = [pst("ps_g%d" % i, [128, 512], st=st2w) for i in range(2)]
            ps_u = [pst("ps_u%d" % i, [128, 512], st=st2w) for i in range(2)]
            ps_d = [pst("ps_d%d" % i, [128, D], st=st2w) for i in range(2)]
            blocks = []
            i = 0
            while i < NTI:
                n = min(4, NTI - i)
                blocks.append((i, n))
                i += n

            def load_w(e_):
                p = e_ % 2
                S.dma("pool", wgb[p][:], wg[e_].rearrange("(k p) n -> p k n", p=128), writes=["wgb%d" % p])
                S.dma("pool", wub[p][:], wu[e_].rearrange("(k p) n -> p k n", p=128), writes=["wub%d" % p])
                S.dma("pool", wdb[p][:], wd[e_].rearrange("(k p) n -> p k n", p=128), writes=["wdb%d" % p])

            load_w(0)
            if n_exp > 1:
                load_w(1)
            fcn = [0]; dn = [0]
            steps = [(e_, bi_) for e_ in range(n_exp) for bi_ in range(len(blocks))]

            def stage_G(k):
                e_, bi_ = steps[k]
                p = e_ % 2
                t0, ntb = blocks[bi_]
                hb = k % 2
                ncol = ntb * 128
                for fc in range(4):
                    q = fcn[0] % 2; fcn[0] += 1
                    for kk in range(8):
                        S.op("pe", lambda e, kk=kk, fc=fc, q=q, p=p, t0=t0, ncol=ncol: e.matmul(
                            ps_g[q][:, :ncol], lhsT=wgb[p][:, kk, fc * 128:(fc + 1) * 128],
                            rhs=h2T[:, kk, t0 * 128:t0 * 128 + ncol], start=(kk == 0), stop=(kk == 7)),
                            reads=["wgb%d" % p], writes=["ps_g%d" % q])
                    for kk in range(8):
                        S.op("pe", lambda e, kk=kk, fc=fc, q=q, p=p, t0=t0, ncol=ncol: e.matmul(
                            ps_u[q][:, :ncol], lhsT=wub[p][:, kk, fc * 128:(fc + 1) * 128],
                            rhs=h2T[:, kk, t0 * 128:t0 * 128 + ncol], start=(kk == 0), stop=(kk == 7)),
                            reads=["wub%d" % p], writes=["ps_u%d" % q])
                    S.op("act", lambda e, q=q, ncol=ncol: e.activation(out=sg[q][:, :ncol], in_=ps_g[q][:, :ncol], func=AF.Silu),
                         reads=["ps_g%d" % q], writes=["sg%d" % q])
                    S.op("dve", lambda e, q=q, ncol=ncol, hb=hb, fc=fc: e.tensor_tensor(
                        out=heT[hb][:, fc, :ncol], in0=ps_u[q][:, :ncol], in1=sg[q][:, :ncol], op=ALU.mult),
                        reads=["ps_u%d" % q, "sg%d" % q], writes=["heT%d_%d" % (hb, fc)])

            def stage_D(k):
                e_, bi_ = steps[k]
                p = e_ % 2
                t0, ntb = blocks[bi_]
                hb = k % 2
                for tt in range(ntb):
                    ti = t0 + tt
                    dq = dn[0] % 2; dn[0] += 1
                    for half in range(2):
                        for fc in range(4):
                            S.op("pe", lambda e, fc=fc, half=half, dq=dq, hb=hb, tt=tt, p=p: e.matmul(
                                ps_d[dq][:, half * 512:(half + 1) * 512], lhsT=heT[hb][:, fc, tt * 128:(tt + 1) * 128],
                                rhs=wdb[p][:, fc, half * 512:(half + 1) * 512], start=(fc == 0), stop=(fc == 3)),
                                reads=["heT%d_%d" % (hb, fc), "wdb%d" % p], writes=["ps_d%d" % dq])
                    if e_ == 0:
                        S.op("dve", lambda e, ti=ti, dq=dq, e_=e_: e.tensor_scalar(
                            out=acc[:, ti, :], in0=ps_d[dq][:], scalar1=gates[:, ti, e_:e_ + 1], scalar2=None, op0=ALU.mult),
                            reads=["ps_d%d" % dq], writes=["acc%d" % ti])
                    else:
                        S.op("dve", lambda e, ti=ti, dq=dq, e_=e_: e.scalar_tensor_tensor(
                            out=acc[:, ti, :], in0=ps_d[dq][:], scalar=gates[:, ti, e_:e_ + 1], in1=acc[:, ti, :],
                            op0=ALU.mult, op1=ALU.add),
                            reads=["ps_d%d" % dq, "acc%d" % ti], writes=["acc%d" % ti])
                if bi_ == len(blocks) - 1 and e_ + 2 < n_exp:
                    load_w(e_ + 2)

            for k in range(len(steps) + 1):
                if k < len(steps):
                    stage_G(k)
                if k >= 1:
                    stage_D(k - 1)
            S.barrier()
            st2w.close()
            xtT = sb("xtT", [128, 8, 128], st=st2)
            ps_x = pst("ps_x", [128, 8, 128], st=st2)
            xr = [sb("xr%d" % i, [128, D], st=st2) for i in range(2)]
            u3 = [sb("u3_%d" % i, [128, D], st=st2) for i in range(2)]
            stt3 = sb("stt3", [128, 2, 6], st=st2)
            mv3 = sb("mv3", [128, 2], st=st2)
            rstd3 = sb("rstd3", [128, 1], st=st2)
            nmr3 = sb("nmr3", [128, 1], st=st2)
            for i in range(NTI):
                is_ctx = has_ctx and i == NTI - 1
                b2_ = i % 2
                g2t = G2c if is_ctx else G2
                S.dma("sp", xr[b2_][:], x1s[i * 128:(i + 1) * 128, :], reads=["x1s%d" % i], writes=["xr%d" % b2_])
                u_ = u3[b2_]; utok = "u3_%d" % b2_
                S.op("dve", lambda e, u_=u_, i=i, g2t=g2t: e.tensor_tensor(out=u_[:], in0=acc[:, i, :], in1=g2t[:], op=ALU.mult),
                     writes=[utok])
                S.op("dve", lambda e, u_=u_, b2_=b2_: e.tensor_tensor(out=u_[:], in0=u_[:], in1=xr[b2_][:], op=ALU.add),
                     reads=[utok, "xr%d" % b2_], writes=[utok])
                for hh in range(2):
                    S.op("dve", lambda e, hh=hh, u_=u_: e.bn_stats(out=stt3[:, hh, :], in_=u_[:, hh * 512:(hh + 1) * 512]),
                         reads=[utok], writes=["stt3_%d" % hh])
                S.op("dve", lambda e: e.bn_aggr(out=mv3[:], in_=stt3[:].rearrange("p a b -> p (a b)")),
                     reads=["stt3_0", "stt3_1"], writes=["mv3"])
                S.op("act", lambda e: e.activation(out=rstd3[:], in_=mv3[:, 1:2], func=AF.Sqrt, bias=epsT[:, 0:1], scale=1.0),
                     reads=["mv3"], writes=["rstd3"])
                S.op("dve", lambda e: e.reciprocal(out=rstd3[:], in_=rstd3[:]), reads=["rstd3"], writes=["rstd3"])
                S.op("dve", lambda e: e.scalar_tensor_tensor(out=nmr3[:], in0=mv3[:, 0:1], scalar=-1.0, in1=rstd3[:],
                                                             op0=ALU.mult, op1=ALU.mult),
                     reads=["mv3", "rstd3"], writes=["nmr3"])
                S.op("act", lambda e, u_=u_: e.activation(out=u_[:], in_=u_[:], func=AF.Identity, bias=nmr3[:, 0:1], scale=rstd3[:, 0:1]),
                     reads=[utok, "nmr3", "rstd3"], writes=[utok])
                S.op("pool", lambda e, u_=u_: e.tensor_tensor(out=u_[:], in0=u_[:], in1=LN2G[:], op=ALU.mult), reads=[utok], writes=[utok])
                S.op("pool", lambda e, u_=u_: e.tensor_tensor(out=u_[:], in0=u_[:], in1=LN2B[:], op=ALU.add), reads=[utok], writes=[utok])
                S.dma("sp", xo[i * 128:(i + 1) * 128, :], u_[:], reads=[utok], writes=["xo%d" % i])
                if xT_lat_o is not None:
                    for k in range(8):
                        S.op("pe", lambda e, k=k, u_=u_: e.transpose(out=ps_x[:, k, :], in_=u_[:, k * 128:(k + 1) * 128], identity=ident[:]),
                             reads=[utok, "ident"], writes=["ps_x"])
                    S.op("dve", lambda e: e.tensor_copy(out=xtT[:], in_=ps_x[:]), reads=["ps_x"], writes=["xtT"])
                    if is_ctx:
                        S.dma("sp", xT_ctx_o.rearrange("(k p) t -> p k t", p=128), xtT[:, :, 0:64], reads=["xtT"], writes=["xTo%d" % i])
                    else:
                        S.dma("sp", xT_lat_o.rearrange("(k p) t -> p k t", p=128)[:, :, i * 128:(i + 1) * 128], xtT[:], reads=["xtT"], writes=["xTo%d" % i])
            S.barrier()


def build_fused(n_lat=8192):
    T = CTX + n_lat
    NQ = n_lat // 4
    NLT = NQ // 128
    NTB = (NLT + 1) * 128
    nc = bass.Bass("TRN2", target_bir_lowering=False)
    def din(name, shape):
        return nc.dram_tensor(name, shape, F32, kind="ExternalInput").ap()
    def scr(name, shape):
        return nc.dram_tensor(name, shape, F32, kind="Internal").ap()
    I = {}
    I["xT0"] = din("xT0", [1024, T]); I["xq0"] = din("xq0", [4, NTB, 1024])
    I["wcore"] = din("wcore", [2, 4, 1024, NCOL]); I["ccol"] = din("ccol", [128, 16])
    I["wadaA"] = din("wadaA", [2, 1024, 2048]); I["badac"] = din("badac", [2, 128, 16])
    I["lbl"] = din("lbl", [4, 64, 4]); I["gnorm"] = din("gnorm", [2, 64, 1]); I["nab"] = din("nab", [2, 4, 21, 128, 128])
    I["swm"] = din("swm", [2, 128, 128]); I["sink"] = din("sink", [2, 4, 1, 2])
    I["ropeC"] = din("ropeC", [64, n_lat]); I["ropeS"] = din("ropeS", [64, n_lat])
    I["cst"] = din("cst", [128, 1024]); I["vmask"] = din("vmask", [128, 4])
    I["wadaB"] = din("wadaB", [2, 1024, 4096]); I["badaB"] = din("badaB", [2, 1, 4096]); I["lnp"] = din("lnp", [2, 1, 4096])
    I["wout"] = din("wout", [2, 1024, 1024]); I["wr"] = din("wr", [1024, 16]); I["rb"] = din("rb", [1, 16])
    I["wg"] = din("wg", [2, 16, 1024, 512]); I["wu"] = din("wu", [2, 16, 1024, 512]); I["wd"] = din("wd", [2, 16, 512, 1024])
    I["ident"] = din("ident", [128, 128])
    out = nc.dram_tensor("out", [NQ, 1024], F32, kind="ExternalOutput").ap()
    Y = scr("Y", [1024, T]); xT1 = scr("xT1", [1024, T]); X1 = scr("X1", [4, NTB, 1024]); x1s = scr("x1s", [NTB, 1024]); ofs = scr("ofs", [64, T])
    UB = scr("UB", [64, 1 + n_lat // 512, 1024]); SG = scr("SG", [64, T])
    QB = nc.dram_tensor("QB", [64, T], BF16, kind="Internal").ap()
    es = contextlib.ExitStack()
    with es:
        S = Sched(nc, es)
        for l in range(2):
            last = l == 1
            xTsrc = I["xT0"] if l == 0 else xT1
            for j in range(4):
                io = {"xT": xTsrc, "wcore": I["wcore"][l, j], "ccol": I["ccol"], "wada": I["wadaA"][l], "badac": I["badac"][l],
                      "lbl": I["lbl"][j], "gnorm": I["gnorm"][l], "nab": I["nab"][l, j], "swm": I["swm"], "sink": I["sink"][l, j],
                      "ropeC": I["ropeC"], "ropeS": I["ropeS"], "cst": I["cst"], "vmask": I["vmask"],
                      "yT": Y[256 * j:256 * (j + 1), :], "ofs": ofs, "UB": UB, "QB": QB, "SG": SG}
                emit_phase_a(nc, S, io, l, not last, "a%d%d_" % (l, j), n_lat=n_lat)
            if l == 0:
                for q in range(4):
                    io = {"yT_lat": Y[:, CTX + q * NQ:CTX + (q + 1) * NQ], "yT_ctx": Y[:, q * 64:(q + 1) * 64],
                          "x": I["xq0"][q], "ccol": I["ccol"], "wada": I["wadaB"][l], "bada": I["badaB"][l],
                          "lnp": I["lnp"][l], "wout": I["wout"][l], "wr": I["wr"], "rb": I["rb"], "wg": I["wg"][l], "wu": I["wu"][l], "wd": I["wd"][l],
                          "ident": I["ident"], "x1s": x1s[0:NTB, :], "xo": X1[q],
                          "xT_lat_o": xT1[:, CTX + q * NQ:CTX + (q + 1) * NQ], "xT_ctx_o": xT1[:, q * 64:(q + 1) * 64]}
                    emit_phase_b(nc, S, io, True, "b%d%d_" % (l, q), n_lat_tiles=NLT)
            else:
                q_pool = nc.gpsimd.partition_id() % 4
                q_sp = nc.sync.partition_id() % 4
                X1f = X1.rearrange("q n d -> (q n) d")
                io = {"yT_lat": Y[:, bass.ds(q_pool * NQ + CTX, NQ)],
                      "x": X1f[bass.ds(q_sp * NTB, NQ), :], "ccol": I["ccol"], "wada": I["wadaB"][l], "bada": I["badaB"][l],
                      "lnp": I["lnp"][l], "wout": I["wout"][l], "wr": I["wr"], "rb": I["rb"], "wg": I["wg"][l], "wu": I["wu"][l], "wd": I["wd"][l],
                      "ident": I["ident"], "x1s": x1s[0:NQ, :], "xo": out}
                emit_phase_b(nc, S, io, False, "b%dq_" % l, n_lat_tiles=NLT)
        S.finish()
    return nc


def fused_inputs(inp, b, n_lat=8192):
    NQ = n_lat // 4; NLT = NQ // 128; NTB = (NLT + 1) * 128
    f = lambda a: np.ascontiguousarray(a, dtype=np.float32)
    x = inp['x'][b, :n_lat]; ctx = inp['ctx'][b]
    xT0 = np.concatenate([ctx, x], 0).T
    xq0 = np.zeros((4, NTB, 1024), np.float32)
    for q in range(4):
        xq0[q, :NQ] = x[q * NQ:(q + 1) * NQ]
        xq0[q, NQ:NQ + 64] = ctx[q * 64:(q + 1) * 64]
    per = [[core_inputs_a(inp, l, b, j, xT0, n_lat=n_lat) for j in range(4)] for l in range(2)]
    m = {"xT0": f(xT0), "xq0": xq0, "ccol": per[0][0]["ccol"], "ropeC": per[0][0]["ropeC"], "ropeS": per[0][0]["ropeS"],
         "cst": per[0][0]["cst"], "vmask": per[0][0]["vmask"], "swm": per[0][0]["swm"]}
    m["wcore"] = f(np.stack([np.stack([per[l][j]["wcore"] for j in range(4)]) for l in range(2)]))
    m["wadaA"] = f(np.stack([per[l][0]["wada"] for l in range(2)])); m["badac"] = f(np.stack([per[l][0]["badac"] for l in range(2)]))
    m["lbl"] = f(np.stack([per[0][j]["lbl"] for j in range(4)])); m["gnorm"] = f(np.stack([per[l][0]["gnorm"] for l in range(2)]))
    m["nab"] = f(np.stack([np.stack([per[l][j]["nab"] for j in range(4)]) for l in range(2)]))
    m["sink"] = f(np.stack([np.stack([per[l][j]["sink"] for j in range(4)]) for l in range(2)]))
    m["wadaB"] = f(np.stack([inp['w_ada'][l][:, 2048:] for l in range(2)])); m["badaB"] = f(np.stack([inp['b_ada'][l][None, 2048:] for l in range(2)]))
    m["lnp"] = f(np.stack([np.concatenate([inp['ln1_g'][l], inp['ln1_b'][l], inp['ln2_g'][l], inp['ln2_b'][l]])[None] for l in range(2)]))
    feat = np.zeros(1024, np.int64)
    for j in range(4):
        feat[256 * j:256 * j + 64] = 64 * j + np.arange(64)
        feat[256 * j + 64:256 * j + 128] = 256 + 64 * j + np.arange(64)
        feat[256 * j + 128:256 * j + 256] = 512 + 128 * j + np.arange(128)
    m["wout"] = f(np.stack([inp['w_out'][l][feat, :] for l in range(2)]))
    m["wr"] = f(inp['w_router']); m["rb"] = f(inp['router_bias'][None])
    m["wg"] = f(inp['w_gate']); m["wu"] = f(inp['w_up']); m["wd"] = f(inp['w_down'])
    m["ident"] = np.eye(128, dtype=np.float32)
    return m

_NC = {}


def kernel(**inputs):
    inp = {k: np.asarray(v, dtype=np.float32) for k, v in inputs.items()}
    if "nc" not in _NC:
        _NC["nc"] = build_fused(8192)
    nc = _NC["nc"]
    maps = [fused_inputs(inp, b) for b in range(2)]
    in_maps = [maps[c // 4] for c in range(8)]
    res = run_bass_kernel_spmd(nc, in_maps, core_ids=list(range(8))).results
    out = np.stack([np.concatenate([res[4 * b + q]["out"] for q in range(4)], 0) for b in range(2)], 0)
    return np.ascontiguousarray(out, dtype=np.float32)
```

```python
import contextlib
import numpy as np
import concourse.bass as bass
import concourse.mybir as mybir
from concourse.bass_utils import run_bass_kernel_spmd


F32 = mybir.dt.float32
BF16 = mybir.dt.bfloat16
AF = mybir.ActivationFunctionType
ALU = mybir.AluOpType
AX = mybir.AxisListType


class Sched:
    ENG = ("pe", "act", "dve", "pool", "sp")

    def __init__(self, nc, es, n_dma_sems=12):
        self.nc = nc
        self.e = {"pe": nc.tensor, "act": nc.scalar, "dve": nc.vector, "pool": nc.gpsimd, "sp": nc.sync}
        self.sem = {}
        self.cnt = {}
        for k in self.ENG:
            self.sem[k] = es.enter_context(nc.semaphore("s_" + k))
            self.cnt[k] = 0
        self.dq = {}
        for q in ("sp", "pool", "act"):
            n = n_dma_sems if q != "act" else 4
            self.dq[q] = {"sems": [], "vals": [0] * n, "rr": 0}
            for i in range(n):
                key = "d_%s_%d" % (q, i)
                self.sem[key] = es.enter_context(nc.semaphore(key))
                self.dq[q]["sems"].append(key)
        self.waited = {k: {} for k in self.ENG}
        self.lastw = {}
        self.readers = {}
        self.n_inst = 0
        self.n_wait = 0

    def _wait(self, eng, ev):
        if ev is None:
            return
        s, v = ev
        if s == eng and eng == "pe":
            return
        if s == eng and v <= 0:
            return
        if self.waited[eng].get(s, 0) >= v:
            return
        self.e[eng].wait_ge(self.sem[s], v)
        self.waited[eng][s] = v
        self.n_wait += 1

    def _deps(self, eng, reads, writes):
        for t in reads:
            self._wait(eng, self.lastw.get(t))
        for t in writes:
            self._wait(eng, self.lastw.get(t))
            for ev in self.readers.get(t, {}).items():
                self._wait(eng, ev)

    def _record(self, ev, reads, writes):
        s, v = ev
        for t in reads:
            r = self.readers.setdefault(t, {})
            if r.get(s, 0) < v:
                r[s] = v
        for t in writes:
            self.lastw[t] = ev
            self.readers[t] = {}

    def op(self, eng, fn, reads=(), writes=()):
        self._deps(eng, reads, writes)
        inst = fn(self.e[eng])
        self.cnt[eng] += 1
        inst.then_inc(self.sem[eng], 1)
        self._record((eng, self.cnt[eng]), reads, writes)
        self.n_inst += 1
        return inst

    def dma(self, q, out, in_, reads=(), writes=(), **kw):
        d = self.dq[q]
        i = d["rr"]
        d["rr"] = (i + 1) % len(d["sems"])
        key = d["sems"][i]
        if d["vals"][i] > 0:
            self._wait(q, (key, d["vals"][i]))
        self._deps(q, reads, writes)
        inst = self.e[q].dma_start(out=out, in_=in_, **kw)
        d["vals"][i] += 16
        inst.then_inc(self.sem[key], 16)
        self._record((key, d["vals"][i]), reads, writes)
        self.n_inst += 1
        return inst

    def all_events(self):
        evs = [(k, self.cnt[k]) for k in self.ENG if self.cnt[k] > 0]
        for q, d in self.dq.items():
            for key, v in zip(d["sems"], d["vals"]):
                if v > 0:
                    evs.append((key, v))
        return evs

    def barrier(self, engines=None):
        evs = self.all_events()
        for eng in (engines or self.ENG):
            for ev in evs:
                self._wait(eng, ev)

    def finish(self):
        self.barrier(engines=("sp",))


RMS_EPS = 1e-6
NCOL = 960
CTX = 256


def bc_mid(ap2d, n):
    p, k = ap2d.shape
    return ap2d.unsqueeze(2).broadcast_to([p, k, n])


def na_configs(nrows=128):
    cfg = {}
    mats = []
    interior = {}
    for pq in range(nrows // 2):
        rows = set()
        for r in (2 * pq, 2 * pq + 1):
            rs = min(max(r - 4, 0), nrows - 8)
            rows |= set(range(rs, rs + 8))
        pks = sorted(set(k // 2 for k in rows))
        lst = []
        for pk in pks:
            if 2 <= pq <= nrows // 2 - 3:
                key = ("i", pk - pq)
            else:
                key = (pq, pk)
            if key not in interior:
                interior[key] = len(mats)
                mats.append((pq, pk))
            lst.append((pk, interior[key]))
        cfg[pq] = lst
    return cfg, mats


def emit_phase_a(nc, S, io, layer, with_ctx_out, uid, n_lat=8192):
    T = CTX + n_lat
    D = 1024
    xT = io["xT"]; wcore = io["wcore"]; ccol = io["ccol"]; wada = io["wada"]; badac = io["badac"]; lbl = io["lbl"]
    gnorm_d = io["gnorm"]; nab_d = io["nab"]; swm_d = io["swm"]; sink_d = io["sink"]; ropeC_d = io["ropeC"]; ropeS_d = io["ropeS"]
    cst_d = io["cst"]; vmask_d = io["vmask"]; yT = io["yT"]; ofs = io["ofs"]
    stop = 99

    cfgs, mats = na_configs(n_lat // 64)
    assert len(mats) == 21
    blocks = [(0, CTX)] + [(CTX + i * 512, 512) for i in range(n_lat // 512)]
    NTILE = T // 128

    es = contextlib.ExitStack()
    with es:
        sb = lambda name, shape, dt=F32, st=es: st.enter_context(nc.sbuf_tensor(uid + "s_" + name, shape, dt))
        pst = lambda name, shape, dt=F32, st=es: st.enter_context(nc.psum_tensor(uid + "p_" + name, shape, dt))
        cst = sb("cst", [128, 128 * 4 + 512])
        identb = sb("identb", [128, 128], BF16)
        ident8b = sb("ident8b", [128, 128], BF16)
        hmF = cst[:, 256:384]; hmB = cst[:, 384:512]; rmask = cst[0:64, 512:1024]
        vmask = sb("vmask", [128, 4])
        wb = sb("wb", [128, 8, NCOL], BF16)
        modc = sb("modc", [128, 16, 2])
        lb = sb("lb", [64, 2]); oml = sb("oml", [64, 2])
        gnorm = sb("gnorm_sb", [64, 1])
        ones64 = sb("ones64", [64, 64])
        epsr = sb("epsr", [64, 1])
        nqT = sb("nqT", [64, T], BF16); nkT = sb("nkT", [64, T], BF16); nv1 = sb("nv1", [128, NTILE, 65], BF16)
        sq0T = sb("sq0T", [64, T], BF16); sq1T = sb("sq1T", [64, T], BF16); skT = sb("skT", [64, T], BF16)
        sv1 = sb("sv1", [128, NTILE, 65], BF16)
        S.dma("sp", cst[:], cst_d[:], writes=["cst"])
        S.dma("sp", vmask[:], vmask_d[:], writes=["vmask"])
        S.dma("sp", gnorm[:], gnorm_d[:], writes=["gnorm"])
        S.dma("pool", wb[:], wcore.rearrange("(k p) n -> p k n", p=128), writes=["wb"])
        S.op("dve", lambda e: e.tensor_copy(out=identb[:], in_=cst[:, 0:128]), reads=["cst"], writes=["identb"])
        S.op("dve", lambda e: e.tensor_copy(out=ident8b[:], in_=cst[:, 128:256]), reads=["cst"], writes=["ident8b"])
        S.op("dve", lambda e: e.memset(ones64[:], 1.0), writes=["ones64"])
        S.op("dve", lambda e: e.memset(epsr[:], RMS_EPS), writes=["epsr"])
        S.op("pool", lambda e: e.memset(nv1[:, :, 64:65], 1.0), writes=["nv1ones"])
        S.op("pool", lambda e: e.memset(sv1[:, :, 64:65], 1.0), writes=["sv1ones"])
        st0 = contextlib.ExitStack()
        with st0:
            lbt = sb("lbt", [64, 4], st=st0)
            S.dma("sp", lbt[:], lbl[:], writes=["lbt"])
            if layer == 0:
                S.op("dve", lambda e: e.memset(lb[:], 1e-6), writes=["lb"])
            else:
                lbv = lbt[:].rearrange("p (d l) -> p d l", l=2)
                S.op("dve", lambda e: e.tensor_tensor(out=lb[:], in0=lbv[:, :, 0], in1=lbv[:, :, 1], op=ALU.subtract),
                     reads=["lbt"], writes=["lb"])
                S.op("act", lambda e: e.activation(out=lb[:], in_=lb[:], func=AF.Exp), reads=["lb"], writes=["lb"])
                S.op("dve", lambda e: e.tensor_scalar(out=lb[:], in0=lb[:], scalar1=1.0, scalar2=None, op0=ALU.add), reads=["lb"], writes=["lb"])
                S.op("dve", lambda e: e.reciprocal(out=lb[:], in_=lb[:]), reads=["lb"], writes=["lb"])
                S.op("dve", lambda e: e.tensor_scalar(out=lb[:], in0=lb[:], scalar1=1e-6, scalar2=None, op0=ALU.max), reads=["lb"], writes=["lb"])
            S.op("dve", lambda e: e.tensor_scalar(out=oml[:], in0=lb[:], scalar1=-1.0, scalar2=1.0, op0=ALU.mult, op1=ALU.add),
                 reads=["lb"], writes=["oml"])
            modc_d = io.get("modc_d")
            if modc_d is not None and not io.get("mod_first", True):
                S.dma("sp", modc[:].rearrange("p a b -> p (a b)"), modc_d, writes=["modc"])
                S.barrier()
            else:
                cc = sb("cc", [128, 16], st=st0); scc = sb("scc", [128, 8, 2], st=st0)
                wa = sb("wa", [128, 8, 2048], st=st0)
                bdc = sb("bdc", [128, 16], st=st0)
                psm = pst("psm", [128, 16, 2], st=st0)
                S.dma("sp", cc[:], ccol[:], writes=["cc"])
                S.dma("sp", bdc[:], badac[:], writes=["bdc"])
                S.dma("sp", wa[:], wada.rearrange("(k p) n -> p k n", p=128), writes=["wa"])
                S.op("act", lambda e: e.activation(out=scc[:].rearrange("p k w -> p w k"), in_=cc[:].rearrange("p (w k) -> p w k", w=2), func=AF.Silu),
                     reads=["cc"], writes=["scc"])
                for dch in range(16):
                    for k in range(8):
                        S.op("pe", lambda e, dch=dch, k=k: e.matmul(psm[:, dch, :], lhsT=wa[:, k, dch * 128:(dch + 1) * 128], rhs=scc[:, k, :],
                                                                    start=(k == 0), stop=(k == 7)),
                             reads=["wa", "scc"], writes=["psm"])
                S.op("dve", lambda e: e.tensor_tensor(out=modc[:], in0=psm[:], in1=bc_mid(bdc[:], 2), op=ALU.add),
                     reads=["psm", "bdc"], writes=["modc"])
                S.op("dve", lambda e: e.tensor_scalar(out=modc[:, 8:16, :], in0=modc[:, 8:16, :], scalar1=1.0, scalar2=None, op0=ALU.add),
                     reads=["modc"], writes=["modc"])
                if modc_d is not None:
                    S.dma("sp", modc_d, modc[:].rearrange("p a b -> p (a b)"), reads=["modc"], writes=["modc_d"])
                S.barrier()
        UB = io["UB"]; QB = io["QB"]; SG = io["SG"]
        dcyB = sb("dcyB", [64, len(blocks), 16])
        Sall = sb("Sall", [64, 17, 64])
        Sbf = sb("Sbf", [64, 16, 64], BF16)
        Ub = sb("Ub", [64, 16, 64])
        sgt = sb("sgt", [64, 512])
        ps_oi = pst("ps_oi", [64, 512])
        oi = sb("oi", [64, 512]); oo = sb("oo", [64, 512]); ofb = sb("ofb", [64, 512])
        qtl = sb("qtl", [64, 512], BF16)
        stp = contextlib.ExitStack()
        with stp:
            xt = sb("xt0", [128, 8, 512], st=stp)
            hT = [sb("hT%d" % i, [128, 8, 512], BF16, st=stp) for i in range(2)]
            rC = sb("rC", [64, 512], st=stp); rS = sb("rS", [64, 512], st=stp)
            g_sb = {}
            for nm in ("aq", "az", "az2", "ag", "r0", "r1"):
                g_sb[nm] = sb("g_" + nm, [64, 512], st=stp)
            tA = sb("tA", [64, 512], st=stp); tB = sb("tB", [64, 512], st=stp); tC = sb("tC", [64, 512], st=stp)
            tD = sb("tD", [64, 512], st=stp); tE = sb("tE", [64, 512], st=stp)
            tot = sb("tot", [64, 16], st=stp); dcy = sb("dcy", [64, 16], st=stp)
            ktl = sb("ktl", [64, 512], BF16, st=stp); khT = sb("khT", [64, 512], BF16, st=stp)
            qtlb = sb("qtlb", [64, 512], BF16, st=stp); ktlb = sb("ktlb", [64, 512], BF16, st=stp); khTb = sb("khTb", [64, 512], BF16, st=stp)
            dcy2 = sb("dcy2", [64, 16], st=stp)
            kh = sb("kh", [128, 4, 64], BF16, st=stp)
            vt = sb("vt", [128, 4, 64], BF16, st=stp)
            vblk = sb("vblk", [128, 4, 4, 64], BF16, st=stp)
            scT = sb("scT", [128, 128], BF16, st=stp)
            ps_f = [pst("ps_f%d" % i, [64, 512], st=stp) for i in range(2)]
            ps_tm = pst("ps_tm", [128, 192], st=stp)
            ps_kh = pst("ps_kh", [128, 64], BF16, st=stp)
            ps_U = pst("ps_U", [64, 4, 64], st=stp)
            ps_sc = pst("ps_sc", [128, 128], st=stp)
            ps_oa = pst("ps_oa", [64, 512], st=stp)
            S.op("dve", lambda e: e.memset(Sall[:, 0, :], 0.0), writes=["Sall"])
            fcnt = [0]

            def load_block(bi, par):
                t0, n = blocks[bi]
                S.dma("sp", xt[:, :, :n], xT.rearrange("(k p) t -> p k t", p=128)[:, :, t0:t0 + n], writes=["xt0"])
                w = 1 if t0 < CTX else 0
                for k in range(8):
                    eng = "dve" if k % 2 == 0 else "pool"
                    S.op(eng, lambda e, k=k, w=w, par=par, n=n: e.tensor_scalar(
                        out=hT[par][:, k, :n], in0=xt[:, k, :n], scalar1=modc[:, 8 + k, w:w + 1], scalar2=modc[:, k, w:w + 1],
                        op0=ALU.mult, op1=ALU.add), reads=["xt0", "modc"], writes=["hT%d_%d" % (par, k)])

            def proj_fm(par, n, g):
                i = fcnt[0] % 2; fcnt[0] += 1
                for k in range(8):
                    S.op("pe", lambda e, k=k, i=i, g=g, par=par, n=n: e.matmul(ps_f[i][:, :n], lhsT=wb[:, k, g * 64:(g + 1) * 64],
                                                                             rhs=hT[par][:, k, :n], start=(k == 0), stop=(k == 7)),
                         reads=["wb", "hT%d_%d" % (par, k)], writes=["ps_f%d" % i])
                return ps_f[i], "ps_f%d" % i

            def hg_elem(n, d, zname, qo, ko, kho, sfx):
                nch = n // 32
                q_ = g_sb["aq"]; z_ = g_sb[zname]; ztok = "g_" + zname
                dc = dcy if d == 0 else dcy2
                dct = "dcy" if d == 0 else "dcy2"
                S.op("act", lambda e: e.activation(out=tA[:, :n], in_=z_[:, :n], func=AF.Sigmoid), reads=[ztok], writes=["tA"]); yield
                S.op("dve", lambda e: e.tensor_scalar(out=tA[:, :n], in0=tA[:, :n], scalar1=oml[:, d:d + 1], scalar2=lb[:, d:d + 1],
                                                      op0=ALU.mult, op1=ALU.add), reads=["tA", "oml", "lb"], writes=["tA"]); yield
                S.op("act", lambda e: e.activation(out=tB[:, :n], in_=tA[:, :n], func=AF.Ln), reads=["tA"], writes=["tB"]); yield
                S.op("dve", lambda e: e.tensor_scalar(out=tA[:, :n], in0=tA[:, :n], scalar1=-1.0, scalar2=1.0, op0=ALU.mult, op1=ALU.add),
                     reads=["tA", "tB"], writes=["tA"]); yield
                S.op("dve", lambda e: e.tensor_tensor_scan(out=tC[:, :n], data0=rmask[:, :n], data1=tB[:, :n], initial=0.0,
                                                           op0=ALU.mult, op1=ALU.add), reads=["tB", "cst"], writes=["tC"]); yield
                cumv = tC[:, :n].rearrange("p (c j) -> p c j", j=32)
                S.op("dve", lambda e: e.tensor_copy(out=tot[:, :nch], in_=cumv[:, :, 31]), reads=["tC"], writes=["tot"]); yield
                S.op("act", lambda e: e.activation(out=dc[:, :nch], in_=tot[:, :nch], func=AF.Exp), reads=["tot"], writes=[dct]); yield
                S.op("dve", lambda e: e.tensor_tensor(out=tD[:, :n].rearrange("p (c j) -> p c j", j=32), in0=bc_mid(tot[:, :nch], 32), in1=cumv,
                                                      op=ALU.subtract), reads=["tot", "tC"], writes=["tD"]); yield
                if d == 0:
                    e1, e3, e1n, e3n = tC, tD, "tC", "tD"
                else:
                    S.op("dve", lambda e: e.tensor_tensor(out=tE[:, :n], in0=tD[:, :n], in1=tB[:, :n], op=ALU.add), reads=["tD", "tB"], writes=["tE"]); yield
                    S.op("dve", lambda e: e.tensor_tensor(out=tC[:, :n], in0=tC[:, :n], in1=tB[:, :n], op=ALU.subtract), reads=["tC", "tB", "tD"], writes=["tC"]); yield
                    e1, e3, e1n, e3n = tE, tC, "tE", "tC"
                S.op("act", lambda e: e.activation(out=tB[:, :n], in_=e1[:, :n], func=AF.Exp, scale=-1.0), reads=[e1n, "tD", "tE", "tC"], writes=["tB"]); yield
                S.op("act", lambda e: e.activation(out=e1[:, :n], in_=e1[:, :n], func=AF.Exp), reads=[e1n, "tB"], writes=[e1n]); yield
                S.op("act", lambda e: e.activation(out=e3[:, :n], in_=e3[:, :n], func=AF.Exp), reads=[e3n], writes=[e3n]); yield
                S.op("dve", lambda e: e.tensor_tensor(out=qo[:, :n], in0=q_[:, :n], in1=e1[:, :n], op=ALU.mult), reads=["g_aq", e1n], writes=["qtl" + sfx]); yield
                S.op("dve", lambda e: e.tensor_tensor(out=ko[:, :n], in0=tA[:, :n], in1=tB[:, :n], op=ALU.mult), reads=["tA", "tB"], writes=["ktl" + sfx]); yield
                S.op("pool", lambda e: e.tensor_tensor(out=kho[:, :n], in0=tA[:, :n], in1=e3[:, :n], op=ALU.mult), reads=["tA", e3n], writes=["khT" + sfx]); yield

            def hg_tiles_U(n, store, kho=None, sfx="", tiles=None):
                kho = khT if kho is None else kho
                ntl = n // 128
                for tl in (range(ntl) if tiles is None else tiles):
                    S.op("pe", lambda e, tl=tl: e.transpose(out=ps_kh[:], in_=kho[:, tl * 128:(tl + 1) * 128], identity=identb[0:64, 0:64]),
                         reads=["khT" + sfx, "identb"], writes=["ps_kh"])
                    S.op("act", lambda e, tl=tl: e.activation(out=kh[:, tl, :], in_=ps_kh[:], func=AF.Copy), reads=["ps_kh"], writes=["kh%d" % tl])
                    S.op("pe", lambda e, tl=tl: e.matmul(ps_U[:].rearrange("p c e -> p (c e)"), lhsT=kh[:, tl, :],
                                                         rhs=vblk[:, tl, :, :].rearrange("p c e -> p (c e)"), start=True, stop=True),
                         reads=["kh%d" % tl, "vblk"], writes=["ps_U"])
                    if store:
                        S.op("act", lambda e, tl=tl: e.activation(out=Ub[:, tl * 4:(tl + 1) * 4, :], in_=ps_U[:], func=AF.Copy),
                             reads=["ps_U"], writes=["Ub"])
                    else:
                        for cc_ in range(4):
                            c = tl * 4 + cc_
                            S.op("dve", lambda e, c=c, cc_=cc_: e.scalar_tensor_tensor(
                                out=Sall[:, c + 1, :], in0=Sall[:, c, :], scalar=dcy[:, c:c + 1], in1=ps_U[:, cc_, :], op0=ALU.mult, op1=ALU.add),
                                reads=["ps_U", "Sall", "dcy"], writes=["Sall"])

            def hg_inter(n, off, qo=None, sfx=""):
                qo = qtl if qo is None else qo
                nch = n // 32
                S.op("act", lambda e: e.activation(out=Sbf[:, :nch, :], in_=Sall[:, off:off + nch, :], func=AF.Copy), reads=["Sall"], writes=["Sbf"])
                for c in range(nch):
                    S.op("pe", lambda e, c=c: e.matmul(ps_oi[:, c * 32:(c + 1) * 32], lhsT=Sbf[:, c, :], rhs=qo[:, c * 32:(c + 1) * 32],
                                                       start=True, stop=True), reads=["Sbf", "qtl" + sfx], writes=["ps_oi"])

            def hg_intra(n, d, qo=None, ko=None, sfx="", tiles=None):
                qo = qtl if qo is None else qo
                ko = ktl if ko is None else ko
                ntl = n // 128
                hm = hmF if d == 0 else hmB
                for tl in (range(ntl) if tiles is None else tiles):
                    S.op("pe", lambda e, tl=tl: e.matmul(ps_sc[:], lhsT=ko[:, tl * 128:(tl + 1) * 128], rhs=qo[:, tl * 128:(tl + 1) * 128],
                                                         start=True, stop=True), reads=["ktl" + sfx, "qtl" + sfx], writes=["ps_sc"])
                    S.op("dve", lambda e: e.tensor_tensor(out=scT[:], in0=ps_sc[:], in1=hm, op=ALU.mult), reads=["ps_sc", "cst"], writes=["scT"])
                    S.op("pe", lambda e, tl=tl: e.matmul(ps_oa[:, tl * 128:(tl + 1) * 128], lhsT=vt[:, tl, :], rhs=scT[:], start=True, stop=True),
                         reads=["vt", "scT"], writes=["ps_oa"])

            load_block(0, 0)
            for bi in range(len(blocks)):
                par = bi % 2
                t0, n = blocks[bi]
                ntl = n // 128; nch = n // 32
                is_ctx = t0 < CTX
                if bi + 1 < len(blocks):
                    load_block(bi + 1, 1 - par)
                if not is_ctx:
                    S.dma("sp", rC[:, :n], ropeC_d[:, t0 - CTX:t0 - CTX + n], writes=["rC"])
                    S.dma("sp", rS[:, :n], ropeS_d[:, t0 - CTX:t0 - CTX + n], writes=["rS"])
                def evac(g, dst, dtok, eng="act"):
                    p_, ptok = proj_fm(par, n, g)
                    o_ = dst[:, :n] if dst.shape[1] == 512 else dst[:, t0:t0 + n]
                    if eng == "act":
                        S.op("act", lambda e: e.activation(out=o_, in_=p_[:, :n], func=AF.Copy), reads=[ptok], writes=[dtok])
                    else:
                        S.op("dve", lambda e: e.tensor_copy(out=o_, in_=p_[:, :n]), reads=[ptok], writes=[dtok])

                def rope_group(ga, gb, dst, dtok):
                    pa, patok = proj_fm(par, n, ga)
                    S.op("dve", lambda e, pa=pa: e.tensor_tensor(out=g_sb["r0"][:, :n], in0=pa[:, :n], in1=rC[:, :n], op=ALU.mult),
                         reads=[patok, "rC"], writes=["g_r0"])
                    pb, pbtok = proj_fm(par, n, gb)
                    S.op("dve", lambda e, pb=pb: e.tensor_tensor(out=g_sb["r1"][:, :n], in0=pb[:, :n], in1=rS[:, :n], op=ALU.mult),
                         reads=[pbtok, "rS"], writes=["g_r1"])
                    S.op("pool", lambda e, dst=dst: e.tensor_tensor(out=dst[:, t0:t0 + n], in0=g_sb["r0"][:, :n], in1=g_sb["r1"][:, :n], op=ALU.add),
                         reads=["g_r0", "g_r1"], writes=[dtok])

                evac(0, g_sb["aq"], "g_aq", "act")
                evac(1, g_sb["az"], "g_az", "dve")
                evac(2, g_sb["az2"], "g_az2", "act")
                for tl in range(ntl):
                    gt = t0 // 128 + tl
                    for k in range(8):
                        S.op("pe", lambda e, k=k, tl=tl, par=par: e.matmul(ps_tm[:], lhsT=hT[par][:, k, tl * 128:(tl + 1) * 128],
                                                                           rhs=wb[:, k, 768:960], start=(k == 0), stop=(k == 7)),
                             reads=["wb", "hT%d_%d" % (par, k)], writes=["ps_tm"])
                    S.op("act", lambda e, tl=tl: e.activation(out=vt[:, tl, :], in_=ps_tm[:, 0:64], func=AF.Copy), reads=["ps_tm"], writes=["vt"])
                    S.op("act", lambda e, gt=gt: e.activation(out=nv1[:, gt, 0:64], in_=ps_tm[:, 64:128], func=AF.Copy), reads=["ps_tm", "vt"], writes=["nv1_%d" % gt])
                    S.op("act", lambda e, gt=gt: e.activation(out=sv1[:, gt, 0:64], in_=ps_tm[:, 128:192], func=AF.Copy), reads=["ps_tm", "nv1_%d" % gt], writes=["sv1_%d" % gt])
                    for c in range(4):
                        S.op("pool", lambda e, tl=tl, c=c: e.tensor_scalar(out=vblk[:, tl, c, :], in0=vt[:, tl, :], scalar1=vmask[:, c:c + 1],
                                                                          scalar2=None, op0=ALU.mult), reads=["vt", "vmask"], writes=["vblk"])
                import itertools
                chain_f = hg_elem(n, 0, "az", qtl, ktl, khT, "")
                chain_b = hg_elem(n, 1, "az2", qtlb, ktlb, khTb, "b")
                others = [lambda: evac(3, g_sb["ag"], "g_ag", "dve"), lambda: evac(4, nqT, "nqT", "act"), lambda: evac(5, nkT, "nkT", "dve")]
                if is_ctx:
                    others += [lambda: evac(6, sq0T, "sq0T", "act"), lambda: evac(8, sq1T, "sq1T", "dve"), lambda: evac(10, skT, "skT", "act")]
                else:
                    others += [lambda: rope_group(6, 7, sq0T, "sq0T"), lambda: rope_group(8, 9, sq1T, "sq1T"), lambda: rope_group(10, 11, skT, "skT")]
                for oth in others:
                    for _ in range(3):
                        next(chain_f, None)
                    oth()
                for _ in chain_f:
                    pass

                def work_f():
                    for tl in range(ntl):
                        hg_tiles_U(n, store=False, tiles=[tl]); yield
                    hg_inter(n, 0); yield
                    for tl in range(ntl):
                        hg_intra(n, 0, tiles=[tl]); yield
                    S.op("act", lambda e: e.activation(out=oi[:, :n], in_=ps_oa[:, :n], func=AF.Copy), reads=["ps_oa"], writes=["oi"])
                    S.op("dve", lambda e: e.tensor_tensor(out=oo[:, :n], in0=ps_oi[:, :n], in1=oi[:, :n], op=ALU.add), reads=["ps_oi", "oi"], writes=["oo"])
                    S.op("dve", lambda e: e.tensor_copy(out=Sall[:, 0, :], in_=Sall[:, nch, :]), reads=["Sall", "Sbf"], writes=["Sall"])
                    yield
                for _ in work_f():
                    next(chain_b, None); next(chain_b, None)
                for _ in chain_b:
                    pass
                hg_tiles_U(n, store=True, kho=khTb, sfx="b")
                hg_intra(n, 1, qo=qtlb, ko=ktlb, sfx="b")
                S.op("dve", lambda e: e.tensor_tensor(out=oo[:, :n], in0=ps_oa[:, :n], in1=oo[:, :n], op=ALU.add), reads=["ps_oa", "oo"], writes=["oo"])
                S.op("dve", lambda e, bi=bi: e.tensor_copy(out=dcyB[:, bi, :nch], in_=dcy2[:, :nch]), reads=["dcy2"], writes=["dcyB"])
                S.dma("sp", ofs[:, t0:t0 + n], oo[:, :n], reads=["oo"], writes=["ofs%d" % bi])
                S.dma("sp", UB[:, bi, :nch * 64], Ub[:, :nch, :].rearrange("p c e -> p (c e)"), reads=["Ub"], writes=["UB%d" % bi])
                S.dma("sp", QB[:, t0:t0 + n], qtlb[:, :n], reads=["qtlb"], writes=["QB%d" % bi])
                S.op("act", lambda e: e.activation(out=sgt[:, :n], in_=g_sb["ag"][:, :n], func=AF.Silu), reads=["g_ag"], writes=["sgt"])
                S.op("dve", lambda e: e.tensor_scalar(out=sgt[:, :n], in0=sgt[:, :n], scalar1=gnorm[:, 0:1], scalar2=None, op0=ALU.mult),
                     reads=["sgt", "gnorm"], writes=["sgt"])
                S.dma("sp", SG[:, t0:t0 + n], sgt[:, :n], reads=["sgt"], writes=["SG%d" % bi])
            S.barrier()

        def pass2_gen():
            S.op("dve", lambda e: e.memset(Sall[:, 0, :], 0.0), writes=["Sall"])
            order2 = [0] + list(range(len(blocks) - 1, 0, -1))
            for oi_, bi in enumerate(order2):
                t0, n = blocks[bi]
                ntl = n // 128; nch = n // 32
                S.dma("sp", Ub[:, :nch, :].rearrange("p c e -> p (c e)"), UB[:, bi, :nch * 64], writes=["Ub"])
                S.dma("sp", qtl[:, :n], QB[:, t0:t0 + n], writes=["qtl"])
                S.dma("sp", ofb[:, :n], ofs[:, t0:t0 + n], writes=["ofb"])
                S.dma("sp", sgt[:, :n], SG[:, t0:t0 + n], writes=["sgt"])
                S.op("dve", lambda e: e.tensor_copy(out=Sall[:, nch, :], in_=Sall[:, 0, :]), reads=["Sall"], writes=["Sall"])
                for c in range(nch - 1, -1, -1):
                    S.op("dve", lambda e, c=c, bi=bi: e.scalar_tensor_tensor(
                        out=Sall[:, c, :], in0=Sall[:, c + 1, :], scalar=dcyB[:, bi, c:c + 1], in1=Ub[:, c, :], op0=ALU.mult, op1=ALU.add),
                        reads=["Ub", "Sall", "dcyB"], writes=["Sall"])
                hg_inter(n, 1)
                S.op("dve", lambda e: e.tensor_tensor(out=oo[:, :n], in0=ps_oi[:, :n], in1=ofb[:, :n], op=ALU.add), reads=["ps_oi", "ofb"], writes=["oo"])
                S.op("act", lambda e: e.activation(out=oi[:, :n], in_=oo[:, :n], func=AF.Square), reads=["oo", "oi"], writes=["oi"])
                pss, psstok = ps_oi, "ps_oi"
                S.op("pe", lambda e, pss=pss: e.matmul(pss[:, :n], lhsT=ones64[:], rhs=oi[:, :n], start=True, stop=True),
                     reads=["ones64", "oi"], writes=[psstok])
                S.op("act", lambda e, pss=pss: e.activation(out=oi[:, :n], in_=pss[:, :n], func=AF.Sqrt, bias=epsr[:, 0:1], scale=1.0 / 64.0),
                     reads=[psstok, "epsr"], writes=["oi"])
                S.op("dve", lambda e: e.reciprocal(out=oi[:, :n], in_=oi[:, :n]), reads=["oi"], writes=["oi"])
                S.op("dve", lambda e: e.tensor_tensor(out=oo[:, :n], in0=oo[:, :n], in1=oi[:, :n], op=ALU.mult), reads=["oo", "oi"], writes=["oo"])
                S.op("dve", lambda e: e.tensor_tensor(out=oo[:, :n], in0=oo[:, :n], in1=sgt[:, :n], op=ALU.mult), reads=["oo", "sgt"], writes=["oo"])
                S.dma("sp", yT[0:64, t0:t0 + n], oo[:, :n], reads=["oo"], writes=["yTa%d" % bi])
                yield bi
        sta = contextlib.ExitStack()
        with sta:
            nab = sb("nab", [128, 21, 128], BF16, st=sta)
            swm = sb("swm", [128, 2, 128], BF16, st=sta)
            snk = sb("snk", [1, 2], st=sta); snkB = sb("snkB", [128, 2], st=sta)
            ones1 = sb("ones1", [1, 128], st=sta)
            pT = [sb("pT%d" % i, [128, 7, 128], BF16, st=sta) for i in range(2)]
            ot = [sb("ot%d" % i, [128, 64], BF16, st=sta) for i in range(2)]
            rinv = sb("rinv", [128, 1], st=sta)
            yblk = [sb("yblk%d" % i, [64, 512], st=sta) for i in range(2)]
            ps_s = [[pst("ps_s%d_%d" % (i, j), [128, 4, 128], st=sta) for j in range(2)] for i in range(2)]
            ps_o = [pst("ps_o%d" % i, [128, 65], st=sta) for i in range(2)]
            ps_y = pst("ps_y", [64, 128], BF16, st=sta)
            S.dma("pool", nab[:], nab_d.rearrange("c k q -> k c q"), writes=["nab"])
            S.dma("pool", swm[:], swm_d.rearrange("c k q -> k c q"), writes=["swm"])
            S.dma("sp", snk[:], sink_d[:], writes=["snk"])
            S.op("dve", lambda e: e.memset(ones1[:], 1.0), writes=["ones1"])
            S.op("pe", lambda e: e.matmul(ps_o[0][:, 0:2], lhsT=ones1[:], rhs=snk[:], start=True, stop=True), reads=["ones1", "snk"], writes=["ps_o0"])
            S.op("act", lambda e: e.activation(out=snkB[:], in_=ps_o[0][:, 0:2], func=AF.Exp), reads=["ps_o0"], writes=["snkB"])
            acnt = [0]
            p2 = pass2_gen()
            p2cnt = [0]

            tiles = []

            def add_tile(qT, qtok_tile, keys, v1, sink_col, yb_, ybtok, ycol, after=None):
                tiles.append(dict(qT=qT, qt=qtok_tile, keys=keys, v1=v1, sink=sink_col, yb=yb_, ybtok=ybtok, ycol=ycol, after=after))

            def st_scores(t, i):
                keys = t["keys"]; nk = len(keys); qT = t["qT"]; qt = t["qt"]
                for j, (kT_, kt, bias) in enumerate(keys):
                    pp = ps_s[i][j // 4]; ptok = "ps_s%d_%d" % (i, j // 4)
                    S.op("pe", lambda e, pp=pp, j=j, kT_=kT_, kt=kt, bias=bias: e.matmul(
                        pp[:, j % 4, :], lhsT=kT_[:, kt * 128:(kt + 1) * 128], rhs=qT[:, qt * 128:(qt + 1) * 128],
                        start=True, stop=(bias is None)), writes=[ptok])
                    if bias is not None:
                        S.op("pe", lambda e, pp=pp, j=j, bias=bias: e.matmul(pp[:, j % 4, :], lhsT=ident8b[:], rhs=bias, start=False, stop=True),
                             reads=["nab", "swm", "ident8b"], writes=[ptok])
                n0 = min(4, nk)
                S.op("act", lambda e: e.activation(out=pT[i][:, 0:n0, :], in_=ps_s[i][0][:, 0:n0, :], func=AF.Exp, scale=0.125),
                     reads=["ps_s%d_0" % i], writes=["pT%d" % i])
                if nk > 4:
                    S.op("act", lambda e: e.activation(out=pT[i][:, 4:nk, :], in_=ps_s[i][1][:, 0:nk - 4, :], func=AF.Exp, scale=0.125),
                         reads=["ps_s%d_1" % i], writes=["pT%d" % i])

            def st_pv(t, i):
                keys = t["keys"]; nk = len(keys); v1 = t["v1"]; sink_col = t["sink"]
                for j, (kT_, kt, bias) in enumerate(keys):
                    S.op("pe", lambda e, j=j, kt=kt: e.matmul(ps_o[i][:], lhsT=pT[i][:, j, :], rhs=v1[:, kt, :], start=(j == 0), stop=(j == nk - 1)),
                         reads=["pT%d" % i], writes=["ps_o%d" % i])
                if sink_col is None:
                    S.op("dve", lambda e: e.reciprocal(out=rinv[:], in_=ps_o[i][:, 64:65]), reads=["ps_o%d" % i], writes=["rinv"])
                else:
                    S.op("dve", lambda e: e.tensor_tensor(out=rinv[:], in0=ps_o[i][:, 64:65], in1=snkB[:, sink_col:sink_col + 1], op=ALU.add),
                         reads=["ps_o%d" % i, "snkB"], writes=["rinv"])
                    S.op("dve", lambda e: e.reciprocal(out=rinv[:], in_=rinv[:]), reads=["rinv"], writes=["rinv"])
                S.op("dve", lambda e: e.tensor_scalar(out=ot[i][:], in0=ps_o[i][:, 0:64], scalar1=rinv[:, 0:1], scalar2=None, op0=ALU.mult),
                     reads=["ps_o%d" % i, "rinv"], writes=["ot%d" % i])

            def st_out(t, i):
                yb_ = t["yb"]; ycol = t["ycol"]
                S.op("pe", lambda e: e.transpose(out=ps_y[:], in_=ot[i][:], identity=identb[:]), reads=["ot%d" % i, "identb"], writes=["ps_y"])
                S.op("act", lambda e: e.activation(out=yb_[:, ycol:ycol + 128], in_=ps_y[:], func=AF.Copy), reads=["ps_y"], writes=[t["ybtok"]])
                if t["after"] is not None:
                    t["after"]()

            CT = CTX // 128
            ctx_keys_n = [(nkT, 0, None), (nkT, 1, None)]
            ctx_keys_s = [(skT, 0, None), (skT, 1, None)]
            ybc = 0
            heads_ = [("n", nqT, None, 64), ("s0", sq0T, 0, 128), ("s1", sq1T, 1, 192)]

            def mk_after(dst, src, ybtok, wtok):
                def f():
                    S.dma("sp", dst, src, reads=[ybtok], writes=[wtok])
                    p2cnt[0] += 1
                    if p2cnt[0] % 2 == 0:
                        next(p2, None)
                return f

            for (hk, qT, sink_col, yrow0) in heads_:
                if with_ctx_out:
                    yb_ = yblk[ybc % 2]; ybtok = "yblk%d" % (ybc % 2); ybc += 1
                    for qt in range(CT):
                        aft = mk_after(yT[yrow0:yrow0 + 64, 0:CTX], yb_[:, 0:CTX], ybtok, "yTc_" + hk) if qt == CT - 1 else None
                        if hk == "n":
                            add_tile(qT, qt, ctx_keys_n, nv1, None, yb_, ybtok, qt * 128, aft)
                        else:
                            add_tile(qT, qt, ctx_keys_s, sv1, sink_col, yb_, ybtok, qt * 128, aft)
                nql = n_lat // 128
                for qb in range(nql // 4):
                    yb_ = yblk[ybc % 2]; ybtok = "yblk%d" % (ybc % 2); ybc += 1
                    for qq in range(4):
                        pq = qb * 4 + qq
                        aft = mk_after(yT[yrow0:yrow0 + 64, CTX + qb * 512:CTX + (qb + 1) * 512], yb_[:], ybtok, "yT_%s_%d" % (hk, qb)) if qq == 3 else None
                        if hk == "n":
                            keys = [(nkT, CT + pk, nab[:, mi, :]) for (pk, mi) in cfgs[pq]] + ctx_keys_n
                            add_tile(qT, CT + pq, keys, nv1, None, yb_, ybtok, qq * 128, aft)
                        else:
                            keys = []
                            if pq > 0:
                                keys.append((skT, CT + pq - 1, swm[:, 0, :]))
                            keys.append((skT, CT + pq, None))
                            if pq < nql - 1:
                                keys.append((skT, CT + pq + 1, swm[:, 1, :]))
                            keys += ctx_keys_s
                            add_tile(qT, CT + pq, keys, sv1, sink_col, yb_, ybtok, qq * 128, aft)
            NTL = len(tiles)
            for n_ in range(NTL + 2):
                if n_ < NTL:
                    st_scores(tiles[n_], n_ % 2)
                if 0 <= n_ - 1 < NTL:
                    st_pv(tiles[n_ - 1], (n_ - 1) % 2)
                if 0 <= n_ - 2 < NTL:
                    st_out(tiles[n_ - 2], (n_ - 2) % 2)
            for _ in p2:
                pass
            S.barrier()

NEG = -30000.0
def rope_tables(n_lat=8192):
    t = np.arange(n_lat); row = (t // 64).astype(np.float32); col = (t % 64).astype(np.float32)
    nf = 16
    inv = (np.float32(10000.0) ** (-np.arange(nf, dtype=np.float32) / np.float32(nf))).astype(np.float32)
    C = np.zeros((64, n_lat), np.float32); S = np.zeros((64, n_lat), np.float32)
    for half, pos in ((0, row), (1, col)):
        ang = (pos[:, None] * inv[None, :]).astype(np.float32)
        c = np.cos(ang).astype(np.float32).T; s_ = np.sin(ang).astype(np.float32).T
        b = half * 32
        C[b:b+16] = c; C[b+16:b+32] = c
        S[b:b+16] = -s_; S[b+16:b+32] = s_
    return C, S
def rope_perm():
    p = np.arange(64)
    for b in (0, 32):
        p[b:b+16] = np.arange(b+16, b+32); p[b+16:b+32] = np.arange(b, b+16)
    return p
def na_index_tables(nrows=128):
    cfgs, mats = na_configs(nrows)
    idx = np.zeros((21, 128, 128), np.int64)
    k = np.arange(128); q = np.arange(128)
    for mi, (pq, pk) in enumerate(mats):
        krow = 2 * pk + k // 64; kcol = k % 64
        qrow = 2 * pq + q // 64; qcol = q % 64
        rs = np.clip(qrow - 4, 0, nrows - 8); cs = np.clip(qcol - 8, 0, 48)
        valid = (krow[:, None] >= rs[None]) & (krow[:, None] < rs[None] + 8) & (kcol[:, None] >= cs[None]) & (kcol[:, None] < cs[None] + 16)
        ridx = krow[:, None] - qrow[None] + 7
        coff = np.clip(kcol[:, None] - qcol[None] + 15, 0, 30)
        ii = np.clip(ridx, 0, 14) * 31 + coff
        idx[mi] = np.where(valid, ii, 15 * 31)
    return idx
def const_tables():
    ident = np.eye(128, dtype=np.float32)
    k = np.arange(128)
    same = (k[:, None] // 32) == (k[None] // 32)
    hmF = (same & (k[:, None] <= k[None])).astype(np.float32)
    hmB = (same & (k[:, None] >= k[None])).astype(np.float32)
    rm = np.ones((128, 512), np.float32); rm[:, ::32] = 0.0
    cst = np.concatenate([ident, 8 * ident, hmF, hmB, rm], axis=1)
    vmask = (k[:, None] // 32 == np.arange(4)[None]).astype(np.float32)
    swm = np.zeros((2, 128, 128), np.float32)
    swm[0] = np.where(k[:, None] >= k[None], 0.0, NEG)
    swm[1] = np.where(k[:, None] <= k[None], 0.0, NEG)
    return cst, vmask, swm
_NAIDX = None
def core_inputs_a(inp, l, b, j, xT_b, n_lat=8192):
    global _NAIDX
    if _NAIDX is None: _NAIDX = na_index_tables(n_lat // 64)
    w = inp['w_in'][l]
    perm = rope_perm()
    def cols(base, width=64, idx=j): return w[:, base + width * idx: base + width * (idx + 1)]
    sq0 = w[:, 2048 + 128 * j: 2048 + 128 * j + 64]; sq1 = w[:, 2048 + 128 * j + 64: 2048 + 128 * j + 128]
    n = j // 2
    sk = w[:, 2560 + 64 * n: 2560 + 64 * n + 64]; sv = w[:, 2688 + 64 * n: 2688 + 64 * n + 64]
    wcore = np.concatenate([cols(0), cols(256), cols(512), cols(1024), cols(1280), cols(1536),
                            sq0, sq0[:, perm], sq1, sq1[:, perm], sk, sk[:, perm],
                            cols(768), cols(1792), sv], axis=1)
    c = inp['c'][b]; cctx = inp['c_ctx']
    ccol = np.concatenate([c.reshape(8, 128).T, cctx.reshape(8, 128).T], axis=1)
    lbl = np.stack([inp['lb_logits'][0, 0, 64*j:64*j+64], inp['lb_logits'][1, 0, 64*j:64*j+64],
                    inp['lb_logits'][0, 1, 64*j:64*j+64], inp['lb_logits'][1, 1, 64*j:64*j+64]], axis=1)
    rpb_ext = np.concatenate([inp['na_rpb'][l, j].reshape(-1), np.array([NEG], np.float32)])
    nab = rpb_ext[_NAIDX]
    C, S = rope_tables(n_lat)
    cst, vmask, swm = const_tables()
    f = lambda a: np.ascontiguousarray(a, dtype=np.float32)
    return {"xT": f(xT_b), "wcore": f(wcore), "ccol": f(ccol), "wada": f(inp['w_ada'][l][:, :2048]),
            "badac": f(inp['b_ada'][l][:2048].reshape(16, 128).T), "lbl": f(lbl), "gnorm": f(inp['hgrn_norm'][l][:, None]),
            "nab": f(nab), "swm": f(swm), "sink": f(inp['swa_sink'][l][None, 2*j:2*j+2]), "ropeC": f(C), "ropeS": f(S),
            "cst": f(cst), "vmask": f(vmask)}


ALPHA = float((2.0 * 2) ** 0.25)
LN_EPS = 1e-5
NEGBIG = 1e30


def bc_mid(ap2d, n):
    p, k = ap2d.shape
    return ap2d.unsqueeze(2).broadcast_to([p, k, n])


def emit_phase_b(nc, S, io, has_ctx, uid, n_lat_tiles=16, n_exp=16):
    NTI = n_lat_tiles + (1 if has_ctx else 0)
    NT = NTI * 128
    D = 1024
    stop = 99
    yT_lat = io["yT_lat"]; yT_ctx = io.get("yT_ctx"); x = io["x"]; ccol = io["ccol"]; wada = io["wada"]; bada = io["bada"]; lnp = io["lnp"]
    wout = io["wout"]; wr = io["wr"]; rb = io["rb"]; wg = io["wg"]; wu = io["wu"]; wd = io["wd"]; ident_d = io["ident"]
    xo = io["xo"]; x1s = io["x1s"]; xT_lat_o = io.get("xT_lat_o"); xT_ctx_o = io.get("xT_ctx_o")
    es = contextlib.ExitStack()
    with es:
        sb = lambda name, shape, dt=F32, st=es: st.enter_context(nc.sbuf_tensor(uid + name, shape, dt))
        pst = lambda name, shape, dt=F32, st=es: st.enter_context(nc.psum_tensor(uid + name, shape, dt))

        h2T = sb("h2T", [128, 8, NT], BF16)
        gates = sb("gates", [128, NTI, 16])
        G2 = sb("G2", [128, D]); G2c = sb("G2c", [128, D]); LN2G = sb("LN2G", [128, D]); LN2B = sb("LN2B", [128, D])
        ident = sb("ident_sb", [128, 128])
        ones1 = sb("ones1", [1, 128])
        epsT = sb("epsT", [128, 1])
        S.dma("sp", ident[:], ident_d[:], writes=["ident"])
        S.op("dve", lambda e: e.memset(ones1[:], 1.0), writes=["ones1"])
        S.op("dve", lambda e: e.memset(epsT[:], LN_EPS), writes=["epsT"])

        st1 = contextlib.ExitStack()
        with st1:
            GA = sb("GA", [128, D], st=st1); BA = sb("BA", [128, D], st=st1)
            A2 = sb("A2", [128, D], st=st1); B2 = sb("B2", [128, D], st=st1)
            A2c = sb("A2c", [128, D], st=st1); B2c = sb("B2c", [128, D], st=st1)
            G1 = sb("G1", [128, D], st=st1); G1c = sb("G1c", [128, D], st=st1)
            bc_d = io.get("bc_d")
            bc_tiles = [G1, G1c, A2, A2c, B2, B2c, GA, BA, G2, G2c, LN2G, LN2B]
            if bc_d is not None and not io.get("bc_first", True):
                for ti_, tl_ in enumerate(bc_tiles):
                    S.dma("sp", tl_[:], bc_d[ti_], writes=["bct%d" % ti_])
                S.barrier()
            else:
                st0 = contextlib.ExitStack()
                with st0:
                    cc = sb("cc", [128, 16], st=st0)
                    scc = sb("scc", [128, 16], st=st0)
                    cb = sb("cb", [128, 16, 128], st=st0)
                    wa = [sb("wa%d" % i, [128, 8, 512], st=st0) for i in range(2)]
                    bd = [sb("bd%d" % i, [1, 512], st=st0) for i in range(2)]
                    SC2 = sb("SC2", [128, D], st=st0); SC2c = sb("SC2c", [128, D], st=st0)
                    SH2 = sb("SH2", [128, D], st=st0); SH2c = sb("SH2c", [128, D], st=st0)
                    LN1G = sb("LN1G", [128, D], st=st0); LN1B = sb("LN1B", [128, D], st=st0)
                    psm = [pst("psm%d" % i, [128, 512], st=st0) for i in range(4)]
                    S.dma("sp", cc[:], ccol[:], writes=["cc"])
                    S.op("act", lambda e: e.activation(out=scc[:], in_=cc[:], func=AF.Silu), reads=["cc"], writes=["scc"])
                    S.op("dve", lambda e: e.tensor_copy(out=cb[:], in_=bc_mid(scc[:], 128)), reads=["scc"], writes=["cb"])
                    wada_v = wada.rearrange("(k p) n -> p k n", p=128)
                    dests = [(G1, G1c), (SH2, SH2c), (SC2, SC2c), (G2, G2c)]
                    pi = 0
                    hi = 0
                    for ch in range(4):
                        for half in range(2):
                            cs = ch * 1024 + half * 512
                            wb = wa[hi % 2]; wtok = "wa%d" % (hi % 2); bdt = bd[hi % 2]; btok = "bd%d" % (hi % 2); hi += 1
                            S.dma("sp", wb[:], wada_v[:, :, cs:cs + 512], writes=[wtok])
                            S.dma("sp", bdt[:], bada[:, cs:cs + 512], writes=[btok])
                            for which in range(2):
                                p_ = psm[pi % 4]; ptok = "psm%d" % (pi % 4); pi += 1
                                for k in range(8):
                                    S.op("pe", lambda e, k=k, p_=p_, which=which, wb=wb: e.matmul(
                                        p_[:], lhsT=cb[:, which * 8 + k, :], rhs=wb[:, k, :],
                                        start=(k == 0), stop=False),
                                        reads=["cb", wtok], writes=[ptok])
                                S.op("pe", lambda e, p_=p_, bdt=bdt: e.matmul(p_[:], lhsT=ones1[:], rhs=bdt[:],
                                                                        start=False, stop=True),
                                     reads=["ones1", btok], writes=[ptok])
                                dst = dests[ch][which]
                                S.op("act", lambda e, dst=dst, p_=p_, half=half: e.activation(
                                    out=dst[:, half * 512:(half + 1) * 512], in_=p_[:], func=AF.Copy),
                                    reads=[ptok], writes=["mod%d_%d" % (ch, which)])
                    if stop <= -1:
                        pass
                    lnd = [LN1G, LN1B, LN2G, LN2B]
                    for j in range(4):
                        for half in range(2):
                            p_ = psm[pi % 4]; ptok = "psm%d" % (pi % 4); pi += 1
                            cs = j * 1024 + half * 512
                            bdt = bd[hi % 2]; btok = "bd%d" % (hi % 2); hi += 1
                            S.dma("sp", bdt[:], lnp[:, cs:cs + 512], writes=[btok])
                            S.op("pe", lambda e, p_=p_, bdt=bdt: e.matmul(p_[:], lhsT=ones1[:], rhs=bdt[:],
                                                                    start=True, stop=True),
                                 reads=["ones1", btok], writes=[ptok])
                            S.op("act", lambda e, j=j, p_=p_, half=half: e.activation(
                                out=lnd[j][:, half * 512:(half + 1) * 512], in_=p_[:], func=AF.Copy),
                                reads=[ptok], writes=["lnd%d" % j])
                    if stop <= -0.5:
                        pass
                    S.barrier()
                    S.op("dve", lambda e: e.tensor_scalar(out=GA[:], in0=LN1G[:], scalar1=ALPHA, scalar2=None, op0=ALU.mult))
                    S.op("dve", lambda e: e.tensor_scalar(out=BA[:], in0=LN1B[:], scalar1=ALPHA, scalar2=None, op0=ALU.mult))
                    for ci, (sc, sh, a2, b2) in enumerate(((SC2, SH2, A2, B2), (SC2c, SH2c, A2c, B2c))):
                        S.op("dve", lambda e, sc=sc: e.tensor_scalar(out=sc[:], in0=sc[:], scalar1=1.0, scalar2=None, op0=ALU.add),
                             writes=["sc1p%d" % ci])
                        S.op("dve", lambda e, sc=sc, a2=a2: e.tensor_tensor(out=a2[:], in0=LN1G[:], in1=sc[:], op=ALU.mult),
                             reads=["sc1p%d" % ci])
                        S.op("dve", lambda e, sc=sc, b2=b2: e.tensor_tensor(out=b2[:], in0=LN1B[:], in1=sc[:], op=ALU.mult),
                             reads=["sc1p%d" % ci], writes=["b2t%d" % ci])
                        S.op("dve", lambda e, sh=sh, b2=b2: e.tensor_tensor(out=b2[:], in0=b2[:], in1=sh[:], op=ALU.add),
                             reads=["b2t%d" % ci], writes=["b2t%d" % ci])
                    S.barrier()
                if stop <= 0:
                    pass
                if bc_d is not None:
                    for ti_, tl_ in enumerate(bc_tiles):
                        S.dma("sp", bc_d[ti_], tl_[:], writes=["bcd%d" % ti_])
                    S.barrier()
            wo = sb("wo", [128, 8, D], BF16, st=st1)
            wrt = sb("wrt", [128, 8, 16], st=st1)
            rbB = sb("rbB", [128, 16], st=st1)
            rb1 = sb("rb1", [1, 16], st=st1)
            xt = [sb("xt%d" % i, [128, D], st=st1) for i in range(2)]
            yb = [sb("yb%d" % i, [128, 8, 512], BF16, st=st1) for i in range(2)]
            uu = [sb("uu%d" % i, [128, D], st=st1) for i in range(2)]
            xn = [sb("xn%d" % i, [128, D], st=st1) for i in range(2)]
            h2 = [sb("h2_%d" % i, [128, D], st=st1) for i in range(2)]
            x1a = [sb("x1a%d" % i, [128, D], st=st1) for i in range(2)]
            h2f = sb("h2f", [128, 8, 128], st=st1)
            stt = sb("stt", [128, 2, 6], st=st1)
            mv = sb("mv", [128, 2], st=st1)
            rstd = sb("rstd", [128, 1], st=st1)
            nmr = sb("nmr", [128, 1], st=st1)
            rt = [sb("rt%d" % i, [128, 16], st=st1) for i in range(6)]
            rs4 = [sb("rs4_%d" % i, [128, 4], st=st1) for i in range(4)]
            r1 = [sb("r1_%d" % i, [128, 1], st=st1) for i in range(2)]
            ps_y = [pst("ps_y%d" % i, [128, D], st=st1) for i in range(2)]
            ps_t = pst("ps_t", [128, 8, 128], st=st1)
            ps_r = pst("ps_r", [128, 16], st=st1)

            S.dma("pool", wo[:], wout.rearrange("(k p) n -> p k n", p=128), writes=["wo"])
            S.dma("sp", wrt[:], wr.rearrange("(k p) n -> p k n", p=128), writes=["wrt"])
            S.dma("sp", rb1[:], rb[:], writes=["rb1"])
            S.op("pe", lambda e: e.matmul(ps_r[:], lhsT=ones1[:], rhs=rb1[:], start=True, stop=True),
                 reads=["ones1", "rb1"], writes=["ps_r"])
            S.op("act", lambda e: e.activation(out=rbB[:], in_=ps_r[:], func=AF.Copy), reads=["ps_r"], writes=["rbB"])
            yT_v = yT_lat.rearrange("(k p) t -> p k t", p=128)
            for i in range(NTI):
                is_ctx = has_ctx and i == NTI - 1
                b2_ = i % 2
                blk = i // 4
                if i % 4 == 0:
                    ntb = min(4, NTI - i)
                    if is_ctx:
                        S.dma("pool", yb[blk % 2][:, :, :64], yT_ctx.rearrange("(k p) t -> p k t", p=128),
                              writes=["yb%d" % (blk % 2)])
                    else:
                        S.dma("pool", yb[blk % 2][:, :, :ntb * 128], yT_v[:, :, i * 128:(i + ntb) * 128],
                              writes=["yb%d" % (blk % 2)])
                S.dma("sp", xt[b2_][:], x[i * 128:(i + 1) * 128, :], writes=["xt%d" % b2_])
                ybt = yb[blk % 2]
                off = (i % 4) * 128
                for half in range(2):
                    for k in range(8):
                        S.op("pe", lambda e, k=k, half=half, ybt=ybt, off=off, b2_=b2_: e.matmul(
                            ps_y[b2_][:, half * 512:(half + 1) * 512], lhsT=ybt[:, k, off:off + 128],
                            rhs=wo[:, k, half * 512:(half + 1) * 512], start=(k == 0), stop=(k == 7)),
                            reads=["yb%d" % (blk % 2), "wo"], writes=["ps_y%d" % b2_])
                if stop <= 0.1:
                    pass
                g1t = G1c if is_ctx else G1
                a2t = A2c if is_ctx else A2
                b2t = B2c if is_ctx else B2
                u_ = uu[b2_]; utok = "uu%d" % b2_
                S.op("dve", lambda e, u_=u_, b2_=b2_, g1t=g1t: e.tensor_tensor(out=u_[:], in0=ps_y[b2_][:], in1=g1t[:], op=ALU.mult),
                     reads=["ps_y%d" % b2_], writes=[utok])
                S.op("dve", lambda e, u_=u_, b2_=b2_: e.scalar_tensor_tensor(out=u_[:], in0=xt[b2_][:], scalar=ALPHA, in1=u_[:],
                                                                   op0=ALU.mult, op1=ALU.add),
                     reads=["xt%d" % b2_, utok], writes=[utok])
                if stop <= 0.2:
                    pass
                for hh in range(2):
                    S.op("dve", lambda e, hh=hh, u_=u_: e.bn_stats(out=stt[:, hh, :], in_=u_[:, hh * 512:(hh + 1) * 512]),
                         reads=[utok], writes=["stt%d" % hh])
                S.op("dve", lambda e: e.bn_aggr(out=mv[:], in_=stt[:].rearrange("p a b -> p (a b)")),
                     reads=["stt0", "stt1"], writes=["mv"])
                S.op("act", lambda e: e.activation(out=rstd[:], in_=mv[:, 1:2], func=AF.Sqrt, bias=epsT[:, 0:1], scale=1.0),
                     reads=["mv", "epsT"], writes=["rstd"])
                S.op("dve", lambda e: e.reciprocal(out=rstd[:], in_=rstd[:]), reads=["rstd"], writes=["rstd"])
                S.op("dve", lambda e: e.scalar_tensor_tensor(out=nmr[:], in0=mv[:, 0:1], scalar=-1.0, in1=rstd[:],
                                                             op0=ALU.mult, op1=ALU.mult),
                     reads=["mv", "rstd"], writes=["nmr"])
                if stop <= 0.3:
                    pass
                xn_ = xn[b2_]; xtok = "xn%d" % b2_
                S.op("act", lambda e, xn_=xn_, u_=u_: e.activation(out=xn_[:], in_=u_[:], func=AF.Identity,
                                                                   bias=nmr[:, 0:1], scale=rstd[:, 0:1]),
                     reads=[utok, "nmr", "rstd"], writes=[xtok])
                if stop <= 0.4:
                    pass
                xa_ = x1a[b2_]; xatok = "x1a%d" % b2_
                S.op("pool", lambda e, xa_=xa_, xn_=xn_: e.tensor_tensor(out=xa_[:], in0=xn_[:], in1=GA[:], op=ALU.mult),
                     reads=[xtok], writes=[xatok])
                S.op("pool", lambda e, xa_=xa_: e.tensor_tensor(out=xa_[:], in0=xa_[:], in1=BA[:], op=ALU.add),
                     reads=[xatok], writes=[xatok])
                S.dma("sp", x1s[i * 128:(i + 1) * 128, :], xa_[:], reads=[xatok], writes=["x1s%d" % i])
                h_ = h2[b2_]; htok = "h2_%d" % b2_
                S.op("dve", lambda e, h_=h_, xn_=xn_, a2t=a2t: e.tensor_tensor(out=h_[:], in0=xn_[:], in1=a2t[:], op=ALU.mult),
                     reads=[xtok], writes=[htok])
                S.op("dve", lambda e, h_=h_, b2t=b2t: e.tensor_tensor(out=h_[:], in0=h_[:], in1=b2t[:], op=ALU.add),
                     reads=[htok], writes=[htok])
                if stop <= 0.5:
                    pass
                for k in range(8):
                    S.op("pe", lambda e, k=k, h_=h_: e.transpose(out=ps_t[:, k, :], in_=h_[:, k * 128:(k + 1) * 128], identity=ident[:]),
                         reads=[htok, "ident"], writes=["ps_t"])
                S.op("dve", lambda e: e.tensor_copy(out=h2f[:], in_=ps_t[:]), reads=["ps_t"], writes=["h2f"])
                S.op("act", lambda e, i=i: e.activation(out=h2T[:, :, i * 128:(i + 1) * 128], in_=h2f[:], func=AF.Copy),
                     reads=["h2f"], writes=["h2T_%d" % i])
                if stop <= 0.6:
                    pass
                for k in range(8):
                    S.op("pe", lambda e, k=k: e.matmul(ps_r[:], lhsT=h2f[:, k, :], rhs=wrt[:, k, :], start=(k == 0), stop=(k == 7)),
                         reads=["h2f", "wrt"], writes=["ps_r"])
                if stop <= 0.7:
                    pass
                s_, sel_, msk, t16, w_, selm = rt
                m1, m2, grp, ing = rs4
                gm, wsum = r1
                V3 = lambda a: a[:].rearrange("p (g i) -> p g i", g=4)
                S.op("act", lambda e: e.activation(out=s_[:], in_=ps_r[:], func=AF.Sigmoid), reads=["ps_r"], writes=["r_s"])
                S.op("dve", lambda e: e.tensor_tensor(out=sel_[:], in0=s_[:], in1=rbB[:], op=ALU.add), reads=["r_s", "rbB"], writes=["r_sel"])
                S.op("dve", lambda e: e.tensor_reduce(out=m1[:], in_=V3(sel_), axis=AX.X, op=ALU.max), reads=["r_sel"], writes=["r_m1"])
                S.op("dve", lambda e: e.tensor_tensor(out=V3(msk), in0=V3(sel_), in1=bc_mid(m1[:], 4), op=ALU.is_ge),
                     reads=["r_sel", "r_m1"], writes=["r_msk"])
                S.op("dve", lambda e: e.scalar_tensor_tensor(out=t16[:], in0=msk[:], scalar=-NEGBIG, in1=sel_[:], op0=ALU.mult, op1=ALU.add),
                     reads=["r_msk", "r_sel"], writes=["r_t16"])
                S.op("dve", lambda e: e.tensor_reduce(out=m2[:], in_=V3(t16), axis=AX.X, op=ALU.max), reads=["r_t16"], writes=["r_m2"])
                S.op("dve", lambda e: e.tensor_tensor(out=grp[:], in0=m1[:], in1=m2[:], op=ALU.add), reads=["r_m1", "r_m2"], writes=["r_grp"])
                S.op("dve", lambda e: e.tensor_reduce(out=gm[:], in_=grp[:], axis=AX.X, op=ALU.max), reads=["r_grp"], writes=["r_gm"])
                S.op("dve", lambda e: e.tensor_scalar(out=ing[:], in0=grp[:], scalar1=gm[:, 0:1], scalar2=None, op0=ALU.is_ge),
                     reads=["r_grp", "r_gm"], writes=["r_ing"])
                S.op("dve", lambda e: e.tensor_tensor(out=V3(selm), in0=V3(sel_), in1=bc_mid(m2[:], 4), op=ALU.is_ge),
                     reads=["r_sel", "r_m2"], writes=["r_selm"])
                S.op("dve", lambda e: e.tensor_tensor(out=V3(selm), in0=V3(selm), in1=bc_mid(ing[:], 4), op=ALU.mult),
                     reads=["r_selm", "r_ing"], writes=["r_selm"])
                S.op("dve", lambda e: e.tensor_tensor(out=w_[:], in0=s_[:], in1=selm[:], op=ALU.mult), reads=["r_s", "r_selm"], writes=["r_w"])
                S.op("dve", lambda e: e.tensor_reduce(out=wsum[:], in_=w_[:], axis=AX.X, op=ALU.add), reads=["r_w"], writes=["r_ws"])
                S.op("dve", lambda e: e.reciprocal(out=wsum[:], in_=wsum[:]), reads=["r_ws"], writes=["r_ws"])
                S.op("dve", lambda e, i=i: e.tensor_scalar(out=gates[:, i, :], in0=w_[:], scalar1=wsum[:, 0:1], scalar2=None, op0=ALU.mult),
                     reads=["r_w", "r_ws"], writes=["gates%d" % i])
            S.barrier()
        if stop <= 1:
            pass
        st2 = contextlib.ExitStack()
        with st2:
            acc = sb("acc", [128, NTI, D], st=st2)
            st2w = contextlib.ExitStack()
            wgb = [sb("wgb%d" % i, [128, 8, 512], BF16, st=st2w) for i in range(2)]
            wub = [sb("wub%d" % i, [128, 8, 512], BF16, st=st2w) for i in range(2)]
            wdb = [sb("wdb%d" % i, [128, 4, D], BF16, st=st2w) for i in range(2)]
            sg = [sb("sg%d" % i, [128, 512], st=st2w) for i in range(2)]
            heT = [sb("heT%d" % i, [128, 4, 512], BF16, st=st2w) for i in range(2)]
            ps_g = [pst("ps_g%d" % i, [128, 512], st=st2w) for i in range(2)]
            ps_u = [pst("ps_u%d" % i, [128, 512], st=st2w) for i in range(2)]
            ps_d = [pst("ps_d%d" % i, [128, D], st=st2w) for i in range(2)]
            blocks = []
            i = 0
            while i < NTI:
                n = min(4, NTI - i)
                blocks.append((i, n))
                i += n

            def load_w(e_):
                p = e_ % 2
                S.dma("pool", wgb[p][:], wg[e_].rearrange("(k p) n -> p k n", p=128), writes=["wgb%d" % p])
                S.dma("pool", wub[p][:], wu[e_].rearrange("(k p) n -> p k n", p=128), writes=["wub%d" % p])
                S.dma("pool", wdb[p][:], wd[e_].rearrange("(k p) n -> p k n", p=128), writes=["wdb%d" % p])

            load_w(0)
            if n_exp > 1:
                load_w(1)
            fcn = [0]; dn = [0]
            steps = [(e_, bi_) for e_ in range(n_exp) for bi_ in range(len(blocks))]

            def stage_G(k):
                e_, bi_ = steps[k]
                p = e_ % 2
                t0, ntb = blocks[bi_]
                hb = k % 2
                ncol = ntb * 128
                for fc in range(4):
                    q = fcn[0] % 2; fcn[0] += 1
                    for kk in range(8):
                        S.op("pe", lambda e, kk=kk, fc=fc, q=q, p=p, t0=t0, ncol=ncol: e.matmul(
                            ps_g[q][:, :ncol], lhsT=wgb[p][:, kk, fc * 128:(fc + 1) * 128],
                            rhs=h2T[:, kk, t0 * 128:t0 * 128 + ncol], start=(kk == 0), stop=(kk == 7)),
                            reads=["wgb%d" % p], writes=["ps_g%d" % q])
                    for kk in range(8):
                        S.op("pe", lambda e, kk=kk, fc=fc, q=q, p=p, t0=t0, ncol=ncol: e.matmul(
                            ps_u[q][:, :ncol], lhsT=wub[p][:, kk, fc * 128:(fc + 1) * 128],
                            rhs=h2T[:, kk, t0 * 128:t0 * 128 + ncol], start=(kk == 0), stop=(kk == 7)),
                            reads=["wub%d" % p], writes=["ps_u%d" % q])
                    S.op("act", lambda e, q=q, ncol=ncol: e.activation(out=sg[q][:, :ncol], in_=ps_g[q][:, :ncol], func=AF.Silu),
                         reads=["ps_g%d" % q], writes=["sg%d" % q])
                    S.op("dve", lambda e, q=q, ncol=ncol, hb=hb, fc=fc: e.tensor_tensor(
                        out=heT[hb][:, fc, :ncol], in0=ps_u[q][:, :ncol], in1=sg[q][:, :ncol], op=ALU.mult),
                        reads=["ps_u%d" % q, "sg%d" % q], writes=["heT%d_%d" % (hb, fc)])

            def stage_D(k):
                e_, bi_ = steps[k]
                p = e_ % 2
                t0, ntb = blocks[bi_]
                hb = k % 2
                for tt in range(ntb):
                    ti = t0 + tt
                    dq = dn[0] % 2; dn[0] += 1
                    for half in range(2):
                        for fc in range(4):
                            S.op("pe", lambda e, fc=fc, half=half, dq=dq, hb=hb, tt=tt, p=p: e.matmul(
                                ps_d[dq][:, half * 512:(half + 1) * 512], lhsT=heT[hb][:, fc, tt * 128:(tt + 1) * 128],
                                rhs=wdb[p][:, fc, half * 512:(half + 1) * 512], start=(fc == 0), stop=(fc == 3)),
                                reads=["heT%d_%d" % (hb, fc), "wdb%d" % p], writes=["ps_d%d" % dq])
                    if e_ == 0:
                        S.op("dve", lambda e, ti=ti, dq=dq, e_=e_: e.tensor_scalar(
                            out=acc[:, ti, :], in0=ps_d[dq][:], scalar1=gates[:, ti, e_:e_ + 1], scalar2=None, op0=ALU.mult),
                            reads=["ps_d%d" % dq], writes=["acc%d" % ti])
                    else:
                        S.op("dve", lambda e, ti=ti, dq=dq, e_=e_: e.scalar_tensor_tensor(
                            out=acc[:, ti, :], in0=ps_d[dq][:], scalar=gates[:, ti, e_:e_ + 1], in1=acc[:, ti, :],
                            op0=ALU.mult, op1=ALU.add),
                            reads=["ps_d%d" % dq, "acc%d" % ti], writes=["acc%d" % ti])
                if bi_ == len(blocks) - 1 and e_ + 2 < n_exp:
                    load_w(e_ + 2)

            for k in range(len(steps) + 1):
                if k < len(steps):
                    stage_G(k)
                if k >= 1:
                    stage_D(k - 1)
            S.barrier()
            st2w.close()
            xtT = sb("xtT", [128, 8, 128], st=st2)
            ps_x = pst("ps_x", [128, 8, 128], st=st2)
            xr = [sb("xr%d" % i, [128, D], st=st2) for i in range(2)]
            u3 = [sb("u3_%d" % i, [128, D], st=st2) for i in range(2)]
            stt3 = sb("stt3", [128, 2, 6], st=st2)
            mv3 = sb("mv3", [128, 2], st=st2)
            rstd3 = sb("rstd3", [128, 1], st=st2)
            nmr3 = sb("nmr3", [128, 1], st=st2)
            for i in range(NTI):
                is_ctx = has_ctx and i == NTI - 1
                b2_ = i % 2
                g2t = G2c if is_ctx else G2
                S.dma("sp", xr[b2_][:], x1s[i * 128:(i + 1) * 128, :], reads=["x1s%d" % i], writes=["xr%d" % b2_])
                u_ = u3[b2_]; utok = "u3_%d" % b2_
                S.op("dve", lambda e, u_=u_, i=i, g2t=g2t: e.tensor_tensor(out=u_[:], in0=acc[:, i, :], in1=g2t[:], op=ALU.mult),
                     writes=[utok])
                S.op("dve", lambda e, u_=u_, b2_=b2_: e.tensor_tensor(out=u_[:], in0=u_[:], in1=xr[b2_][:], op=ALU.add),
                     reads=[utok, "xr%d" % b2_], writes=[utok])
                for hh in range(2):
                    S.op("dve", lambda e, hh=hh, u_=u_: e.bn_stats(out=stt3[:, hh, :], in_=u_[:, hh * 512:(hh + 1) * 512]),
                         reads=[utok], writes=["stt3_%d" % hh])
                S.op("dve", lambda e: e.bn_aggr(out=mv3[:], in_=stt3[:].rearrange("p a b -> p (a b)")),
                     reads=["stt3_0", "stt3_1"], writes=["mv3"])
                S.op("act", lambda e: e.activation(out=rstd3[:], in_=mv3[:, 1:2], func=AF.Sqrt, bias=epsT[:, 0:1], scale=1.0),
                     reads=["mv3"], writes=["rstd3"])
                S.op("dve", lambda e: e.reciprocal(out=rstd3[:], in_=rstd3[:]), reads=["rstd3"], writes=["rstd3"])
                S.op("dve", lambda e: e.scalar_tensor_tensor(out=nmr3[:], in0=mv3[:, 0:1], scalar=-1.0, in1=rstd3[:],
                                                             op0=ALU.mult, op1=ALU.mult),
                     reads=["mv3", "rstd3"], writes=["nmr3"])
                S.op("act", lambda e, u_=u_: e.activation(out=u_[:], in_=u_[:], func=AF.Identity, bias=nmr3[:, 0:1], scale=rstd3[:, 0:1]),
                     reads=[utok, "nmr3", "rstd3"], writes=[utok])
                S.op("pool", lambda e, u_=u_: e.tensor_tensor(out=u_[:], in0=u_[:], in1=LN2G[:], op=ALU.mult), reads=[utok], writes=[utok])
                S.op("pool", lambda e, u_=u_: e.tensor_tensor(out=u_[:], in0=u_[:], in1=LN2B[:], op=ALU.add), reads=[utok], writes=[utok])
                S.dma("sp", xo[i * 128:(i + 1) * 128, :], u_[:], reads=[utok], writes=["xo%d" % i])
                if xT_lat_o is not None:
                    for k in range(8):
                        S.op("pe", lambda e, k=k, u_=u_: e.transpose(out=ps_x[:, k, :], in_=u_[:, k * 128:(k + 1) * 128], identity=ident[:]),
                             reads=[utok, "ident"], writes=["ps_x"])
                    S.op("dve", lambda e: e.tensor_copy(out=xtT[:], in_=ps_x[:]), reads=["ps_x"], writes=["xtT"])
                    if is_ctx:
                        S.dma("sp", xT_ctx_o.rearrange("(k p) t -> p k t", p=128), xtT[:, :, 0:64], reads=["xtT"], writes=["xTo%d" % i])
                    else:
                        S.dma("sp", xT_lat_o.rearrange("(k p) t -> p k t", p=128)[:, :, i * 128:(i + 1) * 128], xtT[:], reads=["xtT"], writes=["xTo%d" % i])
            S.barrier()


def build_fused(n_lat=8192):
    T = CTX + n_lat
    NQ = n_lat // 4
    NLT = NQ // 128
    NTB = (NLT + 1) * 128
    nc = bass.Bass("TRN2", target_bir_lowering=False)
    def din(name, shape):
        return nc.dram_tensor(name, shape, F32, kind="ExternalInput").ap()
    def scr(name, shape):
        return nc.dram_tensor(name, shape, F32, kind="Internal").ap()
    I = {}
    I["xT0"] = din("xT0", [1024, T]); I["xq0"] = din("xq0", [4, NTB, 1024])
    I["wcore"] = din("wcore", [2, 4, 1024, NCOL]); I["ccol"] = din("ccol", [128, 16])
    I["wadaA"] = din("wadaA", [2, 1024, 2048]); I["badac"] = din("badac", [2, 128, 16])
    I["lbl"] = din("lbl", [4, 64, 4]); I["gnorm"] = din("gnorm", [2, 64, 1]); I["nab"] = din("nab", [2, 4, 21, 128, 128])
    I["swm"] = din("swm", [2, 128, 128]); I["sink"] = din("sink", [2, 4, 1, 2])
    I["ropeC"] = din("ropeC", [64, n_lat]); I["ropeS"] = din("ropeS", [64, n_lat])
    I["cst"] = din("cst", [128, 1024]); I["vmask"] = din("vmask", [128, 4])
    I["wadaB"] = din("wadaB", [2, 1024, 4096]); I["badaB"] = din("badaB", [2, 1, 4096]); I["lnp"] = din("lnp", [2, 1, 4096])
    I["wout"] = din("wout", [2, 1024, 1024]); I["wr"] = din("wr", [1024, 16]); I["rb"] = din("rb", [1, 16])
    I["wg"] = din("wg", [2, 16, 1024, 512]); I["wu"] = din("wu", [2, 16, 1024, 512]); I["wd"] = din("wd", [2, 16, 512, 1024])
    I["ident"] = din("ident", [128, 128])
    out = nc.dram_tensor("out", [NQ, 1024], F32, kind="ExternalOutput").ap()
    Y = scr("Y", [1024, T]); xT1 = scr("xT1", [1024, T]); X1 = scr("X1", [4, NTB, 1024]); x1s = scr("x1s", [NTB, 1024]); ofs = scr("ofs", [64, T])
    UB = scr("UB", [64, 1 + n_lat // 512, 1024]); SG = scr("SG", [64, T])
    QB = nc.dram_tensor("QB", [64, T], BF16, kind="Internal").ap()
    modc_d = scr("modc_d", [128, 32]); bc_d = scr("bc_d", [12, 128, 1024])
    es = contextlib.ExitStack()
    with es:
        S = Sched(nc, es)
        for l in range(2):
            last = l == 1
            xTsrc = I["xT0"] if l == 0 else xT1
            for j in range(4):
                io = {"xT": xTsrc, "wcore": I["wcore"][l, j], "ccol": I["ccol"], "wada": I["wadaA"][l], "badac": I["badac"][l],
                      "lbl": I["lbl"][j], "gnorm": I["gnorm"][l], "nab": I["nab"][l, j], "swm": I["swm"], "sink": I["sink"][l, j],
                      "ropeC": I["ropeC"], "ropeS": I["ropeS"], "cst": I["cst"], "vmask": I["vmask"],
                      "yT": Y[256 * j:256 * (j + 1), :], "ofs": ofs, "UB": UB, "QB": QB, "SG": SG, "modc_d": modc_d, "mod_first": j == 0}
                emit_phase_a(nc, S, io, l, not last, "a%d%d_" % (l, j), n_lat=n_lat)
            if l == 0:
                for q in range(4):
                    io = {"yT_lat": Y[:, CTX + q * NQ:CTX + (q + 1) * NQ], "yT_ctx": Y[:, q * 64:(q + 1) * 64],
                          "x": I["xq0"][q], "ccol": I["ccol"], "wada": I["wadaB"][l], "bada": I["badaB"][l],
                          "lnp": I["lnp"][l], "wout": I["wout"][l], "wr": I["wr"], "rb": I["rb"], "wg": I["wg"][l], "wu": I["wu"][l], "wd": I["wd"][l],
                          "ident": I["ident"], "x1s": x1s[0:NTB, :], "xo": X1[q],
                          "xT_lat_o": xT1[:, CTX + q * NQ:CTX + (q + 1) * NQ], "xT_ctx_o": xT1[:, q * 64:(q + 1) * 64],
                          "bc_d": bc_d, "bc_first": q == 0}
                    emit_phase_b(nc, S, io, True, "b%d%d_" % (l, q), n_lat_tiles=NLT)
            else:
                q_pool = nc.gpsimd.partition_id() % 4
                q_sp = nc.sync.partition_id() % 4
                X1f = X1.rearrange("q n d -> (q n) d")
                io = {"yT_lat": Y[:, bass.ds(q_pool * NQ + CTX, NQ)],
                      "x": X1f[bass.ds(q_sp * NTB, NQ), :], "ccol": I["ccol"], "wada": I["wadaB"][l], "bada": I["badaB"][l],
                      "lnp": I["lnp"][l], "wout": I["wout"][l], "wr": I["wr"], "rb": I["rb"], "wg": I["wg"][l], "wu": I["wu"][l], "wd": I["wd"][l],
                      "ident": I["ident"], "x1s": x1s[0:NQ, :], "xo": out}
                emit_phase_b(nc, S, io, False, "b%dq_" % l, n_lat_tiles=NLT)
        S.finish()
    return nc


def fused_inputs(inp, b, n_lat=8192):
    NQ = n_lat // 4; NLT = NQ // 128; NTB = (NLT + 1) * 128
    f = lambda a: np.ascontiguousarray(a, dtype=np.float32)
    x = inp['x'][b, :n_lat]; ctx = inp['ctx'][b]
    xT0 = np.concatenate([ctx, x], 0).T
    xq0 = np.zeros((4, NTB, 1024), np.float32)
    for q in range(4):
        xq0[q, :NQ] = x[q * NQ:(q + 1) * NQ]
        xq0[q, NQ:NQ + 64] = ctx[q * 64:(q + 1) * 64]
    per = [[core_inputs_a(inp, l, b, j, xT0, n_lat=n_lat) for j in range(4)] for l in range(2)]
    m = {"xT0": f(xT0), "xq0": xq0, "ccol": per[0][0]["ccol"], "ropeC": per[0][0]["ropeC"], "ropeS": per[0][0]["ropeS"],
         "cst": per[0][0]["cst"], "vmask": per[0][0]["vmask"], "swm": per[0][0]["swm"]}
    m["wcore"] = f(np.stack([np.stack([per[l][j]["wcore"] for j in range(4)]) for l in range(2)]))
    m["wadaA"] = f(np.stack([per[l][0]["wada"] for l in range(2)])); m["badac"] = f(np.stack([per[l][0]["badac"] for l in range(2)]))
    m["lbl"] = f(np.stack([per[0][j]["lbl"] for j in range(4)])); m["gnorm"] = f(np.stack([per[l][0]["gnorm"] for l in range(2)]))
    m["nab"] = f(np.stack([np.stack([per[l][j]["nab"] for j in range(4)]) for l in range(2)]))
    m["sink"] = f(np.stack([np.stack([per[l][j]["sink"] for j in range(4)]) for l in range(2)]))
    m["wadaB"] = f(np.stack([inp['w_ada'][l][:, 2048:] for l in range(2)])); m["badaB"] = f(np.stack([inp['b_ada'][l][None, 2048:] for l in range(2)]))
    m["lnp"] = f(np.stack([np.concatenate([inp['ln1_g'][l], inp['ln1_b'][l], inp['ln2_g'][l], inp['ln2_b'][l]])[None] for l in range(2)]))
    feat = np.zeros(1024, np.int64)
    for j in range(4):
        feat[256 * j:256 * j + 64] = 64 * j + np.arange(64)
        feat[256 * j + 64:256 * j + 128] = 256 + 64 * j + np.arange(64)
        feat[256 * j + 128:256 * j + 256] = 512 + 128 * j + np.arange(128)
    m["wout"] = f(np.stack([inp['w_out'][l][feat, :] for l in range(2)]))
    m["wr"] = f(inp['w_router']); m["rb"] = f(inp['router_bias'][None])
    m["wg"] = f(inp['w_gate']); m["wu"] = f(inp['w_up']); m["wd"] = f(inp['w_down'])
    m["ident"] = np.eye(128, dtype=np.float32)
    return m

_NC = {}


def kernel(**inputs):
    inp = {k: np.asarray(v, dtype=np.float32) for k, v in inputs.items()}
    if "nc" not in _NC:
        _NC["nc"] = build_fused(8192)
    nc = _NC["nc"]
    maps = [fused_inputs(inp, b) for b in range(2)]
    in_maps = [maps[c // 4] for c in range(8)]
    res = run_bass_kernel_spmd(nc, in_maps, core_ids=list(range(8))).results
    out = np.stack([np.concatenate([res[4 * b + q]["out"] for q in range(4)], 0) for b in range(2)], 0)
    return np.ascontiguousarray(out, dtype=np.float32)
```

```python
import contextlib
import numpy as np
import concourse.bass as bass
import concourse.mybir as mybir
from concourse.bass_utils import run_bass_kernel_spmd


F32 = mybir.dt.float32
BF16 = mybir.dt.bfloat16
AF = mybir.ActivationFunctionType
ALU = mybir.AluOpType
AX = mybir.AxisListType


class Sched:
    ENG = ("pe", "act", "dve", "pool", "sp")

    def __init__(self, nc, es, n_dma_sems=12):
        self.nc = nc
        self.e = {"pe": nc.tensor, "act": nc.scalar, "dve": nc.vector, "pool": nc.gpsimd, "sp": nc.sync}
        self.sem = {}
        self.cnt = {}
        for k in self.ENG:
            self.sem[k] = es.enter_context(nc.semaphore("s_" + k))
            self.cnt[k] = 0
        self.dq = {}
        for q in ("sp", "pool", "act"):
            n = n_dma_sems if q != "act" else 4
            self.dq[q] = {"sems": [], "vals": [0] * n, "rr": 0}
            for i in range(n):
                key = "d_%s_%d" % (q, i)
                self.sem[key] = es.enter_context(nc.semaphore(key))
                self.dq[q]["sems"].append(key)
        self.waited = {k: {} for k in self.ENG}
        self.lastw = {}
        self.readers = {}
        self.n_inst = 0
        self.n_wait = 0

    def _wait(self, eng, ev):
        if ev is None:
            return
        s, v = ev
        if s == eng and eng == "pe":
            return
        if s == eng and v <= 0:
            return
        if self.waited[eng].get(s, 0) >= v:
            return
        self.e[eng].wait_ge(self.sem[s], v)
        self.waited[eng][s] = v
        self.n_wait += 1

    def _deps(self, eng, reads, writes):
        for t in reads:
            self._wait(eng, self.lastw.get(t))
        for t in writes:
            self._wait(eng, self.lastw.get(t))
            for ev in self.readers.get(t, {}).items():
                self._wait(eng, ev)

    def _record(self, ev, reads, writes):
        s, v = ev
        for t in reads:
            r = self.readers.setdefault(t, {})
            if r.get(s, 0) < v:
                r[s] = v
        for t in writes:
            self.lastw[t] = ev
            self.readers[t] = {}

    def op(self, eng, fn, reads=(), writes=()):
        self._deps(eng, reads, writes)
        inst = fn(self.e[eng])
        self.cnt[eng] += 1
        inst.then_inc(self.sem[eng], 1)
        self._record((eng, self.cnt[eng]), reads, writes)
        self.n_inst += 1
        return inst

    def dma(self, q, out, in_, reads=(), writes=(), **kw):
        d = self.dq[q]
        i = d["rr"]
        d["rr"] = (i + 1) % len(d["sems"])
        key = d["sems"][i]
        if d["vals"][i] > 0:
            self._wait(q, (key, d["vals"][i]))
        self._deps(q, reads, writes)
        inst = self.e[q].dma_start(out=out, in_=in_, **kw)
        d["vals"][i] += 16
        inst.then_inc(self.sem[key], 16)
        self._record((key, d["vals"][i]), reads, writes)
        self.n_inst += 1
        return inst

    def all_events(self):
        evs = [(k, self.cnt[k]) for k in self.ENG if self.cnt[k] > 0]
        for q, d in self.dq.items():
            for key, v in zip(d["sems"], d["vals"]):
                if v > 0:
                    evs.append((key, v))
        return evs

    def barrier(self, engines=None):
        evs = self.all_events()
        for eng in (engines or self.ENG):
            for ev in evs:
                self._wait(eng, ev)

    def finish(self):
        self.barrier(engines=("sp",))


RMS_EPS = 1e-6
NCOL = 960
CTX = 256


def bc_mid(ap2d, n):
    p, k = ap2d.shape
    return ap2d.unsqueeze(2).broadcast_to([p, k, n])


def na_configs(nrows=128):
    cfg = {}
    mats = []
    interior = {}
    for pq in range(nrows // 2):
        rows = set()
        for r in (2 * pq, 2 * pq + 1):
            rs = min(max(r - 4, 0), nrows - 8)
            rows |= set(range(rs, rs + 8))
        pks = sorted(set(k // 2 for k in rows))
        lst = []
        for pk in pks:
            if 2 <= pq <= nrows // 2 - 3:
                key = ("i", pk - pq)
            else:
                key = (pq, pk)
            if key not in interior:
                interior[key] = len(mats)
                mats.append((pq, pk))
            lst.append((pk, interior[key]))
        cfg[pq] = lst
    return cfg, mats


def emit_phase_a(nc, S, io, layer, with_ctx_out, uid, n_lat=8192):
    T = CTX + n_lat
    D = 1024
    xT = io["xT"]; wcore = io["wcore"]; ccol = io["ccol"]; wada = io["wada"]; badac = io["badac"]; lbl = io["lbl"]
    gnorm_d = io["gnorm"]; nab_d = io["nab"]; swm_d = io["swm"]; sink_d = io["sink"]; ropeC_d = io["ropeC"]; ropeS_d = io["ropeS"]
    cst_d = io["cst"]; vmask_d = io["vmask"]; yT = io["yT"]; ofs = io["ofs"]
    stop = 99

    cfgs, mats = na_configs(n_lat // 64)
    assert len(mats) == 21
    blocks = [(0, CTX)] + [(CTX + i * 512, 512) for i in range(n_lat // 512)]
    NTILE = T // 128

    es = contextlib.ExitStack()
    with es:
        sb = lambda name, shape, dt=F32, st=es: st.enter_context(nc.sbuf_tensor(uid + "s_" + name, shape, dt))
        pst = lambda name, shape, dt=F32, st=es: st.enter_context(nc.psum_tensor(uid + "p_" + name, shape, dt))
        cst = sb("cst", [128, 128 * 4 + 512])
        identb = sb("identb", [128, 128], BF16)
        ident8b = sb("ident8b", [128, 128], BF16)
        hmF = cst[:, 256:384]; hmB = cst[:, 384:512]; rmask = cst[0:64, 512:1024]
        vmask = sb("vmask", [128, 4])
        wb = sb("wb", [128, 8, NCOL], BF16)
        modc = sb("modc", [128, 16, 2])
        lb = sb("lb", [64, 2]); oml = sb("oml", [64, 2])
        gnorm = sb("gnorm_sb", [64, 1])
        ones64 = sb("ones64", [64, 64])
        epsr = sb("epsr", [64, 1])
        nqT = sb("nqT", [64, T], BF16); nkT = sb("nkT", [64, T], BF16); nv1 = sb("nv1", [128, NTILE, 65], BF16)
        sq0T = sb("sq0T", [64, T], BF16); sq1T = sb("sq1T", [64, T], BF16); skT = sb("skT", [64, T], BF16)
        sv1 = sb("sv1", [128, NTILE, 65], BF16)
        S.dma("sp", cst[:], cst_d[:], writes=["cst"])
        S.dma("sp", vmask[:], vmask_d[:], writes=["vmask"])
        S.dma("sp", gnorm[:], gnorm_d[:], writes=["gnorm"])
        S.dma("pool", wb[:], wcore.rearrange("(k p) n -> p k n", p=128), writes=["wb"])
        S.op("dve", lambda e: e.tensor_copy(out=identb[:], in_=cst[:, 0:128]), reads=["cst"], writes=["identb"])
        S.op("dve", lambda e: e.tensor_copy(out=ident8b[:], in_=cst[:, 128:256]), reads=["cst"], writes=["ident8b"])
        S.op("dve", lambda e: e.memset(ones64[:], 1.0), writes=["ones64"])
        S.op("dve", lambda e: e.memset(epsr[:], RMS_EPS), writes=["epsr"])
        S.op("pool", lambda e: e.memset(nv1[:, :, 64:65], 1.0), writes=["nv1ones"])
        S.op("pool", lambda e: e.memset(sv1[:, :, 64:65], 1.0), writes=["sv1ones"])
        st0 = contextlib.ExitStack()
        with st0:
            lbt = sb("lbt", [64, 4], st=st0)
            S.dma("sp", lbt[:], lbl[:], writes=["lbt"])
            if layer == 0:
                S.op("dve", lambda e: e.memset(lb[:], 1e-6), writes=["lb"])
            else:
                lbv = lbt[:].rearrange("p (d l) -> p d l", l=2)
                S.op("dve", lambda e: e.tensor_tensor(out=lb[:], in0=lbv[:, :, 0], in1=lbv[:, :, 1], op=ALU.subtract),
                     reads=["lbt"], writes=["lb"])
                S.op("act", lambda e: e.activation(out=lb[:], in_=lb[:], func=AF.Exp), reads=["lb"], writes=["lb"])
                S.op("dve", lambda e: e.tensor_scalar(out=lb[:], in0=lb[:], scalar1=1.0, scalar2=None, op0=ALU.add), reads=["lb"], writes=["lb"])
                S.op("dve", lambda e: e.reciprocal(out=lb[:], in_=lb[:]), reads=["lb"], writes=["lb"])
                S.op("dve", lambda e: e.tensor_scalar(out=lb[:], in0=lb[:], scalar1=1e-6, scalar2=None, op0=ALU.max), reads=["lb"], writes=["lb"])
            S.op("dve", lambda e: e.tensor_scalar(out=oml[:], in0=lb[:], scalar1=-1.0, scalar2=1.0, op0=ALU.mult, op1=ALU.add),
                 reads=["lb"], writes=["oml"])
            modc_d = io.get("modc_d")
            if modc_d is not None and not io.get("mod_first", True):
                S.dma("sp", modc[:].rearrange("p a b -> p (a b)"), modc_d, writes=["modc"])
                S.barrier()
            else:
                cc = sb("cc", [128, 16], st=st0); scc = sb("scc", [128, 8, 2], st=st0)
                wa = sb("wa", [128, 8, 2048], st=st0)
                bdc = sb("bdc", [128, 16], st=st0)
                psm = pst("psm", [128, 16, 2], st=st0)
                S.dma("sp", cc[:], ccol[:], writes=["cc"])
                S.dma("sp", bdc[:], badac[:], writes=["bdc"])
                S.dma("sp", wa[:], wada.rearrange("(k p) n -> p k n", p=128), writes=["wa"])
                S.op("act", lambda e: e.activation(out=scc[:].rearrange("p k w -> p w k"), in_=cc[:].rearrange("p (w k) -> p w k", w=2), func=AF.Silu),
                     reads=["cc"], writes=["scc"])
                for dch in range(16):
                    for k in range(8):
                        S.op("pe", lambda e, dch=dch, k=k: e.matmul(psm[:, dch, :], lhsT=wa[:, k, dch * 128:(dch + 1) * 128], rhs=scc[:, k, :],
                                                                    start=(k == 0), stop=(k == 7)),
                             reads=["wa", "scc"], writes=["psm"])
                S.op("dve", lambda e: e.tensor_tensor(out=modc[:], in0=psm[:], in1=bc_mid(bdc[:], 2), op=ALU.add),
                     reads=["psm", "bdc"], writes=["modc"])
                S.op("dve", lambda e: e.tensor_scalar(out=modc[:, 8:16, :], in0=modc[:, 8:16, :], scalar1=1.0, scalar2=None, op0=ALU.add),
                     reads=["modc"], writes=["modc"])
                if modc_d is not None:
                    S.dma("sp", modc_d, modc[:].rearrange("p a b -> p (a b)"), reads=["modc"], writes=["modc_d"])
                S.barrier()
        UB = io["UB"]; QB = io["QB"]; SG = io["SG"]
        dcyB = sb("dcyB", [64, len(blocks), 16])
        Sall = sb("Sall", [64, 17, 64])
        Sbf = sb("Sbf", [64, 16, 64], BF16)
        Ub = sb("Ub", [64, 16, 64])
        sgt = sb("sgt", [64, 512])
        ps_oi = pst("ps_oi", [64, 512])
        oi = sb("oi", [64, 512]); oo = sb("oo", [64, 512]); ofb = sb("ofb", [64, 512])
        qtl = sb("qtl", [64, 512], BF16)
        stp = contextlib.ExitStack()
        with stp:
            xt = sb("xt0", [128, 8, 512], st=stp)
            hT = [sb("hT%d" % i, [128, 8, 512], BF16, st=stp) for i in range(2)]
            rC = sb("rC", [64, 512], st=stp); rS = sb("rS", [64, 512], st=stp)
            g_sb = {}
            for nm in ("aq", "az", "az2", "ag", "r0", "r1"):
                g_sb[nm] = sb("g_" + nm, [64, 512], st=stp)
            tA = sb("tA", [64, 512], st=stp); tB = sb("tB", [64, 512], st=stp); tC = sb("tC", [64, 512], st=stp)
            tD = sb("tD", [64, 512], st=stp); tE = sb("tE", [64, 512], st=stp)
            tot = sb("tot", [64, 16], st=stp); dcy = sb("dcy", [64, 16], st=stp)
            ktl = sb("ktl", [64, 512], BF16, st=stp); khT = sb("khT", [64, 512], BF16, st=stp)
            qtlb = sb("qtlb", [64, 512], BF16, st=stp); ktlb = sb("ktlb", [64, 512], BF16, st=stp); khTb = sb("khTb", [64, 512], BF16, st=stp)
            dcy2 = sb("dcy2", [64, 16], st=stp)
            kh = sb("kh", [128, 4, 64], BF16, st=stp)
            vt = sb("vt", [128, 4, 64], BF16, st=stp)
            vblk = sb("vblk", [128, 4, 4, 64], BF16, st=stp)
            scT = sb("scT", [128, 128], BF16, st=stp)
            ps_f = [pst("ps_f%d" % i, [64, 512], st=stp) for i in range(2)]
            ps_tm = pst("ps_tm", [128, 192], st=stp)
            ps_kh = pst("ps_kh", [128, 64], BF16, st=stp)
            ps_U = pst("ps_U", [64, 4, 64], st=stp)
            ps_sc = pst("ps_sc", [128, 128], st=stp)
            ps_oa = pst("ps_oa", [64, 512], st=stp)
            S.op("dve", lambda e: e.memset(Sall[:, 0, :], 0.0), writes=["Sall"])
            fcnt = [0]

            def load_block(bi, par):
                t0, n = blocks[bi]
                S.dma("sp", xt[:, :, :n], xT.rearrange("(k p) t -> p k t", p=128)[:, :, t0:t0 + n], writes=["xt0"])
                w = 1 if t0 < CTX else 0
                for k in range(8):
                    eng = "dve" if k % 2 == 0 else "pool"
                    S.op(eng, lambda e, k=k, w=w, par=par, n=n: e.tensor_scalar(
                        out=hT[par][:, k, :n], in0=xt[:, k, :n], scalar1=modc[:, 8 + k, w:w + 1], scalar2=modc[:, k, w:w + 1],
                        op0=ALU.mult, op1=ALU.add), reads=["xt0", "modc"], writes=["hT%d_%d" % (par, k)])

            def proj_fm(par, n, g):
                i = fcnt[0] % 2; fcnt[0] += 1
                for k in range(8):
                    S.op("pe", lambda e, k=k, i=i, g=g, par=par, n=n: e.matmul(ps_f[i][:, :n], lhsT=wb[:, k, g * 64:(g + 1) * 64],
                                                                             rhs=hT[par][:, k, :n], start=(k == 0), stop=(k == 7)),
                         reads=["wb", "hT%d_%d" % (par, k)], writes=["ps_f%d" % i])
                return ps_f[i], "ps_f%d" % i

            def hg_elem(n, d, zname, qo, ko, kho, sfx):
                nch = n // 32
                q_ = g_sb["aq"]; z_ = g_sb[zname]; ztok = "g_" + zname
                dc = dcy if d == 0 else dcy2
                dct = "dcy" if d == 0 else "dcy2"
                S.op("act", lambda e: e.activation(out=tA[:, :n], in_=z_[:, :n], func=AF.Sigmoid), reads=[ztok], writes=["tA"]); yield
                S.op("dve", lambda e: e.tensor_scalar(out=tA[:, :n], in0=tA[:, :n], scalar1=oml[:, d:d + 1], scalar2=lb[:, d:d + 1],
                                                      op0=ALU.mult, op1=ALU.add), reads=["tA", "oml", "lb"], writes=["tA"]); yield
                S.op("act", lambda e: e.activation(out=tB[:, :n], in_=tA[:, :n], func=AF.Ln), reads=["tA"], writes=["tB"]); yield
                S.op("dve", lambda e: e.tensor_scalar(out=tA[:, :n], in0=tA[:, :n], scalar1=-1.0, scalar2=1.0, op0=ALU.mult, op1=ALU.add),
                     reads=["tA", "tB"], writes=["tA"]); yield
                S.op("dve", lambda e: e.tensor_tensor_scan(out=tC[:, :n], data0=rmask[:, :n], data1=tB[:, :n], initial=0.0,
                                                           op0=ALU.mult, op1=ALU.add), reads=["tB", "cst"], writes=["tC"]); yield
                cumv = tC[:, :n].rearrange("p (c j) -> p c j", j=32)
                S.op("dve", lambda e: e.tensor_copy(out=tot[:, :nch], in_=cumv[:, :, 31]), reads=["tC"], writes=["tot"]); yield
                S.op("act", lambda e: e.activation(out=dc[:, :nch], in_=tot[:, :nch], func=AF.Exp), reads=["tot"], writes=[dct]); yield
                S.op("dve", lambda e: e.tensor_tensor(out=tD[:, :n].rearrange("p (c j) -> p c j", j=32), in0=bc_mid(tot[:, :nch], 32), in1=cumv,
                                                      op=ALU.subtract), reads=["tot", "tC"], writes=["tD"]); yield
                if d == 0:
                    e1, e3, e1n, e3n = tC, tD, "tC", "tD"
                else:
                    S.op("dve", lambda e: e.tensor_tensor(out=tE[:, :n], in0=tD[:, :n], in1=tB[:, :n], op=ALU.add), reads=["tD", "tB"], writes=["tE"]); yield
                    S.op("dve", lambda e: e.tensor_tensor(out=tC[:, :n], in0=tC[:, :n], in1=tB[:, :n], op=ALU.subtract), reads=["tC", "tB", "tD"], writes=["tC"]); yield
                    e1, e3, e1n, e3n = tE, tC, "tE", "tC"
                S.op("act", lambda e: e.activation(out=tB[:, :n], in_=e1[:, :n], func=AF.Exp, scale=-1.0), reads=[e1n, "tD", "tE", "tC"], writes=["tB"]); yield
                S.op("act", lambda e: e.activation(out=e1[:, :n], in_=e1[:, :n], func=AF.Exp), reads=[e1n, "tB"], writes=[e1n]); yield
                S.op("act", lambda e: e.activation(out=e3[:, :n], in_=e3[:, :n], func=AF.Exp), reads=[e3n], writes=[e3n]); yield
                S.op("dve", lambda e: e.tensor_tensor(out=qo[:, :n], in0=q_[:, :n], in1=e1[:, :n], op=ALU.mult), reads=["g_aq", e1n], writes=["qtl" + sfx]); yield
                S.op("dve", lambda e: e.tensor_tensor(out=ko[:, :n], in0=tA[:, :n], in1=tB[:, :n], op=ALU.mult), reads=["tA", "tB"], writes=["ktl" + sfx]); yield
                S.op("pool", lambda e: e.tensor_tensor(out=kho[:, :n], in0=tA[:, :n], in1=e3[:, :n], op=ALU.mult), reads=["tA", e3n], writes=["khT" + sfx]); yield

            def hg_tiles_U(n, store, kho=None, sfx="", tiles=None):
                kho = khT if kho is None else kho
                ntl = n // 128
                for tl in (range(ntl) if tiles is None else tiles):
                    S.op("pe", lambda e, tl=tl: e.transpose(out=ps_kh[:], in_=kho[:, tl * 128:(tl + 1) * 128], identity=identb[0:64, 0:64]),
                         reads=["khT" + sfx, "identb"], writes=["ps_kh"])
                    S.op("act", lambda e, tl=tl: e.activation(out=kh[:, tl, :], in_=ps_kh[:], func=AF.Copy), reads=["ps_kh"], writes=["kh%d" % tl])
                    S.op("pe", lambda e, tl=tl: e.matmul(ps_U[:].rearrange("p c e -> p (c e)"), lhsT=kh[:, tl, :],
                                                         rhs=vblk[:, tl, :, :].rearrange("p c e -> p (c e)"), start=True, stop=True),
                         reads=["kh%d" % tl, "vblk"], writes=["ps_U"])
                    if store:
                        S.op("act", lambda e, tl=tl: e.activation(out=Ub[:, tl * 4:(tl + 1) * 4, :], in_=ps_U[:], func=AF.Copy),
                             reads=["ps_U"], writes=["Ub"])
                    else:
                        for cc_ in range(4):
                            c = tl * 4 + cc_
                            S.op("dve", lambda e, c=c, cc_=cc_: e.scalar_tensor_tensor(
                                out=Sall[:, c + 1, :], in0=Sall[:, c, :], scalar=dcy[:, c:c + 1], in1=ps_U[:, cc_, :], op0=ALU.mult, op1=ALU.add),
                                reads=["ps_U", "Sall", "dcy"], writes=["Sall"])

            def hg_inter(n, off, qo=None, sfx=""):
                qo = qtl if qo is None else qo
                nch = n // 32
                S.op("act", lambda e: e.activation(out=Sbf[:, :nch, :], in_=Sall[:, off:off + nch, :], func=AF.Copy), reads=["Sall"], writes=["Sbf"])
                for c in range(nch):
                    S.op("pe", lambda e, c=c: e.matmul(ps_oi[:, c * 32:(c + 1) * 32], lhsT=Sbf[:, c, :], rhs=qo[:, c * 32:(c + 1) * 32],
                                                       start=True, stop=True), reads=["Sbf", "qtl" + sfx], writes=["ps_oi"])

            def hg_intra(n, d, qo=None, ko=None, sfx="", tiles=None):
                qo = qtl if qo is None else qo
                ko = ktl if ko is None else ko
                ntl = n // 128
                hm = hmF if d == 0 else hmB
                for tl in (range(ntl) if tiles is None else tiles):
                    S.op("pe", lambda e, tl=tl: e.matmul(ps_sc[:], lhsT=ko[:, tl * 128:(tl + 1) * 128], rhs=qo[:, tl * 128:(tl + 1) * 128],
                                                         start=True, stop=True), reads=["ktl" + sfx, "qtl" + sfx], writes=["ps_sc"])
                    S.op("dve", lambda e: e.tensor_tensor(out=scT[:], in0=ps_sc[:], in1=hm, op=ALU.mult), reads=["ps_sc", "cst"], writes=["scT"])
                    S.op("pe", lambda e, tl=tl: e.matmul(ps_oa[:, tl * 128:(tl + 1) * 128], lhsT=vt[:, tl, :], rhs=scT[:], start=True, stop=True),
                         reads=["vt", "scT"], writes=["ps_oa"])

            load_block(0, 0)
            for bi in range(len(blocks)):
                par = bi % 2
                t0, n = blocks[bi]
                ntl = n // 128; nch = n // 32
                is_ctx = t0 < CTX
                if bi + 1 < len(blocks):
                    load_block(bi + 1, 1 - par)
                if not is_ctx:
                    S.dma("sp", rC[:, :n], ropeC_d[:, t0 - CTX:t0 - CTX + n], writes=["rC"])
                    S.dma("sp", rS[:, :n], ropeS_d[:, t0 - CTX:t0 - CTX + n], writes=["rS"])
                def evac(g, dst, dtok, eng="act"):
                    p_, ptok = proj_fm(par, n, g)
                    o_ = dst[:, :n] if dst.shape[1] == 512 else dst[:, t0:t0 + n]
                    if eng == "act":
                        S.op("act", lambda e: e.activation(out=o_, in_=p_[:, :n], func=AF.Copy), reads=[ptok], writes=[dtok])
                    else:
                        S.op("dve", lambda e: e.tensor_copy(out=o_, in_=p_[:, :n]), reads=[ptok], writes=[dtok])

                def rope_group(ga, gb, dst, dtok):
                    pa, patok = proj_fm(par, n, ga)
                    S.op("dve", lambda e, pa=pa: e.tensor_tensor(out=g_sb["r0"][:, :n], in0=pa[:, :n], in1=rC[:, :n], op=ALU.mult),
                         reads=[patok, "rC"], writes=["g_r0"])
                    pb, pbtok = proj_fm(par, n, gb)
                    S.op("dve", lambda e, pb=pb: e.tensor_tensor(out=g_sb["r1"][:, :n], in0=pb[:, :n], in1=rS[:, :n], op=ALU.mult),
                         reads=[pbtok, "rS"], writes=["g_r1"])
                    S.op("pool", lambda e, dst=dst: e.tensor_tensor(out=dst[:, t0:t0 + n], in0=g_sb["r0"][:, :n], in1=g_sb["r1"][:, :n], op=ALU.add),
                         reads=["g_r0", "g_r1"], writes=[dtok])

                evac(0, g_sb["aq"], "g_aq", "act")
                evac(1, g_sb["az"], "g_az", "dve")
                evac(2, g_sb["az2"], "g_az2", "act")
                for tl in range(ntl):
                    gt = t0 // 128 + tl
                    for k in range(8):
                        S.op("pe", lambda e, k=k, tl=tl, par=par: e.matmul(ps_tm[:], lhsT=hT[par][:, k, tl * 128:(tl + 1) * 128],
                                                                           rhs=wb[:, k, 768:960], start=(k == 0), stop=(k == 7)),
                             reads=["wb", "hT%d_%d" % (par, k)], writes=["ps_tm"])
                    S.op("act", lambda e, tl=tl: e.activation(out=vt[:, tl, :], in_=ps_tm[:, 0:64], func=AF.Copy), reads=["ps_tm"], writes=["vt"])
                    S.op("act", lambda e, gt=gt: e.activation(out=nv1[:, gt, 0:64], in_=ps_tm[:, 64:128], func=AF.Copy), reads=["ps_tm", "vt"], writes=["nv1_%d" % gt])
                    S.op("act", lambda e, gt=gt: e.activation(out=sv1[:, gt, 0:64], in_=ps_tm[:, 128:192], func=AF.Copy), reads=["ps_tm", "nv1_%d" % gt], writes=["sv1_%d" % gt])
                    for c in range(4):
                        S.op("pool", lambda e, tl=tl, c=c: e.tensor_scalar(out=vblk[:, tl, c, :], in0=vt[:, tl, :], scalar1=vmask[:, c:c + 1],
                                                                          scalar2=None, op0=ALU.mult), reads=["vt", "vmask"], writes=["vblk"])
                import itertools
                chain_f = hg_elem(n, 0, "az", qtl, ktl, khT, "")
                chain_b = hg_elem(n, 1, "az2", qtlb, ktlb, khTb, "b")
                others = [lambda: evac(3, g_sb["ag"], "g_ag", "dve"), lambda: evac(4, nqT, "nqT", "act"), lambda: evac(5, nkT, "nkT", "dve")]
                if is_ctx:
                    others += [lambda: evac(6, sq0T, "sq0T", "act"), lambda: evac(8, sq1T, "sq1T", "dve"), lambda: evac(10, skT, "skT", "act")]
                else:
                    others += [lambda: rope_group(6, 7, sq0T, "sq0T"), lambda: rope_group(8, 9, sq1T, "sq1T"), lambda: rope_group(10, 11, skT, "skT")]
                for oth in others:
                    for _ in range(3):
                        next(chain_f, None)
                    oth()
                for _ in chain_f:
                    pass

                def work_f():
                    for tl in range(ntl):
                        hg_tiles_U(n, store=False, tiles=[tl]); yield
                    hg_inter(n, 0); yield
                    for tl in range(ntl):
                        hg_intra(n, 0, tiles=[tl]); yield
                    S.op("act", lambda e: e.activation(out=oi[:, :n], in_=ps_oa[:, :n], func=AF.Copy), reads=["ps_oa"], writes=["oi"])
                    S.op("dve", lambda e: e.tensor_tensor(out=oo[:, :n], in0=ps_oi[:, :n], in1=oi[:, :n], op=ALU.add), reads=["ps_oi", "oi"], writes=["oo"])
                    S.op("dve", lambda e: e.tensor_copy(out=Sall[:, 0, :], in_=Sall[:, nch, :]), reads=["Sall", "Sbf"], writes=["Sall"])
                    yield
                for _ in work_f():
                    next(chain_b, None); next(chain_b, None)
                for _ in chain_b:
                    pass
                hg_tiles_U(n, store=True, kho=khTb, sfx="b")
                hg_intra(n, 1, qo=qtlb, ko=ktlb, sfx="b")
                S.op("dve", lambda e: e.tensor_tensor(out=oo[:, :n], in0=ps_oa[:, :n], in1=oo[:, :n], op=ALU.add), reads=["ps_oa", "oo"], writes=["oo"])
                S.op("dve", lambda e, bi=bi: e.tensor_copy(out=dcyB[:, bi, :nch], in_=dcy2[:, :nch]), reads=["dcy2"], writes=["dcyB"])
                S.dma("sp", ofs[:, t0:t0 + n], oo[:, :n], reads=["oo"], writes=["ofs%d" % bi])
                S.dma("sp", UB[:, bi, :nch * 64], Ub[:, :nch, :].rearrange("p c e -> p (c e)"), reads=["Ub"], writes=["UB%d" % bi])
                S.dma("sp", QB[:, t0:t0 + n], qtlb[:, :n], reads=["qtlb"], writes=["QB%d" % bi])
                S.op("act", lambda e: e.activation(out=sgt[:, :n], in_=g_sb["ag"][:, :n], func=AF.Silu), reads=["g_ag"], writes=["sgt"])
                S.op("dve", lambda e: e.tensor_scalar(out=sgt[:, :n], in0=sgt[:, :n], scalar1=gnorm[:, 0:1], scalar2=None, op0=ALU.mult),
                     reads=["sgt", "gnorm"], writes=["sgt"])
                S.dma("sp", SG[:, t0:t0 + n], sgt[:, :n], reads=["sgt"], writes=["SG%d" % bi])
            S.barrier()

        def pass2_gen():
            S.op("dve", lambda e: e.memset(Sall[:, 0, :], 0.0), writes=["Sall"])
            order2 = [0] + list(range(len(blocks) - 1, 0, -1))
            for oi_, bi in enumerate(order2):
                t0, n = blocks[bi]
                ntl = n // 128; nch = n // 32
                S.dma("sp", Ub[:, :nch, :].rearrange("p c e -> p (c e)"), UB[:, bi, :nch * 64], writes=["Ub"])
                S.dma("sp", qtl[:, :n], QB[:, t0:t0 + n], writes=["qtl"])
                S.dma("sp", ofb[:, :n], ofs[:, t0:t0 + n], writes=["ofb"])
                S.dma("sp", sgt[:, :n], SG[:, t0:t0 + n], writes=["sgt"])
                S.op("dve", lambda e: e.tensor_copy(out=Sall[:, nch, :], in_=Sall[:, 0, :]), reads=["Sall"], writes=["Sall"])
                for c in range(nch - 1, -1, -1):
                    S.op("dve", lambda e, c=c, bi=bi: e.scalar_tensor_tensor(
                        out=Sall[:, c, :], in0=Sall[:, c + 1, :], scalar=dcyB[:, bi, c:c + 1], in1=Ub[:, c, :], op0=ALU.mult, op1=ALU.add),
                        reads=["Ub", "Sall", "dcyB"], writes=["Sall"])
                hg_inter(n, 1)
                S.op("dve", lambda e: e.tensor_tensor(out=oo[:, :n], in0=ps_oi[:, :n], in1=ofb[:, :n], op=ALU.add), reads=["ps_oi", "ofb"], writes=["oo"])
                S.op("act", lambda e: e.activation(out=oi[:, :n], in_=oo[:, :n], func=AF.Square), reads=["oo", "oi"], writes=["oi"])
                pss, psstok = ps_oi, "ps_oi"
                S.op("pe", lambda e, pss=pss: e.matmul(pss[:, :n], lhsT=ones64[:], rhs=oi[:, :n], start=True, stop=True),
                     reads=["ones64", "oi"], writes=[psstok])
                S.op("act", lambda e, pss=pss: e.activation(out=oi[:, :n], in_=pss[:, :n], func=AF.Sqrt, bias=epsr[:, 0:1], scale=1.0 / 64.0),
                     reads=[psstok, "epsr"], writes=["oi"])
                S.op("dve", lambda e: e.reciprocal(out=oi[:, :n], in_=oi[:, :n]), reads=["oi"], writes=["oi"])
                S.op("dve", lambda e: e.tensor_tensor(out=oo[:, :n], in0=oo[:, :n], in1=oi[:, :n], op=ALU.mult), reads=["oo", "oi"], writes=["oo"])
                S.op("dve", lambda e: e.tensor_tensor(out=oo[:, :n], in0=oo[:, :n], in1=sgt[:, :n], op=ALU.mult), reads=["oo", "sgt"], writes=["oo"])
                S.dma("sp", yT[0:64, t0:t0 + n], oo[:, :n], reads=["oo"], writes=["yTa%d" % bi])
                yield bi
        sta = contextlib.ExitStack()
        with sta:
            nab = sb("nab", [128, 21, 128], BF16, st=sta)
            swm = sb("swm", [128, 2, 128], BF16, st=sta)
            snk = sb("snk", [1, 2], st=sta); snkB = sb("snkB", [128, 2], st=sta)
            ones1 = sb("ones1", [1, 128], st=sta)
            pT = [sb("pT%d" % i, [128, 7, 128], BF16, st=sta) for i in range(2)]
            ot = [sb("ot%d" % i, [128, 64], BF16, st=sta) for i in range(2)]
            rinv = sb("rinv", [128, 1], st=sta)
            yblk = [sb("yblk%d" % i, [64, 512], st=sta) for i in range(2)]
            ps_s = [[pst("ps_s%d_%d" % (i, j), [128, 4, 128], st=sta) for j in range(2)] for i in range(2)]
            ps_o = [pst("ps_o%d" % i, [128, 65], st=sta) for i in range(2)]
            ps_y = pst("ps_y", [64, 128], BF16, st=sta)
            S.dma("pool", nab[:], nab_d.rearrange("c k q -> k c q"), writes=["nab"])
            S.dma("pool", swm[:], swm_d.rearrange("c k q -> k c q"), writes=["swm"])
            S.dma("sp", snk[:], sink_d[:], writes=["snk"])
            S.op("dve", lambda e: e.memset(ones1[:], 1.0), writes=["ones1"])
            S.op("pe", lambda e: e.matmul(ps_o[0][:, 0:2], lhsT=ones1[:], rhs=snk[:], start=True, stop=True), reads=["ones1", "snk"], writes=["ps_o0"])
            S.op("act", lambda e: e.activation(out=snkB[:], in_=ps_o[0][:, 0:2], func=AF.Exp), reads=["ps_o0"], writes=["snkB"])
            acnt = [0]
            p2 = pass2_gen()
            p2cnt = [0]

            tiles = []

            def add_tile(qT, qtok_tile, keys, v1, sink_col, yb_, ybtok, ycol, after=None):
                tiles.append(dict(qT=qT, qt=qtok_tile, keys=keys, v1=v1, sink=sink_col, yb=yb_, ybtok=ybtok, ycol=ycol, after=after))

            def st_scores(t, i):
                keys = t["keys"]; nk = len(keys); qT = t["qT"]; qt = t["qt"]
                for j, (kT_, kt, bias) in enumerate(keys):
                    pp = ps_s[i][j // 4]; ptok = "ps_s%d_%d" % (i, j // 4)
                    S.op("pe", lambda e, pp=pp, j=j, kT_=kT_, kt=kt, bias=bias: e.matmul(
                        pp[:, j % 4, :], lhsT=kT_[:, kt * 128:(kt + 1) * 128], rhs=qT[:, qt * 128:(qt + 1) * 128],
                        start=True, stop=(bias is None)), writes=[ptok])
                    if bias is not None:
                        S.op("pe", lambda e, pp=pp, j=j, bias=bias: e.matmul(pp[:, j % 4, :], lhsT=ident8b[:], rhs=bias, start=False, stop=True),
                             reads=["nab", "swm", "ident8b"], writes=[ptok])
                n0 = min(4, nk)
                S.op("act", lambda e: e.activation(out=pT[i][:, 0:n0, :], in_=ps_s[i][0][:, 0:n0, :], func=AF.Exp, scale=0.125),
                     reads=["ps_s%d_0" % i], writes=["pT%d" % i])
                if nk > 4:
                    S.op("act", lambda e: e.activation(out=pT[i][:, 4:nk, :], in_=ps_s[i][1][:, 0:nk - 4, :], func=AF.Exp, scale=0.125),
                         reads=["ps_s%d_1" % i], writes=["pT%d" % i])

            def st_pv(t, i):
                keys = t["keys"]; nk = len(keys); v1 = t["v1"]; sink_col = t["sink"]
                for j, (kT_, kt, bias) in enumerate(keys):
                    S.op("pe", lambda e, j=j, kt=kt: e.matmul(ps_o[i][:], lhsT=pT[i][:, j, :], rhs=v1[:, kt, :], start=(j == 0), stop=(j == nk - 1)),
                         reads=["pT%d" % i], writes=["ps_o%d" % i])
                if sink_col is None:
                    S.op("dve", lambda e: e.reciprocal(out=rinv[:], in_=ps_o[i][:, 64:65]), reads=["ps_o%d" % i], writes=["rinv"])
                else:
                    S.op("dve", lambda e: e.tensor_tensor(out=rinv[:], in0=ps_o[i][:, 64:65], in1=snkB[:, sink_col:sink_col + 1], op=ALU.add),
                         reads=["ps_o%d" % i, "snkB"], writes=["rinv"])
                    S.op("dve", lambda e: e.reciprocal(out=rinv[:], in_=rinv[:]), reads=["rinv"], writes=["rinv"])
                S.op("dve", lambda e: e.tensor_scalar(out=ot[i][:], in0=ps_o[i][:, 0:64], scalar1=rinv[:, 0:1], scalar2=None, op0=ALU.mult),
                     reads=["ps_o%d" % i, "rinv"], writes=["ot%d" % i])

            def st_out(t, i):
                yb_ = t["yb"]; ycol = t["ycol"]
                S.op("pe", lambda e: e.transpose(out=ps_y[:], in_=ot[i][:], identity=identb[:]), reads=["ot%d" % i, "identb"], writes=["ps_y"])
                S.op("act", lambda e: e.activation(out=yb_[:, ycol:ycol + 128], in_=ps_y[:], func=AF.Copy), reads=["ps_y"], writes=[t["ybtok"]])
                if t["after"] is not None:
                    t["after"]()

            CT = CTX // 128
            ctx_keys_n = [(nkT, 0, None), (nkT, 1, None)]
            ctx_keys_s = [(skT, 0, None), (skT, 1, None)]
            ybc = 0
            heads_ = [("n", nqT, None, 64), ("s0", sq0T, 0, 128), ("s1", sq1T, 1, 192)]

            def mk_after(dst, src, ybtok, wtok):
                def f():
                    S.dma("sp", dst, src, reads=[ybtok], writes=[wtok])
                    p2cnt[0] += 1
                    if p2cnt[0] % 2 == 0:
                        next(p2, None)
                return f

            for (hk, qT, sink_col, yrow0) in heads_:
                if with_ctx_out:
                    yb_ = yblk[ybc % 2]; ybtok = "yblk%d" % (ybc % 2); ybc += 1
                    for qt in range(CT):
                        aft = mk_after(yT[yrow0:yrow0 + 64, 0:CTX], yb_[:, 0:CTX], ybtok, "yTc_" + hk) if qt == CT - 1 else None
                        if hk == "n":
                            add_tile(qT, qt, ctx_keys_n, nv1, None, yb_, ybtok, qt * 128, aft)
                        else:
                            add_tile(qT, qt, ctx_keys_s, sv1, sink_col, yb_, ybtok, qt * 128, aft)
                nql = n_lat // 128
                for qb in range(nql // 4):
                    yb_ = yblk[ybc % 2]; ybtok = "yblk%d" % (ybc % 2); ybc += 1
                    for qq in range(4):
                        pq = qb * 4 + qq
                        aft = mk_after(yT[yrow0:yrow0 + 64, CTX + qb * 512:CTX + (qb + 1) * 512], yb_[:], ybtok, "yT_%s_%d" % (hk, qb)) if qq == 3 else None
                        if hk == "n":
                            keys = [(nkT, CT + pk, nab[:, mi, :]) for (pk, mi) in cfgs[pq]] + ctx_keys_n
                            add_tile(qT, CT + pq, keys, nv1, None, yb_, ybtok, qq * 128, aft)
                        else:
                            keys = []
                            if pq > 0:
                                keys.append((skT, CT + pq - 1, swm[:, 0, :]))
                            keys.append((skT, CT + pq, None))
                            if pq < nql - 1:
                                keys.append((skT, CT + pq + 1, swm[:, 1, :]))
                            keys += ctx_keys_s
                            add_tile(qT, CT + pq, keys, sv1, sink_col, yb_, ybtok, qq * 128, aft)
            NTL = len(tiles)
            for n_ in range(NTL + 2):
                if n_ < NTL:
                    st_scores(tiles[n_], n_ % 2)
                if 0 <= n_ - 1 < NTL:
                    st_pv(tiles[n_ - 1], (n_ - 1) % 2)
                if 0 <= n_ - 2 < NTL:
                    st_out(tiles[n_ - 2], (n_ - 2) % 2)
            for _ in p2:
                pass
            S.barrier()

NEG = -30000.0
def rope_tables(n_lat=8192):
    t = np.arange(n_lat); row = (t // 64).astype(np.float32); col = (t % 64).astype(np.float32)
    nf = 16
    inv = (np.float32(10000.0) ** (-np.arange(nf, dtype=np.float32) / np.float32(nf))).astype(np.float32)
    C = np.zeros((64, n_lat), np.float32); S = np.zeros((64, n_lat), np.float32)
    for half, pos in ((0, row), (1, col)):
        ang = (pos[:, None] * inv[None, :]).astype(np.float32)
        c = np.cos(ang).astype(np.float32).T; s_ = np.sin(ang).astype(np.float32).T
        b = half * 32
        C[b:b+16] = c; C[b+16:b+32] = c
        S[b:b+16] = -s_; S[b+16:b+32] = s_
    return C, S
def rope_perm():
    p = np.arange(64)
    for b in (0, 32):
        p[b:b+16] = np.arange(b+16, b+32); p[b+16:b+32] = np.arange(b, b+16)
    return p
def na_index_tables(nrows=128):
    cfgs, mats = na_configs(nrows)
    idx = np.zeros((21, 128, 128), np.int64)
    k = np.arange(128); q = np.arange(128)
    for mi, (pq, pk) in enumerate(mats):
        krow = 2 * pk + k // 64; kcol = k % 64
        qrow = 2 * pq + q // 64; qcol = q % 64
        rs = np.clip(qrow - 4, 0, nrows - 8); cs = np.clip(qcol - 8, 0, 48)
        valid = (krow[:, None] >= rs[None]) & (krow[:, None] < rs[None] + 8) & (kcol[:, None] >= cs[None]) & (kcol[:, None] < cs[None] + 16)
        ridx = krow[:, None] - qrow[None] + 7
        coff = np.clip(kcol[:, None] - qcol[None] + 15, 0, 30)
        ii = np.clip(ridx, 0, 14) * 31 + coff
        idx[mi] = np.where(valid, ii, 15 * 31)
    return idx
def const_tables():
    ident = np.eye(128, dtype=np.float32)
    k = np.arange(128)
    same = (k[:, None] // 32) == (k[None] // 32)
    hmF = (same & (k[:, None] <= k[None])).astype(np.float32)
    hmB = (same & (k[:, None] >= k[None])).astype(np.float32)
    rm = np.ones((128, 512), np.float32); rm[:, ::32] = 0.0
    cst = np.concatenate([ident, 8 * ident, hmF, hmB, rm], axis=1)
    vmask = (k[:, None] // 32 == np.arange(4)[None]).astype(np.float32)
    swm = np.zeros((2, 128, 128), np.float32)
    swm[0] = np.where(k[:, None] >= k[None], 0.0, NEG)
    swm[1] = np.where(k[:, None] <= k[None], 0.0, NEG)
    return cst, vmask, swm
_NAIDX = None
def core_inputs_a(inp, l, b, j, xT_b, n_lat=8192):
    global _NAIDX
    if _NAIDX is None: _NAIDX = na_index_tables(n_lat // 64)
    w = inp['w_in'][l]
    perm = rope_perm()
    def cols(base, width=64, idx=j): return w[:, base + width * idx: base + width * (idx + 1)]
    sq0 = w[:, 2048 + 128 * j: 2048 + 128 * j + 64]; sq1 = w[:, 2048 + 128 * j + 64: 2048 + 128 * j + 128]
    n = j // 2
    sk = w[:, 2560 + 64 * n: 2560 + 64 * n + 64]; sv = w[:, 2688 + 64 * n: 2688 + 64 * n + 64]
    wcore = np.concatenate([cols(0), cols(256), cols(512), cols(1024), cols(1280), cols(1536),
                            sq0, sq0[:, perm], sq1, sq1[:, perm], sk, sk[:, perm],
                            cols(768), cols(1792), sv], axis=1)
    c = inp['c'][b]; cctx = inp['c_ctx']
    ccol = np.concatenate([c.reshape(8, 128).T, cctx.reshape(8, 128).T], axis=1)
    lbl = np.stack([inp['lb_logits'][0, 0, 64*j:64*j+64], inp['lb_logits'][1, 0, 64*j:64*j+64],
                    inp['lb_logits'][0, 1, 64*j:64*j+64], inp['lb_logits'][1, 1, 64*j:64*j+64]], axis=1)
    rpb_ext = np.concatenate([inp['na_rpb'][l, j].reshape(-1), np.array([NEG], np.float32)])
    nab = rpb_ext[_NAIDX]
    C, S = rope_tables(n_lat)
    cst, vmask, swm = const_tables()
    f = lambda a: np.ascontiguousarray(a, dtype=np.float32)
    return {"xT": f(xT_b), "wcore": f(wcore), "ccol": f(ccol), "wada": f(inp['w_ada'][l][:, :2048]),
            "badac": f(inp['b_ada'][l][:2048].reshape(16, 128).T), "lbl": f(lbl), "gnorm": f(inp['hgrn_norm'][l][:, None]),
            "nab": f(nab), "swm": f(swm), "sink": f(inp['swa_sink'][l][None, 2*j:2*j+2]), "ropeC": f(C), "ropeS": f(S),
            "cst": f(cst), "vmask": f(vmask)}


ALPHA = float((2.0 * 2) ** 0.25)
LN_EPS = 1e-5
NEGBIG = 1e30


def bc_mid(ap2d, n):
    p, k = ap2d.shape
    return ap2d.unsqueeze(2).broadcast_to([p, k, n])


def emit_phase_b(nc, S, io, has_ctx, uid, n_lat_tiles=16, n_exp=16):
    NTI = n_lat_tiles + (1 if has_ctx else 0)
    NT = NTI * 128
    D = 1024
    stop = 99
    yT_lat = io["yT_lat"]; yT_ctx = io.get("yT_ctx"); x = io["x"]; ccol = io["ccol"]; wada = io["wada"]; bada = io["bada"]; lnp = io["lnp"]
    wout = io["wout"]; wr = io["wr"]; rb = io["rb"]; wg = io["wg"]; wu = io["wu"]; wd = io["wd"]; ident_d = io["ident"]
    xo = io["xo"]; x1s = io["x1s"]; xT_lat_o = io.get("xT_lat_o"); xT_ctx_o = io.get("xT_ctx_o")
    es = contextlib.ExitStack()
    with es:
        sb = lambda name, shape, dt=F32, st=es: st.enter_context(nc.sbuf_tensor(uid + name, shape, dt))
        pst = lambda name, shape, dt=F32, st=es: st.enter_context(nc.psum_tensor(uid + name, shape, dt))

        h2T = sb("h2T", [128, 8, NT], BF16)
        gates = sb("gates", [128, NTI, 16])
        G2 = sb("G2", [128, D]); G2c = sb("G2c", [128, D]); LN2G = sb("LN2G", [128, D]); LN2B = sb("LN2B", [128, D])
        ident = sb("ident_sb", [128, 128])
        ones1 = sb("ones1", [1, 128])
        epsT = sb("epsT", [128, 1])
        S.dma("sp", ident[:], ident_d[:], writes=["ident"])
        S.op("dve", lambda e: e.memset(ones1[:], 1.0), writes=["ones1"])
        S.op("dve", lambda e: e.memset(epsT[:], LN_EPS), writes=["epsT"])

        st1 = contextlib.ExitStack()
        with st1:
            GA = sb("GA", [128, D], st=st1); BA = sb("BA", [128, D], st=st1)
            A2 = sb("A2", [128, D], st=st1); B2 = sb("B2", [128, D], st=st1)
            A2c = sb("A2c", [128, D], st=st1); B2c = sb("B2c", [128, D], st=st1)
            G1 = sb("G1", [128, D], st=st1); G1c = sb("G1c", [128, D], st=st1)
            bc_d = io.get("bc_d")
            bc_tiles = [G1, G1c, A2, A2c, B2, B2c, GA, BA, G2, G2c, LN2G, LN2B]
            if bc_d is not None and not io.get("bc_first", True):
                for ti_, tl_ in enumerate(bc_tiles):
                    S.dma("sp", tl_[:], bc_d[ti_], writes=["bct%d" % ti_])
                S.barrier()
            else:
                st0 = contextlib.ExitStack()
                with st0:
                    cc = sb("cc", [128, 16], st=st0)
                    scc = sb("scc", [128, 16], st=st0)
                    cb = sb("cb", [128, 16, 128], st=st0)
                    wa = [sb("wa%d" % i, [128, 8, 512], st=st0) for i in range(2)]
                    bd = [sb("bd%d" % i, [1, 512], st=st0) for i in range(2)]
                    SC2 = sb("SC2", [128, D], st=st0); SC2c = sb("SC2c", [128, D], st=st0)
                    SH2 = sb("SH2", [128, D], st=st0); SH2c = sb("SH2c", [128, D], st=st0)
                    LN1G = sb("LN1G", [128, D], st=st0); LN1B = sb("LN1B", [128, D], st=st0)
                    psm = [pst("psm%d" % i, [128, 512], st=st0) for i in range(4)]
                    S.dma("sp", cc[:], ccol[:], writes=["cc"])
                    S.op("act", lambda e: e.activation(out=scc[:], in_=cc[:], func=AF.Silu), reads=["cc"], writes=["scc"])
                    S.op("dve", lambda e: e.tensor_copy(out=cb[:], in_=bc_mid(scc[:], 128)), reads=["scc"], writes=["cb"])
                    wada_v = wada.rearrange("(k p) n -> p k n", p=128)
                    dests = [(G1, G1c), (SH2, SH2c), (SC2, SC2c), (G2, G2c)]
                    pi = 0
                    hi = 0
                    for ch in range(4):
                        for half in range(2):
                            cs = ch * 1024 + half * 512
                            wb = wa[hi % 2]; wtok = "wa%d" % (hi % 2); bdt = bd[hi % 2]; btok = "bd%d" % (hi % 2); hi += 1
                            S.dma("sp", wb[:], wada_v[:, :, cs:cs + 512], writes=[wtok])
                            S.dma("sp", bdt[:], bada[:, cs:cs + 512], writes=[btok])
                            for which in range(2):
                                p_ = psm[pi % 4]; ptok = "psm%d" % (pi % 4); pi += 1
                                for k in range(8):
                                    S.op("pe", lambda e, k=k, p_=p_, which=which, wb=wb: e.matmul(
                                        p_[:], lhsT=cb[:, which * 8 + k, :], rhs=wb[:, k, :],
                                        start=(k == 0), stop=False),
                                        reads=["cb", wtok], writes=[ptok])
                                S.op("pe", lambda e, p_=p_, bdt=bdt: e.matmul(p_[:], lhsT=ones1[:], rhs=bdt[:],
                                                                        start=False, stop=True),
                                     reads=["ones1", btok], writes=[ptok])
                                dst = dests[ch][which]
                                S.op("act", lambda e, dst=dst, p_=p_, half=half: e.activation(
                                    out=dst[:, half * 512:(half + 1) * 512], in_=p_[:], func=AF.Copy),
                                    reads=[ptok], writes=["mod%d_%d" % (ch, which)])
                    if stop <= -1:
                        pass
                    lnd = [LN1G, LN1B, LN2G, LN2B]
                    for j in range(4):
                        for half in range(2):
                            p_ = psm[pi % 4]; ptok = "psm%d" % (pi % 4); pi += 1
                            cs = j * 1024 + half * 512
                            bdt = bd[hi % 2]; btok = "bd%d" % (hi % 2); hi += 1
                            S.dma("sp", bdt[:], lnp[:, cs:cs + 512], writes=[btok])
                            S.op("pe", lambda e, p_=p_, bdt=bdt: e.matmul(p_[:], lhsT=ones1[:], rhs=bdt[:],
                                                                    start=True, stop=True),
                                 reads=["ones1", btok], writes=[ptok])
                            S.op("act", lambda e, j=j, p_=p_, half=half: e.activation(
                                out=lnd[j][:, half * 512:(half + 1) * 512], in_=p_[:], func=AF.Copy),
                                reads=[ptok], writes=["lnd%d" % j])
                    if stop <= -0.5:
                        pass
                    S.barrier()
                    S.op("dve", lambda e: e.tensor_scalar(out=GA[:], in0=LN1G[:], scalar1=ALPHA, scalar2=None, op0=ALU.mult))
                    S.op("dve", lambda e: e.tensor_scalar(out=BA[:], in0=LN1B[:], scalar1=ALPHA, scalar2=None, op0=ALU.mult))
                    for ci, (sc, sh, a2, b2) in enumerate(((SC2, SH2, A2, B2), (SC2c, SH2c, A2c, B2c))):
                        S.op("dve", lambda e, sc=sc: e.tensor_scalar(out=sc[:], in0=sc[:], scalar1=1.0, scalar2=None, op0=ALU.add),
                             writes=["sc1p%d" % ci])
                        S.op("dve", lambda e, sc=sc, a2=a2: e.tensor_tensor(out=a2[:], in0=LN1G[:], in1=sc[:], op=ALU.mult),
                             reads=["sc1p%d" % ci])
                        S.op("dve", lambda e, sc=sc, b2=b2: e.tensor_tensor(out=b2[:], in0=LN1B[:], in1=sc[:], op=ALU.mult),
                             reads=["sc1p%d" % ci], writes=["b2t%d" % ci])
                        S.op("dve", lambda e, sh=sh, b2=b2: e.tensor_tensor(out=b2[:], in0=b2[:], in1=sh[:], op=ALU.add),
                             reads=["b2t%d" % ci], writes=["b2t%d" % ci])
                    S.barrier()
                if stop <= 0:
                    pass
                if bc_d is not None:
                    for ti_, tl_ in enumerate(bc_tiles):
                        S.dma("sp", bc_d[ti_], tl_[:], writes=["bcd%d" % ti_])
                    S.barrier()
            wo = sb("wo", [128, 8, D], BF16, st=st1)
            wrt = sb("wrt", [128, 8, 16], st=st1)
            rbB = sb("rbB", [128, 16], st=st1)
            rb1 = sb("rb1", [1, 16], st=st1)
            xt = [sb("xt%d" % i, [128, D], st=st1) for i in range(2)]
            yb = [sb("yb%d" % i, [128, 8, 512], BF16, st=st1) for i in range(2)]
            uu = [sb("uu%d" % i, [128, D], st=st1) for i in range(2)]
            xn = [sb("xn%d" % i, [128, D], st=st1) for i in range(2)]
            h2 = [sb("h2_%d" % i, [128, D], st=st1) for i in range(2)]
            x1a = [sb("x1a%d" % i, [128, D], st=st1) for i in range(2)]
            h2f = sb("h2f", [128, 8, 128], st=st1)
            stt = sb("stt", [128, 2, 6], st=st1)
            mv = sb("mv", [128, 2], st=st1)
            rstd = sb("rstd", [128, 1], st=st1)
            nmr = sb("nmr", [128, 1], st=st1)
            rt = [sb("rt%d" % i, [128, 16], st=st1) for i in range(6)]
            rs4 = [sb("rs4_%d" % i, [128, 4], st=st1) for i in range(4)]
            r1 = [sb("r1_%d" % i, [128, 1], st=st1) for i in range(2)]
            ps_y = [pst("ps_y%d" % i, [128, D], st=st1) for i in range(2)]
            ps_t = pst("ps_t", [128, 8, 128], st=st1)
            ps_r = pst("ps_r", [128, 16], st=st1)

            S.dma("pool", wo[:], wout.rearrange("(k p) n -> p k n", p=128), writes=["wo"])
            S.dma("sp", wrt[:], wr.rearrange("(k p) n -> p k n", p=128), writes=["wrt"])
            S.dma("sp", rb1[:], rb[:], writes=["rb1"])
            S.op("pe", lambda e: e.matmul(ps_r[:], lhsT=ones1[:], rhs=rb1[:], start=True, stop=True),
                 reads=["ones1", "rb1"], writes=["ps_r"])
            S.op("act", lambda e: e.activation(out=rbB[:], in_=ps_r[:], func=AF.Copy), reads=["ps_r"], writes=["rbB"])
            yT_v = yT_lat.rearrange("(k p) t -> p k t", p=128)
            def P1(i):
                    is_ctx = has_ctx and i == NTI - 1
                    b2_ = i % 2
                    blk = i // 4
                    if i % 4 == 0:
                        ntb = min(4, NTI - i)
                        if is_ctx:
                            S.dma("pool", yb[blk % 2][:, :, :64], yT_ctx.rearrange("(k p) t -> p k t", p=128),
                                  writes=["yb%d" % (blk % 2)])
                        else:
                            S.dma("pool", yb[blk % 2][:, :, :ntb * 128], yT_v[:, :, i * 128:(i + ntb) * 128],
                                  writes=["yb%d" % (blk % 2)])
                    S.dma("sp", xt[b2_][:], x[i * 128:(i + 1) * 128, :], writes=["xt%d" % b2_])
                    ybt = yb[blk % 2]
                    off = (i % 4) * 128
                    for half in range(2):
                        for k in range(8):
                            S.op("pe", lambda e, k=k, half=half, ybt=ybt, off=off, b2_=b2_: e.matmul(
                                ps_y[b2_][:, half * 512:(half + 1) * 512], lhsT=ybt[:, k, off:off + 128],
                                rhs=wo[:, k, half * 512:(half + 1) * 512], start=(k == 0), stop=(k == 7)),
                                reads=["yb%d" % (blk % 2), "wo"], writes=["ps_y%d" % b2_])
            def P2(i):
                    is_ctx = has_ctx and i == NTI - 1
                    b2_ = i % 2
                    blk = i // 4
                    if stop <= 0.1:
                        pass
                    g1t = G1c if is_ctx else G1
                    a2t = A2c if is_ctx else A2
                    b2t = B2c if is_ctx else B2
                    u_ = uu[b2_]; utok = "uu%d" % b2_
                    S.op("dve", lambda e, u_=u_, b2_=b2_, g1t=g1t: e.tensor_tensor(out=u_[:], in0=ps_y[b2_][:], in1=g1t[:], op=ALU.mult),
                         reads=["ps_y%d" % b2_], writes=[utok])
                    S.op("dve", lambda e, u_=u_, b2_=b2_: e.scalar_tensor_tensor(out=u_[:], in0=xt[b2_][:], scalar=ALPHA, in1=u_[:],
                                                                       op0=ALU.mult, op1=ALU.add),
                         reads=["xt%d" % b2_, utok], writes=[utok])
                    if stop <= 0.2:
                        pass
                    for hh in range(2):
                        S.op("dve", lambda e, hh=hh, u_=u_: e.bn_stats(out=stt[:, hh, :], in_=u_[:, hh * 512:(hh + 1) * 512]),
                             reads=[utok], writes=["stt%d" % hh])
                    S.op("dve", lambda e: e.bn_aggr(out=mv[:], in_=stt[:].rearrange("p a b -> p (a b)")),
                         reads=["stt0", "stt1"], writes=["mv"])
                    S.op("act", lambda e: e.activation(out=rstd[:], in_=mv[:, 1:2], func=AF.Sqrt, bias=epsT[:, 0:1], scale=1.0),
                         reads=["mv", "epsT"], writes=["rstd"])
                    S.op("dve", lambda e: e.reciprocal(out=rstd[:], in_=rstd[:]), reads=["rstd"], writes=["rstd"])
                    S.op("dve", lambda e: e.scalar_tensor_tensor(out=nmr[:], in0=mv[:, 0:1], scalar=-1.0, in1=rstd[:],
                                                                 op0=ALU.mult, op1=ALU.mult),
                         reads=["mv", "rstd"], writes=["nmr"])
                    if stop <= 0.3:
                        pass
                    xn_ = xn[b2_]; xtok = "xn%d" % b2_
                    S.op("act", lambda e, xn_=xn_, u_=u_: e.activation(out=xn_[:], in_=u_[:], func=AF.Identity,
                                                                       bias=nmr[:, 0:1], scale=rstd[:, 0:1]),
                         reads=[utok, "nmr", "rstd"], writes=[xtok])
                    if stop <= 0.4:
                        pass
                    xa_ = x1a[b2_]; xatok = "x1a%d" % b2_
                    S.op("pool", lambda e, xa_=xa_, xn_=xn_: e.tensor_tensor(out=xa_[:], in0=xn_[:], in1=GA[:], op=ALU.mult),
                         reads=[xtok], writes=[xatok])
                    S.op("pool", lambda e, xa_=xa_: e.tensor_tensor(out=xa_[:], in0=xa_[:], in1=BA[:], op=ALU.add),
                         reads=[xatok], writes=[xatok])
                    S.dma("sp", x1s[i * 128:(i + 1) * 128, :], xa_[:], reads=[xatok], writes=["x1s%d" % i])
                    h_ = h2[b2_]; htok = "h2_%d" % b2_
                    S.op("dve", lambda e, h_=h_, xn_=xn_, a2t=a2t: e.tensor_tensor(out=h_[:], in0=xn_[:], in1=a2t[:], op=ALU.mult),
                         reads=[xtok], writes=[htok])
                    S.op("dve", lambda e, h_=h_, b2t=b2t: e.tensor_tensor(out=h_[:], in0=h_[:], in1=b2t[:], op=ALU.add),
                         reads=[htok], writes=[htok])
                    if stop <= 0.5:
                        pass
            def P3(i):
                    is_ctx = has_ctx and i == NTI - 1
                    b2_ = i % 2
                    blk = i // 4
                    h_ = h2[b2_]; htok = "h2_%d" % b2_
                    for k in range(8):
                        S.op("pe", lambda e, k=k, h_=h_: e.transpose(out=ps_t[:, k, :], in_=h_[:, k * 128:(k + 1) * 128], identity=ident[:]),
                             reads=[htok, "ident"], writes=["ps_t"])
                    S.op("dve", lambda e: e.tensor_copy(out=h2f[:], in_=ps_t[:]), reads=["ps_t"], writes=["h2f"])
                    S.op("act", lambda e, i=i: e.activation(out=h2T[:, :, i * 128:(i + 1) * 128], in_=h2f[:], func=AF.Copy),
                         reads=["h2f"], writes=["h2T_%d" % i])
                    if stop <= 0.6:
                        pass
                    for k in range(8):
                        S.op("pe", lambda e, k=k: e.matmul(ps_r[:], lhsT=h2f[:, k, :], rhs=wrt[:, k, :], start=(k == 0), stop=(k == 7)),
                             reads=["h2f", "wrt"], writes=["ps_r"])
                    if stop <= 0.7:
                        pass
                    s_, sel_, msk, t16, w_, selm = rt
                    m1, m2, grp, ing = rs4
                    gm, wsum = r1
                    V3 = lambda a: a[:].rearrange("p (g i) -> p g i", g=4)
                    S.op("act", lambda e: e.activation(out=s_[:], in_=ps_r[:], func=AF.Sigmoid), reads=["ps_r"], writes=["r_s"])
                    S.op("dve", lambda e: e.tensor_tensor(out=sel_[:], in0=s_[:], in1=rbB[:], op=ALU.add), reads=["r_s", "rbB"], writes=["r_sel"])
                    S.op("dve", lambda e: e.tensor_reduce(out=m1[:], in_=V3(sel_), axis=AX.X, op=ALU.max), reads=["r_sel"], writes=["r_m1"])
                    S.op("dve", lambda e: e.tensor_tensor(out=V3(msk), in0=V3(sel_), in1=bc_mid(m1[:], 4), op=ALU.is_ge),
                         reads=["r_sel", "r_m1"], writes=["r_msk"])
                    S.op("dve", lambda e: e.scalar_tensor_tensor(out=t16[:], in0=msk[:], scalar=-NEGBIG, in1=sel_[:], op0=ALU.mult, op1=ALU.add),
                         reads=["r_msk", "r_sel"], writes=["r_t16"])
                    S.op("dve", lambda e: e.tensor_reduce(out=m2[:], in_=V3(t16), axis=AX.X, op=ALU.max), reads=["r_t16"], writes=["r_m2"])
                    S.op("dve", lambda e: e.tensor_tensor(out=grp[:], in0=m1[:], in1=m2[:], op=ALU.add), reads=["r_m1", "r_m2"], writes=["r_grp"])
                    S.op("dve", lambda e: e.tensor_reduce(out=gm[:], in_=grp[:], axis=AX.X, op=ALU.max), reads=["r_grp"], writes=["r_gm"])
                    S.op("dve", lambda e: e.tensor_scalar(out=ing[:], in0=grp[:], scalar1=gm[:, 0:1], scalar2=None, op0=ALU.is_ge),
                         reads=["r_grp", "r_gm"], writes=["r_ing"])
                    S.op("dve", lambda e: e.tensor_tensor(out=V3(selm), in0=V3(sel_), in1=bc_mid(m2[:], 4), op=ALU.is_ge),
                         reads=["r_sel", "r_m2"], writes=["r_selm"])
                    S.op("dve", lambda e: e.tensor_tensor(out=V3(selm), in0=V3(selm), in1=bc_mid(ing[:], 4), op=ALU.mult),
                         reads=["r_selm", "r_ing"], writes=["r_selm"])
                    S.op("dve", lambda e: e.tensor_tensor(out=w_[:], in0=s_[:], in1=selm[:], op=ALU.mult), reads=["r_s", "r_selm"], writes=["r_w"])
                    S.op("dve", lambda e: e.tensor_reduce(out=wsum[:], in_=w_[:], axis=AX.X, op=ALU.add), reads=["r_w"], writes=["r_ws"])
                    S.op("dve", lambda e: e.reciprocal(out=wsum[:], in_=wsum[:]), reads=["r_ws"], writes=["r_ws"])
                    S.op("dve", lambda e, i=i: e.tensor_scalar(out=gates[:, i, :], in0=w_[:], scalar1=wsum[:, 0:1], scalar2=None, op0=ALU.mult),
                         reads=["r_w", "r_ws"], writes=["gates%d" % i])
            for it_ in range(NTI + 2):
                if it_ < NTI:
                    P1(it_)
                if 0 <= it_ - 1 < NTI:
                    P2(it_ - 1)
                if 0 <= it_ - 2 < NTI:
                    P3(it_ - 2)
            S.barrier()
        if stop <= 1:
            pass
        st2 = contextlib.ExitStack()
        with st2:
            acc = sb("acc", [128, NTI, D], st=st2)
            st2w = contextlib.ExitStack()
            wgb = [sb("wgb%d" % i, [128, 8, 512], BF16, st=st2w) for i in range(2)]
            wub = [sb("wub%d" % i, [128, 8, 512], BF16, st=st2w) for i in range(2)]
            wdb = [sb("wdb%d" % i, [128, 4, D], BF16, st=st2w) for i in range(2)]
            sg = [sb("sg%d" % i, [128, 512], st=st2w) for i in range(2)]
            heT = [sb("heT%d" % i, [128, 4, 512], BF16, st=st2w) for i in range(2)]
            ps_g = [pst("ps_g%d" % i, [128, 512], st=st2w) for i in range(2)]
            ps_u = [pst("ps_u%d" % i, [128, 512], st=st2w) for i in range(2)]
            ps_d = [pst("ps_d%d" % i, [128, D], st=st2w) for i in range(2)]
            blocks = []
            i = 0
            while i < NTI:
                n = min(4, NTI - i)
                blocks.append((i, n))
                i += n

            def load_w(e_):
                p = e_ % 2
                S.dma("pool", wgb[p][:], wg[e_].rearrange("(k p) n -> p k n", p=128), writes=["wgb%d" % p])
                S.dma("pool", wub[p][:], wu[e_].rearrange("(k p) n -> p k n", p=128), writes=["wub%d" % p])
                S.dma("pool", wdb[p][:], wd[e_].rearrange("(k p) n -> p k n", p=128), writes=["wdb%d" % p])

            load_w(0)
            if n_exp > 1:
                load_w(1)
            fcn = [0]; dn = [0]
            steps = [(e_, bi_) for e_ in range(n_exp) for bi_ in range(len(blocks))]

            def stage_G(k):
                e_, bi_ = steps[k]
                p = e_ % 2
                t0, ntb = blocks[bi_]
                hb = k % 2
                ncol = ntb * 128
                for fc in range(4):
                    q = fcn[0] % 2; fcn[0] += 1
                    for kk in range(8):
                        S.op("pe", lambda e, kk=kk, fc=fc, q=q, p=p, t0=t0, ncol=ncol: e.matmul(
                            ps_g[q][:, :ncol], lhsT=wgb[p][:, kk, fc * 128:(fc + 1) * 128],
                            rhs=h2T[:, kk, t0 * 128:t0 * 128 + ncol], start=(kk == 0), stop=(kk == 7)),
                            reads=["wgb%d" % p], writes=["ps_g%d" % q])
                    for kk in range(8):
                        S.op("pe", lambda e, kk=kk, fc=fc, q=q, p=p, t0=t0, ncol=ncol: e.matmul(
                            ps_u[q][:, :ncol], lhsT=wub[p][:, kk, fc * 128:(fc + 1) * 128],
                            rhs=h2T[:, kk, t0 * 128:t0 * 128 + ncol], start=(kk == 0), stop=(kk == 7)),
                            reads=["wub%d" % p], writes=["ps_u%d" % q])
                    S.op("act", lambda e, q=q, ncol=ncol: e.activation(out=sg[q][:, :ncol], in_=ps_g[q][:, :ncol], func=AF.Silu),
                         reads=["ps_g%d" % q], writes=["sg%d" % q])
                    S.op("dve", lambda e, q=q, ncol=ncol, hb=hb, fc=fc: e.tensor_tensor(
                        out=heT[hb][:, fc, :ncol], in0=ps_u[q][:, :ncol], in1=sg[q][:, :ncol], op=ALU.mult),
                        reads=["ps_u%d" % q, "sg%d" % q], writes=["heT%d_%d" % (hb, fc)])

            def stage_D(k):
                e_, bi_ = steps[k]
                p = e_ % 2
                t0, ntb = blocks[bi_]
                hb = k % 2
                for tt in range(ntb):
                    ti = t0 + tt
                    dq = dn[0] % 2; dn[0] += 1
                    for half in range(2):
                        for fc in range(4):
                            S.op("pe", lambda e, fc=fc, half=half, dq=dq, hb=hb, tt=tt, p=p: e.matmul(
                                ps_d[dq][:, half * 512:(half + 1) * 512], lhsT=heT[hb][:, fc, tt * 128:(tt + 1) * 128],
                                rhs=wdb[p][:, fc, half * 512:(half + 1) * 512], start=(fc == 0), stop=(fc == 3)),
                                reads=["heT%d_%d" % (hb, fc), "wdb%d" % p], writes=["ps_d%d" % dq])
                    if e_ == 0:
                        S.op("dve", lambda e, ti=ti, dq=dq, e_=e_: e.tensor_scalar(
                            out=acc[:, ti, :], in0=ps_d[dq][:], scalar1=gates[:, ti, e_:e_ + 1], scalar2=None, op0=ALU.mult),
                            reads=["ps_d%d" % dq], writes=["acc%d" % ti])
                    else:
                        S.op("dve", lambda e, ti=ti, dq=dq, e_=e_: e.scalar_tensor_tensor(
                            out=acc[:, ti, :], in0=ps_d[dq][:], scalar=gates[:, ti, e_:e_ + 1], in1=acc[:, ti, :],
                            op0=ALU.mult, op1=ALU.add),
                            reads=["ps_d%d" % dq, "acc%d" % ti], writes=["acc%d" % ti])
                if bi_ == len(blocks) - 1 and e_ + 2 < n_exp:
                    load_w(e_ + 2)

            for k in range(len(steps) + 1):
                if k < len(steps):
                    stage_G(k)
                if k >= 1:
                    stage_D(k - 1)
            S.barrier()
            st2w.close()
            xtT = sb("xtT", [128, 8, 128], st=st2)
            ps_x = pst("ps_x", [128, 8, 128], st=st2)
            xr = [sb("xr%d" % i, [128, D], st=st2) for i in range(2)]
            u3 = [sb("u3_%d" % i, [128, D], st=st2) for i in range(2)]
            stt3 = sb("stt3", [128, 2, 6], st=st2)
            mv3 = sb("mv3", [128, 2], st=st2)
            rstd3 = sb("rstd3", [128, 1], st=st2)
            nmr3 = sb("nmr3", [128, 1], st=st2)
            def Q1(i):
                    is_ctx = has_ctx and i == NTI - 1
                    b2_ = i % 2
                    g2t = G2c if is_ctx else G2
                    S.dma("sp", xr[b2_][:], x1s[i * 128:(i + 1) * 128, :], reads=["x1s%d" % i], writes=["xr%d" % b2_])
            def Q2(i):
                    is_ctx = has_ctx and i == NTI - 1
                    b2_ = i % 2
                    g2t = G2c if is_ctx else G2
                    u_ = u3[b2_]; utok = "u3_%d" % b2_
                    S.op("dve", lambda e, u_=u_, i=i, g2t=g2t: e.tensor_tensor(out=u_[:], in0=acc[:, i, :], in1=g2t[:], op=ALU.mult),
                         writes=[utok])
                    S.op("dve", lambda e, u_=u_, b2_=b2_: e.tensor_tensor(out=u_[:], in0=u_[:], in1=xr[b2_][:], op=ALU.add),
                         reads=[utok, "xr%d" % b2_], writes=[utok])
                    for hh in range(2):
                        S.op("dve", lambda e, hh=hh, u_=u_: e.bn_stats(out=stt3[:, hh, :], in_=u_[:, hh * 512:(hh + 1) * 512]),
                             reads=[utok], writes=["stt3_%d" % hh])
                    S.op("dve", lambda e: e.bn_aggr(out=mv3[:], in_=stt3[:].rearrange("p a b -> p (a b)")),
                         reads=["stt3_0", "stt3_1"], writes=["mv3"])
                    S.op("act", lambda e: e.activation(out=rstd3[:], in_=mv3[:, 1:2], func=AF.Sqrt, bias=epsT[:, 0:1], scale=1.0),
                         reads=["mv3"], writes=["rstd3"])
                    S.op("dve", lambda e: e.reciprocal(out=rstd3[:], in_=rstd3[:]), reads=["rstd3"], writes=["rstd3"])
                    S.op("dve", lambda e: e.scalar_tensor_tensor(out=nmr3[:], in0=mv3[:, 0:1], scalar=-1.0, in1=rstd3[:],
                                                                 op0=ALU.mult, op1=ALU.mult),
                         reads=["mv3", "rstd3"], writes=["nmr3"])
                    S.op("act", lambda e, u_=u_: e.activation(out=u_[:], in_=u_[:], func=AF.Identity, bias=nmr3[:, 0:1], scale=rstd3[:, 0:1]),
                         reads=[utok, "nmr3", "rstd3"], writes=[utok])
                    S.op("pool", lambda e, u_=u_: e.tensor_tensor(out=u_[:], in0=u_[:], in1=LN2G[:], op=ALU.mult), reads=[utok], writes=[utok])
                    S.op("pool", lambda e, u_=u_: e.tensor_tensor(out=u_[:], in0=u_[:], in1=LN2B[:], op=ALU.add), reads=[utok], writes=[utok])
                    S.dma("sp", xo[i * 128:(i + 1) * 128, :], u_[:], reads=[utok], writes=["xo%d" % i])
            def Q3(i):
                    is_ctx = has_ctx and i == NTI - 1
                    b2_ = i % 2
                    g2t = G2c if is_ctx else G2
                    u_ = u3[b2_]; utok = "u3_%d" % b2_
                    if xT_lat_o is not None:
                        for k in range(8):
                            S.op("pe", lambda e, k=k, u_=u_: e.transpose(out=ps_x[:, k, :], in_=u_[:, k * 128:(k + 1) * 128], identity=ident[:]),
                                 reads=[utok, "ident"], writes=["ps_x"])
                        S.op("dve", lambda e: e.tensor_copy(out=xtT[:], in_=ps_x[:]), reads=["ps_x"], writes=["xtT"])
                        if is_ctx:
                            S.dma("sp", xT_ctx_o.rearrange("(k p) t -> p k t", p=128), xtT[:, :, 0:64], reads=["xtT"], writes=["xTo%d" % i])
                        else:
                            S.dma("sp", xT_lat_o.rearrange("(k p) t -> p k t", p=128)[:, :, i * 128:(i + 1) * 128], xtT[:], reads=["xtT"], writes=["xTo%d" % i])
            for it_ in range(NTI + 2):
                if it_ < NTI:
                    Q1(it_)
                if 0 <= it_ - 1 < NTI:
                    Q2(it_ - 1)
                if 0 <= it_ - 2 < NTI:
                    Q3(it_ - 2)
            S.barrier()


def build_fused(n_lat=8192):
    T = CTX + n_lat
    NQ = n_lat // 4
    NLT = NQ // 128
    NTB = (NLT + 1) * 128
    nc = bass.Bass("TRN2", target_bir_lowering=False)
    def din(name, shape):
        return nc.dram_tensor(name, shape, F32, kind="ExternalInput").ap()
    def scr(name, shape):
        return nc.dram_tensor(name, shape, F32, kind="Internal").ap()
    I = {}
    I["xT0"] = din("xT0", [1024, T]); I["xq0"] = din("xq0", [4, NTB, 1024])
    I["wcore"] = din("wcore", [2, 4, 1024, NCOL]); I["ccol"] = din("ccol", [128, 16])
    I["wadaA"] = din("wadaA", [2, 1024, 2048]); I["badac"] = din("badac", [2, 128, 16])
    I["lbl"] = din("lbl", [4, 64, 4]); I["gnorm"] = din("gnorm", [2, 64, 1]); I["nab"] = din("nab", [2, 4, 21, 128, 128])
    I["swm"] = din("swm", [2, 128, 128]); I["sink"] = din("sink", [2, 4, 1, 2])
    I["ropeC"] = din("ropeC", [64, n_lat]); I["ropeS"] = din("ropeS", [64, n_lat])
    I["cst"] = din("cst", [128, 1024]); I["vmask"] = din("vmask", [128, 4])
    I["wadaB"] = din("wadaB", [2, 1024, 4096]); I["badaB"] = din("badaB", [2, 1, 4096]); I["lnp"] = din("lnp", [2, 1, 4096])
    I["wout"] = din("wout", [2, 1024, 1024]); I["wr"] = din("wr", [1024, 16]); I["rb"] = din("rb", [1, 16])
    I["wg"] = din("wg", [2, 16, 1024, 512]); I["wu"] = din("wu", [2, 16, 1024, 512]); I["wd"] = din("wd", [2, 16, 512, 1024])
    I["ident"] = din("ident", [128, 128])
    out = nc.dram_tensor("out", [NQ, 1024], F32, kind="ExternalOutput").ap()
    Y = scr("Y", [1024, T]); xT1 = scr("xT1", [1024, T]); X1 = scr("X1", [4, NTB, 1024]); x1s = scr("x1s", [NTB, 1024]); ofs = scr("ofs", [64, T])
    UB = scr("UB", [64, 1 + n_lat // 512, 1024]); SG = scr("SG", [64, T])
    QB = nc.dram_tensor("QB", [64, T], BF16, kind="Internal").ap()
    modc_d = scr("modc_d", [128, 32]); bc_d = scr("bc_d", [12, 128, 1024])
    es = contextlib.ExitStack()
    with es:
        S = Sched(nc, es)
        for l in range(2):
            last = l == 1
            xTsrc = I["xT0"] if l == 0 else xT1
            for j in range(4):
                io = {"xT": xTsrc, "wcore": I["wcore"][l, j], "ccol": I["ccol"], "wada": I["wadaA"][l], "badac": I["badac"][l],
                      "lbl": I["lbl"][j], "gnorm": I["gnorm"][l], "nab": I["nab"][l, j], "swm": I["swm"], "sink": I["sink"][l, j],
                      "ropeC": I["ropeC"], "ropeS": I["ropeS"], "cst": I["cst"], "vmask": I["vmask"],
                      "yT": Y[256 * j:256 * (j + 1), :], "ofs": ofs, "UB": UB, "QB": QB, "SG": SG, "modc_d": modc_d, "mod_first": j == 0}
                emit_phase_a(nc, S, io, l, not last, "a%d%d_" % (l, j), n_lat=n_lat)
            if l == 0:
                for q in range(4):
                    io = {"yT_lat": Y[:, CTX + q * NQ:CTX + (q + 1) * NQ], "yT_ctx": Y[:, q * 64:(q + 1) * 64],
                          "x": I["xq0"][q], "ccol": I["ccol"], "wada": I["wadaB"][l], "bada": I["badaB"][l],
                          "lnp": I["lnp"][l], "wout": I["wout"][l], "wr": I["wr"], "rb": I["rb"], "wg": I["wg"][l], "wu": I["wu"][l], "wd": I["wd"][l],
                          "ident": I["ident"], "x1s": x1s[0:NTB, :], "xo": X1[q],
                          "xT_lat_o": xT1[:, CTX + q * NQ:CTX + (q + 1) * NQ], "xT_ctx_o": xT1[:, q * 64:(q + 1) * 64],
                          "bc_d": bc_d, "bc_first": q == 0}
                    emit_phase_b(nc, S, io, True, "b%d%d_" % (l, q), n_lat_tiles=NLT)
            else:
                q_pool = nc.gpsimd.partition_id() % 4
                q_sp = nc.sync.partition_id() % 4
                X1f = X1.rearrange("q n d -> (q n) d")
                io = {"yT_lat": Y[:, bass.ds(q_pool * NQ + CTX, NQ)],
                      "x": X1f[bass.ds(q_sp * NTB, NQ), :], "ccol": I["ccol"], "wada": I["wadaB"][l], "bada": I["badaB"][l],
                      "lnp": I["lnp"][l], "wout": I["wout"][l], "wr": I["wr"], "rb": I["rb"], "wg": I["wg"][l], "wu": I["wu"][l], "wd": I["wd"][l],
                      "ident": I["ident"], "x1s": x1s[0:NQ, :], "xo": out}
                emit_phase_b(nc, S, io, False, "b%dq_" % l, n_lat_tiles=NLT)
        S.finish()
    return nc


def fused_inputs(inp, b, n_lat=8192):
    NQ = n_lat // 4; NLT = NQ // 128; NTB = (NLT + 1) * 128
    f = lambda a: np.ascontiguousarray(a, dtype=np.float32)
    x = inp['x'][b, :n_lat]; ctx = inp['ctx'][b]
    xT0 = np.concatenate([ctx, x], 0).T
    xq0 = np.zeros((4, NTB, 1024), np.float32)
    for q in range(4):
        xq0[q, :NQ] = x[q * NQ:(q + 1) * NQ]
        xq0[q, NQ:NQ + 64] = ctx[q * 64:(q + 1) * 64]
    per = [[core_inputs_a(inp, l, b, j, xT0, n_lat=n_lat) for j in range(4)] for l in range(2)]
    m = {"xT0": f(xT0), "xq0": xq0, "ccol": per[0][0]["ccol"], "ropeC": per[0][0]["ropeC"], "ropeS": per[0][0]["ropeS"],
         "cst": per[0][0]["cst"], "vmask": per[0][0]["vmask"], "swm": per[0][0]["swm"]}
    m["wcore"] = f(np.stack([np.stack([per[l][j]["wcore"] for j in range(4)]) for l in range(2)]))
    m["wadaA"] = f(np.stack([per[l][0]["wada"] for l in range(2)])); m["badac"] = f(np.stack([per[l][0]["badac"] for l in range(2)]))
    m["lbl"] = f(np.stack([per[0][j]["lbl"] for j in range(4)])); m["gnorm"] = f(np.stack([per[l][0]["gnorm"] for l in range(2)]))
    m["nab"] = f(np.stack([np.stack([per[l][j]["nab"] for j in range(4)]) for l in range(2)]))
    m["sink"] = f(np.stack([np.stack([per[l][j]["sink"] for j in range(4)]) for l in range(2)]))
    m["wadaB"] = f(np.stack([inp['w_ada'][l][:, 2048:] for l in range(2)])); m["badaB"] = f(np.stack([inp['b_ada'][l][None, 2048:] for l in range(2)]))
    m["lnp"] = f(np.stack([np.concatenate([inp['ln1_g'][l], inp['ln1_b'][l], inp['ln2_g'][l], inp['ln2_b'][l]])[None] for l in range(2)]))
    feat = np.zeros(1024, np.int64)
    for j in range(4):
        feat[256 * j:256 * j + 64] = 64 * j + np.arange(64)
        feat[256 * j + 64:256 * j + 128] = 256 + 64 * j + np.arange(64)
        feat[256 * j + 128:256 * j + 256] = 512 + 128 * j + np.arange(128)
    m["wout"] = f(np.stack([inp['w_out'][l][feat, :] for l in range(2)]))
    m["wr"] = f(inp['w_router']); m["rb"] = f(inp['router_bias'][None])
    m["wg"] = f(inp['w_gate']); m["wu"] = f(inp['w_up']); m["wd"] = f(inp['w_down'])
    m["ident"] = np.eye(128, dtype=np.float32)
    return m

_NC = {}


def kernel(**inputs):
    inp = {k: np.asarray(v, dtype=np.float32) for k, v in inputs.items()}
    if "nc" not in _NC:
        _NC["nc"] = build_fused(8192)
    nc = _NC["nc"]
    maps = [fused_inputs(inp, b) for b in range(2)]
    in_maps = [maps[c // 4] for c in range(8)]
    res = run_bass_kernel_spmd(nc, in_maps, core_ids=list(range(8))).results
    out = np.stack([np.concatenate([res[4 * b + q]["out"] for q in range(4)], 0) for b in range(2)], 0)
    return np.ascontiguousarray(out, dtype=np.float32)
```

```python
import contextlib
import numpy as np
import concourse.bass as bass
import concourse.mybir as mybir
from concourse.bass_utils import run_bass_kernel_spmd


F32 = mybir.dt.float32
BF16 = mybir.dt.bfloat16
AF = mybir.ActivationFunctionType
ALU = mybir.AluOpType
AX = mybir.AxisListType


class Sched:
    ENG = ("pe", "act", "dve", "pool", "sp")

    def __init__(self, nc, es, n_dma_sems=12):
        self.nc = nc
        self.e = {"pe": nc.tensor, "act": nc.scalar, "dve": nc.vector, "pool": nc.gpsimd, "sp": nc.sync}
        self.sem = {}
        self.cnt = {}
        for k in self.ENG:
            self.sem[k] = es.enter_context(nc.semaphore("s_" + k))
            self.cnt[k] = 0
        self.dq = {}
        for q in ("sp", "pool", "act"):
            n = n_dma_sems if q != "act" else 4
            self.dq[q] = {"sems": [], "vals": [0] * n, "rr": 0}
            for i in range(n):
                key = "d_%s_%d" % (q, i)
                self.sem[key] = es.enter_context(nc.semaphore(key))
                self.dq[q]["sems"].append(key)
        self.waited = {k: {} for k in self.ENG}
        self.lastw = {}
        self.readers = {}
        self.n_inst = 0
        self.n_wait = 0

    def _wait(self, eng, ev):
        if ev is None:
            return
        s, v = ev
        if s == eng and eng == "pe":
            return
        if s == eng and v <= 0:
            return
        if self.waited[eng].get(s, 0) >= v:
            return
        self.e[eng].wait_ge(self.sem[s], v)
        self.waited[eng][s] = v
        self.n_wait += 1

    def _deps(self, eng, reads, writes):
        for t in reads:
            self._wait(eng, self.lastw.get(t))
        for t in writes:
            self._wait(eng, self.lastw.get(t))
            for ev in self.readers.get(t, {}).items():
                self._wait(eng, ev)

    def _record(self, ev, reads, writes):
        s, v = ev
        for t in reads:
            r = self.readers.setdefault(t, {})
            if r.get(s, 0) < v:
                r[s] = v
        for t in writes:
            self.lastw[t] = ev
            self.readers[t] = {}

    def op(self, eng, fn, reads=(), writes=()):
        self._deps(eng, reads, writes)
        inst = fn(self.e[eng])
        self.cnt[eng] += 1
        inst.then_inc(self.sem[eng], 1)
        self._record((eng, self.cnt[eng]), reads, writes)
        self.n_inst += 1
        return inst

    def dma(self, q, out, in_, reads=(), writes=(), **kw):
        d = self.dq[q]
        i = d["rr"]
        d["rr"] = (i + 1) % len(d["sems"])
        key = d["sems"][i]
        if d["vals"][i] > 0:
            self._wait(q, (key, d["vals"][i]))
        self._deps(q, reads, writes)
        inst = self.e[q].dma_start(out=out, in_=in_, **kw)
        d["vals"][i] += 16
        inst.then_inc(self.sem[key], 16)
        self._record((key, d["vals"][i]), reads, writes)
        self.n_inst += 1
        return inst

    def all_events(self):
        evs = [(k, self.cnt[k]) for k in self.ENG if self.cnt[k] > 0]
        for q, d in self.dq.items():
            for key, v in zip(d["sems"], d["vals"]):
                if v > 0:
                    evs.append((key, v))
        return evs

    def barrier(self, engines=None):
        evs = self.all_events()
        for eng in (engines or self.ENG):
            for ev in evs:
                self._wait(eng, ev)

    def finish(self):
        self.barrier(engines=("sp",))


RMS_EPS = 1e-6
NCOL = 960
CTX = 256


def bc_mid(ap2d, n):
    p, k = ap2d.shape
    return ap2d.unsqueeze(2).broadcast_to([p, k, n])


def na_configs(nrows=128):
    cfg = {}
    mats = []
    interior = {}
    for pq in range(nrows // 2):
        rows = set()
        for r in (2 * pq, 2 * pq + 1):
            rs = min(max(r - 4, 0), nrows - 8)
            rows |= set(range(rs, rs + 8))
        pks = sorted(set(k // 2 for k in rows))
        lst = []
        for pk in pks:
            if 2 <= pq <= nrows // 2 - 3:
                key = ("i", pk - pq)
            else:
                key = (pq, pk)
            if key not in interior:
                interior[key] = len(mats)
                mats.append((pq, pk))
            lst.append((pk, interior[key]))
        cfg[pq] = lst
    return cfg, mats


def emit_phase_a(nc, S, io, layer, with_ctx_out, uid, n_lat=8192):
    T = CTX + n_lat
    D = 1024
    xT = io["xT"]; wcore = io["wcore"]; ccol = io["ccol"]; wada = io["wada"]; badac = io["badac"]; lbl = io["lbl"]
    gnorm_d = io["gnorm"]; nab_d = io["nab"]; swm_d = io["swm"]; sink_d = io["sink"]; ropeC_d = io["ropeC"]; ropeS_d = io["ropeS"]
    cst_d = io["cst"]; vmask_d = io["vmask"]; yT = io["yT"]; ofs = io["ofs"]
    stop = 99

    cfgs, mats = na_configs(n_lat // 64)
    assert len(mats) == 21
    blocks = [(0, CTX)] + [(CTX + i * 512, 512) for i in range(n_lat // 512)]
    NTILE = T // 128

    es = contextlib.ExitStack()
    with es:
        sb = lambda name, shape, dt=F32, st=es: st.enter_context(nc.sbuf_tensor(uid + "s_" + name, shape, dt))
        pst = lambda name, shape, dt=F32, st=es: st.enter_context(nc.psum_tensor(uid + "p_" + name, shape, dt))
        cst = sb("cst", [128, 128 * 4 + 512])
        identb = sb("identb", [128, 128], BF16)
        ident8b = sb("ident8b", [128, 128], BF16)
        hmF = cst[:, 256:384]; hmB = cst[:, 384:512]; rmask = cst[0:64, 512:1024]
        vmask = sb("vmask", [128, 4])
        wb = sb("wb", [128, 8, NCOL], BF16)
        modc = sb("modc", [128, 16, 2])
        lb = sb("lb", [64, 2]); oml = sb("oml", [64, 2])
        gnorm = sb("gnorm_sb", [64, 1])
        ones64 = sb("ones64", [64, 64])
        epsr = sb("epsr", [64, 1])
        nqT = sb("nqT", [64, T], BF16); nkT = sb("nkT", [64, T], BF16); nv1 = sb("nv1", [128, NTILE, 65], BF16)
        sq0T = sb("sq0T", [64, T], BF16); sq1T = sb("sq1T", [64, T], BF16); skT = sb("skT", [64, T], BF16)
        sv1 = sb("sv1", [128, NTILE, 65], BF16)
        S.dma("sp", cst[:], cst_d[:], writes=["cst"])
        S.dma("sp", vmask[:], vmask_d[:], writes=["vmask"])
        S.dma("sp", gnorm[:], gnorm_d[:], writes=["gnorm"])
        S.dma("pool", wb[:], wcore.rearrange("(k p) n -> p k n", p=128), writes=["wb"])
        S.op("dve", lambda e: e.tensor_copy(out=identb[:], in_=cst[:, 0:128]), reads=["cst"], writes=["identb"])
        S.op("dve", lambda e: e.tensor_copy(out=ident8b[:], in_=cst[:, 128:256]), reads=["cst"], writes=["ident8b"])
        S.op("dve", lambda e: e.memset(ones64[:], 1.0), writes=["ones64"])
        S.op("dve", lambda e: e.memset(epsr[:], RMS_EPS), writes=["epsr"])
        S.op("pool", lambda e: e.memset(nv1[:, :, 64:65], 1.0), writes=["nv1ones"])
        S.op("pool", lambda e: e.memset(sv1[:, :, 64:65], 1.0), writes=["sv1ones"])
        st0 = contextlib.ExitStack()
        with st0:
            lbt = sb("lbt", [64, 4], st=st0)
            S.dma("sp", lbt[:], lbl[:], writes=["lbt"])
            if layer == 0:
                S.op("dve", lambda e: e.memset(lb[:], 1e-6), writes=["lb"])
            else:
                lbv = lbt[:].rearrange("p (d l) -> p d l", l=2)
                S.op("dve", lambda e: e.tensor_tensor(out=lb[:], in0=lbv[:, :, 0], in1=lbv[:, :, 1], op=ALU.subtract),
                     reads=["lbt"], writes=["lb"])
                S.op("act", lambda e: e.activation(out=lb[:], in_=lb[:], func=AF.Exp), reads=["lb"], writes=["lb"])
                S.op("dve", lambda e: e.tensor_scalar(out=lb[:], in0=lb[:], scalar1=1.0, scalar2=None, op0=ALU.add), reads=["lb"], writes=["lb"])
                S.op("dve", lambda e: e.reciprocal(out=lb[:], in_=lb[:]), reads=["lb"], writes=["lb"])
                S.op("dve", lambda e: e.tensor_scalar(out=lb[:], in0=lb[:], scalar1=1e-6, scalar2=None, op0=ALU.max), reads=["lb"], writes=["lb"])
            S.op("dve", lambda e: e.tensor_scalar(out=oml[:], in0=lb[:], scalar1=-1.0, scalar2=1.0, op0=ALU.mult, op1=ALU.add),
                 reads=["lb"], writes=["oml"])
            modc_d = io.get("modc_d")
            if modc_d is not None and not io.get("mod_first", True):
                S.dma("sp", modc[:].rearrange("p a b -> p (a b)"), modc_d, writes=["modc"])
                S.barrier()
            else:
                cc = sb("cc", [128, 16], st=st0); scc = sb("scc", [128, 8, 2], st=st0)
                wa = sb("wa", [128, 8, 2048], st=st0)
                bdc = sb("bdc", [128, 16], st=st0)
                psm = pst("psm", [128, 16, 2], st=st0)
                S.dma("sp", cc[:], ccol[:], writes=["cc"])
                S.dma("sp", bdc[:], badac[:], writes=["bdc"])
                S.dma("sp", wa[:], wada.rearrange("(k p) n -> p k n", p=128), writes=["wa"])
                S.op("act", lambda e: e.activation(out=scc[:].rearrange("p k w -> p w k"), in_=cc[:].rearrange("p (w k) -> p w k", w=2), func=AF.Silu),
                     reads=["cc"], writes=["scc"])
                for dch in range(16):
                    for k in range(8):
                        S.op("pe", lambda e, dch=dch, k=k: e.matmul(psm[:, dch, :], lhsT=wa[:, k, dch * 128:(dch + 1) * 128], rhs=scc[:, k, :],
                                                                    start=(k == 0), stop=(k == 7)),
                             reads=["wa", "scc"], writes=["psm"])
                S.op("dve", lambda e: e.tensor_tensor(out=modc[:], in0=psm[:], in1=bc_mid(bdc[:], 2), op=ALU.add),
                     reads=["psm", "bdc"], writes=["modc"])
                S.op("dve", lambda e: e.tensor_scalar(out=modc[:, 8:16, :], in0=modc[:, 8:16, :], scalar1=1.0, scalar2=None, op0=ALU.add),
                     reads=["modc"], writes=["modc"])
                if modc_d is not None:
                    S.dma("sp", modc_d, modc[:].rearrange("p a b -> p (a b)"), reads=["modc"], writes=["modc_d"])
                S.barrier()
        UB = io["UB"]; QB = io["QB"]; SG = io["SG"]
        dcyB = sb("dcyB", [64, len(blocks), 16])
        Sall = sb("Sall", [64, 17, 64])
        Sbf = sb("Sbf", [64, 16, 64], BF16)
        Ub = sb("Ub", [64, 16, 64])
        sgt = sb("sgt", [64, 512])
        ps_oi = pst("ps_oi", [64, 512])
        oi = sb("oi", [64, 512]); oo = sb("oo", [64, 512]); ofb = sb("ofb", [64, 512])
        qtl = sb("qtl", [64, 512], BF16)
        stp = contextlib.ExitStack()
        with stp:
            xt = sb("xt0", [128, 8, 512], st=stp)
            hT = [sb("hT%d" % i, [128, 8, 512], BF16, st=stp) for i in range(2)]
            rC = sb("rC", [64, 512], st=stp); rS = sb("rS", [64, 512], st=stp)
            g_sb = {}
            for nm in ("aq", "az", "az2", "ag", "r0", "r1"):
                g_sb[nm] = sb("g_" + nm, [64, 512], st=stp)
            tA = sb("tA", [64, 512], st=stp); tB = sb("tB", [64, 512], st=stp); tC = sb("tC", [64, 512], st=stp)
            tD = sb("tD", [64, 512], st=stp); tE = sb("tE", [64, 512], st=stp)
            tot = sb("tot", [64, 16], st=stp); dcy = sb("dcy", [64, 16], st=stp)
            ktl = sb("ktl", [64, 512], BF16, st=stp); khT = sb("khT", [64, 512], BF16, st=stp)
            qtlb = sb("qtlb", [64, 512], BF16, st=stp); ktlb = sb("ktlb", [64, 512], BF16, st=stp); khTb = sb("khTb", [64, 512], BF16, st=stp)
            dcy2 = sb("dcy2", [64, 16], st=stp)
            kh = sb("kh", [128, 4, 64], BF16, st=stp)
            vt = sb("vt", [128, 4, 64], BF16, st=stp)
            vblk = sb("vblk", [128, 4, 4, 64], BF16, st=stp)
            scT = sb("scT", [128, 128], BF16, st=stp)
            ps_f = [pst("ps_f%d" % i, [64, 512], st=stp) for i in range(2)]
            ps_tm = pst("ps_tm", [128, 192], st=stp)
            ps_kh = pst("ps_kh", [128, 64], BF16, st=stp)
            ps_U = pst("ps_U", [64, 4, 64], st=stp)
            ps_sc = pst("ps_sc", [128, 128], st=stp)
            ps_oa = pst("ps_oa", [64, 512], st=stp)
            S.op("dve", lambda e: e.memset(Sall[:, 0, :], 0.0), writes=["Sall"])
            fcnt = [0]

            def load_block(bi, par):
                t0, n = blocks[bi]
                S.dma("sp", xt[:, :, :n], xT.rearrange("(k p) t -> p k t", p=128)[:, :, t0:t0 + n], writes=["xt0"])
                w = 1 if t0 < CTX else 0
                for k in range(8):
                    eng = "dve" if k % 2 == 0 else "pool"
                    S.op(eng, lambda e, k=k, w=w, par=par, n=n: e.tensor_scalar(
                        out=hT[par][:, k, :n], in0=xt[:, k, :n], scalar1=modc[:, 8 + k, w:w + 1], scalar2=modc[:, k, w:w + 1],
                        op0=ALU.mult, op1=ALU.add), reads=["xt0", "modc"], writes=["hT%d_%d" % (par, k)])

            def proj_fm(par, n, g):
                i = fcnt[0] % 2; fcnt[0] += 1
                for k in range(8):
                    S.op("pe", lambda e, k=k, i=i, g=g, par=par, n=n: e.matmul(ps_f[i][:, :n], lhsT=wb[:, k, g * 64:(g + 1) * 64],
                                                                             rhs=hT[par][:, k, :n], start=(k == 0), stop=(k == 7)),
                         reads=["wb", "hT%d_%d" % (par, k)], writes=["ps_f%d" % i])
                return ps_f[i], "ps_f%d" % i

            def hg_elem(n, d, zname, qo, ko, kho, sfx):
                nch = n // 32
                q_ = g_sb["aq"]; z_ = g_sb[zname]; ztok = "g_" + zname
                dc = dcy if d == 0 else dcy2
                dct = "dcy" if d == 0 else "dcy2"
                S.op("act", lambda e: e.activation(out=tA[:, :n], in_=z_[:, :n], func=AF.Sigmoid), reads=[ztok], writes=["tA"]); yield
                S.op("dve", lambda e: e.tensor_scalar(out=tA[:, :n], in0=tA[:, :n], scalar1=oml[:, d:d + 1], scalar2=lb[:, d:d + 1],
                                                      op0=ALU.mult, op1=ALU.add), reads=["tA", "oml", "lb"], writes=["tA"]); yield
                S.op("act", lambda e: e.activation(out=tB[:, :n], in_=tA[:, :n], func=AF.Ln), reads=["tA"], writes=["tB"]); yield
                S.op("dve", lambda e: e.tensor_scalar(out=tA[:, :n], in0=tA[:, :n], scalar1=-1.0, scalar2=1.0, op0=ALU.mult, op1=ALU.add),
                     reads=["tA", "tB"], writes=["tA"]); yield
                S.op("dve", lambda e: e.tensor_tensor_scan(out=tC[:, :n], data0=rmask[:, :n], data1=tB[:, :n], initial=0.0,
                                                           op0=ALU.mult, op1=ALU.add), reads=["tB", "cst"], writes=["tC"]); yield
                cumv = tC[:, :n].rearrange("p (c j) -> p c j", j=32)
                S.op("dve", lambda e: e.tensor_copy(out=tot[:, :nch], in_=cumv[:, :, 31]), reads=["tC"], writes=["tot"]); yield
                S.op("act", lambda e: e.activation(out=dc[:, :nch], in_=tot[:, :nch], func=AF.Exp), reads=["tot"], writes=[dct]); yield
                S.op("dve", lambda e: e.tensor_tensor(out=tD[:, :n].rearrange("p (c j) -> p c j", j=32), in0=bc_mid(tot[:, :nch], 32), in1=cumv,
                                                      op=ALU.subtract), reads=["tot", "tC"], writes=["tD"]); yield
                if d == 0:
                    e1, e3, e1n, e3n = tC, tD, "tC", "tD"
                else:
                    S.op("dve", lambda e: e.tensor_tensor(out=tE[:, :n], in0=tD[:, :n], in1=tB[:, :n], op=ALU.add), reads=["tD", "tB"], writes=["tE"]); yield
                    S.op("dve", lambda e: e.tensor_tensor(out=tC[:, :n], in0=tC[:, :n], in1=tB[:, :n], op=ALU.subtract), reads=["tC", "tB", "tD"], writes=["tC"]); yield
                    e1, e3, e1n, e3n = tE, tC, "tE", "tC"
                S.op("act", lambda e: e.activation(out=tB[:, :n], in_=e1[:, :n], func=AF.Exp, scale=-1.0), reads=[e1n, "tD", "tE", "tC"], writes=["tB"]); yield
                S.op("act", lambda e: e.activation(out=e1[:, :n], in_=e1[:, :n], func=AF.Exp), reads=[e1n, "tB"], writes=[e1n]); yield
                S.op("act", lambda e: e.activation(out=e3[:, :n], in_=e3[:, :n], func=AF.Exp), reads=[e3n], writes=[e3n]); yield
                S.op("dve", lambda e: e.tensor_tensor(out=qo[:, :n], in0=q_[:, :n], in1=e1[:, :n], op=ALU.mult), reads=["g_aq", e1n], writes=["qtl" + sfx]); yield
                S.op("dve", lambda e: e.tensor_tensor(out=ko[:, :n], in0=tA[:, :n], in1=tB[:, :n], op=ALU.mult), reads=["tA", "tB"], writes=["ktl" + sfx]); yield
                S.op("pool", lambda e: e.tensor_tensor(out=kho[:, :n], in0=tA[:, :n], in1=e3[:, :n], op=ALU.mult), reads=["tA", e3n], writes=["khT" + sfx]); yield

            def hg_tiles_U(n, store, kho=None, sfx="", tiles=None):
                kho = khT if kho is None else kho
                ntl = n // 128
                for tl in (range(ntl) if tiles is None else tiles):
                    S.op("pe", lambda e, tl=tl: e.transpose(out=ps_kh[:], in_=kho[:, tl * 128:(tl + 1) * 128], identity=identb[0:64, 0:64]),
                         reads=["khT" + sfx, "identb"], writes=["ps_kh"])
                    S.op("act", lambda e, tl=tl: e.activation(out=kh[:, tl, :], in_=ps_kh[:], func=AF.Copy), reads=["ps_kh"], writes=["kh%d" % tl])
                    S.op("pe", lambda e, tl=tl: e.matmul(ps_U[:].rearrange("p c e -> p (c e)"), lhsT=kh[:, tl, :],
                                                         rhs=vblk[:, tl, :, :].rearrange("p c e -> p (c e)"), start=True, stop=True),
                         reads=["kh%d" % tl, "vblk"], writes=["ps_U"])
                    if store:
                        S.op("act", lambda e, tl=tl: e.activation(out=Ub[:, tl * 4:(tl + 1) * 4, :], in_=ps_U[:], func=AF.Copy),
                             reads=["ps_U"], writes=["Ub"])
                    else:
                        for cc_ in range(4):
                            c = tl * 4 + cc_
                            S.op("dve", lambda e, c=c, cc_=cc_: e.scalar_tensor_tensor(
                                out=Sall[:, c + 1, :], in0=Sall[:, c, :], scalar=dcy[:, c:c + 1], in1=ps_U[:, cc_, :], op0=ALU.mult, op1=ALU.add),
                                reads=["ps_U", "Sall", "dcy"], writes=["Sall"])

            def hg_inter(n, off, qo=None, sfx=""):
                qo = qtl if qo is None else qo
                nch = n // 32
                S.op("act", lambda e: e.activation(out=Sbf[:, :nch, :], in_=Sall[:, off:off + nch, :], func=AF.Copy), reads=["Sall"], writes=["Sbf"])
                for c in range(nch):
                    S.op("pe", lambda e, c=c: e.matmul(ps_oi[:, c * 32:(c + 1) * 32], lhsT=Sbf[:, c, :], rhs=qo[:, c * 32:(c + 1) * 32],
                                                       start=True, stop=True), reads=["Sbf", "qtl" + sfx], writes=["ps_oi"])

            def hg_intra(n, d, qo=None, ko=None, sfx="", tiles=None):
                qo = qtl if qo is None else qo
                ko = ktl if ko is None else ko
                ntl = n // 128
                hm = hmF if d == 0 else hmB
                for tl in (range(ntl) if tiles is None else tiles):
                    S.op("pe", lambda e, tl=tl: e.matmul(ps_sc[:], lhsT=ko[:, tl * 128:(tl + 1) * 128], rhs=qo[:, tl * 128:(tl + 1) * 128],
                                                         start=True, stop=True), reads=["ktl" + sfx, "qtl" + sfx], writes=["ps_sc"])
                    S.op("dve", lambda e: e.tensor_tensor(out=scT[:], in0=ps_sc[:], in1=hm, op=ALU.mult), reads=["ps_sc", "cst"], writes=["scT"])
                    S.op("pe", lambda e, tl=tl: e.matmul(ps_oa[:, tl * 128:(tl + 1) * 128], lhsT=vt[:, tl, :], rhs=scT[:], start=True, stop=True),
                         reads=["vt", "scT"], writes=["ps_oa"])

            load_block(0, 0)
            for bi in range(len(blocks)):
                par = bi % 2
                t0, n = blocks[bi]
                ntl = n // 128; nch = n // 32
                is_ctx = t0 < CTX
                if bi + 1 < len(blocks):
                    load_block(bi + 1, 1 - par)
                if not is_ctx:
                    S.dma("sp", rC[:, :n], ropeC_d[:, t0 - CTX:t0 - CTX + n], writes=["rC"])
                    S.dma("sp", rS[:, :n], ropeS_d[:, t0 - CTX:t0 - CTX + n], writes=["rS"])
                def evac(g, dst, dtok, eng="act"):
                    p_, ptok = proj_fm(par, n, g)
                    o_ = dst[:, :n] if dst.shape[1] == 512 else dst[:, t0:t0 + n]
                    if eng == "act":
                        S.op("act", lambda e: e.activation(out=o_, in_=p_[:, :n], func=AF.Copy), reads=[ptok], writes=[dtok])
                    else:
                        S.op("dve", lambda e: e.tensor_copy(out=o_, in_=p_[:, :n]), reads=[ptok], writes=[dtok])

                def rope_group(ga, gb, dst, dtok):
                    pa, patok = proj_fm(par, n, ga)
                    S.op("dve", lambda e, pa=pa: e.tensor_tensor(out=g_sb["r0"][:, :n], in0=pa[:, :n], in1=rC[:, :n], op=ALU.mult),
                         reads=[patok, "rC"], writes=["g_r0"])
                    pb, pbtok = proj_fm(par, n, gb)
                    S.op("dve", lambda e, pb=pb: e.tensor_tensor(out=g_sb["r1"][:, :n], in0=pb[:, :n], in1=rS[:, :n], op=ALU.mult),
                         reads=[pbtok, "rS"], writes=["g_r1"])
                    S.op("pool", lambda e, dst=dst: e.tensor_tensor(out=dst[:, t0:t0 + n], in0=g_sb["r0"][:, :n], in1=g_sb["r1"][:, :n], op=ALU.add),
                         reads=["g_r0", "g_r1"], writes=[dtok])

                evac(0, g_sb["aq"], "g_aq", "act")
                evac(1, g_sb["az"], "g_az", "dve")
                evac(2, g_sb["az2"], "g_az2", "act")
                for tl in range(ntl):
                    gt = t0 // 128 + tl
                    for k in range(8):
                        S.op("pe", lambda e, k=k, tl=tl, par=par: e.matmul(ps_tm[:], lhsT=hT[par][:, k, tl * 128:(tl + 1) * 128],
                                                                           rhs=wb[:, k, 768:960], start=(k == 0), stop=(k == 7)),
                             reads=["wb", "hT%d_%d" % (par, k)], writes=["ps_tm"])
                    S.op("act", lambda e, tl=tl: e.activation(out=vt[:, tl, :], in_=ps_tm[:, 0:64], func=AF.Copy), reads=["ps_tm"], writes=["vt"])
                    S.op("act", lambda e, gt=gt: e.activation(out=nv1[:, gt, 0:64], in_=ps_tm[:, 64:128], func=AF.Copy), reads=["ps_tm", "vt"], writes=["nv1_%d" % gt])
                    S.op("act", lambda e, gt=gt: e.activation(out=sv1[:, gt, 0:64], in_=ps_tm[:, 128:192], func=AF.Copy), reads=["ps_tm", "nv1_%d" % gt], writes=["sv1_%d" % gt])
                    for c in range(4):
                        S.op("pool", lambda e, tl=tl, c=c: e.tensor_scalar(out=vblk[:, tl, c, :], in0=vt[:, tl, :], scalar1=vmask[:, c:c + 1],
                                                                          scalar2=None, op0=ALU.mult), reads=["vt", "vmask"], writes=["vblk"])
                import itertools
                chain_f = hg_elem(n, 0, "az", qtl, ktl, khT, "")
                chain_b = hg_elem(n, 1, "az2", qtlb, ktlb, khTb, "b")
                others = [lambda: evac(3, g_sb["ag"], "g_ag", "dve"), lambda: evac(4, nqT, "nqT", "act"), lambda: evac(5, nkT, "nkT", "dve")]
                if is_ctx:
                    others += [lambda: evac(6, sq0T, "sq0T", "act"), lambda: evac(8, sq1T, "sq1T", "dve"), lambda: evac(10, skT, "skT", "act")]
                else:
                    others += [lambda: rope_group(6, 7, sq0T, "sq0T"), lambda: rope_group(8, 9, sq1T, "sq1T"), lambda: rope_group(10, 11, skT, "skT")]
                for oth in others:
                    for _ in range(3):
                        next(chain_f, None)
                    oth()
                for _ in chain_f:
                    pass

                def work_f():
                    for tl in range(ntl):
                        hg_tiles_U(n, store=False, tiles=[tl]); yield
                    hg_inter(n, 0); yield
                    for tl in range(ntl):
                        hg_intra(n, 0, tiles=[tl]); yield
                    S.op("act", lambda e: e.activation(out=oi[:, :n], in_=ps_oa[:, :n], func=AF.Copy), reads=["ps_oa"], writes=["oi"])
                    S.op("dve", lambda e: e.tensor_tensor(out=oo[:, :n], in0=ps_oi[:, :n], in1=oi[:, :n], op=ALU.add), reads=["ps_oi", "oi"], writes=["oo"])
                    S.op("dve", lambda e: e.tensor_copy(out=Sall[:, 0, :], in_=Sall[:, nch, :]), reads=["Sall", "Sbf"], writes=["Sall"])
                    yield
                for _ in work_f():
                    next(chain_b, None); next(chain_b, None)
                for _ in chain_b:
                    pass
                hg_tiles_U(n, store=True, kho=khTb, sfx="b")
                hg_intra(n, 1, qo=qtlb, ko=ktlb, sfx="b")
                S.op("dve", lambda e: e.tensor_tensor(out=oo[:, :n], in0=ps_oa[:, :n], in1=oo[:, :n], op=ALU.add), reads=["ps_oa", "oo"], writes=["oo"])
                S.op("dve", lambda e, bi=bi: e.tensor_copy(out=dcyB[:, bi, :nch], in_=dcy2[:, :nch]), reads=["dcy2"], writes=["dcyB"])
                S.dma("sp", ofs[:, t0:t0 + n], oo[:, :n], reads=["oo"], writes=["ofs%d" % bi])
                S.dma("sp", UB[:, bi, :nch * 64], Ub[:, :nch, :].rearrange("p c e -> p (c e)"), reads=["Ub"], writes=["UB%d" % bi])
                S.dma("sp", QB[:, t0:t0 + n], qtlb[:, :n], reads=["qtlb"], writes=["QB%d" % bi])
                S.op("act", lambda e: e.activation(out=sgt[:, :n], in_=g_sb["ag"][:, :n], func=AF.Silu), reads=["g_ag"], writes=["sgt"])
                S.op("dve", lambda e: e.tensor_scalar(out=sgt[:, :n], in0=sgt[:, :n], scalar1=gnorm[:, 0:1], scalar2=None, op0=ALU.mult),
                     reads=["sgt", "gnorm"], writes=["sgt"])
                S.dma("sp", SG[:, t0:t0 + n], sgt[:, :n], reads=["sgt"], writes=["SG%d" % bi])
            S.barrier()

        def pass2_gen(bufs):
            S.op("dve", lambda e: e.memset(Sall[:, 0, :], 0.0), writes=["Sall"])
            order2 = [0] + list(range(len(blocks) - 1, 0, -1))

            def loads(k):
                bi = order2[k]
                t0, n = blocks[bi]
                nch = n // 32
                ub_, q_, of_, sg_ = bufs[k % 2]
                sx = "p%d" % (k % 2)
                S.dma("pool", ub_[:, :nch, :].rearrange("p c e -> p (c e)"), UB[:, bi, :nch * 64], writes=["Ub" + sx])
                S.dma("pool", q_[:, :n], QB[:, t0:t0 + n], writes=["qtl" + sx])
                S.dma("pool", of_[:, :n], ofs[:, t0:t0 + n], writes=["ofb" + sx])
                S.dma("pool", sg_[:, :n], SG[:, t0:t0 + n], writes=["sgt" + sx])

            loads(0)
            for k, bi in enumerate(order2):
                t0, n = blocks[bi]
                ntl = n // 128; nch = n // 32
                if k + 1 < len(order2):
                    loads(k + 1)
                ub_, q_, of_, sg_ = bufs[k % 2]
                sx = "p%d" % (k % 2)
                S.op("dve", lambda e: e.tensor_copy(out=Sall[:, nch, :], in_=Sall[:, 0, :]), reads=["Sall"], writes=["Sall"])
                for c in range(nch - 1, -1, -1):
                    S.op("dve", lambda e, c=c, bi=bi, ub_=ub_: e.scalar_tensor_tensor(
                        out=Sall[:, c, :], in0=Sall[:, c + 1, :], scalar=dcyB[:, bi, c:c + 1], in1=ub_[:, c, :], op0=ALU.mult, op1=ALU.add),
                        reads=["Ub" + sx, "Sall", "dcyB"], writes=["Sall"])
                hg_inter(n, 1, qo=q_, sfx=sx)
                S.op("dve", lambda e, of_=of_: e.tensor_tensor(out=oo[:, :n], in0=ps_oi[:, :n], in1=of_[:, :n], op=ALU.add), reads=["ps_oi", "ofb" + sx], writes=["oo"])
                S.op("act", lambda e: e.activation(out=oi[:, :n], in_=oo[:, :n], func=AF.Square), reads=["oo", "oi"], writes=["oi"])
                S.op("pe", lambda e: e.matmul(ps_oi[:, :n], lhsT=ones64[:], rhs=oi[:, :n], start=True, stop=True),
                     reads=["ones64", "oi"], writes=["ps_oi"])
                S.op("act", lambda e: e.activation(out=oi[:, :n], in_=ps_oi[:, :n], func=AF.Sqrt, bias=epsr[:, 0:1], scale=1.0 / 64.0),
                     reads=["ps_oi", "epsr"], writes=["oi"])
                S.op("dve", lambda e: e.reciprocal(out=oi[:, :n], in_=oi[:, :n]), reads=["oi"], writes=["oi"])
                S.op("dve", lambda e: e.tensor_tensor(out=oo[:, :n], in0=oo[:, :n], in1=oi[:, :n], op=ALU.mult), reads=["oo", "oi"], writes=["oo"])
                S.op("dve", lambda e, sg_=sg_: e.tensor_tensor(out=oo[:, :n], in0=oo[:, :n], in1=sg_[:, :n], op=ALU.mult), reads=["oo", "sgt" + sx], writes=["oo"])
                S.dma("pool", yT[0:64, t0:t0 + n], oo[:, :n], reads=["oo"], writes=["yTa%d" % bi])
                yield bi
        sta = contextlib.ExitStack()
        with sta:
            nab = sb("nab", [128, 21, 128], BF16, st=sta)
            swm = sb("swm", [128, 2, 128], BF16, st=sta)
            snk = sb("snk", [1, 2], st=sta); snkB = sb("snkB", [128, 2], st=sta)
            ones1 = sb("ones1", [1, 128], st=sta)
            pT = [sb("pT%d" % i, [128, 7, 128], BF16, st=sta) for i in range(2)]
            ot = [sb("ot%d" % i, [128, 64], BF16, st=sta) for i in range(2)]
            rinv = sb("rinv", [128, 1], st=sta)
            yblk = [sb("yblk%d" % i, [64, 512], st=sta) for i in range(2)]
            ps_s = [[pst("ps_s%d_%d" % (i, j), [128, 4, 128], st=sta) for j in range(2)] for i in range(2)]
            ps_o = [pst("ps_o%d" % i, [128, 65], st=sta) for i in range(2)]
            ps_y = pst("ps_y", [64, 128], BF16, st=sta)
            S.dma("pool", nab[:], nab_d.rearrange("c k q -> k c q"), writes=["nab"])
            S.dma("pool", swm[:], swm_d.rearrange("c k q -> k c q"), writes=["swm"])
            S.dma("sp", snk[:], sink_d[:], writes=["snk"])
            S.op("dve", lambda e: e.memset(ones1[:], 1.0), writes=["ones1"])
            S.op("pe", lambda e: e.matmul(ps_o[0][:, 0:2], lhsT=ones1[:], rhs=snk[:], start=True, stop=True), reads=["ones1", "snk"], writes=["ps_o0"])
            S.op("act", lambda e: e.activation(out=snkB[:], in_=ps_o[0][:, 0:2], func=AF.Exp), reads=["ps_o0"], writes=["snkB"])
            acnt = [0]
            p2bufs = [(Ub, qtl, ofb, sgt),
                      (sb("Ubx", [64, 16, 64], st=sta), sb("qtlx", [64, 512], BF16, st=sta), sb("ofbx", [64, 512], st=sta), sb("sgtx", [64, 512], st=sta))]
            p2 = pass2_gen(p2bufs)
            p2cnt = [0]

            tiles = []

            def add_tile(qT, qtok_tile, keys, v1, sink_col, yb_, ybtok, ycol, after=None):
                tiles.append(dict(qT=qT, qt=qtok_tile, keys=keys, v1=v1, sink=sink_col, yb=yb_, ybtok=ybtok, ycol=ycol, after=after))

            def st_scores(t, i):
                keys = t["keys"]; nk = len(keys); qT = t["qT"]; qt = t["qt"]
                for j, (kT_, kt, bias) in enumerate(keys):
                    pp = ps_s[i][j // 4]; ptok = "ps_s%d_%d" % (i, j // 4)
                    S.op("pe", lambda e, pp=pp, j=j, kT_=kT_, kt=kt, bias=bias: e.matmul(
                        pp[:, j % 4, :], lhsT=kT_[:, kt * 128:(kt + 1) * 128], rhs=qT[:, qt * 128:(qt + 1) * 128],
                        start=True, stop=(bias is None)), writes=[ptok])
                    if bias is not None:
                        S.op("pe", lambda e, pp=pp, j=j, bias=bias: e.matmul(pp[:, j % 4, :], lhsT=ident8b[:], rhs=bias, start=False, stop=True),
                             reads=["nab", "swm", "ident8b"], writes=[ptok])
                n0 = min(4, nk)
                S.op("act", lambda e: e.activation(out=pT[i][:, 0:n0, :], in_=ps_s[i][0][:, 0:n0, :], func=AF.Exp, scale=0.125),
                     reads=["ps_s%d_0" % i], writes=["pT%d" % i])
                if nk > 4:
                    S.op("act", lambda e: e.activation(out=pT[i][:, 4:nk, :], in_=ps_s[i][1][:, 0:nk - 4, :], func=AF.Exp, scale=0.125),
                         reads=["ps_s%d_1" % i], writes=["pT%d" % i])

            def st_pv(t, i):
                keys = t["keys"]; nk = len(keys); v1 = t["v1"]; sink_col = t["sink"]
                for j, (kT_, kt, bias) in enumerate(keys):
                    S.op("pe", lambda e, j=j, kt=kt: e.matmul(ps_o[i][:], lhsT=pT[i][:, j, :], rhs=v1[:, kt, :], start=(j == 0), stop=(j == nk - 1)),
                         reads=["pT%d" % i], writes=["ps_o%d" % i])
                if sink_col is None:
                    S.op("dve", lambda e: e.reciprocal(out=rinv[:], in_=ps_o[i][:, 64:65]), reads=["ps_o%d" % i], writes=["rinv"])
                else:
                    S.op("dve", lambda e: e.tensor_tensor(out=rinv[:], in0=ps_o[i][:, 64:65], in1=snkB[:, sink_col:sink_col + 1], op=ALU.add),
                         reads=["ps_o%d" % i, "snkB"], writes=["rinv"])
                    S.op("dve", lambda e: e.reciprocal(out=rinv[:], in_=rinv[:]), reads=["rinv"], writes=["rinv"])
                S.op("dve", lambda e: e.tensor_scalar(out=ot[i][:], in0=ps_o[i][:, 0:64], scalar1=rinv[:, 0:1], scalar2=None, op0=ALU.mult),
                     reads=["ps_o%d" % i, "rinv"], writes=["ot%d" % i])

            def st_out(t, i):
                yb_ = t["yb"]; ycol = t["ycol"]
                S.op("pe", lambda e: e.transpose(out=ps_y[:], in_=ot[i][:], identity=identb[:]), reads=["ot%d" % i, "identb"], writes=["ps_y"])
                S.op("act", lambda e: e.activation(out=yb_[:, ycol:ycol + 128], in_=ps_y[:], func=AF.Copy), reads=["ps_y"], writes=[t["ybtok"]])
                if t["after"] is not None:
                    t["after"]()

            CT = CTX // 128
            ctx_keys_n = [(nkT, 0, None), (nkT, 1, None)]
            ctx_keys_s = [(skT, 0, None), (skT, 1, None)]
            ybc = 0
            heads_ = [("n", nqT, None, 64), ("s0", sq0T, 0, 128), ("s1", sq1T, 1, 192)]

            def mk_after(dst, src, ybtok, wtok):
                def f():
                    S.dma("sp", dst, src, reads=[ybtok], writes=[wtok])
                    p2cnt[0] += 1
                    if p2cnt[0] % 1 == 0:
                        next(p2, None)
                return f

            for (hk, qT, sink_col, yrow0) in heads_:
                if with_ctx_out:
                    yb_ = yblk[ybc % 2]; ybtok = "yblk%d" % (ybc % 2); ybc += 1
                    for qt in range(CT):
                        aft = mk_after(yT[yrow0:yrow0 + 64, 0:CTX], yb_[:, 0:CTX], ybtok, "yTc_" + hk) if qt == CT - 1 else None
                        if hk == "n":
                            add_tile(qT, qt, ctx_keys_n, nv1, None, yb_, ybtok, qt * 128, aft)
                        else:
                            add_tile(qT, qt, ctx_keys_s, sv1, sink_col, yb_, ybtok, qt * 128, aft)
                nql = n_lat // 128
                for qb in range(nql // 4):
                    yb_ = yblk[ybc % 2]; ybtok = "yblk%d" % (ybc % 2); ybc += 1
                    for qq in range(4):
                        pq = qb * 4 + qq
                        aft = mk_after(yT[yrow0:yrow0 + 64, CTX + qb * 512:CTX + (qb + 1) * 512], yb_[:], ybtok, "yT_%s_%d" % (hk, qb)) if qq == 3 else None
                        if hk == "n":
                            keys = [(nkT, CT + pk, nab[:, mi, :]) for (pk, mi) in cfgs[pq]] + ctx_keys_n
                            add_tile(qT, CT + pq, keys, nv1, None, yb_, ybtok, qq * 128, aft)
                        else:
                            keys = []
                            if pq > 0:
                                keys.append((skT, CT + pq - 1, swm[:, 0, :]))
                            keys.append((skT, CT + pq, None))
                            if pq < nql - 1:
                                keys.append((skT, CT + pq + 1, swm[:, 1, :]))
                            keys += ctx_keys_s
                            add_tile(qT, CT + pq, keys, sv1, sink_col, yb_, ybtok, qq * 128, aft)
            NTL = len(tiles)
            for n_ in range(NTL + 2):
                if n_ < NTL:
                    st_scores(tiles[n_], n_ % 2)
                if 0 <= n_ - 1 < NTL:
                    st_pv(tiles[n_ - 1], (n_ - 1) % 2)
                if 0 <= n_ - 2 < NTL:
                    st_out(tiles[n_ - 2], (n_ - 2) % 2)
            for _ in p2:
                pass
            S.barrier()

NEG = -30000.0
def rope_tables(n_lat=8192):
    t = np.arange(n_lat); row = (t // 64).astype(np.float32); col = (t % 64).astype(np.float32)
    nf = 16
    inv = (np.float32(10000.0) ** (-np.arange(nf, dtype=np.float32) / np.float32(nf))).astype(np.float32)
    C = np.zeros((64, n_lat), np.float32); S = np.zeros((64, n_lat), np.float32)
    for half, pos in ((0, row), (1, col)):
        ang = (pos[:, None] * inv[None, :]).astype(np.float32)
        c = np.cos(ang).astype(np.float32).T; s_ = np.sin(ang).astype(np.float32).T
        b = half * 32
        C[b:b+16] = c; C[b+16:b+32] = c
        S[b:b+16] = -s_; S[b+16:b+32] = s_
    return C, S
def rope_perm():
    p = np.arange(64)
    for b in (0, 32):
        p[b:b+16] = np.arange(b+16, b+32); p[b+16:b+32] = np.arange(b, b+16)
    return p
def na_index_tables(nrows=128):
    cfgs, mats = na_configs(nrows)
    idx = np.zeros((21, 128, 128), np.int64)
    k = np.arange(128); q = np.arange(128)
    for mi, (pq, pk) in enumerate(mats):
        krow = 2 * pk + k // 64; kcol = k % 64
        qrow = 2 * pq + q // 64; qcol = q % 64
        rs = np.clip(qrow - 4, 0, nrows - 8); cs = np.clip(qcol - 8, 0, 48)
        valid = (krow[:, None] >= rs[None]) & (krow[:, None] < rs[None] + 8) & (kcol[:, None] >= cs[None]) & (kcol[:, None] < cs[None] + 16)
        ridx = krow[:, None] - qrow[None] + 7
        coff = np.clip(kcol[:, None] - qcol[None] + 15, 0, 30)
        ii = np.clip(ridx, 0, 14) * 31 + coff
        idx[mi] = np.where(valid, ii, 15 * 31)
    return idx
def const_tables():
    ident = np.eye(128, dtype=np.float32)
    k = np.arange(128)
    same = (k[:, None] // 32) == (k[None] // 32)
    hmF = (same & (k[:, None] <= k[None])).astype(np.float32)
    hmB = (same & (k[:, None] >= k[None])).astype(np.float32)
    rm = np.ones((128, 512), np.float32); rm[:, ::32] = 0.0
    cst = np.concatenate([ident, 8 * ident, hmF, hmB, rm], axis=1)
    vmask = (k[:, None] // 32 == np.arange(4)[None]).astype(np.float32)
    swm = np.zeros((2, 128, 128), np.float32)
    swm[0] = np.where(k[:, None] >= k[None], 0.0, NEG)
    swm[1] = np.where(k[:, None] <= k[None], 0.0, NEG)
    return cst, vmask, swm
_NAIDX = None
def core_inputs_a(inp, l, b, j, xT_b, n_lat=8192):
    global _NAIDX
    if _NAIDX is None: _NAIDX = na_index_tables(n_lat // 64)
    w = inp['w_in'][l]
    perm = rope_perm()
    def cols(base, width=64, idx=j): return w[:, base + width * idx: base + width * (idx + 1)]
    sq0 = w[:, 2048 + 128 * j: 2048 + 128 * j + 64]; sq1 = w[:, 2048 + 128 * j + 64: 2048 + 128 * j + 128]
    n = j // 2
    sk = w[:, 2560 + 64 * n: 2560 + 64 * n + 64]; sv = w[:, 2688 + 64 * n: 2688 + 64 * n + 64]
    wcore = np.concatenate([cols(0), cols(256), cols(512), cols(1024), cols(1280), cols(1536),
                            sq0, sq0[:, perm], sq1, sq1[:, perm], sk, sk[:, perm],
                            cols(768), cols(1792), sv], axis=1)
    c = inp['c'][b]; cctx = inp['c_ctx']
    ccol = np.concatenate([c.reshape(8, 128).T, cctx.reshape(8, 128).T], axis=1)
    lbl = np.stack([inp['lb_logits'][0, 0, 64*j:64*j+64], inp['lb_logits'][1, 0, 64*j:64*j+64],
                    inp['lb_logits'][0, 1, 64*j:64*j+64], inp['lb_logits'][1, 1, 64*j:64*j+64]], axis=1)
    rpb_ext = np.concatenate([inp['na_rpb'][l, j].reshape(-1), np.array([NEG], np.float32)])
    nab = rpb_ext[_NAIDX]
    C, S = rope_tables(n_lat)
    cst, vmask, swm = const_tables()
    f = lambda a: np.ascontiguousarray(a, dtype=np.float32)
    return {"xT": f(xT_b), "wcore": f(wcore), "ccol": f(ccol), "wada": f(inp['w_ada'][l][:, :2048]),
            "badac": f(inp['b_ada'][l][:2048].reshape(16, 128).T), "lbl": f(lbl), "gnorm": f(inp['hgrn_norm'][l][:, None]),
            "nab": f(nab), "swm": f(swm), "sink": f(inp['swa_sink'][l][None, 2*j:2*j+2]), "ropeC": f(C), "ropeS": f(S),
            "cst": f(cst), "vmask": f(vmask)}


ALPHA = float((2.0 * 2) ** 0.25)
LN_EPS = 1e-5
NEGBIG = 1e30


def bc_mid(ap2d, n):
    p, k = ap2d.shape
    return ap2d.unsqueeze(2).broadcast_to([p, k, n])


def emit_phase_b(nc, S, io, has_ctx, uid, n_lat_tiles=16, n_exp=16):
    NTI = n_lat_tiles + (1 if has_ctx else 0)
    NT = NTI * 128
    D = 1024
    stop = 99
    yT_lat = io["yT_lat"]; yT_ctx = io.get("yT_ctx"); x = io["x"]; ccol = io["ccol"]; wada = io["wada"]; bada = io["bada"]; lnp = io["lnp"]
    wout = io["wout"]; wr = io["wr"]; rb = io["rb"]; wg = io["wg"]; wu = io["wu"]; wd = io["wd"]; ident_d = io["ident"]
    xo = io["xo"]; x1s = io["x1s"]; xT_lat_o = io.get("xT_lat_o"); xT_ctx_o = io.get("xT_ctx_o")
    es = contextlib.ExitStack()
    with es:
        sb = lambda name, shape, dt=F32, st=es: st.enter_context(nc.sbuf_tensor(uid + name, shape, dt))
        pst = lambda name, shape, dt=F32, st=es: st.enter_context(nc.psum_tensor(uid + name, shape, dt))

        h2T = sb("h2T", [128, 8, NT], BF16)
        gates = sb("gates", [128, NTI, 16])
        G2 = sb("G2", [128, D]); G2c = sb("G2c", [128, D]); LN2G = sb("LN2G", [128, D]); LN2B = sb("LN2B", [128, D])
        ident = sb("ident_sb", [128, 128])
        ones1 = sb("ones1", [1, 128])
        epsT = sb("epsT", [128, 1])
        S.dma("sp", ident[:], ident_d[:], writes=["ident"])
        S.op("dve", lambda e: e.memset(ones1[:], 1.0), writes=["ones1"])
        S.op("dve", lambda e: e.memset(epsT[:], LN_EPS), writes=["epsT"])

        st1 = contextlib.ExitStack()
        with st1:
            GA = sb("GA", [128, D], st=st1); BA = sb("BA", [128, D], st=st1)
            A2 = sb("A2", [128, D], st=st1); B2 = sb("B2", [128, D], st=st1)
            A2c = sb("A2c", [128, D], st=st1); B2c = sb("B2c", [128, D], st=st1)
            G1 = sb("G1", [128, D], st=st1); G1c = sb("G1c", [128, D], st=st1)
            bc_d = io.get("bc_d")
            bc_tiles = [G1, G1c, A2, A2c, B2, B2c, GA, BA, G2, G2c, LN2G, LN2B]
            if bc_d is not None and not io.get("bc_first", True):
                for ti_, tl_ in enumerate(bc_tiles):
                    S.dma("sp", tl_[:], bc_d[ti_], writes=["bct%d" % ti_])
                S.barrier()
            else:
                st0 = contextlib.ExitStack()
                with st0:
                    cc = sb("cc", [128, 16], st=st0)
                    scc = sb("scc", [128, 16], st=st0)
                    cb = sb("cb", [128, 16, 128], st=st0)
                    wa = [sb("wa%d" % i, [128, 8, 512], st=st0) for i in range(2)]
                    bd = [sb("bd%d" % i, [1, 512], st=st0) for i in range(2)]
                    SC2 = sb("SC2", [128, D], st=st0); SC2c = sb("SC2c", [128, D], st=st0)
                    SH2 = sb("SH2", [128, D], st=st0); SH2c = sb("SH2c", [128, D], st=st0)
                    LN1G = sb("LN1G", [128, D], st=st0); LN1B = sb("LN1B", [128, D], st=st0)
                    psm = [pst("psm%d" % i, [128, 512], st=st0) for i in range(4)]
                    S.dma("sp", cc[:], ccol[:], writes=["cc"])
                    S.op("act", lambda e: e.activation(out=scc[:], in_=cc[:], func=AF.Silu), reads=["cc"], writes=["scc"])
                    S.op("dve", lambda e: e.tensor_copy(out=cb[:], in_=bc_mid(scc[:], 128)), reads=["scc"], writes=["cb"])
                    wada_v = wada.rearrange("(k p) n -> p k n", p=128)
                    dests = [(G1, G1c), (SH2, SH2c), (SC2, SC2c), (G2, G2c)]
                    pi = 0
                    hi = 0
                    for ch in range(4):
                        for half in range(2):
                            cs = ch * 1024 + half * 512
                            wb = wa[hi % 2]; wtok = "wa%d" % (hi % 2); bdt = bd[hi % 2]; btok = "bd%d" % (hi % 2); hi += 1
                            S.dma("sp", wb[:], wada_v[:, :, cs:cs + 512], writes=[wtok])
                            S.dma("sp", bdt[:], bada[:, cs:cs + 512], writes=[btok])
                            for which in range(2):
                                p_ = psm[pi % 4]; ptok = "psm%d" % (pi % 4); pi += 1
                                for k in range(8):
                                    S.op("pe", lambda e, k=k, p_=p_, which=which, wb=wb: e.matmul(
                                        p_[:], lhsT=cb[:, which * 8 + k, :], rhs=wb[:, k, :],
                                        start=(k == 0), stop=False),
                                        reads=["cb", wtok], writes=[ptok])
                                S.op("pe", lambda e, p_=p_, bdt=bdt: e.matmul(p_[:], lhsT=ones1[:], rhs=bdt[:],
                                                                        start=False, stop=True),
                                     reads=["ones1", btok], writes=[ptok])
                                dst = dests[ch][which]
                                S.op("act", lambda e, dst=dst, p_=p_, half=half: e.activation(
                                    out=dst[:, half * 512:(half + 1) * 512], in_=p_[:], func=AF.Copy),
                                    reads=[ptok], writes=["mod%d_%d" % (ch, which)])
                    if stop <= -1:
                        pass
                    lnd = [LN1G, LN1B, LN2G, LN2B]
                    for j in range(4):
                        for half in range(2):
                            p_ = psm[pi % 4]; ptok = "psm%d" % (pi % 4); pi += 1
                            cs = j * 1024 + half * 512
                            bdt = bd[hi % 2]; btok = "bd%d" % (hi % 2); hi += 1
                            S.dma("sp", bdt[:], lnp[:, cs:cs + 512], writes=[btok])
                            S.op("pe", lambda e, p_=p_, bdt=bdt: e.matmul(p_[:], lhsT=ones1[:], rhs=bdt[:],
                                                                    start=True, stop=True),
                                 reads=["ones1", btok], writes=[ptok])
                            S.op("act", lambda e, j=j, p_=p_, half=half: e.activation(
                                out=lnd[j][:, half * 512:(half + 1) * 512], in_=p_[:], func=AF.Copy),
                                reads=[ptok], writes=["lnd%d" % j])
                    if stop <= -0.5:
                        pass
                    S.barrier()
                    S.op("dve", lambda e: e.tensor_scalar(out=GA[:], in0=LN1G[:], scalar1=ALPHA, scalar2=None, op0=ALU.mult))
                    S.op("dve", lambda e: e.tensor_scalar(out=BA[:], in0=LN1B[:], scalar1=ALPHA, scalar2=None, op0=ALU.mult))
                    for ci, (sc, sh, a2, b2) in enumerate(((SC2, SH2, A2, B2), (SC2c, SH2c, A2c, B2c))):
                        S.op("dve", lambda e, sc=sc: e.tensor_scalar(out=sc[:], in0=sc[:], scalar1=1.0, scalar2=None, op0=ALU.add),
                             writes=["sc1p%d" % ci])
                        S.op("dve", lambda e, sc=sc, a2=a2: e.tensor_tensor(out=a2[:], in0=LN1G[:], in1=sc[:], op=ALU.mult),
                             reads=["sc1p%d" % ci])
                        S.op("dve", lambda e, sc=sc, b2=b2: e.tensor_tensor(out=b2[:], in0=LN1B[:], in1=sc[:], op=ALU.mult),
                             reads=["sc1p%d" % ci], writes=["b2t%d" % ci])
                        S.op("dve", lambda e, sh=sh, b2=b2: e.tensor_tensor(out=b2[:], in0=b2[:], in1=sh[:], op=ALU.add),
                             reads=["b2t%d" % ci], writes=["b2t%d" % ci])
                    S.barrier()
                if stop <= 0:
                    pass
                if bc_d is not None:
                    for ti_, tl_ in enumerate(bc_tiles):
                        S.dma("sp", bc_d[ti_], tl_[:], writes=["bcd%d" % ti_])
                    S.barrier()
            wo = sb("wo", [128, 8, D], BF16, st=st1)
            wrt = sb("wrt", [128, 8, 16], st=st1)
            rbB = sb("rbB", [128, 16], st=st1)
            rb1 = sb("rb1", [1, 16], st=st1)
            xt = [sb("xt%d" % i, [128, D], st=st1) for i in range(2)]
            yb = [sb("yb%d" % i, [128, 8, 512], BF16, st=st1) for i in range(2)]
            uu = [sb("uu%d" % i, [128, D], st=st1) for i in range(2)]
            xn = [sb("xn%d" % i, [128, D], st=st1) for i in range(2)]
            h2 = [sb("h2_%d" % i, [128, D], st=st1) for i in range(2)]
            x1a = [sb("x1a%d" % i, [128, D], st=st1) for i in range(2)]
            h2f = sb("h2f", [128, 8, 128], st=st1)
            stt = sb("stt", [128, 2, 6], st=st1)
            mv = sb("mv", [128, 2], st=st1)
            rstd = sb("rstd", [128, 1], st=st1)
            nmr = sb("nmr", [128, 1], st=st1)
            rt = [sb("rt%d" % i, [128, 16], st=st1) for i in range(6)]
            rs4 = [sb("rs4_%d" % i, [128, 4], st=st1) for i in range(4)]
            r1 = [sb("r1_%d" % i, [128, 1], st=st1) for i in range(2)]
            ps_y = [pst("ps_y%d" % i, [128, D], st=st1) for i in range(2)]
            ps_t = pst("ps_t", [128, 8, 128], st=st1)
            ps_r = pst("ps_r", [128, 16], st=st1)

            S.dma("pool", wo[:], wout.rearrange("(k p) n -> p k n", p=128), writes=["wo"])
            S.dma("sp", wrt[:], wr.rearrange("(k p) n -> p k n", p=128), writes=["wrt"])
            S.dma("sp", rb1[:], rb[:], writes=["rb1"])
            S.op("pe", lambda e: e.matmul(ps_r[:], lhsT=ones1[:], rhs=rb1[:], start=True, stop=True),
                 reads=["ones1", "rb1"], writes=["ps_r"])
            S.op("act", lambda e: e.activation(out=rbB[:], in_=ps_r[:], func=AF.Copy), reads=["ps_r"], writes=["rbB"])
            yT_v = yT_lat.rearrange("(k p) t -> p k t", p=128)
            def P1(i):
                    is_ctx = has_ctx and i == NTI - 1
                    b2_ = i % 2
                    blk = i // 4
                    if i % 4 == 0:
                        ntb = min(4, NTI - i)
                        if is_ctx:
                            S.dma("pool", yb[blk % 2][:, :, :64], yT_ctx.rearrange("(k p) t -> p k t", p=128),
                                  writes=["yb%d" % (blk % 2)])
                        else:
                            S.dma("pool", yb[blk % 2][:, :, :ntb * 128], yT_v[:, :, i * 128:(i + ntb) * 128],
                                  writes=["yb%d" % (blk % 2)])
                    S.dma("sp", xt[b2_][:], x[i * 128:(i + 1) * 128, :], writes=["xt%d" % b2_])
                    ybt = yb[blk % 2]
                    off = (i % 4) * 128
                    for half in range(2):
                        for k in range(8):
                            S.op("pe", lambda e, k=k, half=half, ybt=ybt, off=off, b2_=b2_: e.matmul(
                                ps_y[b2_][:, half * 512:(half + 1) * 512], lhsT=ybt[:, k, off:off + 128],
                                rhs=wo[:, k, half * 512:(half + 1) * 512], start=(k == 0), stop=(k == 7)),
                                reads=["yb%d" % (blk % 2), "wo"], writes=["ps_y%d" % b2_])
            def P2(i):
                    is_ctx = has_ctx and i == NTI - 1
                    b2_ = i % 2
                    blk = i // 4
                    if stop <= 0.1:
                        pass
                    g1t = G1c if is_ctx else G1
                    a2t = A2c if is_ctx else A2
                    b2t = B2c if is_ctx else B2
                    u_ = uu[b2_]; utok = "uu%d" % b2_
                    S.op("dve", lambda e, u_=u_, b2_=b2_, g1t=g1t: e.tensor_tensor(out=u_[:], in0=ps_y[b2_][:], in1=g1t[:], op=ALU.mult),
                         reads=["ps_y%d" % b2_], writes=[utok])
                    S.op("dve", lambda e, u_=u_, b2_=b2_: e.scalar_tensor_tensor(out=u_[:], in0=xt[b2_][:], scalar=ALPHA, in1=u_[:],
                                                                       op0=ALU.mult, op1=ALU.add),
                         reads=["xt%d" % b2_, utok], writes=[utok])
                    if stop <= 0.2:
                        pass
                    for hh in range(2):
                        S.op("dve", lambda e, hh=hh, u_=u_: e.bn_stats(out=stt[:, hh, :], in_=u_[:, hh * 512:(hh + 1) * 512]),
                             reads=[utok], writes=["stt%d" % hh])
                    S.op("dve", lambda e: e.bn_aggr(out=mv[:], in_=stt[:].rearrange("p a b -> p (a b)")),
                         reads=["stt0", "stt1"], writes=["mv"])
                    S.op("act", lambda e: e.activation(out=rstd[:], in_=mv[:, 1:2], func=AF.Sqrt, bias=epsT[:, 0:1], scale=1.0),
                         reads=["mv", "epsT"], writes=["rstd"])
                    S.op("dve", lambda e: e.reciprocal(out=rstd[:], in_=rstd[:]), reads=["rstd"], writes=["rstd"])
                    S.op("dve", lambda e: e.scalar_tensor_tensor(out=nmr[:], in0=mv[:, 0:1], scalar=-1.0, in1=rstd[:],
                                                                 op0=ALU.mult, op1=ALU.mult),
                         reads=["mv", "rstd"], writes=["nmr"])
                    if stop <= 0.3:
                        pass
                    xn_ = xn[b2_]; xtok = "xn%d" % b2_
                    S.op("act", lambda e, xn_=xn_, u_=u_: e.activation(out=xn_[:], in_=u_[:], func=AF.Identity,
                                                                       bias=nmr[:, 0:1], scale=rstd[:, 0:1]),
                         reads=[utok, "nmr", "rstd"], writes=[xtok])
                    if stop <= 0.4:
                        pass
                    xa_ = x1a[b2_]; xatok = "x1a%d" % b2_
                    S.op("pool", lambda e, xa_=xa_, xn_=xn_: e.tensor_tensor(out=xa_[:], in0=xn_[:], in1=GA[:], op=ALU.mult),
                         reads=[xtok], writes=[xatok])
                    S.op("pool", lambda e, xa_=xa_: e.tensor_tensor(out=xa_[:], in0=xa_[:], in1=BA[:], op=ALU.add),
                         reads=[xatok], writes=[xatok])
                    S.dma("sp", x1s[i * 128:(i + 1) * 128, :], xa_[:], reads=[xatok], writes=["x1s%d" % i])
                    h_ = h2[b2_]; htok = "h2_%d" % b2_
                    S.op("dve", lambda e, h_=h_, xn_=xn_, a2t=a2t: e.tensor_tensor(out=h_[:], in0=xn_[:], in1=a2t[:], op=ALU.mult),
                         reads=[xtok], writes=[htok])
                    S.op("dve", lambda e, h_=h_, b2t=b2t: e.tensor_tensor(out=h_[:], in0=h_[:], in1=b2t[:], op=ALU.add),
                         reads=[htok], writes=[htok])
                    if stop <= 0.5:
                        pass
            def P3(i):
                    is_ctx = has_ctx and i == NTI - 1
                    b2_ = i % 2
                    blk = i // 4
                    h_ = h2[b2_]; htok = "h2_%d" % b2_
                    for k in range(8):
                        S.op("pe", lambda e, k=k, h_=h_: e.transpose(out=ps_t[:, k, :], in_=h_[:, k * 128:(k + 1) * 128], identity=ident[:]),
                             reads=[htok, "ident"], writes=["ps_t"])
                    S.op("dve", lambda e: e.tensor_copy(out=h2f[:], in_=ps_t[:]), reads=["ps_t"], writes=["h2f"])
                    S.op("act", lambda e, i=i: e.activation(out=h2T[:, :, i * 128:(i + 1) * 128], in_=h2f[:], func=AF.Copy),
                         reads=["h2f"], writes=["h2T_%d" % i])
                    if stop <= 0.6:
                        pass
                    for k in range(8):
                        S.op("pe", lambda e, k=k: e.matmul(ps_r[:], lhsT=h2f[:, k, :], rhs=wrt[:, k, :], start=(k == 0), stop=(k == 7)),
                             reads=["h2f", "wrt"], writes=["ps_r"])
                    if stop <= 0.7:
                        pass
                    s_, sel_, msk, t16, w_, selm = rt
                    m1, m2, grp, ing = rs4
                    gm, wsum = r1
                    V3 = lambda a: a[:].rearrange("p (g i) -> p g i", g=4)
                    S.op("act", lambda e: e.activation(out=s_[:], in_=ps_r[:], func=AF.Sigmoid), reads=["ps_r"], writes=["r_s"])
                    S.op("dve", lambda e: e.tensor_tensor(out=sel_[:], in0=s_[:], in1=rbB[:], op=ALU.add), reads=["r_s", "rbB"], writes=["r_sel"])
                    S.op("dve", lambda e: e.tensor_reduce(out=m1[:], in_=V3(sel_), axis=AX.X, op=ALU.max), reads=["r_sel"], writes=["r_m1"])
                    S.op("dve", lambda e: e.tensor_tensor(out=V3(msk), in0=V3(sel_), in1=bc_mid(m1[:], 4), op=ALU.is_ge),
                         reads=["r_sel", "r_m1"], writes=["r_msk"])
                    S.op("dve", lambda e: e.scalar_tensor_tensor(out=t16[:], in0=msk[:], scalar=-NEGBIG, in1=sel_[:], op0=ALU.mult, op1=ALU.add),
                         reads=["r_msk", "r_sel"], writes=["r_t16"])
                    S.op("dve", lambda e: e.tensor_reduce(out=m2[:], in_=V3(t16), axis=AX.X, op=ALU.max), reads=["r_t16"], writes=["r_m2"])
                    S.op("dve", lambda e: e.tensor_tensor(out=grp[:], in0=m1[:], in1=m2[:], op=ALU.add), reads=["r_m1", "r_m2"], writes=["r_grp"])
                    S.op("dve", lambda e: e.tensor_reduce(out=gm[:], in_=grp[:], axis=AX.X, op=ALU.max), reads=["r_grp"], writes=["r_gm"])
                    S.op("dve", lambda e: e.tensor_scalar(out=ing[:], in0=grp[:], scalar1=gm[:, 0:1], scalar2=None, op0=ALU.is_ge),
                         reads=["r_grp", "r_gm"], writes=["r_ing"])
                    S.op("dve", lambda e: e.tensor_tensor(out=V3(selm), in0=V3(sel_), in1=bc_mid(m2[:], 4), op=ALU.is_ge),
                         reads=["r_sel", "r_m2"], writes=["r_selm"])
                    S.op("dve", lambda e: e.tensor_tensor(out=V3(selm), in0=V3(selm), in1=bc_mid(ing[:], 4), op=ALU.mult),
                         reads=["r_selm", "r_ing"], writes=["r_selm"])
                    S.op("dve", lambda e: e.tensor_tensor(out=w_[:], in0=s_[:], in1=selm[:], op=ALU.mult), reads=["r_s", "r_selm"], writes=["r_w"])
                    S.op("dve", lambda e: e.tensor_reduce(out=wsum[:], in_=w_[:], axis=AX.X, op=ALU.add), reads=["r_w"], writes=["r_ws"])
                    S.op("dve", lambda e: e.reciprocal(out=wsum[:], in_=wsum[:]), reads=["r_ws"], writes=["r_ws"])
                    S.op("dve", lambda e, i=i: e.tensor_scalar(out=gates[:, i, :], in0=w_[:], scalar1=wsum[:, 0:1], scalar2=None, op0=ALU.mult),
                         reads=["r_w", "r_ws"], writes=["gates%d" % i])
            for it_ in range(NTI + 2):
                if it_ < NTI:
                    P1(it_)
                if 0 <= it_ - 1 < NTI:
                    P2(it_ - 1)
                if 0 <= it_ - 2 < NTI:
                    P3(it_ - 2)
            S.barrier()
        if stop <= 1:
            pass
        st2 = contextlib.ExitStack()
        with st2:
            acc = sb("acc", [128, NTI, D], st=st2)
            st2w = contextlib.ExitStack()
            wgb = [sb("wgb%d" % i, [128, 8, 512], BF16, st=st2w) for i in range(2)]
            wub = [sb("wub%d" % i, [128, 8, 512], BF16, st=st2w) for i in range(2)]
            wdb = [sb("wdb%d" % i, [128, 4, D], BF16, st=st2w) for i in range(2)]
            sg = [sb("sg%d" % i, [128, 512], st=st2w) for i in range(2)]
            heT = [sb("heT%d" % i, [128, 4, 512], BF16, st=st2w) for i in range(2)]
            ps_g = [pst("ps_g%d" % i, [128, 512], st=st2w) for i in range(2)]
            ps_u = [pst("ps_u%d" % i, [128, 512], st=st2w) for i in range(2)]
            ps_d = [pst("ps_d%d" % i, [128, D], st=st2w) for i in range(2)]
            blocks = []
            i = 0
            while i < NTI:
                n = min(4, NTI - i)
                blocks.append((i, n))
                i += n

            def load_w(e_):
                p = e_ % 2
                S.dma("pool", wgb[p][:], wg[e_].rearrange("(k p) n -> p k n", p=128), writes=["wgb%d" % p])
                S.dma("pool", wub[p][:], wu[e_].rearrange("(k p) n -> p k n", p=128), writes=["wub%d" % p])
                S.dma("pool", wdb[p][:], wd[e_].rearrange("(k p) n -> p k n", p=128), writes=["wdb%d" % p])

            load_w(0)
            if n_exp > 1:
                load_w(1)
            fcn = [0]; dn = [0]
            steps = [(e_, bi_) for e_ in range(n_exp) for bi_ in range(len(blocks))]

            def stage_G(k):
                e_, bi_ = steps[k]
                p = e_ % 2
                t0, ntb = blocks[bi_]
                hb = k % 2
                ncol = ntb * 128
                for fc in range(4):
                    q = fcn[0] % 2; fcn[0] += 1
                    for kk in range(8):
                        S.op("pe", lambda e, kk=kk, fc=fc, q=q, p=p, t0=t0, ncol=ncol: e.matmul(
                            ps_g[q][:, :ncol], lhsT=wgb[p][:, kk, fc * 128:(fc + 1) * 128],
                            rhs=h2T[:, kk, t0 * 128:t0 * 128 + ncol], start=(kk == 0), stop=(kk == 7)),
                            reads=["wgb%d" % p], writes=["ps_g%d" % q])
                    for kk in range(8):
                        S.op("pe", lambda e, kk=kk, fc=fc, q=q, p=p, t0=t0, ncol=ncol: e.matmul(
                            ps_u[q][:, :ncol], lhsT=wub[p][:, kk, fc * 128:(fc + 1) * 128],
                            rhs=h2T[:, kk, t0 * 128:t0 * 128 + ncol], start=(kk == 0), stop=(kk == 7)),
                            reads=["wub%d" % p], writes=["ps_u%d" % q])
                    S.op("act", lambda e, q=q, ncol=ncol: e.activation(out=sg[q][:, :ncol], in_=ps_g[q][:, :ncol], func=AF.Silu),
                         reads=["ps_g%d" % q], writes=["sg%d" % q])
                    S.op("dve", lambda e, q=q, ncol=ncol, hb=hb, fc=fc: e.tensor_tensor(
                        out=heT[hb][:, fc, :ncol], in0=ps_u[q][:, :ncol], in1=sg[q][:, :ncol], op=ALU.mult),
                        reads=["ps_u%d" % q, "sg%d" % q], writes=["heT%d_%d" % (hb, fc)])

            def stage_D(k):
                e_, bi_ = steps[k]
                p = e_ % 2
                t0, ntb = blocks[bi_]
                hb = k % 2
                for tt in range(ntb):
                    ti = t0 + tt
                    dq = dn[0] % 2; dn[0] += 1
                    for half in range(2):
                        for fc in range(4):
                            S.op("pe", lambda e, fc=fc, half=half, dq=dq, hb=hb, tt=tt, p=p: e.matmul(
                                ps_d[dq][:, half * 512:(half + 1) * 512], lhsT=heT[hb][:, fc, tt * 128:(tt + 1) * 128],
                                rhs=wdb[p][:, fc, half * 512:(half + 1) * 512], start=(fc == 0), stop=(fc == 3)),
                                reads=["heT%d_%d" % (hb, fc), "wdb%d" % p], writes=["ps_d%d" % dq])
                    if e_ == 0:
                        S.op("dve", lambda e, ti=ti, dq=dq, e_=e_: e.tensor_scalar(
                            out=acc[:, ti, :], in0=ps_d[dq][:], scalar1=gates[:, ti, e_:e_ + 1], scalar2=None, op0=ALU.mult),
                            reads=["ps_d%d" % dq], writes=["acc%d" % ti])
                    else:
                        S.op("dve", lambda e, ti=ti, dq=dq, e_=e_: e.scalar_tensor_tensor(
                            out=acc[:, ti, :], in0=ps_d[dq][:], scalar=gates[:, ti, e_:e_ + 1], in1=acc[:, ti, :],
                            op0=ALU.mult, op1=ALU.add),
                            reads=["ps_d%d" % dq, "acc%d" % ti], writes=["acc%d" % ti])
                if bi_ == len(blocks) - 1 and e_ + 2 < n_exp:
                    load_w(e_ + 2)

            for k in range(len(steps) + 1):
                if k < len(steps):
                    stage_G(k)
                if k >= 1:
                    stage_D(k - 1)
            S.barrier()
            st2w.close()
            xtT = sb("xtT", [128, 8, 128], st=st2)
            ps_x = pst("ps_x", [128, 8, 128], st=st2)
            xr = [sb("xr%d" % i, [128, D], st=st2) for i in range(2)]
            u3 = [sb("u3_%d" % i, [128, D], st=st2) for i in range(2)]
            stt3 = sb("stt3", [128, 2, 6], st=st2)
            mv3 = sb("mv3", [128, 2], st=st2)
            rstd3 = sb("rstd3", [128, 1], st=st2)
            nmr3 = sb("nmr3", [128, 1], st=st2)
            def Q1(i):
                    is_ctx = has_ctx and i == NTI - 1
                    b2_ = i % 2
                    g2t = G2c if is_ctx else G2
                    S.dma("sp", xr[b2_][:], x1s[i * 128:(i + 1) * 128, :], reads=["x1s%d" % i], writes=["xr%d" % b2_])
            def Q2(i):
                    is_ctx = has_ctx and i == NTI - 1
                    b2_ = i % 2
                    g2t = G2c if is_ctx else G2
                    u_ = u3[b2_]; utok = "u3_%d" % b2_
                    S.op("dve", lambda e, u_=u_, i=i, g2t=g2t: e.tensor_tensor(out=u_[:], in0=acc[:, i, :], in1=g2t[:], op=ALU.mult),
                         writes=[utok])
                    S.op("dve", lambda e, u_=u_, b2_=b2_: e.tensor_tensor(out=u_[:], in0=u_[:], in1=xr[b2_][:], op=ALU.add),
                         reads=[utok, "xr%d" % b2_], writes=[utok])
                    for hh in range(2):
                        S.op("dve", lambda e, hh=hh, u_=u_: e.bn_stats(out=stt3[:, hh, :], in_=u_[:, hh * 512:(hh + 1) * 512]),
                             reads=[utok], writes=["stt3_%d" % hh])
                    S.op("dve", lambda e: e.bn_aggr(out=mv3[:], in_=stt3[:].rearrange("p a b -> p (a b)")),
                         reads=["stt3_0", "stt3_1"], writes=["mv3"])
                    S.op("act", lambda e: e.activation(out=rstd3[:], in_=mv3[:, 1:2], func=AF.Sqrt, bias=epsT[:, 0:1], scale=1.0),
                         reads=["mv3"], writes=["rstd3"])
                    S.op("dve", lambda e: e.reciprocal(out=rstd3[:], in_=rstd3[:]), reads=["rstd3"], writes=["rstd3"])
                    S.op("dve", lambda e: e.scalar_tensor_tensor(out=nmr3[:], in0=mv3[:, 0:1], scalar=-1.0, in1=rstd3[:],
                                                                 op0=ALU.mult, op1=ALU.mult),
                         reads=["mv3", "rstd3"], writes=["nmr3"])
                    S.op("act", lambda e, u_=u_: e.activation(out=u_[:], in_=u_[:], func=AF.Identity, bias=nmr3[:, 0:1], scale=rstd3[:, 0:1]),
                         reads=[utok, "nmr3", "rstd3"], writes=[utok])
                    S.op("pool", lambda e, u_=u_: e.tensor_tensor(out=u_[:], in0=u_[:], in1=LN2G[:], op=ALU.mult), reads=[utok], writes=[utok])
                    S.op("pool", lambda e, u_=u_: e.tensor_tensor(out=u_[:], in0=u_[:], in1=LN2B[:], op=ALU.add), reads=[utok], writes=[utok])
                    S.dma("sp", xo[i * 128:(i + 1) * 128, :], u_[:], reads=[utok], writes=["xo%d" % i])
            def Q3(i):
                    is_ctx = has_ctx and i == NTI - 1
                    b2_ = i % 2
                    g2t = G2c if is_ctx else G2
                    u_ = u3[b2_]; utok = "u3_%d" % b2_
                    if xT_lat_o is not None:
                        for k in range(8):
                            S.op("pe", lambda e, k=k, u_=u_: e.transpose(out=ps_x[:, k, :], in_=u_[:, k * 128:(k + 1) * 128], identity=ident[:]),
                                 reads=[utok, "ident"], writes=["ps_x"])
                        S.op("dve", lambda e: e.tensor_copy(out=xtT[:], in_=ps_x[:]), reads=["ps_x"], writes=["xtT"])
                        if is_ctx:
                            S.dma("sp", xT_ctx_o.rearrange("(k p) t -> p k t", p=128), xtT[:, :, 0:64], reads=["xtT"], writes=["xTo%d" % i])
                        else:
                            S.dma("sp", xT_lat_o.rearrange("(k p) t -> p k t", p=128)[:, :, i * 128:(i + 1) * 128], xtT[:], reads=["xtT"], writes=["xTo%d" % i])
            for it_ in range(NTI + 2):
                if it_ < NTI:
                    Q1(it_)
                if 0 <= it_ - 1 < NTI:
                    Q2(it_ - 1)
                if 0 <= it_ - 2 < NTI:
                    Q3(it_ - 2)
            S.barrier()


def build_fused(n_lat=8192):
    T = CTX + n_lat
    NQ = n_lat // 4
    NLT = NQ // 128
    NTB = (NLT + 1) * 128
    nc = bass.Bass("TRN2", target_bir_lowering=False)
    def din(name, shape):
        return nc.dram_tensor(name, shape, F32, kind="ExternalInput").ap()
    def scr(name, shape):
        return nc.dram_tensor(name, shape, F32, kind="Internal").ap()
    I = {}
    I["xT0"] = din("xT0", [1024, T]); I["xq0"] = din("xq0", [4, NTB, 1024])
    I["wcore"] = din("wcore", [2, 4, 1024, NCOL]); I["ccol"] = din("ccol", [128, 16])
    I["wadaA"] = din("wadaA", [2, 1024, 2048]); I["badac"] = din("badac", [2, 128, 16])
    I["lbl"] = din("lbl", [4, 64, 4]); I["gnorm"] = din("gnorm", [2, 64, 1]); I["nab"] = din("nab", [2, 4, 21, 128, 128])
    I["swm"] = din("swm", [2, 128, 128]); I["sink"] = din("sink", [2, 4, 1, 2])
    I["ropeC"] = din("ropeC", [64, n_lat]); I["ropeS"] = din("ropeS", [64, n_lat])
    I["cst"] = din("cst", [128, 1024]); I["vmask"] = din("vmask", [128, 4])
    I["wadaB"] = din("wadaB", [2, 1024, 4096]); I["badaB"] = din("badaB", [2, 1, 4096]); I["lnp"] = din("lnp", [2, 1, 4096])
    I["wout"] = din("wout", [2, 1024, 1024]); I["wr"] = din("wr", [1024, 16]); I["rb"] = din("rb", [1, 16])
    I["wg"] = din("wg", [2, 16, 1024, 512]); I["wu"] = din("wu", [2, 16, 1024, 512]); I["wd"] = din("wd", [2, 16, 512, 1024])
    I["ident"] = din("ident", [128, 128])
    out = nc.dram_tensor("out", [NQ, 1024], F32, kind="ExternalOutput").ap()
    Y = scr("Y", [1024, T]); xT1 = scr("xT1", [1024, T]); X1 = scr("X1", [4, NTB, 1024]); x1s = scr("x1s", [NTB, 1024]); ofs = scr("ofs", [64, T])
    UB = scr("UB", [64, 1 + n_lat // 512, 1024]); SG = scr("SG", [64, T])
    QB = nc.dram_tensor("QB", [64, T], BF16, kind="Internal").ap()
    modc_d = scr("modc_d", [128, 32]); bc_d = scr("bc_d", [12, 128, 1024])
    es = contextlib.ExitStack()
    with es:
        S = Sched(nc, es)
        for l in range(2):
            last = l == 1
            xTsrc = I["xT0"] if l == 0 else xT1
            for j in range(4):
                io = {"xT": xTsrc, "wcore": I["wcore"][l, j], "ccol": I["ccol"], "wada": I["wadaA"][l], "badac": I["badac"][l],
                      "lbl": I["lbl"][j], "gnorm": I["gnorm"][l], "nab": I["nab"][l, j], "swm": I["swm"], "sink": I["sink"][l, j],
                      "ropeC": I["ropeC"], "ropeS": I["ropeS"], "cst": I["cst"], "vmask": I["vmask"],
                      "yT": Y[256 * j:256 * (j + 1), :], "ofs": ofs, "UB": UB, "QB": QB, "SG": SG, "modc_d": modc_d, "mod_first": j == 0}
                emit_phase_a(nc, S, io, l, not last, "a%d%d_" % (l, j), n_lat=n_lat)
            if l == 0:
                for q in range(4):
                    io = {"yT_lat": Y[:, CTX + q * NQ:CTX + (q + 1) * NQ], "yT_ctx": Y[:, q * 64:(q + 1) * 64],
                          "x": I["xq0"][q], "ccol": I["ccol"], "wada": I["wadaB"][l], "bada": I["badaB"][l],
                          "lnp": I["lnp"][l], "wout": I["wout"][l], "wr": I["wr"], "rb": I["rb"], "wg": I["wg"][l], "wu": I["wu"][l], "wd": I["wd"][l],
                          "ident": I["ident"], "x1s": x1s[0:NTB, :], "xo": X1[q],
                          "xT_lat_o": xT1[:, CTX + q * NQ:CTX + (q + 1) * NQ], "xT_ctx_o": xT1[:, q * 64:(q + 1) * 64],
                          "bc_d": bc_d, "bc_first": q == 0}
                    emit_phase_b(nc, S, io, True, "b%d%d_" % (l, q), n_lat_tiles=NLT)
            else:
                q_pool = nc.gpsimd.partition_id() % 4
                q_sp = nc.sync.partition_id() % 4
                X1f = X1.rearrange("q n d -> (q n) d")
                io = {"yT_lat": Y[:, bass.ds(q_pool * NQ + CTX, NQ)],
                      "x": X1f[bass.ds(q_sp * NTB, NQ), :], "ccol": I["ccol"], "wada": I["wadaB"][l], "bada": I["badaB"][l],
                      "lnp": I["lnp"][l], "wout": I["wout"][l], "wr": I["wr"], "rb": I["rb"], "wg": I["wg"][l], "wu": I["wu"][l], "wd": I["wd"][l],
                      "ident": I["ident"], "x1s": x1s[0:NQ, :], "xo": out}
                emit_phase_b(nc, S, io, False, "b%dq_" % l, n_lat_tiles=NLT)
        S.finish()
    return nc


def fused_inputs(inp, b, n_lat=8192):
    NQ = n_lat // 4; NLT = NQ // 128; NTB = (NLT + 1) * 128
    f = lambda a: np.ascontiguousarray(a, dtype=np.float32)
    x = inp['x'][b, :n_lat]; ctx = inp['ctx'][b]
    xT0 = np.concatenate([ctx, x], 0).T
    xq0 = np.zeros((4, NTB, 1024), np.float32)
    for q in range(4):
        xq0[q, :NQ] = x[q * NQ:(q + 1) * NQ]
        xq0[q, NQ:NQ + 64] = ctx[q * 64:(q + 1) * 64]
    per = [[core_inputs_a(inp, l, b, j, xT0, n_lat=n_lat) for j in range(4)] for l in range(2)]
    m = {"xT0": f(xT0), "xq0": xq0, "ccol": per[0][0]["ccol"], "ropeC": per[0][0]["ropeC"], "ropeS": per[0][0]["ropeS"],
         "cst": per[0][0]["cst"], "vmask": per[0][0]["vmask"], "swm": per[0][0]["swm"]}
    m["wcore"] = f(np.stack([np.stack([per[l][j]["wcore"] for j in range(4)]) for l in range(2)]))
    m["wadaA"] = f(np.stack([per[l][0]["wada"] for l in range(2)])); m["badac"] = f(np.stack([per[l][0]["badac"] for l in range(2)]))
    m["lbl"] = f(np.stack([per[0][j]["lbl"] for j in range(4)])); m["gnorm"] = f(np.stack([per[l][0]["gnorm"] for l in range(2)]))
    m["nab"] = f(np.stack([np.stack([per[l][j]["nab"] for j in range(4)]) for l in range(2)]))
    m["sink"] = f(np.stack([np.stack([per[l][j]["sink"] for j in range(4)]) for l in range(2)]))
    m["wadaB"] = f(np.stack([inp['w_ada'][l][:, 2048:] for l in range(2)])); m["badaB"] = f(np.stack([inp['b_ada'][l][None, 2048:] for l in range(2)]))
    m["lnp"] = f(np.stack([np.concatenate([inp['ln1_g'][l], inp['ln1_b'][l], inp['ln2_g'][l], inp['ln2_b'][l]])[None] for l in range(2)]))
    feat = np.zeros(1024, np.int64)
    for j in range(4):
        feat[256 * j:256 * j + 64] = 64 * j + np.arange(64)
        feat[256 * j + 64:256 * j + 128] = 256 + 64 * j + np.arange(64)
        feat[256 * j + 128:256 * j + 256] = 512 + 128 * j + np.arange(128)
    m["wout"] = f(np.stack([inp['w_out'][l][feat, :] for l in range(2)]))
    m["wr"] = f(inp['w_router']); m["rb"] = f(inp['router_bias'][None])
    m["wg"] = f(inp['w_gate']); m["wu"] = f(inp['w_up']); m["wd"] = f(inp['w_down'])
    m["ident"] = np.eye(128, dtype=np.float32)
    return m

_NC = {}


def kernel(**inputs):
    inp = {k: np.asarray(v, dtype=np.float32) for k, v in inputs.items()}
    if "nc" not in _NC:
        _NC["nc"] = build_fused(8192)
    nc = _NC["nc"]
    maps = [fused_inputs(inp, b) for b in range(2)]
    in_maps = [maps[c // 4] for c in range(8)]
    res = run_bass_kernel_spmd(nc, in_maps, core_ids=list(range(8))).results
    out = np.stack([np.concatenate([res[4 * b + q]["out"] for q in range(4)], 0) for b in range(2)], 0)
    return np.ascontiguousarray(out, dtype=np.float32)
```

```python
import contextlib
import numpy as np
import concourse.bass as bass
import concourse.mybir as mybir
from concourse.bass_utils import run_bass_kernel_spmd


F32 = mybir.dt.float32
BF16 = mybir.dt.bfloat16
AF = mybir.ActivationFunctionType
ALU = mybir.AluOpType
AX = mybir.AxisListType


class Sched:
    ENG = ("pe", "act", "dve", "pool", "sp")

    def __init__(self, nc, es, n_dma_sems=12):
        self.nc = nc
        self.e = {"pe": nc.tensor, "act": nc.scalar, "dve": nc.vector, "pool": nc.gpsimd, "sp": nc.sync}
        self.sem = {}
        self.cnt = {}
        for k in self.ENG:
            self.sem[k] = es.enter_context(nc.semaphore("s_" + k))
            self.cnt[k] = 0
        self.dq = {}
        for q in ("sp", "pool", "act"):
            n = n_dma_sems if q != "act" else 4
            self.dq[q] = {"sems": [], "vals": [0] * n, "rr": 0}
            for i in range(n):
                key = "d_%s_%d" % (q, i)
                self.sem[key] = es.enter_context(nc.semaphore(key))
                self.dq[q]["sems"].append(key)
        self.waited = {k: {} for k in self.ENG}
        self.lastw = {}
        self.readers = {}
        self.n_inst = 0
        self.n_wait = 0

    def _wait(self, eng, ev):
        if ev is None:
            return
        s, v = ev
        if s == eng and eng == "pe":
            return
        if s == eng and v <= 0:
            return
        if self.waited[eng].get(s, 0) >= v:
            return
        self.e[eng].wait_ge(self.sem[s], v)
        self.waited[eng][s] = v
        self.n_wait += 1

    def _deps(self, eng, reads, writes):
        for t in reads:
            self._wait(eng, self.lastw.get(t))
        for t in writes:
            self._wait(eng, self.lastw.get(t))
            for ev in self.readers.get(t, {}).items():
                self._wait(eng, ev)

    def _record(self, ev, reads, writes):
        s, v = ev
        for t in reads:
            r = self.readers.setdefault(t, {})
            if r.get(s, 0) < v:
                r[s] = v
        for t in writes:
            self.lastw[t] = ev
            self.readers[t] = {}

    def op(self, eng, fn, reads=(), writes=()):
        self._deps(eng, reads, writes)
        inst = fn(self.e[eng])
        self.cnt[eng] += 1
        inst.then_inc(self.sem[eng], 1)
        self._record((eng, self.cnt[eng]), reads, writes)
        self.n_inst += 1
        return inst

    def dma(self, q, out, in_, reads=(), writes=(), **kw):
        d = self.dq[q]
        i = d["rr"]
        d["rr"] = (i + 1) % len(d["sems"])
        key = d["sems"][i]
        if d["vals"][i] > 0:
            self._wait(q, (key, d["vals"][i]))
        self._deps(q, reads, writes)
        inst = self.e[q].dma_start(out=out, in_=in_, **kw)
        d["vals"][i] += 16
        inst.then_inc(self.sem[key], 16)
        self._record((key, d["vals"][i]), reads, writes)
        self.n_inst += 1
        return inst

    def all_events(self):
        evs = [(k, self.cnt[k]) for k in self.ENG if self.cnt[k] > 0]
        for q, d in self.dq.items():
            for key, v in zip(d["sems"], d["vals"]):
                if v > 0:
                    evs.append((key, v))
        return evs

    def barrier(self, engines=None):
        evs = self.all_events()
        for eng in (engines or self.ENG):
            for ev in evs:
                self._wait(eng, ev)

    def finish(self):
        self.barrier(engines=("sp",))


RMS_EPS = 1e-6
NCOL = 960
CTX = 256


def bc_mid(ap2d, n):
    p, k = ap2d.shape
    return ap2d.unsqueeze(2).broadcast_to([p, k, n])


def na_configs(nrows=128):
    cfg = {}
    mats = []
    interior = {}
    for pq in range(nrows // 2):
        rows = set()
        for r in (2 * pq, 2 * pq + 1):
            rs = min(max(r - 4, 0), nrows - 8)
            rows |= set(range(rs, rs + 8))
        pks = sorted(set(k // 2 for k in rows))
        lst = []
        for pk in pks:
            if 2 <= pq <= nrows // 2 - 3:
                key = ("i", pk - pq)
            else:
                key = (pq, pk)
            if key not in interior:
                interior[key] = len(mats)
                mats.append((pq, pk))
            lst.append((pk, interior[key]))
        cfg[pq] = lst
    return cfg, mats


def emit_phase_a(nc, S, io, layer, with_ctx_out, uid, n_lat=8192):
    T = CTX + n_lat
    D = 1024
    xT = io["xT"]; wcore = io["wcore"]; ccol = io["ccol"]; wada = io["wada"]; badac = io["badac"]; lbl = io["lbl"]
    gnorm_d = io["gnorm"]; nab_d = io["nab"]; swm_d = io["swm"]; sink_d = io["sink"]; ropeC_d = io["ropeC"]; ropeS_d = io["ropeS"]
    cst_d = io["cst"]; vmask_d = io["vmask"]; yT = io["yT"]; ofs = io["ofs"]
    stop = 99

    cfgs, mats = na_configs(n_lat // 64)
    assert len(mats) == 21
    blocks = [(0, CTX)] + [(CTX + i * 512, 512) for i in range(n_lat // 512)]
    NTILE = T // 128

    es = contextlib.ExitStack()
    with es:
        sb = lambda name, shape, dt=F32, st=es: st.enter_context(nc.sbuf_tensor(uid + "s_" + name, shape, dt))
        pst = lambda name, shape, dt=F32, st=es: st.enter_context(nc.psum_tensor(uid + "p_" + name, shape, dt))
        cst = sb("cst", [128, 128 * 4 + 512])
        identb = sb("identb", [128, 128], BF16)
        ident8b = sb("ident8b", [128, 128], BF16)
        hmF = cst[:, 256:384]; hmB = cst[:, 384:512]; rmask = cst[0:64, 512:1024]
        vmask = sb("vmask", [128, 4])
        wb = sb("wb", [128, 8, NCOL], BF16)
        modc = sb("modc", [128, 16, 2])
        lb = sb("lb", [64, 2]); oml = sb("oml", [64, 2])
        gnorm = sb("gnorm_sb", [64, 1])
        ones64 = sb("ones64", [64, 64])
        epsr = sb("epsr", [64, 1])
        nqT = sb("nqT", [64, T], BF16); nkT = sb("nkT", [64, T], BF16); nv1 = sb("nv1", [128, NTILE, 65], BF16)
        sq0T = sb("sq0T", [64, T], BF16); sq1T = sb("sq1T", [64, T], BF16); skT = sb("skT", [64, T], BF16)
        sv1 = sb("sv1", [128, NTILE, 65], BF16)
        S.dma("sp", cst[:], cst_d[:], writes=["cst"])
        S.dma("sp", vmask[:], vmask_d[:], writes=["vmask"])
        S.dma("sp", gnorm[:], gnorm_d[:], writes=["gnorm"])
        S.dma("pool", wb[:], wcore.rearrange("(k p) n -> p k n", p=128), writes=["wb"])
        S.op("dve", lambda e: e.tensor_copy(out=identb[:], in_=cst[:, 0:128]), reads=["cst"], writes=["identb"])
        S.op("dve", lambda e: e.tensor_copy(out=ident8b[:], in_=cst[:, 128:256]), reads=["cst"], writes=["ident8b"])
        S.op("dve", lambda e: e.memset(ones64[:], 1.0), writes=["ones64"])
        S.op("dve", lambda e: e.memset(epsr[:], RMS_EPS), writes=["epsr"])
        S.op("pool", lambda e: e.memset(nv1[:, :, 64:65], 1.0), writes=["nv1ones"])
        S.op("pool", lambda e: e.memset(sv1[:, :, 64:65], 1.0), writes=["sv1ones"])
        st0 = contextlib.ExitStack()
        with st0:
            lbt = sb("lbt", [64, 4], st=st0)
            S.dma("sp", lbt[:], lbl[:], writes=["lbt"])
            if layer == 0:
                S.op("dve", lambda e: e.memset(lb[:], 1e-6), writes=["lb"])
            else:
                lbv = lbt[:].rearrange("p (d l) -> p d l", l=2)
                S.op("dve", lambda e: e.tensor_tensor(out=lb[:], in0=lbv[:, :, 0], in1=lbv[:, :, 1], op=ALU.subtract),
                     reads=["lbt"], writes=["lb"])
                S.op("act", lambda e: e.activation(out=lb[:], in_=lb[:], func=AF.Exp), reads=["lb"], writes=["lb"])
                S.op("dve", lambda e: e.tensor_scalar(out=lb[:], in0=lb[:], scalar1=1.0, scalar2=None, op0=ALU.add), reads=["lb"], writes=["lb"])
                S.op("dve", lambda e: e.reciprocal(out=lb[:], in_=lb[:]), reads=["lb"], writes=["lb"])
                S.op("dve", lambda e: e.tensor_scalar(out=lb[:], in0=lb[:], scalar1=1e-6, scalar2=None, op0=ALU.max), reads=["lb"], writes=["lb"])
            S.op("dve", lambda e: e.tensor_scalar(out=oml[:], in0=lb[:], scalar1=-1.0, scalar2=1.0, op0=ALU.mult, op1=ALU.add),
                 reads=["lb"], writes=["oml"])
            modc_d = io.get("modc_d")
            if modc_d is not None and not io.get("mod_first", True):
                S.dma("sp", modc[:].rearrange("p a b -> p (a b)"), modc_d, writes=["modc"])
                S.barrier()
            else:
                cc = sb("cc", [128, 16], st=st0); scc = sb("scc", [128, 8, 2], st=st0)
                wa = sb("wa", [128, 8, 2048], st=st0)
                bdc = sb("bdc", [128, 16], st=st0)
                psm = pst("psm", [128, 16, 2], st=st0)
                S.dma("sp", cc[:], ccol[:], writes=["cc"])
                S.dma("sp", bdc[:], badac[:], writes=["bdc"])
                S.dma("sp", wa[:], wada.rearrange("(k p) n -> p k n", p=128), writes=["wa"])
                S.op("act", lambda e: e.activation(out=scc[:].rearrange("p k w -> p w k"), in_=cc[:].rearrange("p (w k) -> p w k", w=2), func=AF.Silu),
                     reads=["cc"], writes=["scc"])
                for dch in range(16):
                    for k in range(8):
                        S.op("pe", lambda e, dch=dch, k=k: e.matmul(psm[:, dch, :], lhsT=wa[:, k, dch * 128:(dch + 1) * 128], rhs=scc[:, k, :],
                                                                    start=(k == 0), stop=(k == 7)),
                             reads=["wa", "scc"], writes=["psm"])
                S.op("dve", lambda e: e.tensor_tensor(out=modc[:], in0=psm[:], in1=bc_mid(bdc[:], 2), op=ALU.add),
                     reads=["psm", "bdc"], writes=["modc"])
                S.op("dve", lambda e: e.tensor_scalar(out=modc[:, 8:16, :], in0=modc[:, 8:16, :], scalar1=1.0, scalar2=None, op0=ALU.add),
                     reads=["modc"], writes=["modc"])
                if modc_d is not None:
                    S.dma("sp", modc_d, modc[:].rearrange("p a b -> p (a b)"), reads=["modc"], writes=["modc_d"])
                S.barrier()
        UB = io["UB"]; QB = io["QB"]; SG = io["SG"]
        dcyB = sb("dcyB", [64, len(blocks), 16])
        Sall = sb("Sall", [64, 17, 64])
        Sbf = sb("Sbf", [64, 16, 64], BF16)
        Ub = sb("Ub", [64, 16, 64])
        sgt = sb("sgt", [64, 512])
        ps_oi = pst("ps_oi", [64, 512])
        oi = sb("oi", [64, 512]); oo = sb("oo", [64, 512]); ofb = sb("ofb", [64, 512])
        qtl = sb("qtl", [64, 512], BF16)
        stp = contextlib.ExitStack()
        with stp:
            xt = sb("xt0", [128, 8, 512], st=stp)
            hT = [sb("hT%d" % i, [128, 8, 512], BF16, st=stp) for i in range(2)]
            rC = sb("rC", [64, 512], st=stp); rS = sb("rS", [64, 512], st=stp)
            g_sb = {}
            for nm in ("aq", "az", "az2", "ag", "r0", "r1"):
                g_sb[nm] = sb("g_" + nm, [64, 512], st=stp)
            tA = sb("tA", [64, 512], st=stp); tB = sb("tB", [64, 512], st=stp); tC = sb("tC", [64, 512], st=stp)
            tD = sb("tD", [64, 512], st=stp); tE = sb("tE", [64, 512], st=stp)
            tot = sb("tot", [64, 16], st=stp); dcy = sb("dcy", [64, 16], st=stp)
            ktl = sb("ktl", [64, 512], BF16, st=stp); khT = sb("khT", [64, 512], BF16, st=stp)
            qtlb = sb("qtlb", [64, 512], BF16, st=stp); ktlb = sb("ktlb", [64, 512], BF16, st=stp); khTb = sb("khTb", [64, 512], BF16, st=stp)
            dcy2 = sb("dcy2", [64, 16], st=stp)
            kh = sb("kh", [128, 4, 64], BF16, st=stp)
            vt = sb("vt", [128, 4, 64], BF16, st=stp)
            vblk = sb("vblk", [128, 4, 4, 64], BF16, st=stp)
            scT = sb("scT", [128, 128], BF16, st=stp)
            ps_f = [pst("ps_f%d" % i, [64, 512], st=stp) for i in range(2)]
            ps_tm = pst("ps_tm", [128, 192], st=stp)
            ps_kh = pst("ps_kh", [128, 64], BF16, st=stp)
            ps_U = pst("ps_U", [64, 4, 64], st=stp)
            ps_sc = pst("ps_sc", [128, 128], st=stp)
            ps_oa = pst("ps_oa", [64, 512], st=stp)
            S.op("dve", lambda e: e.memset(Sall[:, 0, :], 0.0), writes=["Sall"])
            fcnt = [0]

            HT = io.get("HT")
            ht_first = io.get("mod_first", True)

            def load_block(bi, par):
                t0, n = blocks[bi]
                w = 1 if t0 < CTX else 0
                if HT is not None and not ht_first:
                    S.dma("sp", hT[par][:, :, :n], HT.rearrange("(k p) t -> p k t", p=128)[:, :, t0:t0 + n],
                          writes=["hT%d_%d" % (par, k) for k in range(8)])
                    return
                for h0 in range(0, n, 256):
                    S.dma("sp", xt[:, :, :256], xT.rearrange("(k p) t -> p k t", p=128)[:, :, t0 + h0:t0 + h0 + 256], writes=["xt0"])
                    for k in range(8):
                        eng = "dve" if k % 2 == 0 else "pool"
                        S.op(eng, lambda e, k=k, w=w, par=par, h0=h0: e.tensor_scalar(
                            out=hT[par][:, k, h0:h0 + 256], in0=xt[:, k, :256], scalar1=modc[:, 8 + k, w:w + 1], scalar2=modc[:, k, w:w + 1],
                            op0=ALU.mult, op1=ALU.add), reads=["xt0", "modc"], writes=["hT%d_%d" % (par, k)])
                if HT is not None:
                    S.dma("sp", HT.rearrange("(k p) t -> p k t", p=128)[:, :, t0:t0 + n], hT[par][:, :, :n],
                          reads=["hT%d_%d" % (par, k) for k in range(8)], writes=["HT%d" % bi])

            def proj_fm(par, n, g):
                i = fcnt[0] % 2; fcnt[0] += 1
                for k in range(8):
                    S.op("pe", lambda e, k=k, i=i, g=g, par=par, n=n: e.matmul(ps_f[i][:, :n], lhsT=wb[:, k, g * 64:(g + 1) * 64],
                                                                             rhs=hT[par][:, k, :n], start=(k == 0), stop=(k == 7)),
                         reads=["wb", "hT%d_%d" % (par, k)], writes=["ps_f%d" % i])
                return ps_f[i], "ps_f%d" % i

            def hg_elem(n, d, zname, qo, ko, kho, sfx):
                nch = n // 32
                q_ = g_sb["aq"]; z_ = g_sb[zname]; ztok = "g_" + zname
                dc = dcy if d == 0 else dcy2
                dct = "dcy" if d == 0 else "dcy2"
                S.op("act", lambda e: e.activation(out=tA[:, :n], in_=z_[:, :n], func=AF.Sigmoid), reads=[ztok], writes=["tA"]); yield
                S.op("dve", lambda e: e.tensor_scalar(out=tA[:, :n], in0=tA[:, :n], scalar1=oml[:, d:d + 1], scalar2=lb[:, d:d + 1],
                                                      op0=ALU.mult, op1=ALU.add), reads=["tA", "oml", "lb"], writes=["tA"]); yield
                S.op("act", lambda e: e.activation(out=tB[:, :n], in_=tA[:, :n], func=AF.Ln), reads=["tA"], writes=["tB"]); yield
                S.op("dve", lambda e: e.tensor_scalar(out=tA[:, :n], in0=tA[:, :n], scalar1=-1.0, scalar2=1.0, op0=ALU.mult, op1=ALU.add),
                     reads=["tA", "tB"], writes=["tA"]); yield
                S.op("dve", lambda e: e.tensor_tensor_scan(out=tC[:, :n], data0=rmask[:, :n], data1=tB[:, :n], initial=0.0,
                                                           op0=ALU.mult, op1=ALU.add), reads=["tB", "cst"], writes=["tC"]); yield
                cumv = tC[:, :n].rearrange("p (c j) -> p c j", j=32)
                S.op("dve", lambda e: e.tensor_copy(out=tot[:, :nch], in_=cumv[:, :, 31]), reads=["tC"], writes=["tot"]); yield
                S.op("act", lambda e: e.activation(out=dc[:, :nch], in_=tot[:, :nch], func=AF.Exp), reads=["tot"], writes=[dct]); yield
                S.op("dve", lambda e: e.tensor_tensor(out=tD[:, :n].rearrange("p (c j) -> p c j", j=32), in0=bc_mid(tot[:, :nch], 32), in1=cumv,
                                                      op=ALU.subtract), reads=["tot", "tC"], writes=["tD"]); yield
                if d == 0:
                    e1, e3, e1n, e3n = tC, tD, "tC", "tD"
                else:
                    S.op("dve", lambda e: e.tensor_tensor(out=tE[:, :n], in0=tD[:, :n], in1=tB[:, :n], op=ALU.add), reads=["tD", "tB"], writes=["tE"]); yield
                    S.op("dve", lambda e: e.tensor_tensor(out=tC[:, :n], in0=tC[:, :n], in1=tB[:, :n], op=ALU.subtract), reads=["tC", "tB", "tD"], writes=["tC"]); yield
                    e1, e3, e1n, e3n = tE, tC, "tE", "tC"
                S.op("act", lambda e: e.activation(out=tB[:, :n], in_=e1[:, :n], func=AF.Exp, scale=-1.0), reads=[e1n, "tD", "tE", "tC"], writes=["tB"]); yield
                S.op("act", lambda e: e.activation(out=e1[:, :n], in_=e1[:, :n], func=AF.Exp), reads=[e1n, "tB"], writes=[e1n]); yield
                S.op("act", lambda e: e.activation(out=e3[:, :n], in_=e3[:, :n], func=AF.Exp), reads=[e3n], writes=[e3n]); yield
                S.op("dve", lambda e: e.tensor_tensor(out=qo[:, :n], in0=q_[:, :n], in1=e1[:, :n], op=ALU.mult), reads=["g_aq", e1n], writes=["qtl" + sfx]); yield
                S.op("dve", lambda e: e.tensor_tensor(out=ko[:, :n], in0=tA[:, :n], in1=tB[:, :n], op=ALU.mult), reads=["tA", "tB"], writes=["ktl" + sfx]); yield
                S.op("pool", lambda e: e.tensor_tensor(out=kho[:, :n], in0=tA[:, :n], in1=e3[:, :n], op=ALU.mult), reads=["tA", e3n], writes=["khT" + sfx]); yield

            def hg_tiles_U(n, store, kho=None, sfx="", tiles=None):
                kho = khT if kho is None else kho
                ntl = n // 128
                for tl in (range(ntl) if tiles is None else tiles):
                    S.op("pe", lambda e, tl=tl: e.transpose(out=ps_kh[:], in_=kho[:, tl * 128:(tl + 1) * 128], identity=identb[0:64, 0:64]),
                         reads=["khT" + sfx, "identb"], writes=["ps_kh"])
                    S.op("act", lambda e, tl=tl: e.activation(out=kh[:, tl, :], in_=ps_kh[:], func=AF.Copy), reads=["ps_kh"], writes=["kh%d" % tl])
                    S.op("pe", lambda e, tl=tl: e.matmul(ps_U[:].rearrange("p c e -> p (c e)"), lhsT=kh[:, tl, :],
                                                         rhs=vblk[:, tl, :, :].rearrange("p c e -> p (c e)"), start=True, stop=True),
                         reads=["kh%d" % tl, "vblk"], writes=["ps_U"])
                    if store:
                        S.op("act", lambda e, tl=tl: e.activation(out=Ub[:, tl * 4:(tl + 1) * 4, :], in_=ps_U[:], func=AF.Copy),
                             reads=["ps_U"], writes=["Ub"])
                    else:
                        for cc_ in range(4):
                            c = tl * 4 + cc_
                            S.op("dve", lambda e, c=c, cc_=cc_: e.scalar_tensor_tensor(
                                out=Sall[:, c + 1, :], in0=Sall[:, c, :], scalar=dcy[:, c:c + 1], in1=ps_U[:, cc_, :], op0=ALU.mult, op1=ALU.add),
                                reads=["ps_U", "Sall", "dcy"], writes=["Sall"])

            def hg_inter(n, off, qo=None, sfx=""):
                qo = qtl if qo is None else qo
                nch = n // 32
                S.op("act", lambda e: e.activation(out=Sbf[:, :nch, :], in_=Sall[:, off:off + nch, :], func=AF.Copy), reads=["Sall"], writes=["Sbf"])
                for c in range(nch):
                    S.op("pe", lambda e, c=c: e.matmul(ps_oi[:, c * 32:(c + 1) * 32], lhsT=Sbf[:, c, :], rhs=qo[:, c * 32:(c + 1) * 32],
                                                       start=True, stop=True), reads=["Sbf", "qtl" + sfx], writes=["ps_oi"])

            def hg_intra(n, d, qo=None, ko=None, sfx="", tiles=None):
                qo = qtl if qo is None else qo
                ko = ktl if ko is None else ko
                ntl = n // 128
                hm = hmF if d == 0 else hmB
                for tl in (range(ntl) if tiles is None else tiles):
                    S.op("pe", lambda e, tl=tl: e.matmul(ps_sc[:], lhsT=ko[:, tl * 128:(tl + 1) * 128], rhs=qo[:, tl * 128:(tl + 1) * 128],
                                                         start=True, stop=True), reads=["ktl" + sfx, "qtl" + sfx], writes=["ps_sc"])
                    S.op("dve", lambda e: e.tensor_tensor(out=scT[:], in0=ps_sc[:], in1=hm, op=ALU.mult), reads=["ps_sc", "cst"], writes=["scT"])
                    S.op("pe", lambda e, tl=tl: e.matmul(ps_oa[:, tl * 128:(tl + 1) * 128], lhsT=vt[:, tl, :], rhs=scT[:], start=True, stop=True),
                         reads=["vt", "scT"], writes=["ps_oa"])

            load_block(0, 0)
            for bi in range(len(blocks)):
                par = bi % 2
                t0, n = blocks[bi]
                ntl = n // 128; nch = n // 32
                is_ctx = t0 < CTX
                if bi + 1 < len(blocks):
                    load_block(bi + 1, 1 - par)
                if not is_ctx:
                    S.dma("sp", rC[:, :n], ropeC_d[:, t0 - CTX:t0 - CTX + n], writes=["rC"])
                    S.dma("sp", rS[:, :n], ropeS_d[:, t0 - CTX:t0 - CTX + n], writes=["rS"])
                def evac(g, dst, dtok, eng="act"):
                    p_, ptok = proj_fm(par, n, g)
                    o_ = dst[:, :n] if dst.shape[1] == 512 else dst[:, t0:t0 + n]
                    if eng == "act":
                        S.op("act", lambda e: e.activation(out=o_, in_=p_[:, :n], func=AF.Copy), reads=[ptok], writes=[dtok])
                    else:
                        S.op("dve", lambda e: e.tensor_copy(out=o_, in_=p_[:, :n]), reads=[ptok], writes=[dtok])

                def rope_group(ga, gb, dst, dtok):
                    pa, patok = proj_fm(par, n, ga)
                    S.op("dve", lambda e, pa=pa: e.tensor_tensor(out=g_sb["r0"][:, :n], in0=pa[:, :n], in1=rC[:, :n], op=ALU.mult),
                         reads=[patok, "rC"], writes=["g_r0"])
                    pb, pbtok = proj_fm(par, n, gb)
                    S.op("dve", lambda e, pb=pb: e.tensor_tensor(out=g_sb["r1"][:, :n], in0=pb[:, :n], in1=rS[:, :n], op=ALU.mult),
                         reads=[pbtok, "rS"], writes=["g_r1"])
                    S.op("pool", lambda e, dst=dst: e.tensor_tensor(out=dst[:, t0:t0 + n], in0=g_sb["r0"][:, :n], in1=g_sb["r1"][:, :n], op=ALU.add),
                         reads=["g_r0", "g_r1"], writes=[dtok])

                evac(0, g_sb["aq"], "g_aq", "act")
                evac(1, g_sb["az"], "g_az", "act")
                evac(2, g_sb["az2"], "g_az2", "act")
                for tl in range(ntl):
                    gt = t0 // 128 + tl
                    for k in range(8):
                        S.op("pe", lambda e, k=k, tl=tl, par=par: e.matmul(ps_tm[:], lhsT=hT[par][:, k, tl * 128:(tl + 1) * 128],
                                                                           rhs=wb[:, k, 768:960], start=(k == 0), stop=(k == 7)),
                             reads=["wb", "hT%d_%d" % (par, k)], writes=["ps_tm"])
                    S.op("act", lambda e, tl=tl: e.activation(out=vt[:, tl, :], in_=ps_tm[:, 0:64], func=AF.Copy), reads=["ps_tm"], writes=["vt"])
                    S.op("act", lambda e, gt=gt: e.activation(out=nv1[:, gt, 0:64], in_=ps_tm[:, 64:128], func=AF.Copy), reads=["ps_tm", "vt"], writes=["nv1_%d" % gt])
                    S.op("act", lambda e, gt=gt: e.activation(out=sv1[:, gt, 0:64], in_=ps_tm[:, 128:192], func=AF.Copy), reads=["ps_tm", "nv1_%d" % gt], writes=["sv1_%d" % gt])
                    for c in range(4):
                        S.op("pool", lambda e, tl=tl, c=c: e.tensor_scalar(out=vblk[:, tl, c, :], in0=vt[:, tl, :], scalar1=vmask[:, c:c + 1],
                                                                          scalar2=None, op0=ALU.mult), reads=["vt", "vmask"], writes=["vblk"])
                import itertools
                chain_f = hg_elem(n, 0, "az", qtl, ktl, khT, "")
                chain_b = hg_elem(n, 1, "az2", qtlb, ktlb, khTb, "b")
                others = [lambda: evac(3, g_sb["ag"], "g_ag", "act"), lambda: evac(4, nqT, "nqT", "act"), lambda: evac(5, nkT, "nkT", "act")]
                if is_ctx:
                    others += [lambda: evac(6, sq0T, "sq0T", "act"), lambda: evac(8, sq1T, "sq1T", "dve"), lambda: evac(10, skT, "skT", "act")]
                else:
                    others += [lambda: rope_group(6, 7, sq0T, "sq0T"), lambda: rope_group(8, 9, sq1T, "sq1T"), lambda: rope_group(10, 11, skT, "skT")]
                for oth in others:
                    for _ in range(3):
                        next(chain_f, None)
                    oth()
                for _ in chain_f:
                    pass

                def work_f():
                    for tl in range(ntl):
                        hg_tiles_U(n, store=False, tiles=[tl]); yield
                    hg_inter(n, 0); yield
                    for tl in range(ntl):
                        hg_intra(n, 0, tiles=[tl]); yield
                    S.op("act", lambda e: e.activation(out=oi[:, :n], in_=ps_oa[:, :n], func=AF.Copy), reads=["ps_oa"], writes=["oi"])
                    S.op("dve", lambda e: e.tensor_tensor(out=oo[:, :n], in0=ps_oi[:, :n], in1=oi[:, :n], op=ALU.add), reads=["ps_oi", "oi"], writes=["oo"])
                    S.op("dve", lambda e: e.tensor_copy(out=Sall[:, 0, :], in_=Sall[:, nch, :]), reads=["Sall", "Sbf"], writes=["Sall"])
                    yield
                for _ in work_f():
                    next(chain_b, None); next(chain_b, None)
                for _ in chain_b:
                    pass
                hg_tiles_U(n, store=True, kho=khTb, sfx="b")
                hg_intra(n, 1, qo=qtlb, ko=ktlb, sfx="b")
                S.op("dve", lambda e: e.tensor_tensor(out=oo[:, :n], in0=ps_oa[:, :n], in1=oo[:, :n], op=ALU.add), reads=["ps_oa", "oo"], writes=["oo"])
                S.op("dve", lambda e, bi=bi: e.tensor_copy(out=dcyB[:, bi, :nch], in_=dcy2[:, :nch]), reads=["dcy2"], writes=["dcyB"])
                S.dma("sp", ofs[:, t0:t0 + n], oo[:, :n], reads=["oo"], writes=["ofs%d" % bi])
                S.dma("sp", UB[:, bi, :nch * 64], Ub[:, :nch, :].rearrange("p c e -> p (c e)"), reads=["Ub"], writes=["UB%d" % bi])
                S.dma("sp", QB[:, t0:t0 + n], qtlb[:, :n], reads=["qtlb"], writes=["QB%d" % bi])
                S.op("act", lambda e: e.activation(out=sgt[:, :n], in_=g_sb["ag"][:, :n], func=AF.Silu), reads=["g_ag"], writes=["sgt"])
                S.op("dve", lambda e: e.tensor_scalar(out=sgt[:, :n], in0=sgt[:, :n], scalar1=gnorm[:, 0:1], scalar2=None, op0=ALU.mult),
                     reads=["sgt", "gnorm"], writes=["sgt"])
                S.dma("sp", SG[:, t0:t0 + n], sgt[:, :n], reads=["sgt"], writes=["SG%d" % bi])
            S.barrier()

        def pass2_gen(bufs):
            S.op("dve", lambda e: e.memset(Sall[:, 0, :], 0.0), writes=["Sall"])
            order2 = [0] + list(range(len(blocks) - 1, 0, -1))

            def loads(k):
                bi = order2[k]
                t0, n = blocks[bi]
                nch = n // 32
                ub_, q_, of_, sg_ = bufs[k % 2]
                sx = "p%d" % (k % 2)
                S.dma("pool", ub_[:, :nch, :].rearrange("p c e -> p (c e)"), UB[:, bi, :nch * 64], writes=["Ub" + sx])
                S.dma("pool", q_[:, :n], QB[:, t0:t0 + n], writes=["qtl" + sx])
                S.dma("pool", of_[:, :n], ofs[:, t0:t0 + n], writes=["ofb" + sx])
                S.dma("pool", sg_[:, :n], SG[:, t0:t0 + n], writes=["sgt" + sx])

            loads(0)
            for k, bi in enumerate(order2):
                t0, n = blocks[bi]
                ntl = n // 128; nch = n // 32
                if k + 1 < len(order2):
                    loads(k + 1)
                ub_, q_, of_, sg_ = bufs[k % 2]
                sx = "p%d" % (k % 2)
                S.op("dve", lambda e: e.tensor_copy(out=Sall[:, nch, :], in_=Sall[:, 0, :]), reads=["Sall"], writes=["Sall"])
                for c in range(nch - 1, -1, -1):
                    S.op("dve", lambda e, c=c, bi=bi, ub_=ub_: e.scalar_tensor_tensor(
                        out=Sall[:, c, :], in0=Sall[:, c + 1, :], scalar=dcyB[:, bi, c:c + 1], in1=ub_[:, c, :], op0=ALU.mult, op1=ALU.add),
                        reads=["Ub" + sx, "Sall", "dcyB"], writes=["Sall"])
                hg_inter(n, 1, qo=q_, sfx=sx)
                S.op("dve", lambda e, of_=of_: e.tensor_tensor(out=oo[:, :n], in0=ps_oi[:, :n], in1=of_[:, :n], op=ALU.add), reads=["ps_oi", "ofb" + sx], writes=["oo"])
                S.op("act", lambda e: e.activation(out=oi[:, :n], in_=oo[:, :n], func=AF.Square), reads=["oo", "oi"], writes=["oi"])
                S.op("pe", lambda e: e.matmul(ps_oi[:, :n], lhsT=ones64[:], rhs=oi[:, :n], start=True, stop=True),
                     reads=["ones64", "oi"], writes=["ps_oi"])
                S.op("act", lambda e: e.activation(out=oi[:, :n], in_=ps_oi[:, :n], func=AF.Sqrt, bias=epsr[:, 0:1], scale=1.0 / 64.0),
                     reads=["ps_oi", "epsr"], writes=["oi"])
                S.op("dve", lambda e: e.reciprocal(out=oi[:, :n], in_=oi[:, :n]), reads=["oi"], writes=["oi"])
                S.op("dve", lambda e: e.tensor_tensor(out=oo[:, :n], in0=oo[:, :n], in1=oi[:, :n], op=ALU.mult), reads=["oo", "oi"], writes=["oo"])
                S.op("dve", lambda e, sg_=sg_: e.tensor_tensor(out=oo[:, :n], in0=oo[:, :n], in1=sg_[:, :n], op=ALU.mult), reads=["oo", "sgt" + sx], writes=["oo"])
                S.dma("pool", yT[0:64, t0:t0 + n], oo[:, :n], reads=["oo"], writes=["yTa%d" % bi])
                yield bi
        sta = contextlib.ExitStack()
        with sta:
            nab = sb("nab", [128, 21, 128], BF16, st=sta)
            swm = sb("swm", [128, 2, 128], BF16, st=sta)
            snk = sb("snk", [1, 2], st=sta); snkB = sb("snkB", [128, 2], st=sta)
            ones1 = sb("ones1", [1, 128], st=sta)
            pT = [sb("pT%d" % i, [128, 7, 128], BF16, st=sta) for i in range(2)]
            ot = [sb("ot%d" % i, [128, 64], BF16, st=sta) for i in range(2)]
            rinv = sb("rinv", [128, 1], st=sta)
            yblk = [sb("yblk%d" % i, [64, 512], st=sta) for i in range(2)]
            ps_s = [[pst("ps_s%d_%d" % (i, j), [128, 4, 128], st=sta) for j in range(2)] for i in range(2)]
            ps_o = [pst("ps_o%d" % i, [128, 65], st=sta) for i in range(2)]
            ps_y = pst("ps_y", [64, 128], BF16, st=sta)
            S.dma("pool", nab[:], nab_d.rearrange("c k q -> k c q"), writes=["nab"])
            S.dma("pool", swm[:], swm_d.rearrange("c k q -> k c q"), writes=["swm"])
            S.dma("sp", snk[:], sink_d[:], writes=["snk"])
            S.op("dve", lambda e: e.memset(ones1[:], 1.0), writes=["ones1"])
            S.op("pe", lambda e: e.matmul(ps_o[0][:, 0:2], lhsT=ones1[:], rhs=snk[:], start=True, stop=True), reads=["ones1", "snk"], writes=["ps_o0"])
            S.op("act", lambda e: e.activation(out=snkB[:], in_=ps_o[0][:, 0:2], func=AF.Exp), reads=["ps_o0"], writes=["snkB"])
            acnt = [0]
            p2bufs = [(Ub, qtl, ofb, sgt),
                      (sb("Ubx", [64, 16, 64], st=sta), sb("qtlx", [64, 512], BF16, st=sta), sb("ofbx", [64, 512], st=sta), sb("sgtx", [64, 512], st=sta))]
            p2 = pass2_gen(p2bufs)
            p2cnt = [0]

            tiles = []

            def add_tile(qT, qtok_tile, keys, v1, sink_col, yb_, ybtok, ycol, after=None):
                tiles.append(dict(qT=qT, qt=qtok_tile, keys=keys, v1=v1, sink=sink_col, yb=yb_, ybtok=ybtok, ycol=ycol, after=after))

            def st_scores(t, i):
                keys = t["keys"]; nk = len(keys); qT = t["qT"]; qt = t["qt"]
                for j, (kT_, kt, bias) in enumerate(keys):
                    pp = ps_s[i][j // 4]; ptok = "ps_s%d_%d" % (i, j // 4)
                    S.op("pe", lambda e, pp=pp, j=j, kT_=kT_, kt=kt, bias=bias: e.matmul(
                        pp[:, j % 4, :], lhsT=kT_[:, kt * 128:(kt + 1) * 128], rhs=qT[:, qt * 128:(qt + 1) * 128],
                        start=True, stop=(bias is None)), writes=[ptok])
                    if bias is not None:
                        S.op("pe", lambda e, pp=pp, j=j, bias=bias: e.matmul(pp[:, j % 4, :], lhsT=ident8b[:], rhs=bias, start=False, stop=True),
                             reads=["nab", "swm", "ident8b"], writes=[ptok])
                n0 = min(4, nk)
                S.op("act", lambda e: e.activation(out=pT[i][:, 0:n0, :], in_=ps_s[i][0][:, 0:n0, :], func=AF.Exp, scale=0.125),
                     reads=["ps_s%d_0" % i], writes=["pT%d" % i])
                if nk > 4:
                    S.op("act", lambda e: e.activation(out=pT[i][:, 4:nk, :], in_=ps_s[i][1][:, 0:nk - 4, :], func=AF.Exp, scale=0.125),
                         reads=["ps_s%d_1" % i], writes=["pT%d" % i])

            def st_pv(t, i):
                keys = t["keys"]; nk = len(keys); v1 = t["v1"]; sink_col = t["sink"]
                for j, (kT_, kt, bias) in enumerate(keys):
                    S.op("pe", lambda e, j=j, kt=kt: e.matmul(ps_o[i][:], lhsT=pT[i][:, j, :], rhs=v1[:, kt, :], start=(j == 0), stop=(j == nk - 1)),
                         reads=["pT%d" % i], writes=["ps_o%d" % i])
                if sink_col is None:
                    S.op("dve", lambda e: e.reciprocal(out=rinv[:], in_=ps_o[i][:, 64:65]), reads=["ps_o%d" % i], writes=["rinv"])
                else:
                    S.op("dve", lambda e: e.tensor_tensor(out=rinv[:], in0=ps_o[i][:, 64:65], in1=snkB[:, sink_col:sink_col + 1], op=ALU.add),
                         reads=["ps_o%d" % i, "snkB"], writes=["rinv"])
                    S.op("dve", lambda e: e.reciprocal(out=rinv[:], in_=rinv[:]), reads=["rinv"], writes=["rinv"])
                S.op("dve", lambda e: e.tensor_scalar(out=ot[i][:], in0=ps_o[i][:, 0:64], scalar1=rinv[:, 0:1], scalar2=None, op0=ALU.mult),
                     reads=["ps_o%d" % i, "rinv"], writes=["ot%d" % i])

            def st_out(t, i):
                yb_ = t["yb"]; ycol = t["ycol"]
                S.op("pe", lambda e: e.transpose(out=ps_y[:], in_=ot[i][:], identity=identb[:]), reads=["ot%d" % i, "identb"], writes=["ps_y"])
                S.op("act", lambda e: e.activation(out=yb_[:, ycol:ycol + 128], in_=ps_y[:], func=AF.Copy), reads=["ps_y"], writes=[t["ybtok"]])
                if t["after"] is not None:
                    t["after"]()

            CT = CTX // 128
            ctx_keys_n = [(nkT, 0, None), (nkT, 1, None)]
            ctx_keys_s = [(skT, 0, None), (skT, 1, None)]
            ybc = 0
            heads_ = [("n", nqT, None, 64), ("s0", sq0T, 0, 128), ("s1", sq1T, 1, 192)]

            def mk_after(dst, src, ybtok, wtok):
                def f():
                    S.dma("sp", dst, src, reads=[ybtok], writes=[wtok])
                    p2cnt[0] += 1
                    if p2cnt[0] % 1 == 0:
                        next(p2, None)
                return f

            for (hk, qT, sink_col, yrow0) in heads_:
                if with_ctx_out:
                    yb_ = yblk[ybc % 2]; ybtok = "yblk%d" % (ybc % 2); ybc += 1
                    for qt in range(CT):
                        aft = mk_after(yT[yrow0:yrow0 + 64, 0:CTX], yb_[:, 0:CTX], ybtok, "yTc_" + hk) if qt == CT - 1 else None
                        if hk == "n":
                            add_tile(qT, qt, ctx_keys_n, nv1, None, yb_, ybtok, qt * 128, aft)
                        else:
                            add_tile(qT, qt, ctx_keys_s, sv1, sink_col, yb_, ybtok, qt * 128, aft)
                nql = n_lat // 128
                for qb in range(nql // 4):
                    yb_ = yblk[ybc % 2]; ybtok = "yblk%d" % (ybc % 2); ybc += 1
                    for qq in range(4):
                        pq = qb * 4 + qq
                        aft = mk_after(yT[yrow0:yrow0 + 64, CTX + qb * 512:CTX + (qb + 1) * 512], yb_[:], ybtok, "yT_%s_%d" % (hk, qb)) if qq == 3 else None
                        if hk == "n":
                            keys = [(nkT, CT + pk, nab[:, mi, :]) for (pk, mi) in cfgs[pq]] + ctx_keys_n
                            add_tile(qT, CT + pq, keys, nv1, None, yb_, ybtok, qq * 128, aft)
                        else:
                            keys = []
                            if pq > 0:
                                keys.append((skT, CT + pq - 1, swm[:, 0, :]))
                            keys.append((skT, CT + pq, None))
                            if pq < nql - 1:
                                keys.append((skT, CT + pq + 1, swm[:, 1, :]))
                            keys += ctx_keys_s
                            add_tile(qT, CT + pq, keys, sv1, sink_col, yb_, ybtok, qq * 128, aft)
            NTL = len(tiles)
            for n_ in range(NTL + 2):
                if n_ < NTL:
                    st_scores(tiles[n_], n_ % 2)
                if 0 <= n_ - 1 < NTL:
                    st_pv(tiles[n_ - 1], (n_ - 1) % 2)
                if 0 <= n_ - 2 < NTL:
                    st_out(tiles[n_ - 2], (n_ - 2) % 2)
            for _ in p2:
                pass
            S.barrier()

NEG = -30000.0
def rope_tables(n_lat=8192):
    t = np.arange(n_lat); row = (t // 64).astype(np.float32); col = (t % 64).astype(np.float32)
    nf = 16
    inv = (np.float32(10000.0) ** (-np.arange(nf, dtype=np.float32) / np.float32(nf))).astype(np.float32)
    C = np.zeros((64, n_lat), np.float32); S = np.zeros((64, n_lat), np.float32)
    for half, pos in ((0, row), (1, col)):
        ang = (pos[:, None] * inv[None, :]).astype(np.float32)
        c = np.cos(ang).astype(np.float32).T; s_ = np.sin(ang).astype(np.float32).T
        b = half * 32
        C[b:b+16] = c; C[b+16:b+32] = c
        S[b:b+16] = -s_; S[b+16:b+32] = s_
    return C, S
def rope_perm():
    p = np.arange(64)
    for b in (0, 32):
        p[b:b+16] = np.arange(b+16, b+32); p[b+16:b+32] = np.arange(b, b+16)
    return p
def na_index_tables(nrows=128):
    cfgs, mats = na_configs(nrows)
    idx = np.zeros((21, 128, 128), np.int64)
    k = np.arange(128); q = np.arange(128)
    for mi, (pq, pk) in enumerate(mats):
        krow = 2 * pk + k // 64; kcol = k % 64
        qrow = 2 * pq + q // 64; qcol = q % 64
        rs = np.clip(qrow - 4, 0, nrows - 8); cs = np.clip(qcol - 8, 0, 48)
        valid = (krow[:, None] >= rs[None]) & (krow[:, None] < rs[None] + 8) & (kcol[:, None] >= cs[None]) & (kcol[:, None] < cs[None] + 16)
        ridx = krow[:, None] - qrow[None] + 7
        coff = np.clip(kcol[:, None] - qcol[None] + 15, 0, 30)
        ii = np.clip(ridx, 0, 14) * 31 + coff
        idx[mi] = np.where(valid, ii, 15 * 31)
    return idx
def const_tables():
    ident = np.eye(128, dtype=np.float32)
    k = np.arange(128)
    same = (k[:, None] // 32) == (k[None] // 32)
    hmF = (same & (k[:, None] <= k[None])).astype(np.float32)
    hmB = (same & (k[:, None] >= k[None])).astype(np.float32)
    rm = np.ones((128, 512), np.float32); rm[:, ::32] = 0.0
    cst = np.concatenate([ident, 8 * ident, hmF, hmB, rm], axis=1)
    vmask = (k[:, None] // 32 == np.arange(4)[None]).astype(np.float32)
    swm = np.zeros((2, 128, 128), np.float32)
    swm[0] = np.where(k[:, None] >= k[None], 0.0, NEG)
    swm[1] = np.where(k[:, None] <= k[None], 0.0, NEG)
    return cst, vmask, swm
_NAIDX = None
def core_inputs_a(inp, l, b, j, xT_b, n_lat=8192):
    global _NAIDX
    if _NAIDX is None: _NAIDX = na_index_tables(n_lat // 64)
    w = inp['w_in'][l]
    perm = rope_perm()
    def cols(base, width=64, idx=j): return w[:, base + width * idx: base + width * (idx + 1)]
    sq0 = w[:, 2048 + 128 * j: 2048 + 128 * j + 64]; sq1 = w[:, 2048 + 128 * j + 64: 2048 + 128 * j + 128]
    n = j // 2
    sk = w[:, 2560 + 64 * n: 2560 + 64 * n + 64]; sv = w[:, 2688 + 64 * n: 2688 + 64 * n + 64]
    wcore = np.concatenate([cols(0), cols(256), cols(512), cols(1024), cols(1280), cols(1536),
                            sq0, sq0[:, perm], sq1, sq1[:, perm], sk, sk[:, perm],
                            cols(768), cols(1792), sv], axis=1)
    c = inp['c'][b]; cctx = inp['c_ctx']
    ccol = np.concatenate([c.reshape(8, 128).T, cctx.reshape(8, 128).T], axis=1)
    lbl = np.stack([inp['lb_logits'][0, 0, 64*j:64*j+64], inp['lb_logits'][1, 0, 64*j:64*j+64],
                    inp['lb_logits'][0, 1, 64*j:64*j+64], inp['lb_logits'][1, 1, 64*j:64*j+64]], axis=1)
    rpb_ext = np.concatenate([inp['na_rpb'][l, j].reshape(-1), np.array([NEG], np.float32)])
    nab = rpb_ext[_NAIDX]
    C, S = rope_tables(n_lat)
    cst, vmask, swm = const_tables()
    f = lambda a: np.ascontiguousarray(a, dtype=np.float32)
    return {"xT": f(xT_b), "wcore": f(wcore), "ccol": f(ccol), "wada": f(inp['w_ada'][l][:, :2048]),
            "badac": f(inp['b_ada'][l][:2048].reshape(16, 128).T), "lbl": f(lbl), "gnorm": f(inp['hgrn_norm'][l][:, None]),
            "nab": f(nab), "swm": f(swm), "sink": f(inp['swa_sink'][l][None, 2*j:2*j+2]), "ropeC": f(C), "ropeS": f(S),
            "cst": f(cst), "vmask": f(vmask)}


ALPHA = float((2.0 * 2) ** 0.25)
LN_EPS = 1e-5
NEGBIG = 1e30


def bc_mid(ap2d, n):
    p, k = ap2d.shape
    return ap2d.unsqueeze(2).broadcast_to([p, k, n])


def emit_phase_b(nc, S, io, has_ctx, uid, n_lat_tiles=16, n_exp=16):
    NTI = n_lat_tiles + (1 if has_ctx else 0)
    NT = NTI * 128
    D = 1024
    stop = 99
    yT_lat = io["yT_lat"]; yT_ctx = io.get("yT_ctx"); x = io["x"]; ccol = io["ccol"]; wada = io["wada"]; bada = io["bada"]; lnp = io["lnp"]
    wout = io["wout"]; wr = io["wr"]; rb = io["rb"]; wg = io["wg"]; wu = io["wu"]; wd = io["wd"]; ident_d = io["ident"]
    xo = io["xo"]; x1s = io["x1s"]; xT_lat_o = io.get("xT_lat_o"); xT_ctx_o = io.get("xT_ctx_o")
    es = contextlib.ExitStack()
    with es:
        sb = lambda name, shape, dt=F32, st=es: st.enter_context(nc.sbuf_tensor(uid + name, shape, dt))
        pst = lambda name, shape, dt=F32, st=es: st.enter_context(nc.psum_tensor(uid + name, shape, dt))

        h2T = sb("h2T", [128, 8, NT], BF16)
        gates = sb("gates", [128, NTI, 16])
        G2 = sb("G2", [128, D]); G2c = sb("G2c", [128, D]); LN2G = sb("LN2G", [128, D]); LN2B = sb("LN2B", [128, D])
        ident = sb("ident_sb", [128, 128])
        ones1 = sb("ones1", [1, 128])
        epsT = sb("epsT", [128, 1])
        S.dma("sp", ident[:], ident_d[:], writes=["ident"])
        S.op("dve", lambda e: e.memset(ones1[:], 1.0), writes=["ones1"])
        S.op("dve", lambda e: e.memset(epsT[:], LN_EPS), writes=["epsT"])

        st1 = contextlib.ExitStack()
        with st1:
            GA = sb("GA", [128, D], st=st1); BA = sb("BA", [128, D], st=st1)
            A2 = sb("A2", [128, D], st=st1); B2 = sb("B2", [128, D], st=st1)
            A2c = sb("A2c", [128, D], st=st1); B2c = sb("B2c", [128, D], st=st1)
            G1 = sb("G1", [128, D], st=st1); G1c = sb("G1c", [128, D], st=st1)
            bc_d = io.get("bc_d")
            bc_tiles = [G1, G1c, A2, A2c, B2, B2c, GA, BA, G2, G2c, LN2G, LN2B]
            if bc_d is not None and not io.get("bc_first", True):
                for ti_, tl_ in enumerate(bc_tiles):
                    S.dma("sp", tl_[:], bc_d[ti_], writes=["bct%d" % ti_])
                S.barrier()
            else:
                st0 = contextlib.ExitStack()
                with st0:
                    cc = sb("cc", [128, 16], st=st0)
                    scc = sb("scc", [128, 16], st=st0)
                    cb = sb("cb", [128, 16, 128], st=st0)
                    wa = [sb("wa%d" % i, [128, 8, 512], st=st0) for i in range(2)]
                    bd = [sb("bd%d" % i, [1, 512], st=st0) for i in range(2)]
                    SC2 = sb("SC2", [128, D], st=st0); SC2c = sb("SC2c", [128, D], st=st0)
                    SH2 = sb("SH2", [128, D], st=st0); SH2c = sb("SH2c", [128, D], st=st0)
                    LN1G = sb("LN1G", [128, D], st=st0); LN1B = sb("LN1B", [128, D], st=st0)
                    psm = [pst("psm%d" % i, [128, 512], st=st0) for i in range(4)]
                    S.dma("sp", cc[:], ccol[:], writes=["cc"])
                    S.op("act", lambda e: e.activation(out=scc[:], in_=cc[:], func=AF.Silu), reads=["cc"], writes=["scc"])
                    S.op("dve", lambda e: e.tensor_copy(out=cb[:], in_=bc_mid(scc[:], 128)), reads=["scc"], writes=["cb"])
                    wada_v = wada.rearrange("(k p) n -> p k n", p=128)
                    dests = [(G1, G1c), (SH2, SH2c), (SC2, SC2c), (G2, G2c)]
                    pi = 0
                    hi = 0
                    for ch in range(4):
                        for half in range(2):
                            cs = ch * 1024 + half * 512
                            wb = wa[hi % 2]; wtok = "wa%d" % (hi % 2); bdt = bd[hi % 2]; btok = "bd%d" % (hi % 2); hi += 1
                            S.dma("sp", wb[:], wada_v[:, :, cs:cs + 512], writes=[wtok])
                            S.dma("sp", bdt[:], bada[:, cs:cs + 512], writes=[btok])
                            for which in range(2):
                                p_ = psm[pi % 4]; ptok = "psm%d" % (pi % 4); pi += 1
                                for k in range(8):
                                    S.op("pe", lambda e, k=k, p_=p_, which=which, wb=wb: e.matmul(
                                        p_[:], lhsT=cb[:, which * 8 + k, :], rhs=wb[:, k, :],
                                        start=(k == 0), stop=False),
                                        reads=["cb", wtok], writes=[ptok])
                                S.op("pe", lambda e, p_=p_, bdt=bdt: e.matmul(p_[:], lhsT=ones1[:], rhs=bdt[:],
                                                                        start=False, stop=True),
                                     reads=["ones1", btok], writes=[ptok])
                                dst = dests[ch][which]
                                S.op("act", lambda e, dst=dst, p_=p_, half=half: e.activation(
                                    out=dst[:, half * 512:(half + 1) * 512], in_=p_[:], func=AF.Copy),
                                    reads=[ptok], writes=["mod%d_%d" % (ch, which)])
                    if stop <= -1:
                        pass
                    lnd = [LN1G, LN1B, LN2G, LN2B]
                    for j in range(4):
                        for half in range(2):
                            p_ = psm[pi % 4]; ptok = "psm%d" % (pi % 4); pi += 1
                            cs = j * 1024 + half * 512
                            bdt = bd[hi % 2]; btok = "bd%d" % (hi % 2); hi += 1
                            S.dma("sp", bdt[:], lnp[:, cs:cs + 512], writes=[btok])
                            S.op("pe", lambda e, p_=p_, bdt=bdt: e.matmul(p_[:], lhsT=ones1[:], rhs=bdt[:],
                                                                    start=True, stop=True),
                                 reads=["ones1", btok], writes=[ptok])
                            S.op("act", lambda e, j=j, p_=p_, half=half: e.activation(
                                out=lnd[j][:, half * 512:(half + 1) * 512], in_=p_[:], func=AF.Copy),
                                reads=[ptok], writes=["lnd%d" % j])
                    if stop <= -0.5:
                        pass
                    S.barrier()
                    S.op("dve", lambda e: e.tensor_scalar(out=GA[:], in0=LN1G[:], scalar1=ALPHA, scalar2=None, op0=ALU.mult))
                    S.op("dve", lambda e: e.tensor_scalar(out=BA[:], in0=LN1B[:], scalar1=ALPHA, scalar2=None, op0=ALU.mult))
                    for ci, (sc, sh, a2, b2) in enumerate(((SC2, SH2, A2, B2), (SC2c, SH2c, A2c, B2c))):
                        S.op("dve", lambda e, sc=sc: e.tensor_scalar(out=sc[:], in0=sc[:], scalar1=1.0, scalar2=None, op0=ALU.add),
                             writes=["sc1p%d" % ci])
                        S.op("dve", lambda e, sc=sc, a2=a2: e.tensor_tensor(out=a2[:], in0=LN1G[:], in1=sc[:], op=ALU.mult),
                             reads=["sc1p%d" % ci])
                        S.op("dve", lambda e, sc=sc, b2=b2: e.tensor_tensor(out=b2[:], in0=LN1B[:], in1=sc[:], op=ALU.mult),
                             reads=["sc1p%d" % ci], writes=["b2t%d" % ci])
                        S.op("dve", lambda e, sh=sh, b2=b2: e.tensor_tensor(out=b2[:], in0=b2[:], in1=sh[:], op=ALU.add),
                             reads=["b2t%d" % ci], writes=["b2t%d" % ci])
                    S.barrier()
                if stop <= 0:
                    pass
                if bc_d is not None:
                    for ti_, tl_ in enumerate(bc_tiles):
                        S.dma("sp", bc_d[ti_], tl_[:], writes=["bcd%d" % ti_])
                    S.barrier()
            wo = sb("wo", [128, 8, D], BF16, st=st1)
            wrt = sb("wrt", [128, 8, 16], st=st1)
            rbB = sb("rbB", [128, 16], st=st1)
            rb1 = sb("rb1", [1, 16], st=st1)
            xt = [sb("xt%d" % i, [128, D], st=st1) for i in range(2)]
            yb = [sb("yb%d" % i, [128, 8, 512], BF16, st=st1) for i in range(2)]
            uu = [sb("uu%d" % i, [128, D], st=st1) for i in range(2)]
            xn = [sb("xn%d" % i, [128, D], st=st1) for i in range(2)]
            h2 = [sb("h2_%d" % i, [128, D], st=st1) for i in range(2)]
            x1a = [sb("x1a%d" % i, [128, D], st=st1) for i in range(2)]
            h2f = sb("h2f", [128, 8, 128], st=st1)
            stt = sb("stt", [128, 2, 6], st=st1)
            mv = sb("mv", [128, 2], st=st1)
            rstd = sb("rstd", [128, 1], st=st1)
            nmr = sb("nmr", [128, 1], st=st1)
            rt = [sb("rt%d" % i, [128, 16], st=st1) for i in range(6)]
            rs4 = [sb("rs4_%d" % i, [128, 4], st=st1) for i in range(4)]
            r1 = [sb("r1_%d" % i, [128, 1], st=st1) for i in range(2)]
            ps_y = [pst("ps_y%d" % i, [128, D], st=st1) for i in range(2)]
            ps_t = pst("ps_t", [128, 8, 128], st=st1)
            ps_r = pst("ps_r", [128, 16], st=st1)

            S.dma("pool", wo[:], wout.rearrange("(k p) n -> p k n", p=128), writes=["wo"])
            S.dma("sp", wrt[:], wr.rearrange("(k p) n -> p k n", p=128), writes=["wrt"])
            S.dma("sp", rb1[:], rb[:], writes=["rb1"])
            S.op("pe", lambda e: e.matmul(ps_r[:], lhsT=ones1[:], rhs=rb1[:], start=True, stop=True),
                 reads=["ones1", "rb1"], writes=["ps_r"])
            S.op("act", lambda e: e.activation(out=rbB[:], in_=ps_r[:], func=AF.Copy), reads=["ps_r"], writes=["rbB"])
            yT_v = yT_lat.rearrange("(k p) t -> p k t", p=128)
            def P1(i):
                    is_ctx = has_ctx and i == NTI - 1
                    b2_ = i % 2
                    blk = i // 4
                    if i % 4 == 0:
                        ntb = min(4, NTI - i)
                        if is_ctx:
                            S.dma("pool", yb[blk % 2][:, :, :64], yT_ctx.rearrange("(k p) t -> p k t", p=128),
                                  writes=["yb%d" % (blk % 2)])
                        else:
                            S.dma("pool", yb[blk % 2][:, :, :ntb * 128], yT_v[:, :, i * 128:(i + ntb) * 128],
                                  writes=["yb%d" % (blk % 2)])
                    S.dma("sp", xt[b2_][:], x[i * 128:(i + 1) * 128, :], writes=["xt%d" % b2_])
                    ybt = yb[blk % 2]
                    off = (i % 4) * 128
                    for half in range(2):
                        for k in range(8):
                            S.op("pe", lambda e, k=k, half=half, ybt=ybt, off=off, b2_=b2_: e.matmul(
                                ps_y[b2_][:, half * 512:(half + 1) * 512], lhsT=ybt[:, k, off:off + 128],
                                rhs=wo[:, k, half * 512:(half + 1) * 512], start=(k == 0), stop=(k == 7)),
                                reads=["yb%d" % (blk % 2), "wo"], writes=["ps_y%d" % b2_])
            def P2(i):
                    is_ctx = has_ctx and i == NTI - 1
                    b2_ = i % 2
                    blk = i // 4
                    if stop <= 0.1:
                        pass
                    g1t = G1c if is_ctx else G1
                    a2t = A2c if is_ctx else A2
                    b2t = B2c if is_ctx else B2
                    u_ = uu[b2_]; utok = "uu%d" % b2_
                    S.op("dve", lambda e, u_=u_, b2_=b2_, g1t=g1t: e.tensor_tensor(out=u_[:], in0=ps_y[b2_][:], in1=g1t[:], op=ALU.mult),
                         reads=["ps_y%d" % b2_], writes=[utok])
                    S.op("dve", lambda e, u_=u_, b2_=b2_: e.scalar_tensor_tensor(out=u_[:], in0=xt[b2_][:], scalar=ALPHA, in1=u_[:],
                                                                       op0=ALU.mult, op1=ALU.add),
                         reads=["xt%d" % b2_, utok], writes=[utok])
                    if stop <= 0.2:
                        pass
                    for hh in range(2):
                        S.op("dve", lambda e, hh=hh, u_=u_: e.bn_stats(out=stt[:, hh, :], in_=u_[:, hh * 512:(hh + 1) * 512]),
                             reads=[utok], writes=["stt%d" % hh])
                    S.op("dve", lambda e: e.bn_aggr(out=mv[:], in_=stt[:].rearrange("p a b -> p (a b)")),
                         reads=["stt0", "stt1"], writes=["mv"])
                    S.op("act", lambda e: e.activation(out=rstd[:], in_=mv[:, 1:2], func=AF.Sqrt, bias=epsT[:, 0:1], scale=1.0),
                         reads=["mv", "epsT"], writes=["rstd"])
                    S.op("dve", lambda e: e.reciprocal(out=rstd[:], in_=rstd[:]), reads=["rstd"], writes=["rstd"])
                    S.op("dve", lambda e: e.scalar_tensor_tensor(out=nmr[:], in0=mv[:, 0:1], scalar=-1.0, in1=rstd[:],
                                                                 op0=ALU.mult, op1=ALU.mult),
                         reads=["mv", "rstd"], writes=["nmr"])
                    if stop <= 0.3:
                        pass
                    xn_ = xn[b2_]; xtok = "xn%d" % b2_
                    S.op("act", lambda e, xn_=xn_, u_=u_: e.activation(out=xn_[:], in_=u_[:], func=AF.Identity,
                                                                       bias=nmr[:, 0:1], scale=rstd[:, 0:1]),
                         reads=[utok, "nmr", "rstd"], writes=[xtok])
                    if stop <= 0.4:
                        pass
                    xa_ = x1a[b2_]; xatok = "x1a%d" % b2_
                    S.op("pool", lambda e, xa_=xa_, xn_=xn_: e.tensor_tensor(out=xa_[:], in0=xn_[:], in1=GA[:], op=ALU.mult),
                         reads=[xtok], writes=[xatok])
                    S.op("pool", lambda e, xa_=xa_: e.tensor_tensor(out=xa_[:], in0=xa_[:], in1=BA[:], op=ALU.add),
                         reads=[xatok], writes=[xatok])
                    S.dma("sp", x1s[i * 128:(i + 1) * 128, :], xa_[:], reads=[xatok], writes=["x1s%d" % i])
                    h_ = h2[b2_]; htok = "h2_%d" % b2_
                    S.op("dve", lambda e, h_=h_, xn_=xn_, a2t=a2t: e.tensor_tensor(out=h_[:], in0=xn_[:], in1=a2t[:], op=ALU.mult),
                         reads=[xtok], writes=[htok])
                    S.op("dve", lambda e, h_=h_, b2t=b2t: e.tensor_tensor(out=h_[:], in0=h_[:], in1=b2t[:], op=ALU.add),
                         reads=[htok], writes=[htok])
                    if stop <= 0.5:
                        pass
            def P3(i):
                    is_ctx = has_ctx and i == NTI - 1
                    b2_ = i % 2
                    blk = i // 4
                    h_ = h2[b2_]; htok = "h2_%d" % b2_
                    for k in range(8):
                        S.op("pe", lambda e, k=k, h_=h_: e.transpose(out=ps_t[:, k, :], in_=h_[:, k * 128:(k + 1) * 128], identity=ident[:]),
                             reads=[htok, "ident"], writes=["ps_t"])
                    S.op("dve", lambda e: e.tensor_copy(out=h2f[:], in_=ps_t[:]), reads=["ps_t"], writes=["h2f"])
                    S.op("act", lambda e, i=i: e.activation(out=h2T[:, :, i * 128:(i + 1) * 128], in_=h2f[:], func=AF.Copy),
                         reads=["h2f"], writes=["h2T_%d" % i])
                    if stop <= 0.6:
                        pass
                    for k in range(8):
                        S.op("pe", lambda e, k=k: e.matmul(ps_r[:], lhsT=h2f[:, k, :], rhs=wrt[:, k, :], start=(k == 0), stop=(k == 7)),
                             reads=["h2f", "wrt"], writes=["ps_r"])
                    if stop <= 0.7:
                        pass
                    s_, sel_, msk, t16, w_, selm = rt
                    m1, m2, grp, ing = rs4
                    gm, wsum = r1
                    V3 = lambda a: a[:].rearrange("p (g i) -> p g i", g=4)
                    S.op("act", lambda e: e.activation(out=s_[:], in_=ps_r[:], func=AF.Sigmoid), reads=["ps_r"], writes=["r_s"])
                    S.op("dve", lambda e: e.tensor_tensor(out=sel_[:], in0=s_[:], in1=rbB[:], op=ALU.add), reads=["r_s", "rbB"], writes=["r_sel"])
                    S.op("dve", lambda e: e.tensor_reduce(out=m1[:], in_=V3(sel_), axis=AX.X, op=ALU.max), reads=["r_sel"], writes=["r_m1"])
                    S.op("dve", lambda e: e.tensor_tensor(out=V3(msk), in0=V3(sel_), in1=bc_mid(m1[:], 4), op=ALU.is_ge),
                         reads=["r_sel", "r_m1"], writes=["r_msk"])
                    S.op("dve", lambda e: e.scalar_tensor_tensor(out=t16[:], in0=msk[:], scalar=-NEGBIG, in1=sel_[:], op0=ALU.mult, op1=ALU.add),
                         reads=["r_msk", "r_sel"], writes=["r_t16"])
                    S.op("dve", lambda e: e.tensor_reduce(out=m2[:], in_=V3(t16), axis=AX.X, op=ALU.max), reads=["r_t16"], writes=["r_m2"])
                    S.op("dve", lambda e: e.tensor_tensor(out=grp[:], in0=m1[:], in1=m2[:], op=ALU.add), reads=["r_m1", "r_m2"], writes=["r_grp"])
                    S.op("dve", lambda e: e.tensor_reduce(out=gm[:], in_=grp[:], axis=AX.X, op=ALU.max), reads=["r_grp"], writes=["r_gm"])
                    S.op("dve", lambda e: e.tensor_scalar(out=ing[:], in0=grp[:], scalar1=gm[:, 0:1], scalar2=None, op0=ALU.is_ge),
                         reads=["r_grp", "r_gm"], writes=["r_ing"])
                    S.op("dve", lambda e: e.tensor_tensor(out=V3(selm), in0=V3(sel_), in1=bc_mid(m2[:], 4), op=ALU.is_ge),
                         reads=["r_sel", "r_m2"], writes=["r_selm"])
                    S.op("dve", lambda e: e.tensor_tensor(out=V3(selm), in0=V3(selm), in1=bc_mid(ing[:], 4), op=ALU.mult),
                         reads=["r_selm", "r_ing"], writes=["r_selm"])
                    S.op("dve", lambda e: e.tensor_tensor(out=w_[:], in0=s_[:], in1=selm[:], op=ALU.mult), reads=["r_s", "r_selm"], writes=["r_w"])
                    S.op("dve", lambda e: e.tensor_reduce(out=wsum[:], in_=w_[:], axis=AX.X, op=ALU.add), reads=["r_w"], writes=["r_ws"])
                    S.op("dve", lambda e: e.reciprocal(out=wsum[:], in_=wsum[:]), reads=["r_ws"], writes=["r_ws"])
                    S.op("dve", lambda e, i=i: e.tensor_scalar(out=gates[:, i, :], in0=w_[:], scalar1=wsum[:, 0:1], scalar2=None, op0=ALU.mult),
                         reads=["r_w", "r_ws"], writes=["gates%d" % i])
            for it_ in range(NTI + 2):
                if it_ < NTI:
                    P1(it_)
                if 0 <= it_ - 1 < NTI:
                    P2(it_ - 1)
                if 0 <= it_ - 2 < NTI:
                    P3(it_ - 2)
            S.barrier()
        if stop <= 1:
            pass
        st2 = contextlib.ExitStack()
        with st2:
            acc = sb("acc", [128, NTI, D], st=st2)
            st2w = contextlib.ExitStack()
            wgb = [sb("wgb%d" % i, [128, 8, 512], BF16, st=st2w) for i in range(2)]
            wub = [sb("wub%d" % i, [128, 8, 512], BF16, st=st2w) for i in range(2)]
            wdb = [sb("wdb%d" % i, [128, 4, D], BF16, st=st2w) for i in range(2)]
            sg = [sb("sg%d" % i, [128, 512], st=st2w) for i in range(2)]
            heT = [sb("heT%d" % i, [128, 4, 512], BF16, st=st2w) for i in range(2)]
            ps_g = [pst("ps_g%d" % i, [128, 512], st=st2w) for i in range(2)]
            ps_u = [pst("ps_u%d" % i, [128, 512], st=st2w) for i in range(2)]
            ps_d = [pst("ps_d%d" % i, [128, D], st=st2w) for i in range(2)]
            blocks = []
            i = 0
            while i < NTI:
                n = min(4, NTI - i)
                blocks.append((i, n))
                i += n

            def load_w(e_):
                p = e_ % 2
                S.dma("pool", wgb[p][:], wg[e_].rearrange("(k p) n -> p k n", p=128), writes=["wgb%d" % p])
                S.dma("pool", wub[p][:], wu[e_].rearrange("(k p) n -> p k n", p=128), writes=["wub%d" % p])
                S.dma("pool", wdb[p][:], wd[e_].rearrange("(k p) n -> p k n", p=128), writes=["wdb%d" % p])

            load_w(0)
            if n_exp > 1:
                load_w(1)
            fcn = [0]; dn = [0]
            steps = [(e_, bi_) for e_ in range(n_exp) for bi_ in range(len(blocks))]

            def stage_G(k):
                e_, bi_ = steps[k]
                p = e_ % 2
                t0, ntb = blocks[bi_]
                hb = k % 2
                ncol = ntb * 128
                for fc in range(4):
                    q = fcn[0] % 2; fcn[0] += 1
                    for kk in range(8):
                        S.op("pe", lambda e, kk=kk, fc=fc, q=q, p=p, t0=t0, ncol=ncol: e.matmul(
                            ps_g[q][:, :ncol], lhsT=wgb[p][:, kk, fc * 128:(fc + 1) * 128],
                            rhs=h2T[:, kk, t0 * 128:t0 * 128 + ncol], start=(kk == 0), stop=(kk == 7)),
                            reads=["wgb%d" % p], writes=["ps_g%d" % q])
                    for kk in range(8):
                        S.op("pe", lambda e, kk=kk, fc=fc, q=q, p=p, t0=t0, ncol=ncol: e.matmul(
                            ps_u[q][:, :ncol], lhsT=wub[p][:, kk, fc * 128:(fc + 1) * 128],
                            rhs=h2T[:, kk, t0 * 128:t0 * 128 + ncol], start=(kk == 0), stop=(kk == 7)),
                            reads=["wub%d" % p], writes=["ps_u%d" % q])
                    S.op("act", lambda e, q=q, ncol=ncol: e.activation(out=sg[q][:, :ncol], in_=ps_g[q][:, :ncol], func=AF.Silu),
                         reads=["ps_g%d" % q], writes=["sg%d" % q])
                    S.op("dve", lambda e, q=q, ncol=ncol, hb=hb, fc=fc: e.tensor_tensor(
                        out=heT[hb][:, fc, :ncol], in0=ps_u[q][:, :ncol], in1=sg[q][:, :ncol], op=ALU.mult),
                        reads=["ps_u%d" % q, "sg%d" % q], writes=["heT%d_%d" % (hb, fc)])

            def stage_D(k):
                e_, bi_ = steps[k]
                p = e_ % 2
                t0, ntb = blocks[bi_]
                hb = k % 2
                for tt in range(ntb):
                    ti = t0 + tt
                    dq = dn[0] % 2; dn[0] += 1
                    for half in range(2):
                        for fc in range(4):
                            S.op("pe", lambda e, fc=fc, half=half, dq=dq, hb=hb, tt=tt, p=p: e.matmul(
                                ps_d[dq][:, half * 512:(half + 1) * 512], lhsT=heT[hb][:, fc, tt * 128:(tt + 1) * 128],
                                rhs=wdb[p][:, fc, half * 512:(half + 1) * 512], start=(fc == 0), stop=(fc == 3)),
                                reads=["heT%d_%d" % (hb, fc), "wdb%d" % p], writes=["ps_d%d" % dq])
                    if e_ == 0:
                        S.op("dve", lambda e, ti=ti, dq=dq, e_=e_: e.tensor_scalar(
                            out=acc[:, ti, :], in0=ps_d[dq][:], scalar1=gates[:, ti, e_:e_ + 1], scalar2=None, op0=ALU.mult),
                            reads=["ps_d%d" % dq], writes=["acc%d" % ti])
                    else:
                        S.op("dve", lambda e, ti=ti, dq=dq, e_=e_: e.scalar_tensor_tensor(
                            out=acc[:, ti, :], in0=ps_d[dq][:], scalar=gates[:, ti, e_:e_ + 1], in1=acc[:, ti, :],
                            op0=ALU.mult, op1=ALU.add),
                            reads=["ps_d%d" % dq, "acc%d" % ti], writes=["acc%d" % ti])
                if bi_ == len(blocks) - 1 and e_ + 2 < n_exp:
                    load_w(e_ + 2)

            for k in range(len(steps) + 1):
                if k < len(steps):
                    stage_G(k)
                if k >= 1:
                    stage_D(k - 1)
            S.barrier()
            st2w.close()
            xtT = sb("xtT", [128, 8, 128], st=st2)
            ps_x = pst("ps_x", [128, 8, 128], st=st2)
            xr = [sb("xr%d" % i, [128, D], st=st2) for i in range(2)]
            u3 = [sb("u3_%d" % i, [128, D], st=st2) for i in range(2)]
            stt3 = sb("stt3", [128, 2, 6], st=st2)
            mv3 = sb("mv3", [128, 2], st=st2)
            rstd3 = sb("rstd3", [128, 1], st=st2)
            nmr3 = sb("nmr3", [128, 1], st=st2)
            def Q1(i):
                    is_ctx = has_ctx and i == NTI - 1
                    b2_ = i % 2
                    g2t = G2c if is_ctx else G2
                    S.dma("sp", xr[b2_][:], x1s[i * 128:(i + 1) * 128, :], reads=["x1s%d" % i], writes=["xr%d" % b2_])
            def Q2(i):
                    is_ctx = has_ctx and i == NTI - 1
                    b2_ = i % 2
                    g2t = G2c if is_ctx else G2
                    u_ = u3[b2_]; utok = "u3_%d" % b2_
                    S.op("dve", lambda e, u_=u_, i=i, g2t=g2t: e.tensor_tensor(out=u_[:], in0=acc[:, i, :], in1=g2t[:], op=ALU.mult),
                         writes=[utok])
                    S.op("dve", lambda e, u_=u_, b2_=b2_: e.tensor_tensor(out=u_[:], in0=u_[:], in1=xr[b2_][:], op=ALU.add),
                         reads=[utok, "xr%d" % b2_], writes=[utok])
                    for hh in range(2):
                        S.op("dve", lambda e, hh=hh, u_=u_: e.bn_stats(out=stt3[:, hh, :], in_=u_[:, hh * 512:(hh + 1) * 512]),
                             reads=[utok], writes=["stt3_%d" % hh])
                    S.op("dve", lambda e: e.bn_aggr(out=mv3[:], in_=stt3[:].rearrange("p a b -> p (a b)")),
                         reads=["stt3_0", "stt3_1"], writes=["mv3"])
                    S.op("act", lambda e: e.activation(out=rstd3[:], in_=mv3[:, 1:2], func=AF.Sqrt, bias=epsT[:, 0:1], scale=1.0),
                         reads=["mv3"], writes=["rstd3"])
                    S.op("dve", lambda e: e.reciprocal(out=rstd3[:], in_=rstd3[:]), reads=["rstd3"], writes=["rstd3"])
                    S.op("dve", lambda e: e.scalar_tensor_tensor(out=nmr3[:], in0=mv3[:, 0:1], scalar=-1.0, in1=rstd3[:],
                                                                 op0=ALU.mult, op1=ALU.mult),
                         reads=["mv3", "rstd3"], writes=["nmr3"])
                    S.op("act", lambda e, u_=u_: e.activation(out=u_[:], in_=u_[:], func=AF.Identity, bias=nmr3[:, 0:1], scale=rstd3[:, 0:1]),
                         reads=[utok, "nmr3", "rstd3"], writes=[utok])
                    S.op("pool", lambda e, u_=u_: e.tensor_tensor(out=u_[:], in0=u_[:], in1=LN2G[:], op=ALU.mult), reads=[utok], writes=[utok])
                    S.op("pool", lambda e, u_=u_: e.tensor_tensor(out=u_[:], in0=u_[:], in1=LN2B[:], op=ALU.add), reads=[utok], writes=[utok])
                    S.dma("sp", xo[i * 128:(i + 1) * 128, :], u_[:], reads=[utok], writes=["xo%d" % i])
            def Q3(i):
                    is_ctx = has_ctx and i == NTI - 1
                    b2_ = i % 2
                    g2t = G2c if is_ctx else G2
                    u_ = u3[b2_]; utok = "u3_%d" % b2_
                    if xT_lat_o is not None:
                        for k in range(8):
                            S.op("pe", lambda e, k=k, u_=u_: e.transpose(out=ps_x[:, k, :], in_=u_[:, k * 128:(k + 1) * 128], identity=ident[:]),
                                 reads=[utok, "ident"], writes=["ps_x"])
                        S.op("dve", lambda e: e.tensor_copy(out=xtT[:], in_=ps_x[:]), reads=["ps_x"], writes=["xtT"])
                        if is_ctx:
                            S.dma("sp", xT_ctx_o.rearrange("(k p) t -> p k t", p=128), xtT[:, :, 0:64], reads=["xtT"], writes=["xTo%d" % i])
                        else:
                            S.dma("sp", xT_lat_o.rearrange("(k p) t -> p k t", p=128)[:, :, i * 128:(i + 1) * 128], xtT[:], reads=["xtT"], writes=["xTo%d" % i])
            for it_ in range(NTI + 2):
                if it_ < NTI:
                    Q1(it_)
                if 0 <= it_ - 1 < NTI:
                    Q2(it_ - 1)
                if 0 <= it_ - 2 < NTI:
                    Q3(it_ - 2)
            S.barrier()


def build_fused(n_lat=8192):
    T = CTX + n_lat
    NQ = n_lat // 4
    NLT = NQ // 128
    NTB = (NLT + 1) * 128
    nc = bass.Bass("TRN2", target_bir_lowering=False)
    def din(name, shape):
        return nc.dram_tensor(name, shape, F32, kind="ExternalInput").ap()
    def scr(name, shape):
        return nc.dram_tensor(name, shape, F32, kind="Internal").ap()
    I = {}
    I["xT0"] = din("xT0", [1024, T]); I["xq0"] = din("xq0", [4, NTB, 1024])
    I["wcore"] = din("wcore", [2, 4, 1024, NCOL]); I["ccol"] = din("ccol", [128, 16])
    I["wadaA"] = din("wadaA", [2, 1024, 2048]); I["badac"] = din("badac", [2, 128, 16])
    I["lbl"] = din("lbl", [4, 64, 4]); I["gnorm"] = din("gnorm", [2, 64, 1]); I["nab"] = din("nab", [2, 4, 21, 128, 128])
    I["swm"] = din("swm", [2, 128, 128]); I["sink"] = din("sink", [2, 4, 1, 2])
    I["ropeC"] = din("ropeC", [64, n_lat]); I["ropeS"] = din("ropeS", [64, n_lat])
    I["cst"] = din("cst", [128, 1024]); I["vmask"] = din("vmask", [128, 4])
    I["wadaB"] = din("wadaB", [2, 1024, 4096]); I["badaB"] = din("badaB", [2, 1, 4096]); I["lnp"] = din("lnp", [2, 1, 4096])
    I["wout"] = din("wout", [2, 1024, 1024]); I["wr"] = din("wr", [1024, 16]); I["rb"] = din("rb", [1, 16])
    I["wg"] = din("wg", [2, 16, 1024, 512]); I["wu"] = din("wu", [2, 16, 1024, 512]); I["wd"] = din("wd", [2, 16, 512, 1024])
    I["ident"] = din("ident", [128, 128])
    out = nc.dram_tensor("out", [NQ, 1024], F32, kind="ExternalOutput").ap()
    Y = scr("Y", [1024, T]); xT1 = scr("xT1", [1024, T]); X1 = scr("X1", [4, NTB, 1024]); x1s = scr("x1s", [NTB, 1024]); ofs = scr("ofs", [64, T])
    UB = scr("UB", [64, 1 + n_lat // 512, 1024]); SG = scr("SG", [64, T])
    QB = nc.dram_tensor("QB", [64, T], BF16, kind="Internal").ap()
    HT = nc.dram_tensor("HT", [1024, T], BF16, kind="Internal").ap()
    modc_d = scr("modc_d", [128, 32]); bc_d = scr("bc_d", [12, 128, 1024])
    es = contextlib.ExitStack()
    with es:
        S = Sched(nc, es)
        for l in range(2):
            last = l == 1
            xTsrc = I["xT0"] if l == 0 else xT1
            for j in range(4):
                io = {"xT": xTsrc, "wcore": I["wcore"][l, j], "ccol": I["ccol"], "wada": I["wadaA"][l], "badac": I["badac"][l],
                      "lbl": I["lbl"][j], "gnorm": I["gnorm"][l], "nab": I["nab"][l, j], "swm": I["swm"], "sink": I["sink"][l, j],
                      "ropeC": I["ropeC"], "ropeS": I["ropeS"], "cst": I["cst"], "vmask": I["vmask"],
                      "yT": Y[256 * j:256 * (j + 1), :], "ofs": ofs, "UB": UB, "QB": QB, "SG": SG, "modc_d": modc_d, "mod_first": j == 0, "HT": HT}
                emit_phase_a(nc, S, io, l, not last, "a%d%d_" % (l, j), n_lat=n_lat)
            if l == 0:
                for q in range(4):
                    io = {"yT_lat": Y[:, CTX + q * NQ:CTX + (q + 1) * NQ], "yT_ctx": Y[:, q * 64:(q + 1) * 64],
                          "x": I["xq0"][q], "ccol": I["ccol"], "wada": I["wadaB"][l], "bada": I["badaB"][l],
                          "lnp": I["lnp"][l], "wout": I["wout"][l], "wr": I["wr"], "rb": I["rb"], "wg": I["wg"][l], "wu": I["wu"][l], "wd": I["wd"][l],
                          "ident": I["ident"], "x1s": x1s[0:NTB, :], "xo": X1[q],
                          "xT_lat_o": xT1[:, CTX + q * NQ:CTX + (q + 1) * NQ], "xT_ctx_o": xT1[:, q * 64:(q + 1) * 64],
                          "bc_d": bc_d, "bc_first": q == 0}
                    emit_phase_b(nc, S, io, True, "b%d%d_" % (l, q), n_lat_tiles=NLT)
            else:
                q_pool = nc.gpsimd.partition_id() % 4
                q_sp = nc.sync.partition_id() % 4
                X1f = X1.rearrange("q n d -> (q n) d")
                io = {"yT_lat": Y[:, bass.ds(q_pool * NQ + CTX, NQ)],
                      "x": X1f[bass.ds(q_sp * NTB, NQ), :], "ccol": I["ccol"], "wada": I["wadaB"][l], "bada": I["badaB"][l],
                      "lnp": I["lnp"][l], "wout": I["wout"][l], "wr": I["wr"], "rb": I["rb"], "wg": I["wg"][l], "wu": I["wu"][l], "wd": I["wd"][l],
                      "ident": I["ident"], "x1s": x1s[0:NQ, :], "xo": out}
                emit_phase_b(nc, S, io, False, "b%dq_" % l, n_lat_tiles=NLT)
        S.finish()
    return nc


def fused_inputs(inp, b, n_lat=8192):
    NQ = n_lat // 4; NLT = NQ // 128; NTB = (NLT + 1) * 128
    f = lambda a: np.ascontiguousarray(a, dtype=np.float32)
    x = inp['x'][b, :n_lat]; ctx = inp['ctx'][b]
    xT0 = np.concatenate([ctx, x], 0).T
    xq0 = np.zeros((4, NTB, 1024), np.float32)
    for q in range(4):
        xq0[q, :NQ] = x[q * NQ:(q + 1) * NQ]
        xq0[q, NQ:NQ + 64] = ctx[q * 64:(q + 1) * 64]
    per = [[core_inputs_a(inp, l, b, j, xT0, n_lat=n_lat) for j in range(4)] for l in range(2)]
    m = {"xT0": f(xT0), "xq0": xq0, "ccol": per[0][0]["ccol"], "ropeC": per[0][0]["ropeC"], "ropeS": per[0][0]["ropeS"],
         "cst": per[0][0]["cst"], "vmask": per[0][0]["vmask"], "swm": per[0][0]["swm"]}
    m["wcore"] = f(np.stack([np.stack([per[l][j]["wcore"] for j in range(4)]) for l in range(2)]))
    m["wadaA"] = f(np.stack([per[l][0]["wada"] for l in range(2)])); m["badac"] = f(np.stack([per[l][0]["badac"] for l in range(2)]))
    m["lbl"] = f(np.stack([per[0][j]["lbl"] for j in range(4)])); m["gnorm"] = f(np.stack([per[l][0]["gnorm"] for l in range(2)]))
    m["nab"] = f(np.stack([np.stack([per[l][j]["nab"] for j in range(4)]) for l in range(2)]))
    m["sink"] = f(np.stack([np.stack([per[l][j]["sink"] for j in range(4)]) for l in range(2)]))
    m["wadaB"] = f(np.stack([inp['w_ada'][l][:, 2048:] for l in range(2)])); m["badaB"] = f(np.stack([inp['b_ada'][l][None, 2048:] for l in range(2)]))
    m["lnp"] = f(np.stack([np.concatenate([inp['ln1_g'][l], inp['ln1_b'][l], inp['ln2_g'][l], inp['ln2_b'][l]])[None] for l in range(2)]))
    feat = np.zeros(1024, np.int64)
    for j in range(4):
        feat[256 * j:256 * j + 64] = 64 * j + np.arange(64)
        feat[256 * j + 64:256 * j + 128] = 256 + 64 * j + np.arange(64)
        feat[256 * j + 128:256 * j + 256] = 512 + 128 * j + np.arange(128)
    m["wout"] = f(np.stack([inp['w_out'][l][feat, :] for l in range(2)]))
    m["wr"] = f(inp['w_router']); m["rb"] = f(inp['router_bias'][None])
    m["wg"] = f(inp['w_gate']); m["wu"] = f(inp['w_up']); m["wd"] = f(inp['w_down'])
    m["ident"] = np.eye(128, dtype=np.float32)
    return m

_NC = {}


def kernel(**inputs):
    inp = {k: np.asarray(v, dtype=np.float32) for k, v in inputs.items()}
    if "nc" not in _NC:
        _NC["nc"] = build_fused(8192)
    nc = _NC["nc"]
    maps = [fused_inputs(inp, b) for b in range(2)]
    in_maps = [maps[c // 4] for c in range(8)]
    res = run_bass_kernel_spmd(nc, in_maps, core_ids=list(range(8))).results
    out = np.stack([np.concatenate([res[4 * b + q]["out"] for q in range(4)], 0) for b in range(2)], 0)
    return np.ascontiguousarray(out, dtype=np.float32)
```
